# Optimizing a Trainium2 kernel written in Bass

```python
import math, functools
import jax
import jax.numpy as jnp
from jax import lax
import numpy as np

D_MODEL = 1024
BATCH = 4
SEQ = 8192
DEPTH = 4

CTX_LEN = 256
GRID_W = 64
D_MIX = D_MODEL
MIXER_W = D_MIX // 4
CHUNK = 32
NORM_EPS = 1e-6
ROPE_BASE = 10000.0
LB_FLOOR = 1e-30
LB_CEIL = 1.0 - 1e-6

RET_HEADS = 4
RET_DK = MIXER_W // RET_HEADS
RET_DV = MIXER_W // RET_HEADS

S5_GROUP = 16
S5_GROUPS = MIXER_W // S5_GROUP
S5_STATE = 64

HG_HEADS = 4
HG_DK = MIXER_W // HG_HEADS
HG_DV = MIXER_W // HG_HEADS

GLA_HEADS = 4
GLA_DK = MIXER_W // (2 * GLA_HEADS)
GLA_DV = MIXER_W // GLA_HEADS
GLA_RANK = 16
GLA_TAU = 16.0

D_FF = 2816
N_EXPERTS = 8
TOP_K = 2
D_FF_EXPERT = 2816
MOE_BLOCK = 256
N_DENSE = (DEPTH + 1) // 2
N_MOE = DEPTH // 2

COL_SPLITS = (
    ('ret_q', RET_HEADS * RET_DK), ('ret_k', RET_HEADS * RET_DK),
    ('ret_v', RET_HEADS * RET_DV), ('ret_g', RET_HEADS * RET_DV),
    ('s5_u', MIXER_W),
    ('hg_q', HG_HEADS * HG_DK), ('hg_ff', HG_HEADS * HG_DK), ('hg_fb', HG_HEADS * HG_DK),
    ('hg_i', HG_HEADS * HG_DV), ('hg_g', HG_HEADS * HG_DV),
    ('gla_q', GLA_HEADS * GLA_DK), ('gla_k', GLA_HEADS * GLA_DK),
    ('gla_v', GLA_HEADS * GLA_DV), ('gla_g', GLA_HEADS * GLA_DV),
    ('gla_af', GLA_RANK), ('gla_ab', GLA_RANK),
)
D_IN = sum(w for _, w in COL_SPLITS)

kernel_name = 'hybrid_ret_s5_hgrn2_gla_moe_dit'


def rmsnorm(x, w):
    xf = x.astype(jnp.float32)
    y = xf * lax.rsqrt(jnp.mean(xf * xf, axis=-1, keepdims=True) + NORM_EPS)
    return (y * w.astype(jnp.float32)).astype(x.dtype)


def head_norm(o, w, center):
    of = o.astype(jnp.float32)
    if center:
        of = of - jnp.mean(of, axis=-1, keepdims=True)
    of = of * lax.rsqrt(jnp.mean(of * of, axis=-1, keepdims=True) + NORM_EPS)
    return (of.reshape(o.shape[0], o.shape[1], -1) * w.astype(jnp.float32)).astype(o.dtype)


def modulate(h, shift, scale):
    return h * (1 + scale) + shift


def split_cols(p):
    names = [n for n, _ in COL_SPLITS]
    offsets = np.cumsum([w for _, w in COL_SPLITS])[:-1].tolist()
    return dict(zip(names, jnp.split(p, offsets, axis=-1)))


def to_heads(t, n_heads):
    return t.reshape(t.shape[0], t.shape[1], n_heads, t.shape[-1] // n_heads)


def rope_1d(t, pos):
    half = t.shape[-1] // 2
    freqs = ROPE_BASE ** (-jnp.arange(half, dtype=jnp.float32) / half)
    ang = pos[:, None] * freqs[None, :]
    cos = jnp.cos(ang)[None, :, None, :]
    sin = jnp.sin(ang)[None, :, None, :]
    t1 = t[..., :half].astype(jnp.float32)
    t2 = t[..., half:].astype(jnp.float32)
    return jnp.concatenate([t1 * cos - t2 * sin, t1 * sin + t2 * cos], axis=-1).astype(t.dtype)


def axial_rope(t, row, col):
    d = t.shape[-1] // 2
    return jnp.concatenate([rope_1d(t[..., :d], row), rope_1d(t[..., d:], col)], axis=-1)


def chunk_gated_scan(inputs, h0):
    q, k, v, log_g = (a.astype(jnp.float32) for a in inputs)
    bsz, seq, n_heads, dk = q.shape
    dv = v.shape[-1]
    n_chunks = seq // CHUNK

    def chunks(a):
        return jnp.moveaxis(a.reshape(bsz, n_chunks, CHUNK, *a.shape[2:]), 1, 0)

    if h0 is None:
        h0 = jnp.zeros((bsz, n_heads, dk, dv), jnp.float32)
    causal = jnp.tril(jnp.ones((CHUNK, CHUNK), dtype=bool))[None, :, :, None, None]
    scalar_decay = log_g.shape[-1] == 1

    def step(state, blk):
        qc, kc, vc, gc = blk
        b = jnp.cumsum(gc, axis=1)
        diff = b[:, :, None] - b[:, None, :]
        decay = jnp.where(causal, jnp.exp(jnp.where(causal, diff, 0.0)), 0.0)
        if scalar_decay:
            scores = jnp.einsum('bihk,bjhk->bijh', qc, kc) * decay[..., 0]
        else:
            scores = jnp.sum(qc[:, :, None] * kc[:, None, :] * decay, axis=-1)
        out = (jnp.einsum('bijh,bjhv->bihv', scores, vc)
               + jnp.einsum('bihk,bhkv->bihv', qc * jnp.exp(b), state))
        b_last = b[:, -1]
        state = (jnp.exp(b_last)[..., None] * state
                 + jnp.einsum('bjhk,bjhv->bhkv', kc * jnp.exp(b_last[:, None] - b), vc))
        return state, out

    h_final, out = lax.scan(step, h0, tuple(chunks(a) for a in (q, k, v, log_g)))
    out = jnp.moveaxis(out, 0, 1).reshape(bsz, seq, n_heads, dv)
    return h_final, out.astype(inputs[2].dtype)


def s5_scan(prm, inputs, h0):
    lam_re, lam_im, log_dt, b_re, b_im, c_re, c_im = (a.astype(jnp.float32) for a in prm)
    (u,) = inputs
    seq = u.shape[1]
    lam = lax.complex(lam_re, lam_im)
    a_bar = jnp.exp(lam * jnp.exp(log_dt)[:, None])
    b_bar = ((a_bar - 1) / lam)[..., None] * lax.complex(b_re, b_im)
    c_mat = lax.complex(c_re, c_im)
    bu = jnp.einsum('gpc,btgc->tbgp', b_bar, u.astype(jnp.float32).astype(jnp.complex64))
    if h0 is not None:
        bu = bu.at[0].add(a_bar * h0)
    a_seq = jnp.broadcast_to(a_bar, (seq, 1) + a_bar.shape)

    def combine(e1, e2):
        return e1[0] * e2[0], e2[0] * e1[1] + e2[1]

    _, h = lax.associative_scan(combine, (a_seq, bu), axis=0)
    y = jnp.einsum('gcp,tbgp->btgc', c_mat, h).real
    return h[-1], y.astype(u.dtype)


def prefixed_bidir(run_f, run_b, ctx_f, lat_f, ctx_b, lat_b, want_ctx):
    def flip(ts):
        return tuple(t[:, ::-1] for t in ts)
    h_f, yc_f = run_f(ctx_f, None)
    _, yl_f = run_f(lat_f, h_f)
    h_b, yc_b = run_b(flip(ctx_b), None)
    _, yl_b = run_b(flip(lat_b), h_b)
    y_lat = yl_f + yl_b[:, ::-1]
    y_ctx = yc_f + yc_b[:, ::-1] if want_ctx else None
    return y_ctx, y_lat


def retention_mixer(pc, pl, gn_w, row, col, want_ctx):
    def qkv(p, rotate):
        q = to_heads(p['ret_q'], RET_HEADS)
        k = to_heads(p['ret_k'], RET_HEADS) * (RET_DK ** -0.5)
        v = to_heads(p['ret_v'], RET_HEADS)
        if rotate:
            q, k = axial_rope(q, row, col), axial_rope(k, row, col)
        return q, k, v

    log_gamma_f = jnp.log1p(-(2.0 ** (-5.0 - jnp.arange(RET_HEADS, dtype=jnp.float32))))
    log_gamma_b = log_gamma_f[::-1]

    def decay(p, lg):
        return jnp.broadcast_to(lg[None, None, :, None], (p.shape[0], p.shape[1], RET_HEADS, 1))

    qc, kc, vc = qkv(pc, False)
    ql, kl, vl = qkv(pl, True)
    yc, yl = prefixed_bidir(
        chunk_gated_scan, chunk_gated_scan,
        (qc, kc, vc, decay(pc['ret_q'], log_gamma_f)), (ql, kl, vl, decay(pl['ret_q'], log_gamma_f)),
        (qc, kc, vc, decay(pc['ret_q'], log_gamma_b)), (ql, kl, vl, decay(pl['ret_q'], log_gamma_b)),
        want_ctx)

    def post(y, p):
        return head_norm(y, gn_w, True) * jax.nn.silu(p['ret_g'])
    return (post(yc, pc) if want_ctx else None), post(yl, pl)


def s5_mixer(pc, pl, lp, want_ctx):
    names = ('s5_lam_re', 's5_lam_im', 's5_log_dt', 's5_b_re', 's5_b_im', 's5_c_re', 's5_c_im')
    run_f = functools.partial(s5_scan, tuple(lp[n][0] for n in names))
    run_b = functools.partial(s5_scan, tuple(lp[n][1] for n in names))

    def groups(u):
        return u.reshape(u.shape[0], u.shape[1], S5_GROUPS, S5_GROUP)

    uc, ul = pc['s5_u'], pl['s5_u']
    yc, yl = prefixed_bidir(run_f, run_b, (groups(uc),), (groups(ul),), (groups(uc),), (groups(ul),), want_ctx)

    def post(y, u):
        y = y.reshape(u.shape) + lp['s5_d'] * u
        z = jax.nn.gelu(y)
        return z * jax.nn.sigmoid(z @ lp['s5_glu_w'] + lp['s5_glu_b'])
    return (post(yc, uc) if want_ctx else None), post(yl, ul)


def hgrn2_mixer(pc, pl, lower_bound, norm_w, want_ctx):
    lb = lower_bound.reshape(HG_HEADS, HG_DK).astype(jnp.float32)
    log_lb = jnp.log(jnp.maximum(lb, LB_FLOOR))
    log_1m_lb = jnp.log1p(-lb)

    def inputs(p, gate_name):
        z = to_heads(p[gate_name], HG_HEADS).astype(jnp.float32)
        log_f = jnp.logaddexp(log_lb, log_1m_lb + jax.nn.log_sigmoid(z))
        k = (1.0 - lb) * jax.nn.sigmoid(-z)
        return (to_heads(p['hg_q'], HG_HEADS), k, to_heads(p['hg_i'], HG_HEADS), log_f)

    yc, yl = prefixed_bidir(chunk_gated_scan, chunk_gated_scan,
                            inputs(pc, 'hg_ff'), inputs(pl, 'hg_ff'),
                            inputs(pc, 'hg_fb'), inputs(pl, 'hg_fb'), want_ctx)

    def post(y, p):
        return head_norm(y, norm_w, False) * jax.nn.silu(p['hg_g'])
    return (post(yc, pc) if want_ctx else None), post(yl, pl)


def gla_mixer(pc, pl, wa2, ba, norm_w, want_ctx):
    def inputs(p, d):
        low = p[('gla_af', 'gla_ab')[d]]
        log_a = jax.nn.log_sigmoid((low @ wa2[d] + ba[d]).astype(jnp.float32)) / GLA_TAU
        return (to_heads(p['gla_q'], GLA_HEADS),
                to_heads(p['gla_k'], GLA_HEADS) * (GLA_DK ** -0.5),
                to_heads(p['gla_v'], GLA_HEADS),
                to_heads(log_a, GLA_HEADS))

    yc, yl = prefixed_bidir(chunk_gated_scan, chunk_gated_scan,
                            inputs(pc, 0), inputs(pl, 0), inputs(pc, 1), inputs(pl, 1), want_ctx)

    def post(y, p):
        return head_norm(y, norm_w, False) * jax.nn.silu(p['gla_g'])
    return (post(yc, pc) if want_ctx else None), post(yl, pl)


def token_mix(pc, pl, lp, lower_bound, row, col, want_ctx):
    outs = [
        retention_mixer(pc, pl, lp['ret_gn_w'], row, col, want_ctx),
        s5_mixer(pc, pl, lp, want_ctx),
        hgrn2_mixer(pc, pl, lower_bound, lp['hg_norm_w'], want_ctx),
        gla_mixer(pc, pl, lp['gla_wa2'], lp['gla_ba'], lp['gla_norm_w'], want_ctx),
    ]
    y_lat = jnp.concatenate([o[1] for o in outs], axis=-1)
    y_ctx = jnp.concatenate([o[0] for o in outs], axis=-1) if want_ctx else None
    return y_ctx, y_lat


def swiglu(x, w1, w3, w2):
    return (jax.nn.silu(x @ w1) * (x @ w3)) @ w2


def moe_ffn(xf, router_w, router_b, w1, w3, w2):
    n_tok, d = xf.shape
    logits = (xf @ router_w + router_b).astype(jnp.float32)
    top_v, top_i = lax.top_k(logits, TOP_K)
    gates = jax.nn.softmax(top_v, axis=-1).astype(xf.dtype)
    n_assign = n_tok * TOP_K
    flat_e = top_i.reshape(-1)
    flat_tok = jnp.arange(n_assign, dtype=jnp.int32) // TOP_K
    flat_g = gates.reshape(-1)
    order = jnp.argsort(flat_e)
    se, stok, sg = flat_e[order], flat_tok[order], flat_g[order]
    counts = jnp.bincount(flat_e, length=N_EXPERTS)
    starts = jnp.cumsum(counts) - counts
    padded = (counts + MOE_BLOCK - 1) // MOE_BLOCK * MOE_BLOCK
    pends = jnp.cumsum(padded)
    pstarts = pends - padded
    dest = pstarts[se] + (jnp.arange(n_assign, dtype=jnp.int32) - starts[se])
    n_blocks = -(-n_assign // MOE_BLOCK) + N_EXPERTS
    cap = n_blocks * MOE_BLOCK
    xbuf = jnp.zeros((cap, d), xf.dtype).at[dest].set(xf[stok])
    block_start = jnp.arange(n_blocks, dtype=jnp.int32) * MOE_BLOCK
    block_e = jnp.minimum(jnp.searchsorted(pends, block_start, side='right'), N_EXPERTS - 1)

    def expert_block(args):
        xb, e = args
        return swiglu(xb, w1[e], w3[e], w2[e])

    ybuf = lax.map(expert_block, (xbuf.reshape(n_blocks, MOE_BLOCK, d), block_e)).reshape(cap, d)
    return jnp.zeros_like(xf).at[stok].add(ybuf[dest] * sg[:, None])


def setup_inputs(seed: int = 0) -> dict:
    key = jax.random.key(seed)
    ks = iter(jax.random.split(key, 48))
    f32 = jnp.float32

    def nrm(shape, scale):
        return scale * jax.random.normal(next(ks), shape, f32)

    G, P, Cg = S5_GROUPS, S5_STATE, S5_GROUP
    return {
        'x': nrm((BATCH, SEQ, D_MODEL), 1.0),
        'c': nrm((BATCH, D_MODEL), 1.0),
        'ctx': nrm((BATCH, CTX_LEN, D_MODEL), 1.0),
        'c_ctx': nrm((D_MODEL,), 1.0),
        'ada_w': nrm((DEPTH, D_MODEL, 6 * D_MODEL), 0.5 * D_MODEL ** -0.5),
        'ada_b': nrm((DEPTH, 6 * D_MODEL), 0.02),
        'norm1_w': 1.0 + nrm((DEPTH, D_MODEL), 0.02),
        'norm2_w': 1.0 + nrm((DEPTH, D_MODEL), 0.02),
        'w_in': nrm((DEPTH, D_MODEL, D_IN), D_MODEL ** -0.5),
        'w_out': nrm((DEPTH, D_MIX, D_MODEL), D_MIX ** -0.5),
        'ret_gn_w': 1.0 + nrm((DEPTH, RET_HEADS * RET_DV), 0.02),
        's5_lam_re': -0.5 + nrm((DEPTH, 2, G, P), 0.01),
        's5_lam_im': math.pi * jnp.arange(P, dtype=f32) + nrm((DEPTH, 2, G, P), 0.01),
        's5_log_dt': jax.random.uniform(next(ks), (DEPTH, 2, G), f32, math.log(1e-3), math.log(1e-1)),
        's5_b_re': nrm((DEPTH, 2, G, P, Cg), (2 * Cg) ** -0.5),
        's5_b_im': nrm((DEPTH, 2, G, P, Cg), (2 * Cg) ** -0.5),
        's5_c_re': nrm((DEPTH, 2, G, Cg, P), P ** -0.5),
        's5_c_im': nrm((DEPTH, 2, G, Cg, P), P ** -0.5),
        's5_d': nrm((DEPTH, MIXER_W), 1.0),
        's5_glu_w': nrm((DEPTH, MIXER_W, MIXER_W), MIXER_W ** -0.5),
        's5_glu_b': nrm((DEPTH, MIXER_W), 0.02),
        'hg_lb_logits': nrm((DEPTH, HG_HEADS * HG_DK), 0.5),
        'hg_norm_w': 1.0 + nrm((DEPTH, HG_HEADS * HG_DV), 0.02),
        'gla_wa2': nrm((DEPTH, 2, GLA_RANK, GLA_HEADS * GLA_DK), GLA_RANK ** -0.5),
        'gla_ba': nrm((DEPTH, 2, GLA_HEADS * GLA_DK), 0.5),
        'gla_norm_w': 1.0 + nrm((DEPTH, GLA_HEADS * GLA_DV), 0.02),
        'ffn_w1': nrm((N_DENSE, D_MODEL, D_FF), D_MODEL ** -0.5),
        'ffn_w3': nrm((N_DENSE, D_MODEL, D_FF), D_MODEL ** -0.5),
        'ffn_w2': nrm((N_DENSE, D_FF, D_MODEL), D_FF ** -0.5),
        'router_w': nrm((N_MOE, D_MODEL, N_EXPERTS), D_MODEL ** -0.5),
        'router_b': nrm((N_MOE, N_EXPERTS), 0.01),
        'moe_w1': nrm((N_MOE, N_EXPERTS, D_MODEL, D_FF_EXPERT), D_MODEL ** -0.5),
        'moe_w3': nrm((N_MOE, N_EXPERTS, D_MODEL, D_FF_EXPERT), D_MODEL ** -0.5),
        'moe_w2': nrm((N_MOE, N_EXPERTS, D_FF_EXPERT, D_MODEL), D_FF_EXPERT ** -0.5),
        'final_norm_w': 1.0 + nrm((D_MODEL,), 0.02),
    }


def reference(x, c, ctx, c_ctx, ada_w, ada_b, norm1_w, norm2_w, w_in, w_out, ret_gn_w,
              s5_lam_re, s5_lam_im, s5_log_dt, s5_b_re, s5_b_im, s5_c_re, s5_c_im, s5_d,
              s5_glu_w, s5_glu_b, hg_lb_logits, hg_norm_w, gla_wa2, gla_ba, gla_norm_w,
              ffn_w1, ffn_w3, ffn_w2, router_w, router_b, moe_w1, moe_w3, moe_w2, final_norm_w):
    bsz, seq, d = x.shape
    rows = seq // GRID_W
    t_idx = jnp.arange(rows * GRID_W, dtype=jnp.int32)
    row = (t_idx // GRID_W).astype(jnp.float32)
    col = (t_idx % GRID_W).astype(jnp.float32)

    lb_sm = jax.nn.softmax(hg_lb_logits.astype(jnp.float32), axis=0)
    lower_bounds = jnp.clip(jnp.cumsum(lb_sm, axis=0) - lb_sm[0], 0.0, LB_CEIL)

    cond_lat = jax.nn.silu(c)
    cond_ctx = jax.nn.silu(c_ctx)
    h = ctx
    n_lat = bsz * seq
    for l in range(DEPTH):
        keep_ctx = l < DEPTH - 1
        ml = [m[:, None, :] for m in jnp.split(cond_lat @ ada_w[l] + ada_b[l], 6, axis=-1)]
        mc = jnp.split(cond_ctx @ ada_w[l] + ada_b[l], 6, axis=-1)

        pl = split_cols(modulate(rmsnorm(x, norm1_w[l]), ml[0], ml[1]) @ w_in[l])
        pc = split_cols(modulate(rmsnorm(h, norm1_w[l]), mc[0], mc[1]) @ w_in[l])
        lp = {
            'ret_gn_w': ret_gn_w[l],
            's5_lam_re': s5_lam_re[l], 's5_lam_im': s5_lam_im[l], 's5_log_dt': s5_log_dt[l],
            's5_b_re': s5_b_re[l], 's5_b_im': s5_b_im[l], 's5_c_re': s5_c_re[l], 's5_c_im': s5_c_im[l],
            's5_d': s5_d[l], 's5_glu_w': s5_glu_w[l], 's5_glu_b': s5_glu_b[l],
            'hg_norm_w': hg_norm_w[l],
            'gla_wa2': gla_wa2[l], 'gla_ba': gla_ba[l], 'gla_norm_w': gla_norm_w[l],
        }
        y_ctx, y_lat = token_mix(pc, pl, lp, lower_bounds[l], row, col, keep_ctx)
        x = x + ml[2] * (y_lat @ w_out[l])
        if keep_ctx:
            h = h + mc[2] * (y_ctx @ w_out[l])

        tokens = modulate(rmsnorm(x, norm2_w[l]), ml[3], ml[4]).reshape(-1, d)
        if keep_ctx:
            fc = modulate(rmsnorm(h, norm2_w[l]), mc[3], mc[4]).reshape(-1, d)
            tokens = jnp.concatenate([tokens, fc], axis=0)
        j = l // 2
        if l % 2 == 0:
            f = swiglu(tokens, ffn_w1[j], ffn_w3[j], ffn_w2[j])
        else:
            f = moe_ffn(tokens, router_w[j], router_b[j], moe_w1[j], moe_w3[j], moe_w2[j])
        x = x + ml[5] * f[:n_lat].reshape(bsz, seq, d)
        if keep_ctx:
            h = h + mc[5] * f[n_lat:].reshape(h.shape)

    return rmsnorm(x, final_norm_w)
```

```python
import numpy as np
import ml_dtypes
import concourse.bass as bass
import concourse.mybir as mybir
from concourse.bass_utils import run_bass_kernel_spmd

F32 = mybir.dt.float32
BF16 = mybir.dt.bfloat16
AF = mybir.ActivationFunctionType
ALU = mybir.AluOpType
AX = mybir.AxisListType

D = 1024
KD = 8
CTX = 256
L = 128
NPT = 27
NCOL = NPT * 128
DFF = 2816
HFF = 1408
NFT = 11


class Res:
    __slots__ = ("w", "r")

    def __init__(self):
        self.w = None
        self.r = {}


class V:
    __slots__ = ("ap", "res")

    def __init__(self, ap, res):
        self.ap = ap
        self.res = res

    def __getitem__(self, idx):
        return V(self.ap[idx], self.res)

    def bc(self, shape):
        return V(self.ap.broadcast_to(shape), self.res)

    def re(self, pat, **kw):
        return V(self.ap.rearrange(pat, **kw), self.res)


class T:
    def __init__(self, ap):
        self.ap = ap
        self.res = Res()
        self.keyed = {}

    def __getitem__(self, idx):
        return V(self.ap[idx], self.res)

    def k(self, key, idx):
        r = self.keyed.get(key)
        if r is None:
            r = self.keyed[key] = Res()
        return V(self.ap[idx], r)

    def all(self):
        return V(self.ap, self.res)


class Eng:
    def __init__(self, key, sem):
        self.key = key
        self.sem = sem
        self.cnt = 0
        self.seen = {}
        self.ops = []


class Sched:
    def __init__(self, nc, stack, ndma=12):
        self.nc = nc
        self.stack = stack
        self.eng = {}
        for k in ("pe", "act", "dve", "pool", "sp"):
            self.eng[k] = Eng(k, stack.enter_context(nc.semaphore("s_" + k)))
        self.dsem = {}
        self.dq = {}
        for q in ("sp", "pool", "act"):
            self.dq[q] = 0
            for i in range(ndma):
                self.dsem[(q, i)] = [stack.enter_context(nc.semaphore(f"d_{q}_{i}")), 0]
        self.ndma = ndma
        self.n_inst = 0

    def semof(self, key):
        if isinstance(key, tuple):
            return self.dsem[key][0]
        return self.eng[key].sem

    def _deps(self, e, reads, writes):
        deps = {}

        def add(k, c):
            if deps.get(k, 0) < c:
                deps[k] = c

        for v in reads:
            if v.res.w is not None:
                add(*v.res.w)
        for v in writes:
            if v.res.w is not None:
                add(*v.res.w)
            for k, c in v.res.r.items():
                add(k, c)
        for k, c in deps.items():
            if k == "pe" and e.key == "pe":
                continue
            if e.seen.get(k, 0) < c:
                e.ops.append(("w", self.semof(k), c))
                e.seen[k] = c
                self.n_inst += 1

    def emit(self, ek, fn, reads, writes):
        e = self.eng[ek]
        self._deps(e, reads, writes)
        e.cnt += 1
        e.ops.append(("o", fn))
        self.n_inst += 1
        for v in writes:
            v.res.w = (ek, e.cnt)
            v.res.r = {}
        for v in reads:
            v.res.r[ek] = e.cnt

    def dma(self, q, out, in_, **kw):
        e = self.eng[q]
        self._deps(e, [in_], [out])
        i = self.dq[q]
        self.dq[q] = (i + 1) % self.ndma
        key = (q, i)
        ds = self.dsem[key]
        if e.seen.get(key, 0) < ds[1]:
            e.ops.append(("w", ds[0], ds[1]))
            e.seen[key] = ds[1]
        ds[1] += 16
        e.ops.append(("d", out.ap, in_.ap, ds[0], kw))
        self.n_inst += 1
        out.res.w = (key, ds[1])
        out.res.r = {}
        in_.res.r[key] = ds[1]

    def barrier(self):
        for e in self.eng.values():
            for o in self.eng.values():
                if o.key != e.key and o.cnt > 0 and e.seen.get(o.key, 0) < o.cnt:
                    e.ops.append(("w", o.sem, o.cnt))
                    e.seen[o.key] = o.cnt
            for key, ds in self.dsem.items():
                if ds[1] > 0 and e.seen.get(key, 0) < ds[1]:
                    e.ops.append(("w", ds[0], ds[1]))
                    e.seen[key] = ds[1]

    def finish(self):
        self.barrier()
        nc = self.nc
        with nc.Block() as block:
            def run(e, h):
                for op in e.ops:
                    if op[0] == "w":
                        h.wait_ge(op[1], op[2])
                    elif op[0] == "o":
                        op[1](h).then_inc(e.sem, 1)
                    else:
                        h.dma_start(out=op[1], in_=op[2], **op[4]).then_inc(op[3], 16)

            @block.tensor
            def _(h):
                run(self.eng["pe"], h)

            @block.scalar
            def _(h):
                run(self.eng["act"], h)

            @block.vector
            def _(h):
                run(self.eng["dve"], h)

            @block.gpsimd
            def _(h):
                run(self.eng["pool"], h)

            @block.sync
            def _(h):
                run(self.eng["sp"], h)

    def mm(self, out, lhsT, rhs, start=True, stop=True):
        self.emit("pe", lambda h: h.matmul(out.ap, lhsT.ap, rhs.ap, start=start, stop=stop),
                  [lhsT, rhs] + ([] if start else [out]), [out])

    def tr(self, out, in_, ident):
        self.emit("pe", lambda h: h.transpose(out.ap, in_.ap, ident.ap), [in_, ident], [out])

    def act(self, out, in_, func, bias=None, scale=None, accum=None):
        reads = [in_]
        kw = {}
        if bias is not None:
            if isinstance(bias, V):
                reads.append(bias)
                kw["bias"] = bias.ap
            else:
                kw["bias"] = float(bias)
        if scale is not None:
            if isinstance(scale, V):
                reads.append(scale)
                kw["scale"] = scale.ap
            else:
                kw["scale"] = float(scale)
        writes = [out]
        if accum is not None:
            kw["accum_out"] = accum.ap
            writes.append(accum)
        self.emit("act", lambda h: h.activation(out.ap, in_.ap, func, **kw), reads, writes)

    def ts(self, ek, out, in0, s1, op0, s2=None, op1=None):
        reads = [in0]
        a1 = s1
        a2 = s2
        if isinstance(s1, V):
            reads.append(s1)
            a1 = s1.ap
        if isinstance(s2, V):
            reads.append(s2)
            a2 = s2.ap
        if op1 is None:
            self.emit(ek, lambda h: h.tensor_scalar(out.ap, in0.ap, a1, None, op0), reads, [out])
        else:
            self.emit(ek, lambda h: h.tensor_scalar(out.ap, in0.ap, a1, a2, op0, op1), reads, [out])

    def tt(self, ek, out, a, b, op):
        self.emit(ek, lambda h: h.tensor_tensor(out.ap, a.ap, b.ap, op), [a, b], [out])

    def stt(self, out, in0, sc, in1, op0, op1):
        reads = [in0, in1]
        a = sc
        if isinstance(sc, V):
            reads.append(sc)
            a = sc.ap
        self.emit("dve", lambda h: h.scalar_tensor_tensor(out.ap, in0.ap, a, in1.ap, op0, op1), reads, [out])

    def scan(self, out, d0, d1, init, op0=ALU.mult, op1=ALU.add):
        reads = [d0, d1]
        a = init
        if isinstance(init, V):
            reads.append(init)
            a = init.ap
        self.emit("dve", lambda h: h.tensor_tensor_scan(out.ap, d0.ap, d1.ap, a, op0, op1), reads, [out])

    def copy(self, ek, out, in_):
        if ek == "act":
            self.emit("act", lambda h: h.copy(out.ap, in_.ap), [in_], [out])
        else:
            self.emit(ek, lambda h: h.tensor_copy(out.ap, in_.ap), [in_], [out])

    def memset(self, ek, out, val):
        self.emit(ek, lambda h: h.memset(out.ap, val), [], [out])

    def recip(self, out, in_):
        self.emit("dve", lambda h: h.reciprocal(out.ap, in_.ap), [in_], [out])

    def red(self, out, in_, op):
        self.emit("dve", lambda h: h.tensor_reduce(out.ap, in_.ap, AX.X, op), [in_], [out])


from contextlib import ExitStack


class Prog:
    def __init__(self, TL, debug=False, NL=4):
        self.NL = NL
        self.nc = bass.Bass("TRN2", target_bir_lowering=False)
        self.stack = ExitStack()
        self.S = Sched(self.nc, self.stack)
        self.TL = TL
        self.NT = CTX + TL
        self.tiles = [(0, CTX)] + [(CTX + 512 * i, 512) for i in range(TL // 512)]
        self.debug = debug
        self.scr_kind = "ExternalOutput" if debug else "Internal"

    def din(self, name, shape, dt=F32):
        return T(self.nc.dram_tensor(name, list(shape), dt, kind="ExternalInput").ap())

    def dout(self, name, shape, dt=F32):
        return T(self.nc.dram_tensor(name, list(shape), dt, kind="ExternalOutput").ap())

    def dscr(self, name, shape, dt=F32):
        return T(self.nc.dram_tensor(name, list(shape), dt, kind=self.scr_kind).ap())

    def sb(self, st, name, shape, dt=F32):
        self._uid = getattr(self, "_uid", 0) + 1
        t = st.enter_context(self.nc.sbuf_tensor(f"sb{self._uid}_{name}", list(shape), dt))
        return T(t[tuple(slice(None) for _ in shape)])

    def ps(self, st, name, shape, dt=F32):
        self._uid = getattr(self, "_uid", 0) + 1
        t = st.enter_context(self.nc.psum_tensor(f"ps{self._uid}_{name}", list(shape), dt))
        return T(t[tuple(slice(None) for _ in shape)])

    def flush(self):
        self.S.finish()
        for e in self.S.eng.values():
            e.ops = []


def load_cast(P, st_name, dst, src_ap_fn, npieces, piece_shape, stg, eng_cycle=("dve", "pool")):
    raise NotImplementedError


def phase_mod(P, st, W, nj):
    S = P.S
    cond = P.sb(st, "cond", [128, KD, 2])
    csl = P.sb(st, "csl", [128, KD, 2])
    adab = P.sb(st, "adab", [128, 48])
    MOD = P.sb(st, "MOD", [128, 48, 2])
    S.dma("sp", cond.all(), W["cond"].all())
    S.dma("sp", adab.all(), W["ada_b"].all())
    S.act(csl.all(), cond.all(), AF.Silu)
    with ExitStack() as st2:
        pm = P.ps(st2, "pm", [128, 48, 2])
        stg = [P.sb(st2, f"adaw{i}", [128, KD, 768]) for i in range(2)]
        aw = W["ada_w"].ap.rearrange("(kc p) n -> p kc n", p=128)
        for pc in range((nj + 5) // 6):
            sg = stg[pc % 2]
            S.dma("sp" if pc % 2 == 0 else "pool", sg.all(), V(aw[:, :, pc * 768:(pc + 1) * 768], W["ada_w"].res))
            for jj in range(6):
                j = pc * 6 + jj
                if j >= nj:
                    break
                for kc in range(KD):
                    S.mm(pm[:, j, :], sg[:, kc, jj * 128:(jj + 1) * 128], csl[:, kc, :], start=(kc == 0), stop=(kc == KD - 1))
        S.tt("dve", MOD[:, 0:nj, :], pm[:, 0:nj, :], adab[:, 0:nj].re("p (j o) -> p j o", o=1).bc([128, nj, 2]), ALU.add)
        S.barrier()
        P.flush()
    return MOD


def norm_mod(P, S, X, XN, sqb, psn, rstd, tmpf, ones_bf, s_vec, b_vec, col, Wd):
    S.act(sqb[:, :, :Wd], X[:, :, :Wd], AF.Square)
    for kc in range(KD):
        S.mm(psn[:, :Wd], ones_bf.all(), sqb[:, kc, :Wd], start=(kc == 0), stop=(kc == KD - 1))
    S.ts("dve", rstd[:, :Wd], psn[:, :Wd], 1.0 / D, ALU.mult, 1e-6, ALU.add)
    S.act(rstd[:, :Wd], rstd[:, :Wd], AF.Sqrt)
    S.recip(rstd[:, :Wd], rstd[:, :Wd])
    for kc in range(KD):
        S.stt(tmpf[:, kc, :Wd], X[:, kc, :Wd], s_vec[:, kc, col:col + 1], rstd[:, :Wd], ALU.mult, ALU.mult)
        S.act(XN[:, kc, :Wd], tmpf[:, kc, :Wd], AF.Identity, bias=b_vec[:, kc, col:col + 1])


def consts_np():
    c = {}
    c["ones_bf"] = np.ones((128, 128), ml_dtypes.bfloat16)
    c["ident_f"] = np.eye(128, dtype=np.float32)
    c["ident_bf"] = np.eye(128).astype(ml_dtypes.bfloat16)
    return c


def to_fm(a):
    T_ = a.shape[0]
    return np.ascontiguousarray(a.T.reshape(KD, 128, T_))


def from_fm(a):
    return np.ascontiguousarray(a.reshape(D, -1).T)


def alloc_scratch(P):
    NT = P.NT
    sc = {}
    sc["QS"] = P.dscr("QS", [18, 128, NT], BF16)
    sc["GS"] = P.dscr("GS", [8, 128, NT], F32)
    sc["GT"] = P.dscr("GT", [8, 128, NT], BF16)
    sc["VS"] = P.dscr("VS", [NT, 768], BF16)
    return sc


def phase_A(P, W, MOD, sc, lbv):
    S = P.S
    with ExitStack() as st:
        Win = P.sb(st, "Win", [128, KD, NCOL], BF16)
        Wv = P.sb(st, "Wv", [128, KD, 768], BF16)
        wa2 = P.sb(st, "wa2", [128, 2, 256], BF16)
        wa2f = P.sb(st, "wa2f", [128, 2, 256])
        gba = P.sb(st, "gba", [128, 2, 2])
        n1w = P.sb(st, "n1w", [128, KD])
        s1 = P.sb(st, "s1", [128, KD, 2])
        ones_bf = P.sb(st, "ones", [128, 128], BF16)
        S.dma("sp", ones_bf.all(), W["ones_bf"].all())
        S.dma("sp", wa2f.all(), W["wa2p"].all())
        S.dma("sp", gba.all(), W["gla_ba"].all())
        S.dma("sp", n1w.all(), W["n1w"].all())
        S.copy("pool", wa2.all(), wa2f.all())
        S.ts("dve", s1.all(), MOD[:, 8:16, :], 1.0, ALU.add)
        S.tt("dve", s1.all(), s1.all(), n1w.all().re("p (k o) -> p k o", o=1).bc([128, KD, 2]), ALU.mult)
        win_v = W["w_in_arr"].ap.rearrange("(kc p) n -> p kc n", p=128)
        wv_v = W["w_v"].ap.rearrange("(kc p) n -> p kc n", p=128)
        with ExitStack() as st2:
            stg = [P.sb(st2, f"wstg{i}", [128, KD, 432]) for i in range(2)]
            for pc in range(8):
                sg = stg[pc % 2]
                S.dma("sp" if pc % 2 == 0 else "pool", sg.all(), V(win_v[:, :, pc * 432:(pc + 1) * 432], W["w_in_arr"].res))
                S.copy("dve" if pc % 2 == 0 else "act", Win[:, :, pc * 432:(pc + 1) * 432], sg.all())
            for pc in range(2):
                sg = stg[pc % 2]
                S.dma("sp", sg[:, :, 0:384], V(wv_v[:, :, pc * 384:(pc + 1) * 384], W["w_v"].res))
                S.copy("dve", Wv[:, :, pc * 384:(pc + 1) * 384], sg[:, :, 0:384])
            S.barrier()
            P.flush()
        X = [P.sb(st, f"X{i}", [128, KD, 512]) for i in range(2)]
        XN = P.sb(st, "XN", [128, KD, 512], BF16)
        sqb = P.sb(st, "sqb", [128, KD, 512], BF16)
        tmpf = P.sb(st, "tmpf", [128, KD, 512])
        rstd = P.sb(st, "rstd", [128, 512])
        rope = [P.sb(st, f"rope{i}", [128, 4, 512]) for i in range(1)]
        QSs = [P.sb(st, f"QSs{i}", [128, 18, 512], BF16) for i in range(1)]
        GSs = [P.sb(st, f"GSs{i}", [128, 8, 512]) for i in range(1)]
        GTs = [P.sb(st, f"GTs{i}", [128, 8, 512], BF16) for i in range(1)]
        Vs = [P.sb(st, f"Vs{i}", [128, 768], BF16) for i in range(3)]
        low = P.sb(st, "low", [128, 512], BF16)
        t1 = [P.sb(st, f"t1_{i}", [128, 512]) for i in range(2)]
        t2 = [P.sb(st, f"t2_{i}", [128, 512]) for i in range(2)]
        sg_ = [P.sb(st, f"sg_{i}", [128, 512]) for i in range(2)]
        psn = P.ps(st, "psn", [128, 512])
        pp = [P.ps(st, f"pp{i}", [128, 512]) for i in range(5)]
        pv = P.ps(st, "pv", [128, 1024])
        xv = W["xT"].ap.rearrange("k p t -> p k t")
        qsv = sc["QS"].ap.rearrange("n p t -> p n t")
        gsv = sc["GS"].ap.rearrange("n p t -> p n t")
        gtv = sc["GT"].ap.rearrange("n p t -> p n t")
        ppi = [0]

        def proj(pt, Wd):
            p = pp[ppi[0] % 5]
            ppi[0] += 1
            for kc in range(KD):
                S.mm(p[:, :Wd], Win[:, kc, pt * 128:(pt + 1) * 128], XN[:, kc, :Wd], start=(kc == 0), stop=(kc == KD - 1))
            return p

        for ti, (t0, Wd) in enumerate(P.tiles):
            col = 1 if ti == 0 else 0
            Xt = X[ti % 2]
            rp = rope[0]
            QSt, GSt, GTt = QSs[0], GSs[0], GTs[0]
            S.dma("sp", Xt[:, :, :Wd], V(xv[:, :, t0:t0 + Wd], W["xT"].res))
            S.dma("sp", rp[:, :, :Wd], V(W["rope"].ap[:, :, t0:t0 + Wd], W["rope"].res))
            norm_mod(P, S, Xt, XN, sqb, psn, rstd, tmpf, ones_bf, s1, MOD[:, 0:8, :], col, Wd)
            for which, (pt_a, pt_s, dst) in enumerate(((0, 4, 0), (2, 6, 6))):
                for j in range(2):
                    pa = proj(pt_a + j, Wd)
                    pb = proj(pt_s + j, Wd)
                    a = t1[j]
                    b = t2[j]
                    S.tt("dve", a[:, :Wd], pa[:, :Wd], rp[:, 2 * which, :Wd], ALU.mult)
                    S.tt("dve", b[:, :Wd], pb[:, :Wd], rp[:, 2 * which + 1, :Wd], ALU.mult)
                    S.tt("pool", QSt[:, dst + j, :Wd], a[:, :Wd], b[:, :Wd], ALU.add)
                    if which == 1:
                        S.copy("pool", QSt[:, 12 + j, :Wd], QSt[:, 6 + j, :Wd])
            for j in range(2):
                S.act(GTt[:, 0 + j, :Wd], proj(8 + j, Wd)[:, :Wd], AF.Silu)
                S.act(GTt[:, 2 + j, :Wd], proj(18 + j, Wd)[:, :Wd], AF.Silu)
                S.act(GTt[:, 4 + j, :Wd], proj(24 + j, Wd)[:, :Wd], AF.Silu)
                S.copy("dve", GTt[:, 6 + j, :Wd], proj(10 + j, Wd)[:, :Wd])
                S.copy("dve", QSt[:, 2 + j, :Wd], proj(12 + j, Wd)[:, :Wd])
                S.copy("act", QSt[:, 4 + j, :Wd], proj(20 + j, Wd)[:, :Wd])
                pk = proj(22 + j, Wd)
                S.ts("dve", QSt[:, 10 + j, :Wd], pk[:, :Wd], 32.0 ** -0.5, ALU.mult)
                S.copy("pool", QSt[:, 16 + j, :Wd], QSt[:, 10 + j, :Wd])
            for d in range(2):
                for j in range(2):
                    pz = proj(14 + 2 * d + j, Wd)
                    sg = sg_[j]
                    S.act(sg[:, :Wd], pz[:, :Wd], AF.Sigmoid)
                    f = t1[j]
                    S.ts("dve", f[:, :Wd], sg[:, :Wd], lbv[:, j, 0:1], ALU.mult, lbv[:, j, 1:2], ALU.add)
                    S.act(GSt[:, 4 * d + j, :Wd], f[:, :Wd], AF.Ln)
                    S.ts("dve", QSt[:, (8 if d == 0 else 14) + j, :Wd], sg[:, :Wd], lbv[:, j, 2:3], ALU.mult, lbv[:, j, 0:1], ALU.add)
            S.copy("dve", low[:, :Wd], proj(26, Wd)[:, :Wd])
            for d in range(2):
                for j in range(2):
                    p = pp[ppi[0] % 5]
                    ppi[0] += 1
                    S.mm(p[:, :Wd], wa2[32 * d:32 * d + 16, d, j * 128:(j + 1) * 128], low[32 * d:32 * d + 16, :Wd])
                    sg = sg_[j]
                    S.act(sg[:, :Wd], p[:, :Wd], AF.Sigmoid, bias=gba[:, d, j:j + 1])
                    S.act(sg[:, :Wd], sg[:, :Wd], AF.Ln)
                    S.ts("pool", GSt[:, 4 * d + 2 + j, :Wd], sg[:, :Wd], 1.0 / 16.0, ALU.mult)
            for sub in range(Wd // 128):
                for kc in range(KD):
                    S.mm(pv[:, 0:512], XN[:, kc, sub * 128:(sub + 1) * 128], Wv[:, kc, 0:512], start=(kc == 0), stop=(kc == KD - 1))
                for kc in range(KD):
                    S.mm(pv[:, 512:768], XN[:, kc, sub * 128:(sub + 1) * 128], Wv[:, kc, 512:768], start=(kc == 0), stop=(kc == KD - 1))
                vt = Vs[sub % 3]
                S.copy("act", vt.all(), pv[:, 0:768])
                S.dma("pool", V(sc["VS"].ap[t0 + sub * 128:t0 + (sub + 1) * 128, :], sc["VS"].res), vt.all())
            S.dma("pool", V(qsv[:, :, t0:t0 + Wd], sc["QS"].res), QSt[:, :, :Wd])
            S.dma("pool", V(gsv[:, :, t0:t0 + Wd], sc["GS"].res), GSt[:, :, :Wd])
            S.dma("pool", V(gtv[:, :, t0:t0 + Wd], sc["GT"].res), GTt[:, :, :Wd])
        S.barrier()
        P.flush()


def compute_lbv(P, st, W, l):
    S = P.S
    NL = P.NL
    lg = P.sb(st, "lbl", [128, 2, NL])
    ex = P.sb(st, "lbe", [128, 2, NL])
    mx = P.sb(st, "lbm", [128, 2])
    sm = P.sb(st, "lbs", [128, 2])
    pa = P.sb(st, "lbp", [128, 2])
    lbv = P.sb(st, "lbv", [128, 2, 3])
    S.dma("sp", lg.all(), W["hg_lb"].all())
    S.red(mx.all(), lg.all(), ALU.max)
    S.tt("dve", ex.all(), lg.all(), mx.all().re("p (j o) -> p j o", o=1).bc([128, 2, NL]), ALU.subtract)
    S.act(ex.all(), ex.all(), AF.Exp)
    S.red(sm.all(), ex.all(), ALU.add)
    S.recip(sm.all(), sm.all())
    if l == 0:
        S.memset("dve", pa.all(), 0.0)
    else:
        S.red(pa.all(), ex[:, :, 1:l + 1], ALU.add)
        S.tt("dve", pa.all(), pa.all(), sm.all(), ALU.mult)
        S.ts("dve", pa.all(), pa.all(), 0.0, ALU.max, 1.0 - 1e-6, ALU.min)
    S.copy("dve", lbv[:, :, 1], pa.all())
    S.ts("dve", lbv[:, :, 0], pa.all(), -1.0, ALU.mult, 1.0, ALU.add)
    S.ts("dve", lbv[:, :, 2], pa.all(), 1.0, ALU.mult, -1.0, ALU.add)
    return lbv


def fm_vec(v, n=None):
    v = np.asarray(v, np.float32)
    return np.ascontiguousarray(v.reshape(-1, 128).T)


def pad_gla(a):
    out = np.zeros(a.shape[:-1] + (256,), a.dtype)
    for h in range(4):
        out[..., h * 64:h * 64 + 32] = a[..., h * 32:(h + 1) * 32]
    return out


def rope_tables(TL, half):
    NT = CTX + TL
    r = np.arange(128)
    w = r % 64
    part = w // 32
    u = w % 32
    f = u % 16
    freqs = (10000.0 ** (-(f.astype(np.float64)) / 16.0))
    tg = half * TL + np.arange(TL)
    pos = np.where(part[:, None] == 0, (tg // 64)[None, :], (tg % 64)[None, :]).astype(np.float64)
    ang = pos.astype(np.float32).astype(np.float64) * freqs.astype(np.float32).astype(np.float64)[:, None]
    cos = np.cos(ang)
    sin = np.sin(ang) * np.where(u < 16, -1.0, 1.0)[:, None]
    tab = np.zeros((128, 4, NT), np.float32)
    tab[:, 0, :CTX] = 1.0
    tab[:, 2, :CTX] = 0.125
    tab[:, 0, CTX:] = cos
    tab[:, 1, CTX:] = sin
    tab[:, 2, CTX:] = 0.125 * cos
    tab[:, 3, CTX:] = 0.125 * sin
    return tab


def arrange_w_in(w_in):
    w_in = np.asarray(w_in, np.float32)
    c = np.arange(256)
    h, w = c // 64, c % 64
    perm = h * 64 + (w // 32) * 32 + ((w % 32) + 16) % 32
    lowt = np.zeros((D, 128), np.float32)
    lowt[:, 0:16] = w_in[:, 3328:3344]
    lowt[:, 32:48] = w_in[:, 3344:3360]
    parts = [w_in[:, 0:256], w_in[:, 256:512], w_in[:, 0:256][:, perm], w_in[:, 256:512][:, perm],
             w_in[:, 768:1024], w_in[:, 1024:1280], w_in[:, 1280:1536], w_in[:, 1536:1792], w_in[:, 1792:2048],
             w_in[:, 2304:2560], pad_gla(w_in[:, 2560:2688]), pad_gla(w_in[:, 2688:2816]), w_in[:, 3072:3328], lowt]
    arr = np.ascontiguousarray(np.concatenate(parts, axis=1))
    assert arr.shape == (D, NCOL)
    wv = np.ascontiguousarray(np.concatenate([w_in[:, 512:768], w_in[:, 2048:2304], w_in[:, 2816:3072]], axis=1))
    return arr, wv


def layer_consts(inp, l):
    d = {}
    d["ada_w"] = np.ascontiguousarray(inp["ada_w"][l])
    d["ada_b"] = fm_vec(inp["ada_b"][l])
    d["n1w"] = fm_vec(inp["norm1_w"][l])
    d["n2w"] = fm_vec(inp["norm2_w"][l])
    d["w_in_arr"], d["w_v"] = arrange_w_in(inp["w_in"][l])
    wa2p = np.zeros((128, 2, 256), np.float32)
    wa2p[0:16, 0, :] = pad_gla(np.asarray(inp["gla_wa2"][l][0]))
    wa2p[32:48, 1, :] = pad_gla(np.asarray(inp["gla_wa2"][l][1]))
    d["wa2p"] = wa2p
    ba = pad_gla(np.asarray(inp["gla_ba"][l]))
    d["gla_ba"] = np.ascontiguousarray(ba.reshape(2, 2, 128).transpose(2, 0, 1))
    d["hg_lb"] = np.ascontiguousarray(np.asarray(inp["hg_lb_logits"]).reshape(-1, 2, 128).transpose(2, 1, 0))
    d.update(consts_np())
    return d


def core_cond(inp, b):
    c = np.stack([fm_vec(inp["c"][b]), fm_vec(inp["c_ctx"])], axis=-1)
    return np.ascontiguousarray(c)


def build_test_A(TL, l, NL=4):
    P = Prog(TL, debug=True, NL=NL)
    W = {}
    NT = P.NT
    for name, shape, dt in [("xT", [KD, 128, NT], F32), ("cond", [128, KD, 2], F32), ("ada_w", [D, 6 * D], F32),
                            ("ada_b", [128, 48], F32), ("n1w", [128, KD], F32), ("w_in_arr", [D, NCOL], F32),
                            ("w_v", [D, 768], F32), ("wa2p", [128, 2, 256], F32), ("gla_ba", [128, 2, 2], F32),
                            ("hg_lb", [128, 2, P.NL], F32), ("rope", [128, 4, NT], F32), ("ones_bf", [128, 128], BF16)]:
        W[name] = P.din(name, shape, dt)
    sc = alloc_scratch(P)
    with ExitStack() as st:
        MOD = phase_mod(P, st, W, 16)
        lbv = compute_lbv(P, st, W, l)
        phase_A(P, W, MOD, sc, lbv)
    return P.nc


def attn_consts_np():
    lg = np.log1p(-(2.0 ** (-5.0 - np.arange(4, dtype=np.float32)))).astype(np.float32)
    bret = np.zeros((128, 2, 2, 128), np.float32)
    t = np.arange(128, dtype=np.float32)
    for d in range(2):
        lgd = lg if d == 0 else lg[::-1]
        for tile in range(2):
            for hh in range(2):
                h = tile * 2 + hh
                cnt = (t + 1) if d == 0 else (128 - t)
                bret[hh * 64:(hh + 1) * 64, d, tile, :] = (cnt * lgd[h])[None, :]
    j = np.arange(128)[:, None]
    i = np.arange(128)[None, :]
    masks = np.stack([(j <= i), (j >= i)], axis=1).astype(np.float32)
    return {"bret": bret, "masks": np.ascontiguousarray(masks)}


class AttnBufs:
    pass


def attn_alloc(P, st, W):
    S = P.S
    A = AttnBufs()
    A.ones = P.sb(st, "a_ones", [128, 128])
    S.memset("dve", A.ones.all(), 1.0)
    A.masks = P.sb(st, "a_masks", [128, 2, 128])
    S.dma("sp", A.masks.all(), W["masks"].all())
    A.identb = P.sb(st, "a_identb", [128, 128], BF16)
    S.dma("sp", A.identb.all(), W["ident_bf"].all())
    A.B = []
    bretv = W["bret"]
    for d in range(2):
        b = P.sb(st, f"a_B{d}", [128, 6, 128])
        S.dma("sp", b[:, 0:2, :], bretv[:, d, :, :])
        A.B.append(b)
    A.E = P.sb(st, "a_E", [128, 6, 128])
    A.D1 = P.sb(st, "a_D1", [128, 6, 128])
    A.Eq1 = P.sb(st, "a_Eq1", [128, 6, 64])
    A.Ek1 = P.sb(st, "a_Ek1", [128, 6, 128])
    A.Ek0 = P.sb(st, "a_Ek0", [128, 6, 64])
    A.qh = P.sb(st, "a_qh", [128, 6, 128], BF16)
    A.q1 = P.sb(st, "a_q1", [128, 6, 64], BF16)
    A.k1 = P.sb(st, "a_k1", [128, 6, 128], BF16)
    A.k0 = P.sb(st, "a_k0", [128, 6, 64], BF16)
    A.k1T = P.sb(st, "a_k1T", [128, 6, 128], BF16)
    A.Asb = [[P.sb(st, f"a_A{d}{m}", [128, 4, 128], BF16) for m in range(3)] for d in range(2)]
    for d in range(2):
        for m in range(3):
            S.memset("pool", A.Asb[d][m].all(), 0.0)
    A.S = P.sb(st, "a_S", [128, 6, 64])
    A.Sbf = P.sb(st, "a_Sbf", [128, 6, 64], BF16)
    A.tS = P.sb(st, "a_tS", [128, 6, 64])
    A.Q = [P.sb(st, f"a_Q{i}", [128, 6, 512], BF16) for i in range(2)]
    A.K = [P.sb(st, f"a_K{i}", [128, 6, 512], BF16) for i in range(2)]
    A.G = [P.sb(st, f"a_G{i}", [128, 4, 512]) for i in range(2)]
    A.Vt = [P.sb(st, f"a_V{i}", [128, 4, 768], BF16) for i in range(2)]
    A.Yo = [P.sb(st, f"a_Yo{i}", [128, 6, 512]) for i in range(2)]
    A.Yf = [P.sb(st, f"a_Yf{i}", [128, 6, 512]) for i in range(1)]
    A.psA = [P.ps(st, f"a_psA{m}", [128, 4, 128]) for m in range(3)]
    A.psO = P.ps(st, "a_psO", [128, 6, 128])
    A.psT = P.ps(st, "a_psT", [128, 6, 128], BF16)
    A.psS = P.ps(st, "a_psS", [128, 6, 64])
    return A


def attn_chunk(P, A, d, Q, K, G, Vt, c0, sub, full, Yo):
    S = P.S
    B = A.B[d]
    cs = slice(c0, c0 + 128)
    if d == 0:
        H0, H1, ref, last, last1 = slice(0, 64), slice(64, 128), 63, 127, 63
    else:
        H0, H1, ref, last, last1 = slice(64, 128), slice(0, 64), 64, 0, 0
    rv = (lambda v: v) if d == 0 else (lambda v: v[:, ::-1])
    for j in range(4):
        S.scan(rv(B[:, 2 + j, :]), rv(A.ones.all()), rv(G[:, j, cs]), 0.0)
    bref = B[:, :, ref:ref + 1]
    S.tt("dve", A.D1.all(), B.all(), bref.bc([128, 6, 128]), ALU.subtract)
    S.act(A.Eq1.all(), A.D1[:, :, H1], AF.Exp)
    S.ts("dve", A.D1.all(), A.D1.all(), -1.0, ALU.mult, 80.0, ALU.min)
    S.act(A.Ek1.all(), A.D1.all(), AF.Exp)
    S.tt("dve", A.k1.all(), K[:, :, cs], A.Ek1.all(), ALU.mult)
    if full:
        S.act(A.E.all(), B.all(), AF.Exp)
        S.ts("pool", A.Ek0.all(), B[:, :, H0], -1.0, ALU.mult, 80.0, ALU.min)
        S.act(A.Ek0.all(), A.Ek0.all(), AF.Exp)
        S.tt("dve", A.qh.all(), Q[:, :, cs], A.E.all(), ALU.mult)
        S.tt("pool", A.q1.all(), Q[:, :, cs][:, :, H1], A.Eq1.all(), ALU.mult)
        S.tt("pool", A.k0.all(), K[:, :, cs][:, :, H0], A.Ek0.all(), ALU.mult)
        mask = A.masks[:, d, :]
        for m in range(3):
            psA = A.psA[m]
            for h in range(4):
                tile = 2 * m + h // 2
                rows = slice((h % 2) * 64, (h % 2) * 64 + 64)
                S.mm(psA[H0, h, H0], A.k0[rows, tile, :], A.qh[rows, tile, H0])
                S.mm(psA[:, h, H1], A.k1[rows, tile, :], A.q1[rows, tile, :])
            Asb = A.Asb[d][m]
            S.tt("dve", Asb[H0, :, H0], psA[H0, :, H0], mask[H0, H0].re("p (o i) -> p o i", o=1).bc([64, 4, 64]), ALU.mult)
            S.tt("dve", Asb[:, :, H1], psA[:, :, H1], mask[:, H1].re("p (o i) -> p o i", o=1).bc([128, 4, 64]), ALU.mult)
            for h in range(4):
                tile = 2 * m + h // 2
                rows = slice((h % 2) * 64, (h % 2) * 64 + 64)
                vc = slice(m * 256 + h * 64, m * 256 + h * 64 + 64)
                S.mm(A.psO[rows, tile, :], Vt[:, sub, vc], Asb[:, h, :], start=True, stop=False)
                S.mm(A.psO[rows, tile, :], A.Sbf[rows, tile, :], A.qh[rows, tile, :], start=False, stop=True)
        S.copy("act", Yo[:, :, cs], A.psO.all())
    else:
        S.act(A.E[:, :, last:last + 1], B[:, :, last:last + 1], AF.Exp)
    for j in range(6):
        S.tr(A.psT[:, j, :], A.k1[:, j, :], A.identb.all())
    S.copy("act", A.k1T.all(), A.psT.all())
    for m in range(3):
        for h in range(4):
            tile = 2 * m + h // 2
            rows = slice((h % 2) * 64, (h % 2) * 64 + 64)
            vc = slice(m * 256 + h * 64, m * 256 + h * 64 + 64)
            S.mm(A.psS[rows, tile, :], A.k1T[:, tile, rows], Vt[:, sub, vc])
    e1 = A.E[:, :, last:last + 1].bc([128, 6, 64])
    e2 = A.Eq1[:, :, last1:last1 + 1].bc([128, 6, 64])
    S.tt("dve", A.tS.all(), A.psS.all(), e2, ALU.mult)
    S.tt("pool", A.S.all(), A.S.all(), e1, ALU.mult)
    S.tt("pool", A.S.all(), A.S.all(), A.tS.all(), ALU.add)
    S.copy("pool", A.Sbf.all(), A.S.all())


def attn_pass(P, A, sc, d, full, init_lat, out_ctx, out_fin, YA):
    S = P.S
    qsv = sc["QS"].ap.rearrange("n p t -> p n t")
    gsv = sc["GS"].ap.rearrange("n p t -> p n t")
    yav = YA.ap.rearrange("n p t -> p n t") if YA is not None else None
    S.memset("pool", A.S.all(), 0.0)
    S.memset("pool", A.Sbf.all(), 0.0)
    lat = P.tiles[1:]
    order = [P.tiles[0]] + (lat if d == 0 else lat[::-1])
    for n, (t0, Wd) in enumerate(order):
        Q, K, G, Vt, Yo = A.Q[n % 2], A.K[n % 2], A.G[n % 2], A.Vt[n % 2], A.Yo[n % 2]
        if full:
            S.dma("sp", Q[:, :, :Wd], V(qsv[:, 0:6, t0:t0 + Wd], sc["QS"].res))
        ko = 6 if d == 0 else 12
        S.dma("sp", K[:, :, :Wd], V(qsv[:, ko:ko + 6, t0:t0 + Wd], sc["QS"].res))
        S.dma("sp", G[:, :, :Wd], V(gsv[:, 4 * d:4 * d + 4, t0:t0 + Wd], sc["GS"].res))
        S.dma("sp", Vt[:, :Wd // 128, :], V(sc["VS"].ap[t0:t0 + Wd, :].rearrange("(s p) c -> p s c", p=128), sc["VS"].res))
        if full and d == 1:
            S.dma("sp", A.Yf[0][:, :, :Wd], V(yav[:, :, t0:t0 + Wd], YA.res))
        nch = Wd // 128
        for ci in (range(nch) if d == 0 else range(nch - 1, -1, -1)):
            attn_chunk(P, A, d, Q, K, G, Vt, ci * 128, ci, full, Yo)
        if full:
            if d == 1:
                S.tt("dve", Yo[:, :, :Wd], Yo[:, :, :Wd], A.Yf[0][:, :, :Wd], ALU.add)
            S.dma("pool", V(yav[:, :, t0:t0 + Wd], YA.res), Yo[:, :, :Wd])
        if n == 0:
            if out_ctx is not None:
                S.dma("pool", out_ctx, A.S.all())
            if init_lat is not None:
                S.dma("sp", A.S.all(), init_lat)
                S.copy("pool", A.Sbf.all(), A.S.all())
    if out_fin is not None:
        S.dma("pool", out_fin, A.S.all())


def build_test_B(TL, l, NL=4, full=True):
    P = Prog(TL, debug=True, NL=NL)
    W = {}
    NT = P.NT
    for name, shape, dt in [("xT", [KD, 128, NT], F32), ("cond", [128, KD, 2], F32), ("ada_w", [D, 6 * D], F32),
                            ("ada_b", [128, 48], F32), ("n1w", [128, KD], F32), ("w_in_arr", [D, NCOL], F32),
                            ("w_v", [D, 768], F32), ("wa2p", [128, 2, 256], F32), ("gla_ba", [128, 2, 2], F32),
                            ("hg_lb", [128, 2, P.NL], F32), ("rope", [128, 4, NT], F32), ("ones_bf", [128, 128], BF16),
                            ("ident_bf", [128, 128], BF16), ("bret", [128, 2, 2, 128], F32), ("masks", [128, 2, 128], F32),
                            ("init_att", [2, 128, 6, 64], F32)]:
        W[name] = P.din(name, shape, dt)
    sc = alloc_scratch(P)
    YA = P.dscr("YA", [6, 128, NT], F32)
    st_out = P.dout("st_out", [4, 128, 6, 64], F32)
    with ExitStack() as st:
        MOD = phase_mod(P, st, W, 16)
        lbv = compute_lbv(P, st, W, l)
        phase_A(P, W, MOD, sc, lbv)
        with ExitStack() as st2:
            A = attn_alloc(P, st2, W)
            for d in range(2):
                attn_pass(P, A, sc, d, full, V(W["init_att"].ap[d], W["init_att"].res) if full else None,
                          V(st_out.ap[d], st_out.res), V(st_out.ap[2 + d], st_out.res), YA if full else None)
            P.S.barrier()
            P.flush()
    return P.nc


def s5_layout_np(inp, l):
    d = {}
    G, Pn, Cg = 16, 64, 16

    def sm(a):
        return np.ascontiguousarray(np.asarray(a, np.float32).reshape(2, 8, 128).transpose(2, 0, 1))

    d["s5_lre"] = sm(inp["s5_lam_re"][l])
    d["s5_lim"] = sm(inp["s5_lam_im"][l])
    dt = np.repeat(np.asarray(inp["s5_log_dt"][l], np.float32)[:, :, None], Pn, axis=2)
    d["s5_ldt"] = sm(dt)
    BT = np.zeros((2, 2, 8, 128, 128), np.float32)
    CT = np.zeros((2, 2, 8, 128, 128), np.float32)
    for dd in range(2):
        for ri, (bn, cn) in enumerate((("s5_b_re", "s5_c_re"), ("s5_b_im", "s5_c_im"))):
            Bm = np.asarray(inp[bn][l][dd], np.float32)
            Cm = np.asarray(inp[cn][l][dd], np.float32)
            for k in range(8):
                for gg in range(2):
                    g = 2 * k + gg
                    r0 = (g % 8) * 16
                    BT[dd, ri, k, r0:r0 + 16, gg * 64:(gg + 1) * 64] = Bm[g].T
                    CT[dd, ri, k, gg * 64:(gg + 1) * 64, r0:r0 + 16] = Cm[g].T
    d["s5_BT"] = np.ascontiguousarray(BT.transpose(3, 0, 1, 2, 4).reshape(128, 32, 128))
    d["s5_CT"] = np.ascontiguousarray(CT.transpose(3, 0, 1, 2, 4).reshape(128, 32, 128))
    d["s5_d"] = fm_vec(inp["s5_d"][l])
    d["glu_w"] = np.ascontiguousarray(inp["s5_glu_w"][l])
    d["glu_b"] = fm_vec(inp["s5_glu_b"][l])
    return d


class S5Bufs:
    pass


def cmul(S, ek, o_re, o_im, a_re, a_im, b_re, b_im, t1, t2):
    S.tt(ek, t1, a_re, b_re, ALU.mult)
    S.tt(ek, t2, a_im, b_im, ALU.mult)
    S.tt(ek, o_im, a_re, b_im, ALU.mult)
    S.tt(ek, o_re, t1, t2, ALU.subtract)
    S.tt(ek, t1, a_im, b_re, ALU.mult)
    S.tt(ek, o_im, o_im, t1, ALU.add)


def s5_alloc(P, st, W):
    S = P.S
    B = S5Bufs()
    B.BT = P.sb(st, "s5BT", [128, 32, 128], BF16)
    B.CT = P.sb(st, "s5CT", [128, 32, 128], BF16)
    with ExitStack() as st2:
        stg = P.sb(st2, "s5stg", [128, 32, 128])
        S.dma("sp", stg.all(), W["s5_BT"].all())
        S.copy("dve", B.BT.all(), stg.all())
        stg2 = P.sb(st2, "s5stg2", [128, 32, 128])
        S.dma("sp", stg2.all(), W["s5_CT"].all())
        S.copy("act", B.CT.all(), stg2.all())
        S.barrier()
        P.flush()
    B.lre = P.sb(st, "s5lre", [128, 2, 8])
    B.lim = P.sb(st, "s5lim", [128, 2, 8])
    B.ldt = P.sb(st, "s5ldt", [128, 2, 8])
    S.dma("sp", B.lre.all(), W["s5_lre"].all())
    S.dma("sp", B.lim.all(), W["s5_lim"].all())
    S.dma("sp", B.ldt.all(), W["s5_ldt"].all())
    B.dt = P.sb(st, "s5dt", [128, 2, 8])
    S.act(B.dt.all(), B.ldt.all(), AF.Exp)
    B.r = P.sb(st, "s5r", [128, 2, 8])
    B.u = P.sb(st, "s5u", [128, 2, 2, 8])
    B.coef = P.sb(st, "s5coef", [128, 2, 2, 8])
    B.uW = P.sb(st, "s5uW", [128, 2, 2, 8])
    tmp = [P.sb(st, f"s5tmp{i}", [128, 2, 8]) for i in range(6)]
    th = tmp[0]
    S.tt("dve", th.all(), B.lim.all(), B.dt.all(), ALU.mult)
    S.tt("dve", tmp[1].all(), B.lre.all(), B.dt.all(), ALU.mult)
    S.act(B.r.all(), tmp[1].all(), AF.Exp)
    x, x2, pa, pb_ = tmp[2], tmp[3], tmp[4], tmp[5]
    ure, uim = B.u[:, :, 0, :], B.u[:, :, 1, :]
    S.ts("dve", x.all(), th.all(), 1.0 / 64, ALU.mult)
    S.tt("dve", x2.all(), x.all(), x.all(), ALU.mult)
    S.ts("dve", pa.all(), x2.all(), -1.0 / 5040, ALU.mult)
    S.stt(pa.all(), pa.all(), 1.0 / 120, x2.all(), ALU.add, ALU.mult)
    S.stt(pa.all(), pa.all(), -1.0 / 6, x2.all(), ALU.add, ALU.mult)
    S.stt(uim, pa.all(), 1.0, x.all(), ALU.add, ALU.mult)
    S.ts("dve", pb_.all(), x2.all(), 1.0 / 40320, ALU.mult)
    S.stt(pb_.all(), pb_.all(), -1.0 / 720, x2.all(), ALU.add, ALU.mult)
    S.stt(pb_.all(), pb_.all(), 1.0 / 24, x2.all(), ALU.add, ALU.mult)
    S.stt(pb_.all(), pb_.all(), -0.5, x2.all(), ALU.add, ALU.mult)
    S.ts("dve", ure, pb_.all(), 1.0, ALU.add)
    for _ in range(6):
        S.tt("dve", pa.all(), ure, ure, ALU.mult)
        S.tt("dve", pb_.all(), uim, uim, ALU.mult)
        S.stt(x.all(), ure, 2.0, uim, ALU.mult, ALU.mult)
        S.tt("dve", ure, pa.all(), pb_.all(), ALU.subtract)
        S.copy("dve", uim, x.all())
    S.tt("dve", pa.all(), ure, ure, ALU.mult)
    S.tt("dve", pb_.all(), uim, uim, ALU.mult)
    S.tt("dve", pa.all(), pa.all(), pb_.all(), ALU.add)
    S.act(pa.all(), pa.all(), AF.Sqrt)
    S.recip(pa.all(), pa.all())
    S.tt("dve", ure, ure, pa.all(), ALU.mult)
    S.tt("dve", uim, uim, pa.all(), ALU.mult)
    are, aim = tmp[0], tmp[1]
    S.tt("dve", are.all(), B.u[:, :, 0, :], B.r.all(), ALU.mult)
    S.tt("dve", aim.all(), B.u[:, :, 1, :], B.r.all(), ALU.mult)
    S.ts("dve", are.all(), are.all(), -1.0, ALU.add)
    den = tmp[2]
    S.tt("dve", den.all(), B.lre.all(), B.lre.all(), ALU.mult)
    S.tt("dve", tmp[3].all(), B.lim.all(), B.lim.all(), ALU.mult)
    S.tt("dve", den.all(), den.all(), tmp[3].all(), ALU.add)
    S.recip(den.all(), den.all())
    nl = tmp[3]
    S.ts("dve", nl.all(), B.lim.all(), -1.0, ALU.mult)
    cmul(S, "dve", B.coef[:, :, 0, :], B.coef[:, :, 1, :], are.all(), aim.all(), B.lre.all(), nl.all(), tmp[4].all(), tmp[5].all())
    S.tt("dve", B.coef[:, :, 0, :], B.coef[:, :, 0, :], den.all(), ALU.mult)
    S.tt("dve", B.coef[:, :, 1, :], B.coef[:, :, 1, :], den.all(), ALU.mult)
    B.POST = P.sb(st, "s5POST", [128, 8, 2, 512])
    B.PRE = P.sb(st, "s5PRE", [128, 8, 2, 512])
    B.tt1 = P.sb(st, "s5tt1", [128, 8, 256])
    B.tt2 = P.sb(st, "s5tt2", [128, 8, 256])
    B.G = P.sb(st, "s5G", [128, 8, 2, 512])
    B.T1 = P.sb(st, "s5T1", [128, 2, 512])
    B.T2 = P.sb(st, "s5T2", [128, 2, 512])
    B.Hb = [P.sb(st, f"s5Hb{i}", [128, 2, 512], BF16) for i in range(2)]
    B.U = [P.sb(st, f"s5U{i}", [128, 2, 512], BF16) for i in range(2)]
    B.Wt = [P.sb(st, f"s5Wt{i}", [128, 2, 512]) for i in range(2)]
    B.Yo = [P.sb(st, f"s5Yo{i}", [128, 2, 512]) for i in range(2)]
    B.Yf = P.sb(st, "s5Yf", [128, 2, 512])
    B.h = P.sb(st, "s5h", [128, 2, 8])
    B.gi = P.sb(st, "s5gi", [128, 2, 8])
    B.sm = [P.sb(st, f"s5sm{i}", [128, 8]) for i in range(3)]
    B.pb = [P.ps(st, f"s5pb{i}", [128, 2, 512]) for i in range(2)]
    B.py = [P.ps(st, f"s5py{i}", [128, 512]) for i in range(2)]
    return B


def s5_tables(P, B, d):
    S = P.S
    S.memset("dve", B.POST[:, :, 0, 0:1], 1.0)
    S.memset("dve", B.POST[:, :, 1, 0:1], 0.0)
    S.copy("dve", B.uW[:, d, :, :], B.u[:, d, :, :])
    n = 1
    while n < 512:
        wre = B.uW[:, d, 0, :].re("p (k o) -> p k o", o=1).bc([128, 8, n])
        wim = B.uW[:, d, 1, :].re("p (k o) -> p k o", o=1).bc([128, 8, n])
        cmul(S, "dve", B.POST[:, :, 0, n:2 * n], B.POST[:, :, 1, n:2 * n], B.POST[:, :, 0, 0:n], B.POST[:, :, 1, 0:n],
             wre, wim, B.tt1[:, :, 0:n], B.tt2[:, :, 0:n])
        n *= 2
        if n < 512:
            cmul(S, "dve", B.sm[0].all(), B.sm[1].all(), B.uW[:, d, 0, :], B.uW[:, d, 1, :], B.uW[:, d, 0, :], B.uW[:, d, 1, :], B.sm[2].all(), B.gi[:, 0, :])
            S.copy("dve", B.uW[:, d, 0, :], B.sm[0].all())
            S.copy("dve", B.uW[:, d, 1, :], B.sm[1].all())
    for hlf in range(2):
        sl = slice(hlf * 256, (hlf + 1) * 256)
        cre = B.coef[:, d, 0, :].re("p (k o) -> p k o", o=1).bc([128, 8, 256])
        cim = B.coef[:, d, 1, :].re("p (k o) -> p k o", o=1).bc([128, 8, 256])
        S.tt("dve", B.tt1.all(), B.POST[:, :, 0, sl], cre, ALU.mult)
        S.tt("dve", B.tt2.all(), B.POST[:, :, 1, sl], cim, ALU.mult)
        S.tt("dve", B.PRE[:, :, 0, sl], B.tt1.all(), B.tt2.all(), ALU.add)
        S.tt("dve", B.tt1.all(), B.POST[:, :, 0, sl], cim, ALU.mult)
        S.tt("dve", B.tt2.all(), B.POST[:, :, 1, sl], cre, ALU.mult)
        S.tt("dve", B.PRE[:, :, 1, sl], B.tt1.all(), B.tt2.all(), ALU.subtract)


def s5_pass(P, B, sc, d, full, init_lat, out_ctx, out_fin, YS):
    S = P.S
    gtv = sc["GT"].ap.rearrange("n p t -> p n t")
    ysv = YS.ap.rearrange("n p t -> p n t") if YS is not None else None
    s5_tables(P, B, d)
    S.memset("dve", B.h.all(), 0.0)
    lat = P.tiles[1:]
    order = [P.tiles[0]] + (lat if d == 0 else lat[::-1])
    rv = (lambda v: v) if d == 0 else (lambda v: v[:, ::-1])
    rv3 = (lambda v: v) if d == 0 else (lambda v: v[:, :, ::-1])
    for n, (t0, Wd) in enumerate(order):
        U, Yo = B.U[n % 2], B.Yo[n % 2]
        S.dma("sp", U[:, :, :Wd], V(gtv[:, 6:8, t0:t0 + Wd], sc["GT"].res))
        if full and d == 1:
            S.dma("sp", B.Yf[:, :, :Wd], V(ysv[:, :, t0:t0 + Wd], YS.res))
        cmul(S, "dve", B.gi[:, 0, :], B.gi[:, 1, :], B.u[:, d, 0, :], B.u[:, d, 1, :], B.h[:, 0, :], B.h[:, 1, :], B.sm[0].all(), B.sm[1].all())
        for k in range(8):
            pb = B.pb[k % 2]
            for ri in range(2):
                S.mm(pb[:, ri, :Wd], B.BT[:, (d * 2 + ri) * 8 + k, :], U[:, k // 4, :Wd])
            pre_re = rv3(B.PRE[:, k, 0:1, :Wd]).bc([128, 2, Wd])
            pre_im = rv3(B.PRE[:, k, 1:2, :Wd]).bc([128, 2, Wd])
            Wt = B.Wt[k % 2]
            S.tt("dve", B.T1[:, :, :Wd], pb[:, :, :Wd], pre_re, ALU.mult)
            S.tt("dve", B.T2[:, :, :Wd], pb[:, ::-1, :Wd], pre_im, ALU.mult)
            S.tt("pool", Wt[:, 0, :Wd], B.T1[:, 0, :Wd], B.T2[:, 0, :Wd], ALU.subtract)
            S.tt("pool", Wt[:, 1, :Wd], B.T1[:, 1, :Wd], B.T2[:, 1, :Wd], ALU.add)
            rbc = B.r[:, d, k:k + 1].bc([128, Wd])
            for ri in range(2):
                S.scan(rv(B.G[:, k, ri, :Wd]), rbc, rv(Wt[:, ri, :Wd]), B.gi[:, ri, k:k + 1])
            if full:
                post_re = rv3(B.POST[:, k, 0:1, :Wd]).bc([128, 2, Wd])
                post_im = rv3(B.POST[:, k, 1:2, :Wd]).bc([128, 2, Wd])
                Hb = B.Hb[k % 2]
                S.tt("dve", B.T1[:, :, :Wd], B.G[:, k, :, :Wd], post_re, ALU.mult)
                S.tt("dve", B.T2[:, :, :Wd], B.G[:, k, ::-1, :Wd], post_im, ALU.mult)
                S.tt("pool", Hb[:, 0, :Wd], B.T1[:, 0, :Wd], B.T2[:, 0, :Wd], ALU.subtract)
                S.stt(Hb[:, 1, :Wd], B.T1[:, 1, :Wd], -1.0, B.T2[:, 1, :Wd], ALU.mult, ALU.subtract)
                o = k // 4
                for ri in range(2):
                    S.mm(B.py[o][:, :Wd], B.CT[:, (d * 2 + ri) * 8 + k, :], Hb[:, ri, :Wd],
                         start=(k % 4 == 0 and ri == 0), stop=(k % 4 == 3 and ri == 1))
        mi = Wd - 1 if d == 0 else 0
        cmul(S, "dve", B.h[:, 0, :], B.h[:, 1, :], B.POST[:, :, 0, Wd - 1], B.POST[:, :, 1, Wd - 1],
             B.G[:, :, 0, mi], B.G[:, :, 1, mi], B.sm[0].all(), B.sm[1].all())
        if full:
            for o in range(2):
                if d == 1:
                    S.tt("dve", Yo[:, o, :Wd], B.py[o][:, :Wd], B.Yf[:, o, :Wd], ALU.add)
                else:
                    S.copy("act", Yo[:, o, :Wd], B.py[o][:, :Wd])
            S.dma("pool", V(ysv[:, :, t0:t0 + Wd], YS.res), Yo[:, :, :Wd])
        if n == 0:
            if out_ctx is not None:
                S.dma("pool", out_ctx, B.h.all())
            if init_lat is not None:
                S.dma("sp", B.h.all(), init_lat)
    if out_fin is not None:
        S.dma("pool", out_fin, B.h.all())


W_A = [("xT", lambda P: [KD, 128, P.NT], F32), ("cond", lambda P: [128, KD, 2], F32), ("ada_w", lambda P: [D, 6 * D], F32),
       ("ada_b", lambda P: [128, 48], F32), ("n1w", lambda P: [128, KD], F32), ("w_in_arr", lambda P: [D, NCOL], F32),
       ("w_v", lambda P: [D, 768], F32), ("wa2p", lambda P: [128, 2, 256], F32), ("gla_ba", lambda P: [128, 2, 2], F32),
       ("hg_lb", lambda P: [128, 2, P.NL], F32), ("rope", lambda P: [128, 4, P.NT], F32), ("ones_bf", lambda P: [128, 128], BF16),
       ("ident_bf", lambda P: [128, 128], BF16), ("bret", lambda P: [128, 2, 2, 128], F32), ("masks", lambda P: [128, 2, 128], F32),
       ("s5_lre", lambda P: [128, 2, 8], F32), ("s5_lim", lambda P: [128, 2, 8], F32), ("s5_ldt", lambda P: [128, 2, 8], F32),
       ("s5_BT", lambda P: [128, 32, 128], F32), ("s5_CT", lambda P: [128, 32, 128], F32)]


def declare(P, specs):
    W = {}
    for name, shp, dt in specs:
        W[name] = P.din(name, shp(P), dt)
    return W


def build_test_S(TL, l, NL=4, full=True):
    P = Prog(TL, debug=True, NL=NL)
    W = declare(P, W_A + [("init_s5", lambda P: [2, 128, 2, 8], F32)])
    NT = P.NT
    sc = alloc_scratch(P)
    YS = P.dscr("YS", [2, 128, NT], F32)
    st_out = P.dout("st_out", [4, 128, 2, 8], F32)
    with ExitStack() as st:
        MOD = phase_mod(P, st, W, 16)
        lbv = compute_lbv(P, st, W, l)
        phase_A(P, W, MOD, sc, lbv)
        with ExitStack() as st2:
            B = s5_alloc(P, st2, W)
            for d in range(2):
                s5_pass(P, B, sc, d, full, V(W["init_s5"].ap[d], W["init_s5"].res) if full else None,
                        V(st_out.ap[d], st_out.res), V(st_out.ap[2 + d], st_out.res), YS if full else None)
            P.S.barrier()
            P.flush()
    return P.nc


def norm_mod2(P, S, X, XN, XNf, sqb, psn, rstd, tmpf, ones_bf, s_vec, b_vec, col, Wd):
    S.act(sqb[:, :, :Wd], X[:, :, :Wd], AF.Square)
    for kc in range(KD):
        S.mm(psn[:, :Wd], ones_bf.all(), sqb[:, kc, :Wd], start=(kc == 0), stop=(kc == KD - 1))
    S.ts("dve", rstd[:, :Wd], psn[:, :Wd], 1.0 / D, ALU.mult, 1e-6, ALU.add)
    S.act(rstd[:, :Wd], rstd[:, :Wd], AF.Sqrt)
    S.recip(rstd[:, :Wd], rstd[:, :Wd])
    for kc in range(KD):
        S.stt(tmpf[:, kc, :Wd], X[:, kc, :Wd], s_vec[:, kc, col:col + 1], rstd[:, :Wd], ALU.mult, ALU.mult)
        S.act(XNf[:, kc, :Wd], tmpf[:, kc, :Wd], AF.Identity, bias=b_vec[:, kc, col:col + 1])
    S.copy("pool", XN[:, :, :Wd], XNf[:, :, :Wd])


W_C = [("w_out", lambda P: [D, D], F32), ("hn_w", lambda P: [128, 6], F32), ("s5_d", lambda P: [128, 2], F32),
       ("glu_w", lambda P: [256, 256], F32), ("glu_b", lambda P: [128, 2], F32), ("n2w", lambda P: [128, KD], F32),
       ("blockmean", lambda P: [128, 128], BF16), ("ident_f", lambda P: [128, 128], F32)]
W_MOE = [("router_w", lambda P: [D, 8], F32), ("router_b", lambda P: [1, 8], F32), ("selE", lambda P: [8, 8, 128], BF16)]


def phase_C(P, W, MOD, sc, YA, YS, xT2, XN2, GTS, moe, do_ctx):
    S = P.S
    with ExitStack() as st:
        Wout = P.sb(st, "Wout", [128, KD, D], BF16)
        gluw = P.sb(st, "gluw", [128, 2, 256], BF16)
        Bm = P.sb(st, "Bm", [128, 128], BF16)
        ones_bf = P.sb(st, "onesC", [128, 128], BF16)
        hnw = P.sb(st, "hnw", [128, 6])
        s5d = P.sb(st, "s5d", [128, 2])
        glub = P.sb(st, "glub", [128, 2])
        n2w = P.sb(st, "n2w", [128, KD])
        s2 = P.sb(st, "s2", [128, KD, 2])
        S.dma("sp", Bm.all(), W["blockmean"].all())
        S.dma("sp", ones_bf.all(), W["ones_bf"].all())
        S.dma("sp", hnw.all(), W["hn_w"].all())
        S.dma("sp", s5d.all(), W["s5_d"].all())
        S.dma("sp", glub.all(), W["glu_b"].all())
        S.dma("sp", n2w.all(), W["n2w"].all())
        S.ts("dve", s2.all(), MOD[:, 32:40, :], 1.0, ALU.add)
        S.tt("dve", s2.all(), s2.all(), n2w.all().re("p (k o) -> p k o", o=1).bc([128, KD, 2]), ALU.mult)
        if moe:
            rw = P.sb(st, "rw", [128, KD, 8])
            rb = P.sb(st, "rb", [128, 8])
            identf = P.sb(st, "identf", [128, 128])
            S.dma("sp", rw.all(), V(W["router_w"].ap.rearrange("(kc p) n -> p kc n", p=128), W["router_w"].res))
            S.dma("sp", rb.all(), V(W["router_b"].ap.partition_broadcast(128), W["router_b"].res), allow_slow_non_contiguous=True)
            S.dma("sp", identf.all(), W["ident_f"].all())
        with ExitStack() as st2:
            stg = P.sb(st2, "wostg", [128, KD, D])
            S.dma("sp", stg.all(), V(W["w_out"].ap.rearrange("(kc p) n -> p kc n", p=128), W["w_out"].res))
            S.copy("dve", Wout[:, 0:4, :], stg[:, 0:4, :])
            S.copy("act", Wout[:, 4:8, :], stg[:, 4:8, :])
            stg2 = P.sb(st2, "glstg", [128, 2, 256])
            S.dma("sp", stg2.all(), V(W["glu_w"].ap.rearrange("(kc p) n -> p kc n", p=128), W["glu_w"].res))
            S.copy("pool", gluw.all(), stg2.all())
            S.barrier()
            P.flush()
        X = [P.sb(st, f"cX{i}", [128, KD, 512]) for i in range(2)]
        YAt = [P.sb(st, f"cYA{i}", [128, 6, 512]) for i in range(2)]
        YSt = [P.sb(st, f"cYS{i}", [128, 2, 512]) for i in range(2)]
        GTt = [P.sb(st, f"cGT{i}", [128, 8, 512], BF16) for i in range(2)]
        Y = P.sb(st, "cY", [128, KD, 512], BF16)
        ybf = P.sb(st, "cybf", [128, 512], BF16)
        sqh = P.sb(st, "csqh", [128, 512], BF16)
        yc = P.sb(st, "cyc", [128, 512])
        rs = P.sb(st, "crs", [128, 512])
        o_ = P.sb(st, "co", [128, 512])
        zz = [P.sb(st, f"cz{i}", [128, 512]) for i in range(2)]
        zb = P.sb(st, "czb", [128, 2, 512], BF16)
        ta = P.sb(st, "cta", [128, 512])
        tb = P.sb(st, "ctb", [128, 512])
        sqb = P.sb(st, "csqb", [128, KD, 512], BF16)
        tmpf = P.sb(st, "ctmpf", [128, KD, 512])
        XNf = P.sb(st, "cXNf", [128, KD, 512])
        XN = P.sb(st, "cXN", [128, KD, 512], BF16)
        rstd = P.sb(st, "crstd", [128, 512])
        pp = [P.ps(st, f"cpp{i}", [128, 512]) for i in range(3)]
        po = [P.ps(st, f"cpo{i}", [128, 512]) for i in range(2)]
        psn = P.ps(st, "cpsn", [128, 512])
        if moe:
            pr = P.ps(st, "cpr", [128, 8])
            ptr = P.ps(st, "cptr", [8, 128])
            lg = P.sb(st, "clg", [128, 8])
            l2 = P.sb(st, "cl2", [128, 8])
            ee = P.sb(st, "cee", [128, 8])
            sm = [P.sb(st, f"csm{i}", [128, 1]) for i in range(4)]
            gT = P.sb(st, "cgT", [8, 512], BF16)
        ppi = [0]

        def nextpp():
            p = pp[ppi[0] % 3]
            ppi[0] += 1
            return p

        xv = W["xT"].ap.rearrange("k p t -> p k t")
        x2v = xT2.ap.rearrange("k p t -> p k t")
        xnv = XN2.ap.rearrange("k p t -> p k t")
        yav = YA.ap.rearrange("n p t -> p n t")
        ysv = YS.ap.rearrange("n p t -> p n t")
        gtv = sc["GT"].ap.rearrange("n p t -> p n t")
        tiles = P.tiles if do_ctx else P.tiles[1:]
        for n, (t0, Wd) in enumerate(tiles):
            col = 1 if t0 == 0 else 0
            Xt, ya, ys, gt = X[n % 2], YAt[n % 2], YSt[n % 2], GTt[n % 2]
            S.dma("sp", Xt[:, :, :Wd], V(xv[:, :, t0:t0 + Wd], W["xT"].res))
            S.dma("sp", ya[:, :, :Wd], V(yav[:, :, t0:t0 + Wd], YA.res))
            S.dma("sp", ys[:, :, :Wd], V(ysv[:, :, t0:t0 + Wd], YS.res))
            S.dma("sp", gt[:, :, :Wd], V(gtv[:, :, t0:t0 + Wd], sc["GT"].res))
            for j in range(6):
                m = j // 2
                kc = (0, 4, 6)[m] + (j % 2)
                y = ya[:, j, :Wd]
                if m == 0:
                    S.copy("act", ybf[:, :Wd], y)
                    pm = nextpp()
                    S.mm(pm[:, :Wd], Bm.all(), ybf[:, :Wd])
                    S.tt("dve", yc[:, :Wd], y, pm[:, :Wd], ALU.subtract)
                    y = yc[:, :Wd]
                S.act(sqh[:, :Wd], y, AF.Square)
                pv = nextpp()
                S.mm(pv[:, :Wd], Bm.all(), sqh[:, :Wd])
                S.ts("dve", rs[:, :Wd], pv[:, :Wd], 1e-6, ALU.add)
                S.act(rs[:, :Wd], rs[:, :Wd], AF.Sqrt)
                S.recip(rs[:, :Wd], rs[:, :Wd])
                S.stt(o_[:, :Wd], y, hnw[:, j:j + 1], rs[:, :Wd], ALU.mult, ALU.mult)
                S.tt("pool", Y[:, kc, :Wd], o_[:, :Wd], gt[:, j, :Wd], ALU.mult)
            for j in range(2):
                z = zz[j]
                S.stt(z[:, :Wd], gt[:, 6 + j, :Wd], s5d[:, j:j + 1], ys[:, j, :Wd], ALU.mult, ALU.add)
                S.act(ta[:, :Wd], z[:, :Wd], AF.Square)
                S.ts("dve", ta[:, :Wd], ta[:, :Wd], 0.044715, ALU.mult, 1.0, ALU.add)
                S.tt("dve", ta[:, :Wd], ta[:, :Wd], z[:, :Wd], ALU.mult)
                S.act(tb[:, :Wd], ta[:, :Wd], AF.Sigmoid, scale=1.5957691216057308)
                S.tt("dve", z[:, :Wd], z[:, :Wd], tb[:, :Wd], ALU.mult)
                S.copy("pool", zb[:, j, :Wd], z[:, :Wd])
            for j in range(2):
                pg = nextpp()
                for kk in range(2):
                    S.mm(pg[:, :Wd], gluw[:, kk, j * 128:(j + 1) * 128], zb[:, kk, :Wd], start=(kk == 0), stop=(kk == 1))
                S.act(tb[:, :Wd], pg[:, :Wd], AF.Sigmoid, bias=glub[:, j:j + 1])
                S.tt("dve", Y[:, 2 + j, :Wd], zz[j][:, :Wd], tb[:, :Wd], ALU.mult)
            for dt in range(KD):
                p = po[dt % 2]
                for kc in range(KD):
                    S.mm(p[:, :Wd], Wout[:, kc, dt * 128:(dt + 1) * 128], Y[:, kc, :Wd], start=(kc == 0), stop=(kc == KD - 1))
                S.stt(Xt[:, dt, :Wd], p[:, :Wd], MOD[:, 16 + dt, col:col + 1], Xt[:, dt, :Wd], ALU.mult, ALU.add)
            S.dma("pool", V(x2v[:, :, t0:t0 + Wd], xT2.k(t0, slice(None)).res), Xt[:, :, :Wd])
            norm_mod2(P, S, Xt, XN, XNf, sqb, psn, rstd, tmpf, ones_bf, s2, MOD[:, 24:32, :], col, Wd)
            S.dma("pool", V(xnv[:, :, t0:t0 + Wd], XN2.res), XN[:, :, :Wd])
            if moe:
                for sub in range(Wd // 128):
                    ss = slice(sub * 128, (sub + 1) * 128)
                    for kc in range(KD):
                        S.mm(pr.all(), XNf[:, kc, ss], rw[:, kc, :], start=(kc == 0), stop=(kc == KD - 1))
                    S.tt("dve", lg.all(), pr.all(), rb.all(), ALU.add)
                    S.red(sm[0].all(), lg.all(), ALU.max)
                    S.ts("dve", l2.all(), lg.all(), sm[0].all(), ALU.is_equal)
                    S.stt(l2.all(), l2.all(), -1e30, lg.all(), ALU.mult, ALU.add)
                    S.red(sm[1].all(), l2.all(), ALU.max)
                    S.ts("dve", l2.all(), lg.all(), sm[1].all(), ALU.is_ge)
                    S.ts("dve", sm[2].all(), sm[0].all(), -1.0, ALU.mult)
                    S.act(ee.all(), lg.all(), AF.Exp, bias=sm[2].all())
                    S.tt("dve", ee.all(), ee.all(), l2.all(), ALU.mult)
                    S.red(sm[3].all(), ee.all(), ALU.add)
                    S.recip(sm[3].all(), sm[3].all())
                    S.ts("dve", ee.all(), ee.all(), sm[3].all(), ALU.mult)
                    S.tr(ptr.all(), ee.all(), identf.all())
                    S.copy("act", gT[:, ss], ptr.all())
                S.dma("pool", V(GTS.ap[:, t0:t0 + Wd], GTS.res), gT[:, :Wd])
        S.barrier()
        P.flush()


def phase_D(P, W, MOD, xT2, XN2, GTS, moe, do_ctx):
    S = P.S
    nE = 8 if moe else 1
    with ExitStack() as st:
        w1 = P.sb(st, "fw1", [128, KD, HFF], BF16)
        w3 = P.sb(st, "fw3", [128, KD, HFF], BF16)
        w2 = P.sb(st, "fw2", [128, NFT, D], BF16)
        stg = [P.sb(st, f"fstg{i}", [128, 2816]) for i in range(2)]
        XNt = [P.sb(st, f"fXN{i}", [128, KD, 512], BF16) for i in range(2)]
        Xa = [P.sb(st, f"fXa{i}", [128, KD, 512]) for i in range(2)]
        hT = P.sb(st, "fhT", [128, NFT, 512], BF16)
        sl = [P.sb(st, f"fsl{i}", [128, 512]) for i in range(2)]
        tg = [P.sb(st, f"ftg{i}", [128, 512]) for i in range(2)]
        p1 = [P.ps(st, f"fp1_{i}", [128, 512]) for i in range(2)]
        p3 = [P.ps(st, f"fp3_{i}", [128, 512]) for i in range(2)]
        po = [P.ps(st, f"fpo{i}", [128, 512]) for i in range(2)]
        if moe:
            gTs = P.sb(st, "fgTs", [8, P.NT], BF16)
            selE = P.sb(st, "fselE", [8, 8, 128], BF16)
            gateB = P.sb(st, "fgateB", [128, 512])
            pg = P.ps(st, "fpg", [128, 512])
            S.dma("sp", gTs.all(), GTS.all())
            S.dma("sp", selE.all(), W["selE"].all())
        x2v = xT2.ap.rearrange("k p t -> p k t")
        xnv = XN2.ap.rearrange("k p t -> p k t")
        tiles = P.tiles if do_ctx else P.tiles[1:]
        cast_eng = ("dve", "act", "pool")
        ci = 0
        qi = 0
        for e in range(nE):
            for hh in range(2):
                hs = slice(hh * HFF, (hh + 1) * HFF)
                if moe:
                    a1, a3, a2 = W["ffn_w1"].ap[e], W["ffn_w3"].ap[e], W["ffn_w2"].ap[e]
                else:
                    a1, a3, a2 = W["ffn_w1"].ap, W["ffn_w3"].ap, W["ffn_w2"].ap
                for (src, dst, rw_) in ((a1, w1, W["ffn_w1"]), (a3, w3, W["ffn_w3"])):
                    sv = src[:, hs].rearrange("(kc p) n -> p kc n", p=128)
                    for pc in range(4):
                        sg = stg[qi % 2]
                        qi += 1
                        cs = slice(pc * 352, (pc + 1) * 352)
                        S.dma("sp" if qi % 2 == 0 else "act", sg.all().re("p (k n) -> p k n", k=KD), V(sv[:, :, cs], rw_.res))
                        S.copy(cast_eng[ci % 3], dst[:, :, cs], sg.all().re("p (k n) -> p k n", k=KD))
                        ci += 1
                sv = a2[hs, :].rearrange("(ft p) n -> p ft n", p=128)
                for pc in range(4):
                    sg = stg[qi % 2]
                    qi += 1
                    cs = slice(pc * 256, (pc + 1) * 256)
                    S.dma("sp" if qi % 2 == 0 else "act", sg.all().re("p (k n) -> p k n", k=NFT), V(sv[:, :, cs], W["ffn_w2"].res))
                    S.copy(cast_eng[ci % 3], w2[:, :, cs], sg.all().re("p (k n) -> p k n", k=NFT))
                    ci += 1
                for n, (t0, Wd) in enumerate(tiles):
                    col = 1 if t0 == 0 else 0
                    xn, xa = XNt[n % 2], Xa[n % 2]
                    S.dma("sp", xn[:, :, :Wd], V(xnv[:, :, t0:t0 + Wd], XN2.res))
                    S.dma("sp", xa[:, :, :Wd], V(x2v[:, :, t0:t0 + Wd], xT2.k(t0, slice(None)).res))
                    if moe:
                        S.mm(pg[:, :Wd], selE[:, e, :], gTs[:, t0:t0 + Wd])
                        S.copy("act", gateB[:, :Wd], pg[:, :Wd])
                    for ft in range(NFT):
                        a, b = p1[ft % 2], p3[ft % 2]
                        fs = slice(ft * 128, (ft + 1) * 128)
                        for kc in range(KD):
                            S.mm(a[:, :Wd], w1[:, kc, fs], xn[:, kc, :Wd], start=(kc == 0), stop=(kc == KD - 1))
                        for kc in range(KD):
                            S.mm(b[:, :Wd], w3[:, kc, fs], xn[:, kc, :Wd], start=(kc == 0), stop=(kc == KD - 1))
                        s_ = sl[ft % 2]
                        S.act(s_[:, :Wd], a[:, :Wd], AF.Silu)
                        S.tt("dve", hT[:, ft, :Wd], s_[:, :Wd], b[:, :Wd], ALU.mult)
                    for dt in range(KD):
                        p = po[dt % 2]
                        for ft in range(NFT):
                            S.mm(p[:, :Wd], w2[:, ft, dt * 128:(dt + 1) * 128], hT[:, ft, :Wd], start=(ft == 0), stop=(ft == NFT - 1))
                        if moe:
                            t_ = tg[dt % 2]
                            S.tt("dve", t_[:, :Wd], p[:, :Wd], gateB[:, :Wd], ALU.mult)
                            S.stt(xa[:, dt, :Wd], t_[:, :Wd], MOD[:, 40 + dt, col:col + 1], xa[:, dt, :Wd], ALU.mult, ALU.add)
                        else:
                            S.stt(xa[:, dt, :Wd], p[:, :Wd], MOD[:, 40 + dt, col:col + 1], xa[:, dt, :Wd], ALU.mult, ALU.add)
                    S.dma("pool", V(x2v[:, :, t0:t0 + Wd], xT2.k(t0, slice(None)).res), xa[:, :, :Wd])
        S.barrier()
        P.flush()


def phase_final(P, W, xT2, yT):
    S = P.S
    with ExitStack() as st:
        ones_bf = P.sb(st, "zones", [128, 128], BF16)
        fws = P.sb(st, "zfw", [128, KD, 1])
        zb = P.sb(st, "zzb", [128, KD, 1])
        S.dma("sp", ones_bf.all(), W["ones_bf"].all())
        S.dma("sp", fws.all(), W["fw"].all())
        S.memset("dve", zb.all(), 0.0)
        X = [P.sb(st, f"zX{i}", [128, KD, 512]) for i in range(2)]
        XN = [P.sb(st, f"zXN{i}", [128, KD, 512]) for i in range(2)]
        sqb = P.sb(st, "zsqb", [128, KD, 512], BF16)
        tmpf = P.sb(st, "ztmpf", [128, KD, 512])
        rstd = P.sb(st, "zrstd", [128, 512])
        psn = P.ps(st, "zpsn", [128, 512])
        xv = xT2.ap.rearrange("k p t -> p k t")
        yv = yT.ap.rearrange("k p t -> p k t")
        for i, (t0, Wd) in enumerate(P.tiles[1:]):
            S.dma("sp", X[i % 2].all(), V(xv[:, :, t0:t0 + Wd], xT2.k(t0, slice(None)).res))
            norm_mod(P, S, X[i % 2], XN[i % 2], sqb, psn, rstd, tmpf, ones_bf, fws, zb, 0, 512)
            S.dma("pool", V(yv[:, :, t0 - CTX:t0 - CTX + Wd], yT.res), XN[i % 2].all())
        S.barrier()
        P.flush()


W_ST = [("init_att", lambda P: [2, 128, 6, 64], F32), ("init_s5", lambda P: [2, 128, 2, 8], F32)]


def build_La(TL, l, NL=4):
    P = Prog(TL, NL=NL)
    W = declare(P, W_A)
    sc = alloc_scratch(P)
    st_att = P.dout("st_att", [4, 128, 6, 64], F32)
    st_s5 = P.dout("st_s5", [4, 128, 2, 8], F32)
    with ExitStack() as st:
        MOD = phase_mod(P, st, W, 16)
        lbv = compute_lbv(P, st, W, l)
        phase_A(P, W, MOD, sc, lbv)
        with ExitStack() as st2:
            A = attn_alloc(P, st2, W)
            for d in range(2):
                attn_pass(P, A, sc, d, False, None, V(st_att.ap[d], st_att.res), V(st_att.ap[2 + d], st_att.res), None)
            P.S.barrier()
            P.flush()
        with ExitStack() as st2:
            B = s5_alloc(P, st2, W)
            for d in range(2):
                s5_pass(P, B, sc, d, False, None, V(st_s5.ap[d], st_s5.res), V(st_s5.ap[2 + d], st_s5.res), None)
            P.S.barrier()
            P.flush()
    return P.nc


def build_Lb(TL, l, moe, last, NL=4):
    P = Prog(TL, NL=NL)
    NT = P.NT
    specs = W_A + W_ST + W_C
    if moe:
        specs = specs + W_MOE + [("ffn_w1", lambda P: [8, D, DFF], F32), ("ffn_w3", lambda P: [8, D, DFF], F32), ("ffn_w2", lambda P: [8, DFF, D], F32)]
    else:
        specs = specs + [("ffn_w1", lambda P: [D, DFF], F32), ("ffn_w3", lambda P: [D, DFF], F32), ("ffn_w2", lambda P: [DFF, D], F32)]
    if last:
        specs = specs + [("fw", lambda P: [128, KD, 1], F32)]
    W = declare(P, specs)
    sc = alloc_scratch(P)
    YA = P.dscr("YA", [6, 128, NT], F32)
    YS = P.dscr("YS", [2, 128, NT], F32)
    XN2 = P.dscr("XN2", [KD, 128, NT], BF16)
    GTS = P.dscr("GTS", [8, NT], BF16)
    if last:
        xT2 = P.dscr("xT2", [KD, 128, NT], F32)
        yT = P.dout("yT", [KD, 128, P.TL], F32)
    else:
        xT2 = P.dout("xT2", [KD, 128, NT], F32)
    with ExitStack() as st:
        MOD = phase_mod(P, st, W, 48)
        lbv = compute_lbv(P, st, W, l)
        phase_A(P, W, MOD, sc, lbv)
        with ExitStack() as st2:
            A = attn_alloc(P, st2, W)
            for d in range(2):
                attn_pass(P, A, sc, d, True, V(W["init_att"].ap[d], W["init_att"].res), None, None, YA)
            P.S.barrier()
            P.flush()
        with ExitStack() as st2:
            B = s5_alloc(P, st2, W)
            for d in range(2):
                s5_pass(P, B, sc, d, True, V(W["init_s5"].ap[d], W["init_s5"].res), None, None, YS)
            P.S.barrier()
            P.flush()
        phase_C(P, W, MOD, sc, YA, YS, xT2, XN2, GTS, moe, not last)
        phase_D(P, W, MOD, xT2, XN2, GTS, moe, not last)
        if last:
            phase_final(P, W, xT2, yT)
    return P.nc


def consts_all():
    c = consts_np()
    c.update(attn_consts_np())
    bm = np.zeros((128, 128), np.float32)
    bm[:64, :64] = 1.0 / 64
    bm[64:, 64:] = 1.0 / 64
    c["blockmean"] = bm.astype(ml_dtypes.bfloat16)
    sel = np.zeros((8, 8, 128), np.float32)
    for e in range(8):
        sel[e, e, :] = 1.0
    c["selE"] = sel.astype(ml_dtypes.bfloat16)
    return c


def layer_inputs(inp, l, depth):
    d = layer_consts(inp, l)
    d.update(s5_layout_np(inp, l))
    d.update(consts_all())
    d["w_out"] = np.ascontiguousarray(inp["w_out"][l])
    d["hn_w"] = np.ascontiguousarray(np.concatenate([fm_vec(inp["ret_gn_w"][l]), fm_vec(inp["hg_norm_w"][l]), fm_vec(inp["gla_norm_w"][l])], axis=1))
    j = l // 2
    if l % 2 == 0:
        d["ffn_w1"] = np.ascontiguousarray(inp["ffn_w1"][j])
        d["ffn_w3"] = np.ascontiguousarray(inp["ffn_w3"][j])
        d["ffn_w2"] = np.ascontiguousarray(inp["ffn_w2"][j])
    else:
        d["ffn_w1"] = np.ascontiguousarray(inp["moe_w1"][j])
        d["ffn_w3"] = np.ascontiguousarray(inp["moe_w3"][j])
        d["ffn_w2"] = np.ascontiguousarray(inp["moe_w2"][j])
        d["router_w"] = np.ascontiguousarray(inp["router_w"][j])
        d["router_b"] = np.ascontiguousarray(np.asarray(inp["router_b"][j]).reshape(1, 8))
    d["fw"] = np.ascontiguousarray(fm_vec(inp["final_norm_w"]).reshape(128, KD, 1))
    return d


def names_of(specs):
    return [s[0] for s in specs]


def kernel(**inputs):
    inp = {k: np.asarray(v) for k, v in inputs.items()}
    x = inp["x"]
    B, SEQ, _ = x.shape
    depth = inp["ada_w"].shape[0]
    TL = SEQ // 2
    ncore = 2 * B
    cores = [(b, s) for b in range(B) for s in range(2)]
    xT = [to_fm(np.concatenate([inp["ctx"][b], x[b, s * TL:(s + 1) * TL]], axis=0)) for (b, s) in cores]
    ropes = [rope_tables(TL, s) for s in range(2)]
    conds = [core_cond(inp, b) for b in range(B)]
    out = np.empty_like(x)
    for l in range(depth):
        moe = (l % 2 == 1)
        last = (l == depth - 1)
        li = layer_inputs(inp, l, depth)
        nca = build_La(TL, l, NL=depth)
        maps = []
        for ci, (b, s) in enumerate(cores):
            m = {k: li[k] for k in names_of(W_A) if k in li}
            m["xT"] = xT[ci]
            m["cond"] = conds[b]
            m["rope"] = ropes[s]
            maps.append(m)
        ra = run_bass_kernel_spmd(nca, maps, core_ids=list(range(ncore))).results
        nb_specs = W_A + W_ST + W_C + (W_MOE if moe else [])
        ncb = build_Lb(TL, l, moe, last, NL=depth)
        maps = []
        for ci, (b, s) in enumerate(cores):
            m = {k: li[k] for k in names_of(nb_specs) if k in li}
            for k in ("ffn_w1", "ffn_w3", "ffn_w2"):
                m[k] = li[k]
            if last:
                m["fw"] = li["fw"]
            m["xT"] = xT[ci]
            m["cond"] = conds[b]
            m["rope"] = ropes[s]
            pa = ra[ci ^ 1]
            own = ra[ci]
            if s == 0:
                m["init_att"] = np.stack([np.asarray(own["st_att"])[0], np.asarray(pa["st_att"])[3]])
                m["init_s5"] = np.stack([np.asarray(own["st_s5"])[0], np.asarray(pa["st_s5"])[3]])
            else:
                m["init_att"] = np.stack([np.asarray(pa["st_att"])[2], np.asarray(own["st_att"])[1]])
                m["init_s5"] = np.stack([np.asarray(pa["st_s5"])[2], np.asarray(own["st_s5"])[1]])
            maps.append(m)
        rb = run_bass_kernel_spmd(ncb, maps, core_ids=list(range(ncore))).results
        if last:
            for ci, (b, s) in enumerate(cores):
                out[b, s * TL:(s + 1) * TL] = from_fm(np.asarray(rb[ci]["yT"]))
        else:
            xT = [np.asarray(rb[ci]["xT2"]) for ci in range(ncore)]
    return out
```

```python
import numpy as np
import ml_dtypes
import concourse.bass as bass
import concourse.mybir as mybir
from concourse.bass_utils import run_bass_kernel_spmd

F32 = mybir.dt.float32
BF16 = mybir.dt.bfloat16
AF = mybir.ActivationFunctionType
ALU = mybir.AluOpType
AX = mybir.AxisListType

D = 1024
KD = 8
CTX = 256
L = 128
NPT = 27
NCOL = NPT * 128
DFF = 2816
HFF = 1408
NFT = 11


class Res:
    __slots__ = ("w", "r")

    def __init__(self):
        self.w = None
        self.r = {}


class V:
    __slots__ = ("ap", "res")

    def __init__(self, ap, res):
        self.ap = ap
        self.res = res

    def __getitem__(self, idx):
        return V(self.ap[idx], self.res)

    def bc(self, shape):
        return V(self.ap.broadcast_to(shape), self.res)

    def re(self, pat, **kw):
        return V(self.ap.rearrange(pat, **kw), self.res)


class T:
    def __init__(self, ap):
        self.ap = ap
        self.res = Res()
        self.keyed = {}

    def __getitem__(self, idx):
        return V(self.ap[idx], self.res)

    def k(self, key, idx):
        r = self.keyed.get(key)
        if r is None:
            r = self.keyed[key] = Res()
        return V(self.ap[idx], r)

    def all(self):
        return V(self.ap, self.res)


class Eng:
    def __init__(self, key, sem):
        self.key = key
        self.sem = sem
        self.cnt = 0
        self.seen = {}
        self.ops = []


class Sched:
    def __init__(self, nc, stack, ndma=12):
        self.nc = nc
        self.stack = stack
        self.eng = {}
        for k in ("pe", "act", "dve", "pool", "sp"):
            self.eng[k] = Eng(k, stack.enter_context(nc.semaphore("s_" + k)))
        self.dsem = {}
        self.dq = {}
        for q in ("sp", "pool", "act"):
            self.dq[q] = 0
            for i in range(ndma):
                self.dsem[(q, i)] = [stack.enter_context(nc.semaphore(f"d_{q}_{i}")), 0]
        self.ndma = ndma
        self.n_inst = 0

    def semof(self, key):
        if isinstance(key, tuple):
            return self.dsem[key][0]
        return self.eng[key].sem

    def _deps(self, e, reads, writes):
        deps = {}

        def add(k, c):
            if deps.get(k, 0) < c:
                deps[k] = c

        for v in reads:
            if v.res.w is not None:
                add(*v.res.w)
        for v in writes:
            if v.res.w is not None:
                add(*v.res.w)
            for k, c in v.res.r.items():
                add(k, c)
        for k, c in deps.items():
            if k == "pe" and e.key == "pe":
                continue
            if e.seen.get(k, 0) < c:
                e.ops.append(("w", self.semof(k), c))
                e.seen[k] = c
                self.n_inst += 1

    def emit(self, ek, fn, reads, writes):
        e = self.eng[ek]
        self._deps(e, reads, writes)
        e.cnt += 1
        e.ops.append(("o", fn))
        self.n_inst += 1
        for v in writes:
            v.res.w = (ek, e.cnt)
            v.res.r = {}
        for v in reads:
            v.res.r[ek] = e.cnt

    def dma(self, q, out, in_, **kw):
        e = self.eng[q]
        self._deps(e, [in_], [out])
        i = self.dq[q]
        self.dq[q] = (i + 1) % self.ndma
        key = (q, i)
        ds = self.dsem[key]
        if e.seen.get(key, 0) < ds[1]:
            e.ops.append(("w", ds[0], ds[1]))
            e.seen[key] = ds[1]
        ds[1] += 16
        e.ops.append(("d", out.ap, in_.ap, ds[0], kw))
        self.n_inst += 1
        out.res.w = (key, ds[1])
        out.res.r = {}
        in_.res.r[key] = ds[1]

    def barrier(self):
        for e in self.eng.values():
            for o in self.eng.values():
                if o.key != e.key and o.cnt > 0 and e.seen.get(o.key, 0) < o.cnt:
                    e.ops.append(("w", o.sem, o.cnt))
                    e.seen[o.key] = o.cnt
            for key, ds in self.dsem.items():
                if ds[1] > 0 and e.seen.get(key, 0) < ds[1]:
                    e.ops.append(("w", ds[0], ds[1]))
                    e.seen[key] = ds[1]

    def finish(self):
        self.barrier()
        nc = self.nc
        with nc.Block() as block:
            def run(e, h):
                for op in e.ops:
                    if op[0] == "w":
                        h.wait_ge(op[1], op[2])
                    elif op[0] == "o":
                        op[1](h).then_inc(e.sem, 1)
                    else:
                        h.dma_start(out=op[1], in_=op[2], **op[4]).then_inc(op[3], 16)

            @block.tensor
            def _(h):
                run(self.eng["pe"], h)

            @block.scalar
            def _(h):
                run(self.eng["act"], h)

            @block.vector
            def _(h):
                run(self.eng["dve"], h)

            @block.gpsimd
            def _(h):
                run(self.eng["pool"], h)

            @block.sync
            def _(h):
                run(self.eng["sp"], h)

    def mm(self, out, lhsT, rhs, start=True, stop=True):
        self.emit("pe", lambda h: h.matmul(out.ap, lhsT.ap, rhs.ap, start=start, stop=stop),
                  [lhsT, rhs] + ([] if start else [out]), [out])

    def tr(self, out, in_, ident):
        self.emit("pe", lambda h: h.transpose(out.ap, in_.ap, ident.ap), [in_, ident], [out])

    def act(self, out, in_, func, bias=None, scale=None, accum=None):
        reads = [in_]
        kw = {}
        if bias is not None:
            if isinstance(bias, V):
                reads.append(bias)
                kw["bias"] = bias.ap
            else:
                kw["bias"] = float(bias)
        if scale is not None:
            if isinstance(scale, V):
                reads.append(scale)
                kw["scale"] = scale.ap
            else:
                kw["scale"] = float(scale)
        writes = [out]
        if accum is not None:
            kw["accum_out"] = accum.ap
            writes.append(accum)
        self.emit("act", lambda h: h.activation(out.ap, in_.ap, func, **kw), reads, writes)

    def ts(self, ek, out, in0, s1, op0, s2=None, op1=None):
        reads = [in0]
        a1 = s1
        a2 = s2
        if isinstance(s1, V):
            reads.append(s1)
            a1 = s1.ap
        if isinstance(s2, V):
            reads.append(s2)
            a2 = s2.ap
        if op1 is None:
            self.emit(ek, lambda h: h.tensor_scalar(out.ap, in0.ap, a1, None, op0), reads, [out])
        else:
            self.emit(ek, lambda h: h.tensor_scalar(out.ap, in0.ap, a1, a2, op0, op1), reads, [out])

    def tt(self, ek, out, a, b, op):
        self.emit(ek, lambda h: h.tensor_tensor(out.ap, a.ap, b.ap, op), [a, b], [out])

    def stt(self, out, in0, sc, in1, op0, op1):
        reads = [in0, in1]
        a = sc
        if isinstance(sc, V):
            reads.append(sc)
            a = sc.ap
        self.emit("dve", lambda h: h.scalar_tensor_tensor(out.ap, in0.ap, a, in1.ap, op0, op1), reads, [out])

    def scan(self, out, d0, d1, init, op0=ALU.mult, op1=ALU.add):
        reads = [d0, d1]
        a = init
        if isinstance(init, V):
            reads.append(init)
            a = init.ap
        self.emit("dve", lambda h: h.tensor_tensor_scan(out.ap, d0.ap, d1.ap, a, op0, op1), reads, [out])

    def copy(self, ek, out, in_):
        if ek == "act":
            self.emit("act", lambda h: h.copy(out.ap, in_.ap), [in_], [out])
        else:
            self.emit(ek, lambda h: h.tensor_copy(out.ap, in_.ap), [in_], [out])

    def memset(self, ek, out, val):
        self.emit(ek, lambda h: h.memset(out.ap, val), [], [out])

    def recip(self, out, in_):
        self.emit("dve", lambda h: h.reciprocal(out.ap, in_.ap), [in_], [out])

    def red(self, out, in_, op):
        self.emit("dve", lambda h: h.tensor_reduce(out.ap, in_.ap, AX.X, op), [in_], [out])


from contextlib import ExitStack


class Prog:
    def __init__(self, TL, debug=False, NL=4):
        self.NL = NL
        self.nc = bass.Bass("TRN2", target_bir_lowering=False)
        self.stack = ExitStack()
        self.S = Sched(self.nc, self.stack)
        self.TL = TL
        self.NT = CTX + TL
        self.tiles = [(0, CTX)] + [(CTX + 512 * i, 512) for i in range(TL // 512)]
        self.debug = debug
        self.scr_kind = "ExternalOutput" if debug else "Internal"

    def din(self, name, shape, dt=F32):
        return T(self.nc.dram_tensor(name, list(shape), dt, kind="ExternalInput").ap())

    def dout(self, name, shape, dt=F32):
        return T(self.nc.dram_tensor(name, list(shape), dt, kind="ExternalOutput").ap())

    def dscr(self, name, shape, dt=F32):
        return T(self.nc.dram_tensor(name, list(shape), dt, kind=self.scr_kind).ap())

    def sb(self, st, name, shape, dt=F32):
        self._uid = getattr(self, "_uid", 0) + 1
        t = st.enter_context(self.nc.sbuf_tensor(f"sb{self._uid}_{name}", list(shape), dt))
        return T(t[tuple(slice(None) for _ in shape)])

    def ps(self, st, name, shape, dt=F32):
        self._uid = getattr(self, "_uid", 0) + 1
        t = st.enter_context(self.nc.psum_tensor(f"ps{self._uid}_{name}", list(shape), dt))
        return T(t[tuple(slice(None) for _ in shape)])

    def flush(self):
        self.S.finish()
        for e in self.S.eng.values():
            e.ops = []


def load_cast(P, st_name, dst, src_ap_fn, npieces, piece_shape, stg, eng_cycle=("dve", "pool")):
    raise NotImplementedError


def phase_mod(P, st, W, nj):
    S = P.S
    cond = P.sb(st, "cond", [128, KD, 2])
    csl = P.sb(st, "csl", [128, KD, 2])
    adab = P.sb(st, "adab", [128, 48])
    MOD = P.sb(st, "MOD", [128, 48, 2])
    S.dma("sp", cond.all(), W["cond"].all())
    S.dma("sp", adab.all(), W["ada_b"].all())
    S.act(csl.all(), cond.all(), AF.Silu)
    with ExitStack() as st2:
        pm = P.ps(st2, "pm", [128, 48, 2])
        stg = [P.sb(st2, f"adaw{i}", [128, KD, 768]) for i in range(2)]
        aw = W["ada_w"].ap.rearrange("(kc p) n -> p kc n", p=128)
        for pc in range((nj + 5) // 6):
            sg = stg[pc % 2]
            S.dma("sp" if pc % 2 == 0 else "pool", sg.all(), V(aw[:, :, pc * 768:(pc + 1) * 768], W["ada_w"].res))
            for jj in range(6):
                j = pc * 6 + jj
                if j >= nj:
                    break
                for kc in range(KD):
                    S.mm(pm[:, j, :], sg[:, kc, jj * 128:(jj + 1) * 128], csl[:, kc, :], start=(kc == 0), stop=(kc == KD - 1))
        S.tt("dve", MOD[:, 0:nj, :], pm[:, 0:nj, :], adab[:, 0:nj].re("p (j o) -> p j o", o=1).bc([128, nj, 2]), ALU.add)
        S.barrier()
        P.flush()
    return MOD


def norm_mod(P, S, X, XN, sqb, psn, rstd, tmpf, ones_bf, s_vec, b_vec, col, Wd):
    S.act(sqb[:, :, :Wd], X[:, :, :Wd], AF.Square)
    for kc in range(KD):
        S.mm(psn[:, :Wd], ones_bf.all(), sqb[:, kc, :Wd], start=(kc == 0), stop=(kc == KD - 1))
    S.ts("dve", rstd[:, :Wd], psn[:, :Wd], 1.0 / D, ALU.mult, 1e-6, ALU.add)
    S.act(rstd[:, :Wd], rstd[:, :Wd], AF.Sqrt)
    S.recip(rstd[:, :Wd], rstd[:, :Wd])
    for kc in range(KD):
        S.stt(tmpf[:, kc, :Wd], X[:, kc, :Wd], s_vec[:, kc, col:col + 1], rstd[:, :Wd], ALU.mult, ALU.mult)
        S.act(XN[:, kc, :Wd], tmpf[:, kc, :Wd], AF.Identity, bias=b_vec[:, kc, col:col + 1])


def consts_np():
    c = {}
    c["ones_bf"] = np.ones((128, 128), ml_dtypes.bfloat16)
    c["ident_f"] = np.eye(128, dtype=np.float32)
    c["ident_bf"] = np.eye(128).astype(ml_dtypes.bfloat16)
    return c


def to_fm(a):
    T_ = a.shape[0]
    return np.ascontiguousarray(a.T.reshape(KD, 128, T_))


def from_fm(a):
    return np.ascontiguousarray(a.reshape(D, -1).T)


def alloc_scratch(P):
    NT = P.NT
    sc = {}
    sc["QS"] = P.dscr("QS", [18, 128, NT], BF16)
    sc["GS"] = P.dscr("GS", [8, 128, NT], F32)
    sc["GT"] = P.dscr("GT", [8, 128, NT], BF16)
    sc["VS"] = P.dscr("VS", [NT, 768], BF16)
    return sc


def phase_A(P, W, MOD, sc, lbv, so=False):
    S = P.S
    with ExitStack() as st:
        Win = P.sb(st, "Win", [128, KD, NCOL], BF16)
        Wv = P.sb(st, "Wv", [128, KD, 768], BF16)
        wa2 = P.sb(st, "wa2", [128, 2, 256], BF16)
        wa2f = P.sb(st, "wa2f", [128, 2, 256])
        gba = P.sb(st, "gba", [128, 2, 2])
        n1w = P.sb(st, "n1w", [128, KD])
        s1 = P.sb(st, "s1", [128, KD, 2])
        ones_bf = P.sb(st, "ones", [128, 128], BF16)
        S.dma("sp", ones_bf.all(), W["ones_bf"].all())
        S.dma("sp", wa2f.all(), W["wa2p"].all())
        S.dma("sp", gba.all(), W["gla_ba"].all())
        S.dma("sp", n1w.all(), W["n1w"].all())
        S.copy("pool", wa2.all(), wa2f.all())
        S.ts("dve", s1.all(), MOD[:, 8:16, :], 1.0, ALU.add)
        S.tt("dve", s1.all(), s1.all(), n1w.all().re("p (k o) -> p k o", o=1).bc([128, KD, 2]), ALU.mult)
        win_v = W["w_in_arr"].ap.rearrange("(kc p) n -> p kc n", p=128)
        wv_v = W["w_v"].ap.rearrange("(kc p) n -> p kc n", p=128)
        with ExitStack() as st2:
            stg = [P.sb(st2, f"wstg{i}", [128, KD, 432]) for i in range(2)]
            for pc in range(8):
                sg = stg[pc % 2]
                S.dma("sp" if pc % 2 == 0 else "pool", sg.all(), V(win_v[:, :, pc * 432:(pc + 1) * 432], W["w_in_arr"].res))
                S.copy("dve" if pc % 2 == 0 else "act", Win[:, :, pc * 432:(pc + 1) * 432], sg.all())
            for pc in range(2):
                sg = stg[pc % 2]
                S.dma("sp", sg[:, :, 0:384], V(wv_v[:, :, pc * 384:(pc + 1) * 384], W["w_v"].res))
                S.copy("dve", Wv[:, :, pc * 384:(pc + 1) * 384], sg[:, :, 0:384])
            S.barrier()
            P.flush()
        X = [P.sb(st, f"X{i}", [128, KD, 512]) for i in range(2)]
        XN = P.sb(st, "XN", [128, KD, 512], BF16)
        sqb = P.sb(st, "sqb", [128, KD, 512], BF16)
        tmpf = P.sb(st, "tmpf", [128, KD, 512])
        rstd = P.sb(st, "rstd", [128, 512])
        rope = [P.sb(st, f"rope{i}", [128, 4, 512]) for i in range(1)]
        QSs = [P.sb(st, f"QSs{i}", [128, 18, 512], BF16) for i in range(1)]
        GSs = [P.sb(st, f"GSs{i}", [128, 8, 512]) for i in range(1)]
        GTs = [P.sb(st, f"GTs{i}", [128, 8, 512], BF16) for i in range(1)]
        Vs = [P.sb(st, f"Vs{i}", [128, 768], BF16) for i in range(3)]
        low = P.sb(st, "low", [128, 512], BF16)
        t1 = [P.sb(st, f"t1_{i}", [128, 512]) for i in range(2)]
        t2 = [P.sb(st, f"t2_{i}", [128, 512]) for i in range(2)]
        sg_ = [P.sb(st, f"sg_{i}", [128, 512]) for i in range(2)]
        psn = P.ps(st, "psn", [128, 512])
        pp = [P.ps(st, f"pp{i}", [128, 512]) for i in range(5)]
        pv = P.ps(st, "pv", [128, 1024])
        xv = W["xT"].ap.rearrange("k p t -> p k t")
        qsv = sc["QS"].ap.rearrange("n p t -> p n t")
        gsv = sc["GS"].ap.rearrange("n p t -> p n t")
        gtv = sc["GT"].ap.rearrange("n p t -> p n t")
        ppi = [0]

        def proj(pt, Wd):
            p = pp[ppi[0] % 5]
            ppi[0] += 1
            for kc in range(KD):
                S.mm(p[:, :Wd], Win[:, kc, pt * 128:(pt + 1) * 128], XN[:, kc, :Wd], start=(kc == 0), stop=(kc == KD - 1))
            return p

        for ti, (t0, Wd) in enumerate(P.tiles):
            col = 1 if ti == 0 else 0
            Xt = X[ti % 2]
            rp = rope[0]
            QSt, GSt, GTt = QSs[0], GSs[0], GTs[0]
            S.dma("sp", Xt[:, :, :Wd], V(xv[:, :, t0:t0 + Wd], W["xT"].res))
            S.dma("sp", rp[:, :, :Wd], V(W["rope"].ap[:, :, t0:t0 + Wd], W["rope"].res))
            norm_mod(P, S, Xt, XN, sqb, psn, rstd, tmpf, ones_bf, s1, MOD[:, 0:8, :], col, Wd)
            for which, (pt_a, pt_s, dst) in enumerate(((0, 4, 0), (2, 6, 6))):
                if so and which == 0:
                    continue
                for j in range(2):
                    pa = proj(pt_a + j, Wd)
                    pb = proj(pt_s + j, Wd)
                    a = t1[j]
                    b = t2[j]
                    S.tt("dve", a[:, :Wd], pa[:, :Wd], rp[:, 2 * which, :Wd], ALU.mult)
                    S.tt("dve", b[:, :Wd], pb[:, :Wd], rp[:, 2 * which + 1, :Wd], ALU.mult)
                    S.tt("pool", QSt[:, dst + j, :Wd], a[:, :Wd], b[:, :Wd], ALU.add)
                    if which == 1:
                        S.copy("pool", QSt[:, 12 + j, :Wd], QSt[:, 6 + j, :Wd])
            for j in range(2):
                if not so:
                    S.act(GTt[:, 0 + j, :Wd], proj(8 + j, Wd)[:, :Wd], AF.Silu)
                    S.act(GTt[:, 2 + j, :Wd], proj(18 + j, Wd)[:, :Wd], AF.Silu)
                    S.act(GTt[:, 4 + j, :Wd], proj(24 + j, Wd)[:, :Wd], AF.Silu)
                    S.copy("dve", QSt[:, 2 + j, :Wd], proj(12 + j, Wd)[:, :Wd])
                    S.copy("act", QSt[:, 4 + j, :Wd], proj(20 + j, Wd)[:, :Wd])
                S.copy("dve", GTt[:, 6 + j, :Wd], proj(10 + j, Wd)[:, :Wd])
                pk = proj(22 + j, Wd)
                S.ts("dve", QSt[:, 10 + j, :Wd], pk[:, :Wd], 32.0 ** -0.5, ALU.mult)
                S.copy("pool", QSt[:, 16 + j, :Wd], QSt[:, 10 + j, :Wd])
            for d in range(2):
                for j in range(2):
                    pz = proj(14 + 2 * d + j, Wd)
                    sg = sg_[j]
                    S.act(sg[:, :Wd], pz[:, :Wd], AF.Sigmoid)
                    f = t1[j]
                    S.ts("dve", f[:, :Wd], sg[:, :Wd], lbv[:, j, 0:1], ALU.mult, lbv[:, j, 1:2], ALU.add)
                    S.act(GSt[:, 4 * d + j, :Wd], f[:, :Wd], AF.Ln)
                    S.ts("dve", QSt[:, (8 if d == 0 else 14) + j, :Wd], sg[:, :Wd], lbv[:, j, 2:3], ALU.mult, lbv[:, j, 0:1], ALU.add)
            S.copy("dve", low[:, :Wd], proj(26, Wd)[:, :Wd])
            for d in range(2):
                for j in range(2):
                    p = pp[ppi[0] % 5]
                    ppi[0] += 1
                    S.mm(p[:, :Wd], wa2[32 * d:32 * d + 16, d, j * 128:(j + 1) * 128], low[32 * d:32 * d + 16, :Wd])
                    sg = sg_[j]
                    S.act(sg[:, :Wd], p[:, :Wd], AF.Sigmoid, bias=gba[:, d, j:j + 1])
                    S.act(sg[:, :Wd], sg[:, :Wd], AF.Ln)
                    S.ts("pool", GSt[:, 4 * d + 2 + j, :Wd], sg[:, :Wd], 1.0 / 16.0, ALU.mult)
            for sub in range(Wd // 128):
                for kc in range(KD):
                    S.mm(pv[:, 0:512], XN[:, kc, sub * 128:(sub + 1) * 128], Wv[:, kc, 0:512], start=(kc == 0), stop=(kc == KD - 1))
                for kc in range(KD):
                    S.mm(pv[:, 512:768], XN[:, kc, sub * 128:(sub + 1) * 128], Wv[:, kc, 512:768], start=(kc == 0), stop=(kc == KD - 1))
                vt = Vs[sub % 3]
                S.copy("act", vt.all(), pv[:, 0:768])
                S.dma("pool", V(sc["VS"].ap[t0 + sub * 128:t0 + (sub + 1) * 128, :], sc["VS"].res), vt.all())
            if so:
                S.dma("pool", V(qsv[:, 6:18, t0:t0 + Wd], sc["QS"].res), QSt[:, 6:18, :Wd])
                S.dma("pool", V(gtv[:, 6:8, t0:t0 + Wd], sc["GT"].res), GTt[:, 6:8, :Wd])
            else:
                S.dma("pool", V(qsv[:, :, t0:t0 + Wd], sc["QS"].res), QSt[:, :, :Wd])
                S.dma("pool", V(gtv[:, :, t0:t0 + Wd], sc["GT"].res), GTt[:, :, :Wd])
            S.dma("pool", V(gsv[:, :, t0:t0 + Wd], sc["GS"].res), GSt[:, :, :Wd])
        S.barrier()
        P.flush()


def compute_lbv(P, st, W, l):
    S = P.S
    NL = P.NL
    lg = P.sb(st, "lbl", [128, 2, NL])
    ex = P.sb(st, "lbe", [128, 2, NL])
    mx = P.sb(st, "lbm", [128, 2])
    sm = P.sb(st, "lbs", [128, 2])
    pa = P.sb(st, "lbp", [128, 2])
    lbv = P.sb(st, "lbv", [128, 2, 3])
    S.dma("sp", lg.all(), W["hg_lb"].all())
    S.red(mx.all(), lg.all(), ALU.max)
    S.tt("dve", ex.all(), lg.all(), mx.all().re("p (j o) -> p j o", o=1).bc([128, 2, NL]), ALU.subtract)
    S.act(ex.all(), ex.all(), AF.Exp)
    S.red(sm.all(), ex.all(), ALU.add)
    S.recip(sm.all(), sm.all())
    if l == 0:
        S.memset("dve", pa.all(), 0.0)
    else:
        S.red(pa.all(), ex[:, :, 1:l + 1], ALU.add)
        S.tt("dve", pa.all(), pa.all(), sm.all(), ALU.mult)
        S.ts("dve", pa.all(), pa.all(), 0.0, ALU.max, 1.0 - 1e-6, ALU.min)
    S.copy("dve", lbv[:, :, 1], pa.all())
    S.ts("dve", lbv[:, :, 0], pa.all(), -1.0, ALU.mult, 1.0, ALU.add)
    S.ts("dve", lbv[:, :, 2], pa.all(), 1.0, ALU.mult, -1.0, ALU.add)
    return lbv


def fm_vec(v, n=None):
    v = np.asarray(v, np.float32)
    return np.ascontiguousarray(v.reshape(-1, 128).T)


def pad_gla(a):
    out = np.zeros(a.shape[:-1] + (256,), a.dtype)
    for h in range(4):
        out[..., h * 64:h * 64 + 32] = a[..., h * 32:(h + 1) * 32]
    return out


def rope_tables(TL, half):
    NT = CTX + TL
    r = np.arange(128)
    w = r % 64
    part = w // 32
    u = w % 32
    f = u % 16
    freqs = (10000.0 ** (-(f.astype(np.float64)) / 16.0))
    tg = half * TL + np.arange(TL)
    pos = np.where(part[:, None] == 0, (tg // 64)[None, :], (tg % 64)[None, :]).astype(np.float64)
    ang = pos.astype(np.float32).astype(np.float64) * freqs.astype(np.float32).astype(np.float64)[:, None]
    cos = np.cos(ang)
    sin = np.sin(ang) * np.where(u < 16, -1.0, 1.0)[:, None]
    tab = np.zeros((128, 4, NT), np.float32)
    tab[:, 0, :CTX] = 1.0
    tab[:, 2, :CTX] = 0.125
    tab[:, 0, CTX:] = cos
    tab[:, 1, CTX:] = sin
    tab[:, 2, CTX:] = 0.125 * cos
    tab[:, 3, CTX:] = 0.125 * sin
    return tab


def arrange_w_in(w_in):
    w_in = np.asarray(w_in, np.float32)
    c = np.arange(256)
    h, w = c // 64, c % 64
    perm = h * 64 + (w // 32) * 32 + ((w % 32) + 16) % 32
    lowt = np.zeros((D, 128), np.float32)
    lowt[:, 0:16] = w_in[:, 3328:3344]
    lowt[:, 32:48] = w_in[:, 3344:3360]
    parts = [w_in[:, 0:256], w_in[:, 256:512], w_in[:, 0:256][:, perm], w_in[:, 256:512][:, perm],
             w_in[:, 768:1024], w_in[:, 1024:1280], w_in[:, 1280:1536], w_in[:, 1536:1792], w_in[:, 1792:2048],
             w_in[:, 2304:2560], pad_gla(w_in[:, 2560:2688]), pad_gla(w_in[:, 2688:2816]), w_in[:, 3072:3328], lowt]
    arr = np.ascontiguousarray(np.concatenate(parts, axis=1))
    assert arr.shape == (D, NCOL)
    wv = np.ascontiguousarray(np.concatenate([w_in[:, 512:768], w_in[:, 2048:2304], w_in[:, 2816:3072]], axis=1))
    return arr, wv


def layer_consts(inp, l):
    d = {}
    d["ada_w"] = np.ascontiguousarray(inp["ada_w"][l])
    d["ada_b"] = fm_vec(inp["ada_b"][l])
    d["n1w"] = fm_vec(inp["norm1_w"][l])
    d["n2w"] = fm_vec(inp["norm2_w"][l])
    d["w_in_arr"], d["w_v"] = arrange_w_in(inp["w_in"][l])
    wa2p = np.zeros((128, 2, 256), np.float32)
    wa2p[0:16, 0, :] = pad_gla(np.asarray(inp["gla_wa2"][l][0]))
    wa2p[32:48, 1, :] = pad_gla(np.asarray(inp["gla_wa2"][l][1]))
    d["wa2p"] = wa2p
    ba = pad_gla(np.asarray(inp["gla_ba"][l]))
    d["gla_ba"] = np.ascontiguousarray(ba.reshape(2, 2, 128).transpose(2, 0, 1))
    d["hg_lb"] = np.ascontiguousarray(np.asarray(inp["hg_lb_logits"]).reshape(-1, 2, 128).transpose(2, 1, 0))
    d.update(consts_np())
    return d


def core_cond(inp, b):
    c = np.stack([fm_vec(inp["c"][b]), fm_vec(inp["c_ctx"])], axis=-1)
    return np.ascontiguousarray(c)


def build_test_A(TL, l, NL=4):
    P = Prog(TL, debug=True, NL=NL)
    W = {}
    NT = P.NT
    for name, shape, dt in [("xT", [KD, 128, NT], F32), ("cond", [128, KD, 2], F32), ("ada_w", [D, 6 * D], F32),
                            ("ada_b", [128, 48], F32), ("n1w", [128, KD], F32), ("w_in_arr", [D, NCOL], F32),
                            ("w_v", [D, 768], F32), ("wa2p", [128, 2, 256], F32), ("gla_ba", [128, 2, 2], F32),
                            ("hg_lb", [128, 2, P.NL], F32), ("rope", [128, 4, NT], F32), ("ones_bf", [128, 128], BF16)]:
        W[name] = P.din(name, shape, dt)
    sc = alloc_scratch(P)
    with ExitStack() as st:
        MOD = phase_mod(P, st, W, 16)
        lbv = compute_lbv(P, st, W, l)
        phase_A(P, W, MOD, sc, lbv)
    return P.nc


def attn_consts_np():
    lg = np.log1p(-(2.0 ** (-5.0 - np.arange(4, dtype=np.float32)))).astype(np.float32)
    bret = np.zeros((128, 2, 2, 128), np.float32)
    t = np.arange(128, dtype=np.float32)
    for d in range(2):
        lgd = lg if d == 0 else lg[::-1]
        for tile in range(2):
            for hh in range(2):
                h = tile * 2 + hh
                cnt = (t + 1) if d == 0 else (128 - t)
                bret[hh * 64:(hh + 1) * 64, d, tile, :] = (cnt * lgd[h])[None, :]
    j = np.arange(128)[:, None]
    i = np.arange(128)[None, :]
    masks = np.stack([(j <= i), (j >= i)], axis=1).astype(np.float32)
    return {"bret": bret, "masks": np.ascontiguousarray(masks)}


class AttnBufs:
    pass


def attn_alloc(P, st, W):
    S = P.S
    A = AttnBufs()
    A.ones = P.sb(st, "a_ones", [128, 128])
    S.memset("dve", A.ones.all(), 1.0)
    A.masks = P.sb(st, "a_masks", [128, 2, 128])
    S.dma("sp", A.masks.all(), W["masks"].all())
    A.identb = P.sb(st, "a_identb", [128, 128], BF16)
    S.dma("sp", A.identb.all(), W["ident_bf"].all())
    A.B = []
    bretv = W["bret"]
    for d in range(2):
        bl = []
        for pp_ in range(2):
            b = P.sb(st, f"a_B{d}{pp_}", [128, 6, 128])
            S.dma("sp", b[:, 0:2, :], bretv[:, d, :, :])
            bl.append(b)
        A.B.append(bl)
    A.cc = 0
    A.E_ = [P.sb(st, f"a_E{i}", [128, 6, 128]) for i in range(2)]
    A.D1_ = [P.sb(st, f"a_D1{i}", [128, 6, 128]) for i in range(2)]
    A.Eq1_ = [P.sb(st, f"a_Eq1{i}", [128, 6, 64]) for i in range(2)]
    A.Ek1_ = [P.sb(st, f"a_Ek1{i}", [128, 6, 128]) for i in range(2)]
    A.Ek0_ = [P.sb(st, f"a_Ek0{i}", [128, 6, 64]) for i in range(2)]
    A.qh_ = [P.sb(st, f"a_qh{i}", [128, 6, 128], BF16) for i in range(2)]
    A.q1_ = [P.sb(st, f"a_q1{i}", [128, 6, 64], BF16) for i in range(2)]
    A.k1_ = [P.sb(st, f"a_k1{i}", [128, 6, 128], BF16) for i in range(2)]
    A.k0_ = [P.sb(st, f"a_k0{i}", [128, 6, 64], BF16) for i in range(2)]
    A.k1T_ = [P.sb(st, f"a_k1T{i}", [128, 6, 128], BF16) for i in range(2)]
    A.tS_ = [P.sb(st, f"a_tS{i}", [128, 6, 64]) for i in range(2)]
    A.Asb = [[[P.sb(st, f"a_A{d}{m}{i}", [128, 4, 128], BF16) for i in range(2)] for m in range(3)] for d in range(2)]
    for d in range(2):
        for m in range(3):
            for i in range(2):
                S.memset("pool", A.Asb[d][m][i].all(), 0.0)
    A.S = P.sb(st, "a_S", [128, 6, 64])
    A.Sbf = P.sb(st, "a_Sbf", [128, 6, 64], BF16)
    A.Q = [P.sb(st, f"a_Q{i}", [128, 6, 512], BF16) for i in range(2)]
    A.K = [P.sb(st, f"a_K{i}", [128, 6, 512], BF16) for i in range(2)]
    A.G = [P.sb(st, f"a_G{i}", [128, 4, 512]) for i in range(2)]
    A.Vt = [P.sb(st, f"a_V{i}", [128, 4, 768], BF16) for i in range(2)]
    A.Yo = [P.sb(st, f"a_Yo{i}", [128, 6, 512]) for i in range(2)]
    A.Yf = [P.sb(st, f"a_Yf{i}", [128, 6, 512]) for i in range(2)]
    A.psA = [P.ps(st, f"a_psA{m}", [128, 4, 128]) for m in range(3)]
    A.psO = P.ps(st, "a_psO", [128, 6, 128])
    A.psT = P.ps(st, "a_psT", [128, 6, 128], BF16)
    A.psS = P.ps(st, "a_psS", [128, 6, 64])
    return A


def attn_dirs(d):
    if d == 0:
        return slice(0, 64), slice(64, 128), 63, 127, 63
    return slice(64, 128), slice(0, 64), 64, 0, 0


def attn_stage1(P, A, d, par, Q, K, G, c0, full):
    S = P.S
    B = A.B[d][par]
    E, D1, Eq1, Ek1, Ek0 = A.E_[par], A.D1_[par], A.Eq1_[par], A.Ek1_[par], A.Ek0_[par]
    qh, q1, k1, k0, k1T = A.qh_[par], A.q1_[par], A.k1_[par], A.k0_[par], A.k1T_[par]
    cs = slice(c0, c0 + 128)
    H0, H1, ref, last, last1 = attn_dirs(d)
    rv = (lambda v: v) if d == 0 else (lambda v: v[:, ::-1])
    for j in range(4):
        S.scan(rv(B[:, 2 + j, :]), rv(A.ones.all()), rv(G[:, j, cs]), 0.0)
    bref = B[:, :, ref:ref + 1]
    S.tt("dve", D1.all(), B.all(), bref.bc([128, 6, 128]), ALU.subtract)
    S.act(Eq1.all(), D1[:, :, H1], AF.Exp)
    S.ts("dve", D1.all(), D1.all(), -1.0, ALU.mult, 80.0, ALU.min)
    S.act(Ek1.all(), D1.all(), AF.Exp)
    S.tt("dve", k1.all(), K[:, :, cs], Ek1.all(), ALU.mult)
    for j in range(6):
        S.tr(A.psT[:, j, :], k1[:, j, :], A.identb.all())
    S.copy("act", k1T.all(), A.psT.all())
    if full:
        S.act(E.all(), B.all(), AF.Exp)
        S.ts("pool", Ek0.all(), B[:, :, H0], -1.0, ALU.mult, 80.0, ALU.min)
        S.act(Ek0.all(), Ek0.all(), AF.Exp)
        S.tt("dve", qh.all(), Q[:, :, cs], E.all(), ALU.mult)
        S.tt("pool", q1.all(), Q[:, :, cs][:, :, H1], Eq1.all(), ALU.mult)
        S.tt("pool", k0.all(), K[:, :, cs][:, :, H0], Ek0.all(), ALU.mult)
        mask = A.masks[:, d, :]
        for m in range(3):
            psA = A.psA[m]
            for h in range(4):
                tile = 2 * m + h // 2
                rows = slice((h % 2) * 64, (h % 2) * 64 + 64)
                S.mm(psA[H0, h, H0], k0[rows, tile, :], qh[rows, tile, H0])
                S.mm(psA[:, h, H1], k1[rows, tile, :], q1[rows, tile, :])
            Asb = A.Asb[d][m][par]
            S.tt("dve", Asb[H0, :, H0], psA[H0, :, H0], mask[H0, H0].re("p (o i) -> p o i", o=1).bc([64, 4, 64]), ALU.mult)
            S.tt("dve", Asb[:, :, H1], psA[:, :, H1], mask[:, H1].re("p (o i) -> p o i", o=1).bc([128, 4, 64]), ALU.mult)
    else:
        S.act(E[:, :, last:last + 1], B[:, :, last:last + 1], AF.Exp)


def attn_stage2(P, A, d, par, Vt, c0, sub, full, Yo):
    S = P.S
    E, Eq1, qh, k1T, tS = A.E_[par], A.Eq1_[par], A.qh_[par], A.k1T_[par], A.tS_[par]
    cs = slice(c0, c0 + 128)
    H0, H1, ref, last, last1 = attn_dirs(d)
    if full:
        for m in range(3):
            Asb = A.Asb[d][m][par]
            for h in range(4):
                tile = 2 * m + h // 2
                rows = slice((h % 2) * 64, (h % 2) * 64 + 64)
                vc = slice(m * 256 + h * 64, m * 256 + h * 64 + 64)
                S.mm(A.psO[rows, tile, :], Vt[:, sub, vc], Asb[:, h, :], start=True, stop=False)
                S.mm(A.psO[rows, tile, :], A.Sbf[rows, tile, :], qh[rows, tile, :], start=False, stop=True)
        S.copy("act", Yo[:, :, cs], A.psO.all())
    for m in range(3):
        for h in range(4):
            tile = 2 * m + h // 2
            rows = slice((h % 2) * 64, (h % 2) * 64 + 64)
            vc = slice(m * 256 + h * 64, m * 256 + h * 64 + 64)
            S.mm(A.psS[rows, tile, :], k1T[:, tile, rows], Vt[:, sub, vc])
    e1 = E[:, :, last:last + 1].bc([128, 6, 64])
    e2 = Eq1[:, :, last1:last1 + 1].bc([128, 6, 64])
    S.tt("dve", tS.all(), A.psS.all(), e2, ALU.mult)
    S.tt("pool", A.S.all(), A.S.all(), e1, ALU.mult)
    S.tt("pool", A.S.all(), A.S.all(), tS.all(), ALU.add)
    S.copy("pool", A.Sbf.all(), A.S.all())


def attn_pass(P, A, sc, d, full, init_lat, out_ctx, out_fin, YA):
    S = P.S
    qsv = sc["QS"].ap.rearrange("n p t -> p n t")
    gsv = sc["GS"].ap.rearrange("n p t -> p n t")
    yav = YA.ap.rearrange("n p t -> p n t") if YA is not None else None
    S.memset("pool", A.S.all(), 0.0)
    S.memset("pool", A.Sbf.all(), 0.0)
    lat = P.tiles[1:]
    order = [P.tiles[0]] + (lat if d == 0 else lat[::-1])
    chunks = []
    for n, (t0, Wd) in enumerate(order):
        nch = Wd // 128
        cis = list(range(nch)) if d == 0 else list(range(nch - 1, -1, -1))
        for idx, ci in enumerate(cis):
            chunks.append((n, t0, Wd, ci, idx == 0, idx == nch - 1))

    def s1(i):
        n, t0, Wd, ci, first, lastc = chunks[i]
        Q, K, G, Vt = A.Q[n % 2], A.K[n % 2], A.G[n % 2], A.Vt[n % 2]
        if first:
            if full:
                S.dma("sp", Q[:, :, :Wd], V(qsv[:, 0:6, t0:t0 + Wd], sc["QS"].res))
            ko = 6 if d == 0 else 12
            S.dma("sp", K[:, :, :Wd], V(qsv[:, ko:ko + 6, t0:t0 + Wd], sc["QS"].res))
            S.dma("sp", G[:, :, :Wd], V(gsv[:, 4 * d:4 * d + 4, t0:t0 + Wd], sc["GS"].res))
            S.dma("sp", Vt[:, :Wd // 128, :], V(sc["VS"].ap[t0:t0 + Wd, :].rearrange("(s p) c -> p s c", p=128), sc["VS"].res))
            if full and d == 1:
                S.dma("sp", A.Yf[n % 2][:, :, :Wd], V(yav[:, :, t0:t0 + Wd], YA.res))
        attn_stage1(P, A, d, i % 2, Q, K, G, ci * 128, full)

    def s2(i):
        n, t0, Wd, ci, first, lastc = chunks[i]
        Vt, Yo = A.Vt[n % 2], A.Yo[n % 2]
        attn_stage2(P, A, d, i % 2, Vt, ci * 128, ci, full, Yo)
        if lastc:
            if full:
                if d == 1:
                    S.tt("dve", Yo[:, :, :Wd], Yo[:, :, :Wd], A.Yf[n % 2][:, :, :Wd], ALU.add)
                S.dma("pool", V(yav[:, :, t0:t0 + Wd], YA.res), Yo[:, :, :Wd])
            if n == 0:
                if out_ctx is not None:
                    S.dma("pool", out_ctx, A.S.all())
                if init_lat is not None:
                    S.dma("sp", A.S.all(), init_lat)
                    S.copy("pool", A.Sbf.all(), A.S.all())

    s1(0)
    for i in range(len(chunks)):
        if i + 1 < len(chunks):
            s1(i + 1)
        s2(i)
    if out_fin is not None:
        S.dma("pool", out_fin, A.S.all())


def build_test_B(TL, l, NL=4, full=True):
    P = Prog(TL, debug=True, NL=NL)
    W = {}
    NT = P.NT
    for name, shape, dt in [("xT", [KD, 128, NT], F32), ("cond", [128, KD, 2], F32), ("ada_w", [D, 6 * D], F32),
                            ("ada_b", [128, 48], F32), ("n1w", [128, KD], F32), ("w_in_arr", [D, NCOL], F32),
                            ("w_v", [D, 768], F32), ("wa2p", [128, 2, 256], F32), ("gla_ba", [128, 2, 2], F32),
                            ("hg_lb", [128, 2, P.NL], F32), ("rope", [128, 4, NT], F32), ("ones_bf", [128, 128], BF16),
                            ("ident_bf", [128, 128], BF16), ("bret", [128, 2, 2, 128], F32), ("masks", [128, 2, 128], F32),
                            ("init_att", [2, 128, 6, 64], F32)]:
        W[name] = P.din(name, shape, dt)
    sc = alloc_scratch(P)
    YA = P.dscr("YA", [6, 128, NT], F32)
    st_out = P.dout("st_out", [4, 128, 6, 64], F32)
    with ExitStack() as st:
        MOD = phase_mod(P, st, W, 16)
        lbv = compute_lbv(P, st, W, l)
        phase_A(P, W, MOD, sc, lbv)
        with ExitStack() as st2:
            A = attn_alloc(P, st2, W)
            for d in range(2):
                attn_pass(P, A, sc, d, full, V(W["init_att"].ap[d], W["init_att"].res) if full else None,
                          V(st_out.ap[d], st_out.res), V(st_out.ap[2 + d], st_out.res), YA if full else None)
            P.S.barrier()
            P.flush()
    return P.nc


def s5_layout_np(inp, l):
    d = {}
    G, Pn, Cg = 16, 64, 16

    def sm(a):
        return np.ascontiguousarray(np.asarray(a, np.float32).reshape(2, 8, 128).transpose(2, 0, 1))

    d["s5_lre"] = sm(inp["s5_lam_re"][l])
    d["s5_lim"] = sm(inp["s5_lam_im"][l])
    dt = np.repeat(np.asarray(inp["s5_log_dt"][l], np.float32)[:, :, None], Pn, axis=2)
    d["s5_ldt"] = sm(dt)
    BT = np.zeros((2, 2, 8, 128, 128), np.float32)
    CT = np.zeros((2, 2, 8, 128, 128), np.float32)
    for dd in range(2):
        for ri, (bn, cn) in enumerate((("s5_b_re", "s5_c_re"), ("s5_b_im", "s5_c_im"))):
            Bm = np.asarray(inp[bn][l][dd], np.float32)
            Cm = np.asarray(inp[cn][l][dd], np.float32)
            for k in range(8):
                for gg in range(2):
                    g = 2 * k + gg
                    r0 = (g % 8) * 16
                    BT[dd, ri, k, r0:r0 + 16, gg * 64:(gg + 1) * 64] = Bm[g].T
                    CT[dd, ri, k, gg * 64:(gg + 1) * 64, r0:r0 + 16] = Cm[g].T
    d["s5_BT"] = np.ascontiguousarray(BT.transpose(3, 0, 1, 2, 4).reshape(128, 32, 128))
    d["s5_CT"] = np.ascontiguousarray(CT.transpose(3, 0, 1, 2, 4).reshape(128, 32, 128))
    d["s5_d"] = fm_vec(inp["s5_d"][l])
    d["glu_w"] = np.ascontiguousarray(inp["s5_glu_w"][l])
    d["glu_b"] = fm_vec(inp["s5_glu_b"][l])
    return d


class S5Bufs:
    pass


def cmul(S, ek, o_re, o_im, a_re, a_im, b_re, b_im, t1, t2):
    S.tt(ek, t1, a_re, b_re, ALU.mult)
    S.tt(ek, t2, a_im, b_im, ALU.mult)
    S.tt(ek, o_im, a_re, b_im, ALU.mult)
    S.tt(ek, o_re, t1, t2, ALU.subtract)
    S.tt(ek, t1, a_im, b_re, ALU.mult)
    S.tt(ek, o_im, o_im, t1, ALU.add)


def s5_alloc(P, st, W):
    S = P.S
    B = S5Bufs()
    B.BT = P.sb(st, "s5BT", [128, 32, 128], BF16)
    B.CT = P.sb(st, "s5CT", [128, 32, 128], BF16)
    with ExitStack() as st2:
        stg = P.sb(st2, "s5stg", [128, 32, 128])
        S.dma("sp", stg.all(), W["s5_BT"].all())
        S.copy("dve", B.BT.all(), stg.all())
        stg2 = P.sb(st2, "s5stg2", [128, 32, 128])
        S.dma("sp", stg2.all(), W["s5_CT"].all())
        S.copy("act", B.CT.all(), stg2.all())
        S.barrier()
        P.flush()
    B.lre = P.sb(st, "s5lre", [128, 2, 8])
    B.lim = P.sb(st, "s5lim", [128, 2, 8])
    B.ldt = P.sb(st, "s5ldt", [128, 2, 8])
    S.dma("sp", B.lre.all(), W["s5_lre"].all())
    S.dma("sp", B.lim.all(), W["s5_lim"].all())
    S.dma("sp", B.ldt.all(), W["s5_ldt"].all())
    B.dt = P.sb(st, "s5dt", [128, 2, 8])
    S.act(B.dt.all(), B.ldt.all(), AF.Exp)
    B.r = P.sb(st, "s5r", [128, 2, 8])
    B.u = P.sb(st, "s5u", [128, 2, 2, 8])
    B.coef = P.sb(st, "s5coef", [128, 2, 2, 8])
    B.uW = P.sb(st, "s5uW", [128, 2, 2, 8])
    tmp = [P.sb(st, f"s5tmp{i}", [128, 2, 8]) for i in range(6)]
    th = tmp[0]
    S.tt("dve", th.all(), B.lim.all(), B.dt.all(), ALU.mult)
    S.tt("dve", tmp[1].all(), B.lre.all(), B.dt.all(), ALU.mult)
    S.act(B.r.all(), tmp[1].all(), AF.Exp)
    x, x2, pa, pb_ = tmp[2], tmp[3], tmp[4], tmp[5]
    ure, uim = B.u[:, :, 0, :], B.u[:, :, 1, :]
    S.ts("dve", x.all(), th.all(), 1.0 / 64, ALU.mult)
    S.tt("dve", x2.all(), x.all(), x.all(), ALU.mult)
    S.ts("dve", pa.all(), x2.all(), -1.0 / 5040, ALU.mult)
    S.stt(pa.all(), pa.all(), 1.0 / 120, x2.all(), ALU.add, ALU.mult)
    S.stt(pa.all(), pa.all(), -1.0 / 6, x2.all(), ALU.add, ALU.mult)
    S.stt(uim, pa.all(), 1.0, x.all(), ALU.add, ALU.mult)
    S.ts("dve", pb_.all(), x2.all(), 1.0 / 40320, ALU.mult)
    S.stt(pb_.all(), pb_.all(), -1.0 / 720, x2.all(), ALU.add, ALU.mult)
    S.stt(pb_.all(), pb_.all(), 1.0 / 24, x2.all(), ALU.add, ALU.mult)
    S.stt(pb_.all(), pb_.all(), -0.5, x2.all(), ALU.add, ALU.mult)
    S.ts("dve", ure, pb_.all(), 1.0, ALU.add)
    for _ in range(6):
        S.tt("dve", pa.all(), ure, ure, ALU.mult)
        S.tt("dve", pb_.all(), uim, uim, ALU.mult)
        S.stt(x.all(), ure, 2.0, uim, ALU.mult, ALU.mult)
        S.tt("dve", ure, pa.all(), pb_.all(), ALU.subtract)
        S.copy("dve", uim, x.all())
    S.tt("dve", pa.all(), ure, ure, ALU.mult)
    S.tt("dve", pb_.all(), uim, uim, ALU.mult)
    S.tt("dve", pa.all(), pa.all(), pb_.all(), ALU.add)
    S.act(pa.all(), pa.all(), AF.Sqrt)
    S.recip(pa.all(), pa.all())
    S.tt("dve", ure, ure, pa.all(), ALU.mult)
    S.tt("dve", uim, uim, pa.all(), ALU.mult)
    are, aim = tmp[0], tmp[1]
    S.tt("dve", are.all(), B.u[:, :, 0, :], B.r.all(), ALU.mult)
    S.tt("dve", aim.all(), B.u[:, :, 1, :], B.r.all(), ALU.mult)
    S.ts("dve", are.all(), are.all(), -1.0, ALU.add)
    den = tmp[2]
    S.tt("dve", den.all(), B.lre.all(), B.lre.all(), ALU.mult)
    S.tt("dve", tmp[3].all(), B.lim.all(), B.lim.all(), ALU.mult)
    S.tt("dve", den.all(), den.all(), tmp[3].all(), ALU.add)
    S.recip(den.all(), den.all())
    nl = tmp[3]
    S.ts("dve", nl.all(), B.lim.all(), -1.0, ALU.mult)
    cmul(S, "dve", B.coef[:, :, 0, :], B.coef[:, :, 1, :], are.all(), aim.all(), B.lre.all(), nl.all(), tmp[4].all(), tmp[5].all())
    S.tt("dve", B.coef[:, :, 0, :], B.coef[:, :, 0, :], den.all(), ALU.mult)
    S.tt("dve", B.coef[:, :, 1, :], B.coef[:, :, 1, :], den.all(), ALU.mult)
    B.POST = P.sb(st, "s5POST", [128, 8, 2, 512])
    B.PRE = P.sb(st, "s5PRE", [128, 8, 2, 512])
    B.tt1 = P.sb(st, "s5tt1", [128, 8, 256])
    B.tt2 = P.sb(st, "s5tt2", [128, 8, 256])
    B.G = P.sb(st, "s5G", [128, 8, 2, 512])
    B.Tp1 = [P.sb(st, f"s5Tp1_{i}", [128, 2, 512]) for i in range(2)]
    B.Tp2 = [P.sb(st, f"s5Tp2_{i}", [128, 2, 512]) for i in range(2)]
    B.Tq1 = [P.sb(st, f"s5Tq1_{i}", [128, 2, 512]) for i in range(2)]
    B.Tq2 = [P.sb(st, f"s5Tq2_{i}", [128, 2, 512]) for i in range(2)]
    B.Hb = [P.sb(st, f"s5Hb{i}", [128, 2, 512], BF16) for i in range(2)]
    B.U = [P.sb(st, f"s5U{i}", [128, 2, 512], BF16) for i in range(2)]
    B.Wt = [P.sb(st, f"s5Wt{i}", [128, 2, 512]) for i in range(2)]
    B.Yo = [P.sb(st, f"s5Yo{i}", [128, 2, 512]) for i in range(2)]
    B.Yf = P.sb(st, "s5Yf", [128, 2, 512])
    B.h = P.sb(st, "s5h", [128, 2, 8])
    B.gi = P.sb(st, "s5gi", [128, 2, 8])
    B.sm = [P.sb(st, f"s5sm{i}", [128, 8]) for i in range(3)]
    B.pb = [P.ps(st, f"s5pb{i}", [128, 2, 512]) for i in range(2)]
    B.py = [P.ps(st, f"s5py{i}", [128, 512]) for i in range(2)]
    return B


def s5_tables(P, B, d):
    S = P.S
    S.memset("dve", B.POST[:, :, 0, 0:1], 1.0)
    S.memset("dve", B.POST[:, :, 1, 0:1], 0.0)
    S.copy("dve", B.uW[:, d, :, :], B.u[:, d, :, :])
    n = 1
    while n < 512:
        wre = B.uW[:, d, 0, :].re("p (k o) -> p k o", o=1).bc([128, 8, n])
        wim = B.uW[:, d, 1, :].re("p (k o) -> p k o", o=1).bc([128, 8, n])
        cmul(S, "dve", B.POST[:, :, 0, n:2 * n], B.POST[:, :, 1, n:2 * n], B.POST[:, :, 0, 0:n], B.POST[:, :, 1, 0:n],
             wre, wim, B.tt1[:, :, 0:n], B.tt2[:, :, 0:n])
        n *= 2
        if n < 512:
            cmul(S, "dve", B.sm[0].all(), B.sm[1].all(), B.uW[:, d, 0, :], B.uW[:, d, 1, :], B.uW[:, d, 0, :], B.uW[:, d, 1, :], B.sm[2].all(), B.gi[:, 0, :])
            S.copy("dve", B.uW[:, d, 0, :], B.sm[0].all())
            S.copy("dve", B.uW[:, d, 1, :], B.sm[1].all())
    for hlf in range(2):
        sl = slice(hlf * 256, (hlf + 1) * 256)
        cre = B.coef[:, d, 0, :].re("p (k o) -> p k o", o=1).bc([128, 8, 256])
        cim = B.coef[:, d, 1, :].re("p (k o) -> p k o", o=1).bc([128, 8, 256])
        S.tt("dve", B.tt1.all(), B.POST[:, :, 0, sl], cre, ALU.mult)
        S.tt("dve", B.tt2.all(), B.POST[:, :, 1, sl], cim, ALU.mult)
        S.tt("dve", B.PRE[:, :, 0, sl], B.tt1.all(), B.tt2.all(), ALU.add)
        S.tt("dve", B.tt1.all(), B.POST[:, :, 0, sl], cim, ALU.mult)
        S.tt("dve", B.tt2.all(), B.POST[:, :, 1, sl], cre, ALU.mult)
        S.tt("dve", B.PRE[:, :, 1, sl], B.tt1.all(), B.tt2.all(), ALU.subtract)


def s5_pass(P, B, sc, d, full, init_lat, out_ctx, out_fin, YS):
    S = P.S
    gtv = sc["GT"].ap.rearrange("n p t -> p n t")
    ysv = YS.ap.rearrange("n p t -> p n t") if YS is not None else None
    s5_tables(P, B, d)
    S.memset("dve", B.h.all(), 0.0)
    lat = P.tiles[1:]
    order = [P.tiles[0]] + (lat if d == 0 else lat[::-1])
    rv = (lambda v: v) if d == 0 else (lambda v: v[:, ::-1])
    rv3 = (lambda v: v) if d == 0 else (lambda v: v[:, :, ::-1])
    for n, (t0, Wd) in enumerate(order):
        U, Yo = B.U[n % 2], B.Yo[n % 2]
        S.dma("sp", U[:, :, :Wd], V(gtv[:, 6:8, t0:t0 + Wd], sc["GT"].res))
        if full and d == 1:
            S.dma("sp", B.Yf[:, :, :Wd], V(ysv[:, :, t0:t0 + Wd], YS.res))
        cmul(S, "dve", B.gi[:, 0, :], B.gi[:, 1, :], B.u[:, d, 0, :], B.u[:, d, 1, :], B.h[:, 0, :], B.h[:, 1, :], B.sm[0].all(), B.sm[1].all())
        def Gk(k, idx):
            return B.G.k(k, (slice(None), k) + idx)

        def pre(k):
            pb = B.pb[k % 2]
            for ri in range(2):
                S.mm(pb[:, ri, :Wd], B.BT[:, (d * 2 + ri) * 8 + k, :], U[:, k // 4, :Wd])
            pre_re = rv3(B.PRE[:, k, 0:1, :Wd]).bc([128, 2, Wd])
            pre_im = rv3(B.PRE[:, k, 1:2, :Wd]).bc([128, 2, Wd])
            Wt = B.Wt[k % 2]
            T1, T2 = B.Tp1[k % 2], B.Tp2[k % 2]
            S.tt("dve", T1[:, :, :Wd], pb[:, :, :Wd], pre_re, ALU.mult)
            S.tt("dve", T2[:, :, :Wd], pb[:, ::-1, :Wd], pre_im, ALU.mult)
            S.tt("pool", Wt[:, 0, :Wd], T1[:, 0, :Wd], T2[:, 0, :Wd], ALU.subtract)
            S.tt("pool", Wt[:, 1, :Wd], T1[:, 1, :Wd], T2[:, 1, :Wd], ALU.add)

        def mid(k):
            Wt = B.Wt[k % 2]
            rbc = B.r[:, d, k:k + 1].bc([128, Wd])
            for ri in range(2):
                S.scan(rv(Gk(k, (ri, slice(0, Wd)))), rbc, rv(Wt[:, ri, :Wd]), B.gi[:, ri, k:k + 1])

        def post(k):
            post_re = rv3(B.POST[:, k, 0:1, :Wd]).bc([128, 2, Wd])
            post_im = rv3(B.POST[:, k, 1:2, :Wd]).bc([128, 2, Wd])
            Hb = B.Hb[k % 2]
            T1, T2 = B.Tq1[k % 2], B.Tq2[k % 2]
            S.tt("dve", T1[:, :, :Wd], Gk(k, (slice(None), slice(0, Wd))), post_re, ALU.mult)
            S.tt("dve", T2[:, :, :Wd], Gk(k, (slice(None, None, -1), slice(0, Wd))), post_im, ALU.mult)
            S.tt("pool", Hb[:, 0, :Wd], T1[:, 0, :Wd], T2[:, 0, :Wd], ALU.subtract)
            S.stt(Hb[:, 1, :Wd], T1[:, 1, :Wd], -1.0, T2[:, 1, :Wd], ALU.mult, ALU.subtract)
            o = k // 4
            for ri in range(2):
                S.mm(B.py[o][:, :Wd], B.CT[:, (d * 2 + ri) * 8 + k, :], Hb[:, ri, :Wd],
                     start=(k % 4 == 0 and ri == 0), stop=(k % 4 == 3 and ri == 1))

        pre(0)
        for k in range(8):
            if k + 1 < 8:
                pre(k + 1)
            mid(k)
            if full:
                post(k)
        mi = Wd - 1 if d == 0 else 0
        S.eng["dve"].ops.append(("w", S.eng["dve"].sem, S.eng["dve"].cnt))
        S.eng["dve"].seen["dve"] = S.eng["dve"].cnt
        cmul(S, "dve", B.h[:, 0, :], B.h[:, 1, :], B.POST[:, :, 0, Wd - 1], B.POST[:, :, 1, Wd - 1],
             B.G[:, :, 0, mi], B.G[:, :, 1, mi], B.sm[0].all(), B.sm[1].all())
        for k_ in range(8):
            B.G.k(k_, (slice(None),)).res.r["dve"] = S.eng["dve"].cnt
        if full:
            for o in range(2):
                if d == 1:
                    S.tt("dve", Yo[:, o, :Wd], B.py[o][:, :Wd], B.Yf[:, o, :Wd], ALU.add)
                else:
                    S.copy("act", Yo[:, o, :Wd], B.py[o][:, :Wd])
            S.dma("pool", V(ysv[:, :, t0:t0 + Wd], YS.res), Yo[:, :, :Wd])
        if n == 0:
            if out_ctx is not None:
                S.dma("pool", out_ctx, B.h.all())
            if init_lat is not None:
                S.dma("sp", B.h.all(), init_lat)
    if out_fin is not None:
        S.dma("pool", out_fin, B.h.all())


W_A = [("xT", lambda P: [KD, 128, P.NT], F32), ("cond", lambda P: [128, KD, 2], F32), ("ada_w", lambda P: [D, 6 * D], F32),
       ("ada_b", lambda P: [128, 48], F32), ("n1w", lambda P: [128, KD], F32), ("w_in_arr", lambda P: [D, NCOL], F32),
       ("w_v", lambda P: [D, 768], F32), ("wa2p", lambda P: [128, 2, 256], F32), ("gla_ba", lambda P: [128, 2, 2], F32),
       ("hg_lb", lambda P: [128, 2, P.NL], F32), ("rope", lambda P: [128, 4, P.NT], F32), ("ones_bf", lambda P: [128, 128], BF16),
       ("ident_bf", lambda P: [128, 128], BF16), ("bret", lambda P: [128, 2, 2, 128], F32), ("masks", lambda P: [128, 2, 128], F32),
       ("s5_lre", lambda P: [128, 2, 8], F32), ("s5_lim", lambda P: [128, 2, 8], F32), ("s5_ldt", lambda P: [128, 2, 8], F32),
       ("s5_BT", lambda P: [128, 32, 128], F32), ("s5_CT", lambda P: [128, 32, 128], F32)]


def declare(P, specs):
    W = {}
    for name, shp, dt in specs:
        W[name] = P.din(name, shp(P), dt)
    return W


def build_test_S(TL, l, NL=4, full=True):
    P = Prog(TL, debug=True, NL=NL)
    W = declare(P, W_A + [("init_s5", lambda P: [2, 128, 2, 8], F32)])
    NT = P.NT
    sc = alloc_scratch(P)
    YS = P.dscr("YS", [2, 128, NT], F32)
    st_out = P.dout("st_out", [4, 128, 2, 8], F32)
    with ExitStack() as st:
        MOD = phase_mod(P, st, W, 16)
        lbv = compute_lbv(P, st, W, l)
        phase_A(P, W, MOD, sc, lbv)
        with ExitStack() as st2:
            B = s5_alloc(P, st2, W)
            for d in range(2):
                s5_pass(P, B, sc, d, full, V(W["init_s5"].ap[d], W["init_s5"].res) if full else None,
                        V(st_out.ap[d], st_out.res), V(st_out.ap[2 + d], st_out.res), YS if full else None)
            P.S.barrier()
            P.flush()
    return P.nc


def norm_mod2(P, S, X, XN, XNf, sqb, psn, rstd, tmpf, ones_bf, s_vec, b_vec, col, Wd):
    S.act(sqb[:, :, :Wd], X[:, :, :Wd], AF.Square)
    for kc in range(KD):
        S.mm(psn[:, :Wd], ones_bf.all(), sqb[:, kc, :Wd], start=(kc == 0), stop=(kc == KD - 1))
    S.ts("dve", rstd[:, :Wd], psn[:, :Wd], 1.0 / D, ALU.mult, 1e-6, ALU.add)
    S.act(rstd[:, :Wd], rstd[:, :Wd], AF.Sqrt)
    S.recip(rstd[:, :Wd], rstd[:, :Wd])
    for kc in range(KD):
        S.stt(tmpf[:, kc, :Wd], X[:, kc, :Wd], s_vec[:, kc, col:col + 1], rstd[:, :Wd], ALU.mult, ALU.mult)
        S.act(XNf[:, kc, :Wd], tmpf[:, kc, :Wd], AF.Identity, bias=b_vec[:, kc, col:col + 1])
    S.copy("pool", XN[:, :, :Wd], XNf[:, :, :Wd])


W_C = [("w_out", lambda P: [D, D], F32), ("hn_w", lambda P: [128, 6], F32), ("s5_d", lambda P: [128, 2], F32),
       ("glu_w", lambda P: [256, 256], F32), ("glu_b", lambda P: [128, 2], F32), ("n2w", lambda P: [128, KD], F32),
       ("blockmean", lambda P: [128, 128], BF16), ("ident_f", lambda P: [128, 128], F32)]
W_MOE = [("router_w", lambda P: [D, 8], F32), ("router_b", lambda P: [1, 8], F32), ("selE", lambda P: [8, 8, 128], BF16)]


def phase_C(P, W, MOD, sc, YA, YS, xT2, XN2, GTS, moe, do_ctx):
    S = P.S
    with ExitStack() as st:
        Wout = P.sb(st, "Wout", [128, KD, D], BF16)
        gluw = P.sb(st, "gluw", [128, 2, 256], BF16)
        Bm = P.sb(st, "Bm", [128, 128], BF16)
        ones_bf = P.sb(st, "onesC", [128, 128], BF16)
        hnw = P.sb(st, "hnw", [128, 6])
        s5d = P.sb(st, "s5d", [128, 2])
        glub = P.sb(st, "glub", [128, 2])
        n2w = P.sb(st, "n2w", [128, KD])
        s2 = P.sb(st, "s2", [128, KD, 2])
        S.dma("sp", Bm.all(), W["blockmean"].all())
        S.dma("sp", ones_bf.all(), W["ones_bf"].all())
        S.dma("sp", hnw.all(), W["hn_w"].all())
        S.dma("sp", s5d.all(), W["s5_d"].all())
        S.dma("sp", glub.all(), W["glu_b"].all())
        S.dma("sp", n2w.all(), W["n2w"].all())
        S.ts("dve", s2.all(), MOD[:, 32:40, :], 1.0, ALU.add)
        S.tt("dve", s2.all(), s2.all(), n2w.all().re("p (k o) -> p k o", o=1).bc([128, KD, 2]), ALU.mult)
        if moe:
            rw = P.sb(st, "rw", [128, KD, 8])
            rb = P.sb(st, "rb", [128, 8])
            identf = P.sb(st, "identf", [128, 128])
            S.dma("sp", rw.all(), V(W["router_w"].ap.rearrange("(kc p) n -> p kc n", p=128), W["router_w"].res))
            S.dma("sp", rb.all(), V(W["router_b"].ap.partition_broadcast(128), W["router_b"].res), allow_slow_non_contiguous=True)
            S.dma("sp", identf.all(), W["ident_f"].all())
        with ExitStack() as st2:
            stg = P.sb(st2, "wostg", [128, KD, D])
            S.dma("sp", stg.all(), V(W["w_out"].ap.rearrange("(kc p) n -> p kc n", p=128), W["w_out"].res))
            S.copy("dve", Wout[:, 0:4, :], stg[:, 0:4, :])
            S.copy("act", Wout[:, 4:8, :], stg[:, 4:8, :])
            stg2 = P.sb(st2, "glstg", [128, 2, 256])
            S.dma("sp", stg2.all(), V(W["glu_w"].ap.rearrange("(kc p) n -> p kc n", p=128), W["glu_w"].res))
            S.copy("pool", gluw.all(), stg2.all())
            S.barrier()
            P.flush()
        X = [P.sb(st, f"cX{i}", [128, KD, 512]) for i in range(2)]
        YAt = [P.sb(st, f"cYA{i}", [128, 6, 512]) for i in range(2)]
        YSt = [P.sb(st, f"cYS{i}", [128, 2, 512]) for i in range(2)]
        GTt = [P.sb(st, f"cGT{i}", [128, 8, 512], BF16) for i in range(2)]
        Y = P.sb(st, "cY", [128, KD, 512], BF16)
        ybf = P.sb(st, "cybf", [128, 512], BF16)
        sqh = P.sb(st, "csqh", [128, 512], BF16)
        yc = P.sb(st, "cyc", [128, 512])
        rs = P.sb(st, "crs", [128, 512])
        o_ = P.sb(st, "co", [128, 512])
        zz = [P.sb(st, f"cz{i}", [128, 512]) for i in range(2)]
        zb = P.sb(st, "czb", [128, 2, 512], BF16)
        ta = P.sb(st, "cta", [128, 512])
        tb = P.sb(st, "ctb", [128, 512])
        sqb = P.sb(st, "csqb", [128, KD, 512], BF16)
        tmpf = P.sb(st, "ctmpf", [128, KD, 512])
        XNf = P.sb(st, "cXNf", [128, KD, 512])
        XN = P.sb(st, "cXN", [128, KD, 512], BF16)
        rstd = P.sb(st, "crstd", [128, 512])
        pp = [P.ps(st, f"cpp{i}", [128, 512]) for i in range(3)]
        po = [P.ps(st, f"cpo{i}", [128, 512]) for i in range(2)]
        psn = P.ps(st, "cpsn", [128, 512])
        if moe:
            pr = P.ps(st, "cpr", [128, 8])
            ptr = P.ps(st, "cptr", [8, 128])
            lg = P.sb(st, "clg", [128, 8])
            l2 = P.sb(st, "cl2", [128, 8])
            ee = P.sb(st, "cee", [128, 8])
            sm = [P.sb(st, f"csm{i}", [128, 1]) for i in range(4)]
            gT = P.sb(st, "cgT", [8, 512], BF16)
        ppi = [0]

        def nextpp():
            p = pp[ppi[0] % 3]
            ppi[0] += 1
            return p

        xv = W["xT"].ap.rearrange("k p t -> p k t")
        x2v = xT2.ap.rearrange("k p t -> p k t")
        xnv = XN2.ap.rearrange("k p t -> p k t")
        yav = YA.ap.rearrange("n p t -> p n t")
        ysv = YS.ap.rearrange("n p t -> p n t")
        gtv = sc["GT"].ap.rearrange("n p t -> p n t")
        tiles = P.tiles if do_ctx else P.tiles[1:]
        for n, (t0, Wd) in enumerate(tiles):
            col = 1 if t0 == 0 else 0
            Xt, ya, ys, gt = X[n % 2], YAt[n % 2], YSt[n % 2], GTt[n % 2]
            S.dma("sp", Xt[:, :, :Wd], V(xv[:, :, t0:t0 + Wd], W["xT"].res))
            S.dma("sp", ya[:, :, :Wd], V(yav[:, :, t0:t0 + Wd], YA.res))
            S.dma("sp", ys[:, :, :Wd], V(ysv[:, :, t0:t0 + Wd], YS.res))
            S.dma("sp", gt[:, :, :Wd], V(gtv[:, :, t0:t0 + Wd], sc["GT"].res))
            for j in range(6):
                m = j // 2
                kc = (0, 4, 6)[m] + (j % 2)
                y = ya[:, j, :Wd]
                if m == 0:
                    S.copy("act", ybf[:, :Wd], y)
                    pm = nextpp()
                    S.mm(pm[:, :Wd], Bm.all(), ybf[:, :Wd])
                    S.tt("dve", yc[:, :Wd], y, pm[:, :Wd], ALU.subtract)
                    y = yc[:, :Wd]
                S.act(sqh[:, :Wd], y, AF.Square)
                pv = nextpp()
                S.mm(pv[:, :Wd], Bm.all(), sqh[:, :Wd])
                S.ts("dve", rs[:, :Wd], pv[:, :Wd], 1e-6, ALU.add)
                S.act(rs[:, :Wd], rs[:, :Wd], AF.Sqrt)
                S.recip(rs[:, :Wd], rs[:, :Wd])
                S.stt(o_[:, :Wd], y, hnw[:, j:j + 1], rs[:, :Wd], ALU.mult, ALU.mult)
                S.tt("pool", Y[:, kc, :Wd], o_[:, :Wd], gt[:, j, :Wd], ALU.mult)
            for j in range(2):
                z = zz[j]
                S.stt(z[:, :Wd], gt[:, 6 + j, :Wd], s5d[:, j:j + 1], ys[:, j, :Wd], ALU.mult, ALU.add)
                S.act(ta[:, :Wd], z[:, :Wd], AF.Square)
                S.ts("dve", ta[:, :Wd], ta[:, :Wd], 0.044715, ALU.mult, 1.0, ALU.add)
                S.tt("dve", ta[:, :Wd], ta[:, :Wd], z[:, :Wd], ALU.mult)
                S.act(tb[:, :Wd], ta[:, :Wd], AF.Sigmoid, scale=1.5957691216057308)
                S.tt("dve", z[:, :Wd], z[:, :Wd], tb[:, :Wd], ALU.mult)
                S.copy("pool", zb[:, j, :Wd], z[:, :Wd])
            for j in range(2):
                pg = nextpp()
                for kk in range(2):
                    S.mm(pg[:, :Wd], gluw[:, kk, j * 128:(j + 1) * 128], zb[:, kk, :Wd], start=(kk == 0), stop=(kk == 1))
                S.act(tb[:, :Wd], pg[:, :Wd], AF.Sigmoid, bias=glub[:, j:j + 1])
                S.tt("dve", Y[:, 2 + j, :Wd], zz[j][:, :Wd], tb[:, :Wd], ALU.mult)
            for dt in range(KD):
                p = po[dt % 2]
                for kc in range(KD):
                    S.mm(p[:, :Wd], Wout[:, kc, dt * 128:(dt + 1) * 128], Y[:, kc, :Wd], start=(kc == 0), stop=(kc == KD - 1))
                S.stt(Xt[:, dt, :Wd], p[:, :Wd], MOD[:, 16 + dt, col:col + 1], Xt[:, dt, :Wd], ALU.mult, ALU.add)
            S.dma("pool", V(x2v[:, :, t0:t0 + Wd], xT2.k(t0, slice(None)).res), Xt[:, :, :Wd])
            norm_mod2(P, S, Xt, XN, XNf, sqb, psn, rstd, tmpf, ones_bf, s2, MOD[:, 24:32, :], col, Wd)
            S.dma("pool", V(xnv[:, :, t0:t0 + Wd], XN2.res), XN[:, :, :Wd])
            if moe:
                for sub in range(Wd // 128):
                    ss = slice(sub * 128, (sub + 1) * 128)
                    for kc in range(KD):
                        S.mm(pr.all(), XNf[:, kc, ss], rw[:, kc, :], start=(kc == 0), stop=(kc == KD - 1))
                    S.tt("dve", lg.all(), pr.all(), rb.all(), ALU.add)
                    S.red(sm[0].all(), lg.all(), ALU.max)
                    S.ts("dve", l2.all(), lg.all(), sm[0].all(), ALU.is_equal)
                    S.stt(l2.all(), l2.all(), -1e30, lg.all(), ALU.mult, ALU.add)
                    S.red(sm[1].all(), l2.all(), ALU.max)
                    S.ts("dve", l2.all(), lg.all(), sm[1].all(), ALU.is_ge)
                    S.ts("dve", sm[2].all(), sm[0].all(), -1.0, ALU.mult)
                    S.act(ee.all(), lg.all(), AF.Exp, bias=sm[2].all())
                    S.tt("dve", ee.all(), ee.all(), l2.all(), ALU.mult)
                    S.red(sm[3].all(), ee.all(), ALU.add)
                    S.recip(sm[3].all(), sm[3].all())
                    S.ts("dve", ee.all(), ee.all(), sm[3].all(), ALU.mult)
                    S.tr(ptr.all(), ee.all(), identf.all())
                    S.copy("act", gT[:, ss], ptr.all())
                S.dma("pool", V(GTS.ap[:, t0:t0 + Wd], GTS.res), gT[:, :Wd])
        S.barrier()
        P.flush()


def phase_D(P, W, MOD, xT2, XN2, GTS, moe, do_ctx):
    S = P.S
    nE = 8 if moe else 1
    with ExitStack() as st:
        w1 = P.sb(st, "fw1", [128, KD, HFF], BF16)
        w3 = P.sb(st, "fw3", [128, KD, HFF], BF16)
        w2 = P.sb(st, "fw2", [128, NFT, D], BF16)
        stg = [P.sb(st, f"fstg{i}", [128, 2816]) for i in range(2)]
        XNt = [P.sb(st, f"fXN{i}", [128, KD, 512], BF16) for i in range(2)]
        Xa = [P.sb(st, f"fXa{i}", [128, KD, 512]) for i in range(2)]
        hT = P.sb(st, "fhT", [128, NFT, 512], BF16)
        sl = [P.sb(st, f"fsl{i}", [128, 512]) for i in range(2)]
        tg = [P.sb(st, f"ftg{i}", [128, 512]) for i in range(2)]
        p1 = [P.ps(st, f"fp1_{i}", [128, 512]) for i in range(2)]
        p3 = [P.ps(st, f"fp3_{i}", [128, 512]) for i in range(2)]
        po = [P.ps(st, f"fpo{i}", [128, 512]) for i in range(2)]
        if moe:
            gTs = P.sb(st, "fgTs", [8, P.NT], BF16)
            selE = P.sb(st, "fselE", [8, 8, 128], BF16)
            gateB = P.sb(st, "fgateB", [128, 512])
            pg = P.ps(st, "fpg", [128, 512])
            c0_ = 0 if do_ctx else CTX
            S.dma("sp", gTs[:, c0_:], GTS[:, c0_:])
            S.dma("sp", selE.all(), W["selE"].all())
        x2v = xT2.ap.rearrange("k p t -> p k t")
        xnv = XN2.ap.rearrange("k p t -> p k t")
        tiles = P.tiles if do_ctx else P.tiles[1:]
        cast_eng = ("dve", "act", "pool")
        ci = 0
        qi = 0
        for e in range(nE):
            for hh in range(2):
                hs = slice(hh * HFF, (hh + 1) * HFF)
                if moe:
                    a1, a3, a2 = W["ffn_w1"].ap[e], W["ffn_w3"].ap[e], W["ffn_w2"].ap[e]
                else:
                    a1, a3, a2 = W["ffn_w1"].ap, W["ffn_w3"].ap, W["ffn_w2"].ap
                for (src, dst, rw_) in ((a1, w1, W["ffn_w1"]), (a3, w3, W["ffn_w3"])):
                    sv = src[:, hs].rearrange("(kc p) n -> p kc n", p=128)
                    for pc in range(4):
                        sg = stg[qi % 2]
                        qi += 1
                        cs = slice(pc * 352, (pc + 1) * 352)
                        S.dma("sp" if qi % 2 == 0 else "act", sg.all().re("p (k n) -> p k n", k=KD), V(sv[:, :, cs], rw_.res))
                        S.copy(cast_eng[ci % 3], dst[:, :, cs], sg.all().re("p (k n) -> p k n", k=KD))
                        ci += 1
                sv = a2[hs, :].rearrange("(ft p) n -> p ft n", p=128)
                for pc in range(4):
                    sg = stg[qi % 2]
                    qi += 1
                    cs = slice(pc * 256, (pc + 1) * 256)
                    S.dma("sp" if qi % 2 == 0 else "act", sg.all().re("p (k n) -> p k n", k=NFT), V(sv[:, :, cs], W["ffn_w2"].res))
                    S.copy(cast_eng[ci % 3], w2[:, :, cs], sg.all().re("p (k n) -> p k n", k=NFT))
                    ci += 1
                for n, (t0, Wd) in enumerate(tiles):
                    col = 1 if t0 == 0 else 0
                    xn, xa = XNt[n % 2], Xa[n % 2]
                    S.dma("sp", xn[:, :, :Wd], V(xnv[:, :, t0:t0 + Wd], XN2.res))
                    S.dma("sp", xa[:, :, :Wd], V(x2v[:, :, t0:t0 + Wd], xT2.k(t0, slice(None)).res))
                    if moe:
                        S.mm(pg[:, :Wd], selE[:, e, :], gTs[:, t0:t0 + Wd])
                        S.copy("act", gateB[:, :Wd], pg[:, :Wd])
                    for ft in range(NFT):
                        a, b = p1[ft % 2], p3[ft % 2]
                        fs = slice(ft * 128, (ft + 1) * 128)
                        for kc in range(KD):
                            S.mm(a[:, :Wd], w1[:, kc, fs], xn[:, kc, :Wd], start=(kc == 0), stop=(kc == KD - 1))
                        for kc in range(KD):
                            S.mm(b[:, :Wd], w3[:, kc, fs], xn[:, kc, :Wd], start=(kc == 0), stop=(kc == KD - 1))
                        s_ = sl[ft % 2]
                        S.act(s_[:, :Wd], a[:, :Wd], AF.Silu)
                        S.tt("dve", hT[:, ft, :Wd], s_[:, :Wd], b[:, :Wd], ALU.mult)
                    for dt in range(KD):
                        p = po[dt % 2]
                        for ft in range(NFT):
                            S.mm(p[:, :Wd], w2[:, ft, dt * 128:(dt + 1) * 128], hT[:, ft, :Wd], start=(ft == 0), stop=(ft == NFT - 1))
                        if moe:
                            t_ = tg[dt % 2]
                            S.tt("dve", t_[:, :Wd], p[:, :Wd], gateB[:, :Wd], ALU.mult)
                            S.stt(xa[:, dt, :Wd], t_[:, :Wd], MOD[:, 40 + dt, col:col + 1], xa[:, dt, :Wd], ALU.mult, ALU.add)
                        else:
                            S.stt(xa[:, dt, :Wd], p[:, :Wd], MOD[:, 40 + dt, col:col + 1], xa[:, dt, :Wd], ALU.mult, ALU.add)
                    S.dma("pool", V(x2v[:, :, t0:t0 + Wd], xT2.k(t0, slice(None)).res), xa[:, :, :Wd])
        S.barrier()
        P.flush()


def phase_final(P, W, xT2, yT):
    S = P.S
    with ExitStack() as st:
        ones_bf = P.sb(st, "zones", [128, 128], BF16)
        fws = P.sb(st, "zfw", [128, KD, 1])
        zb = P.sb(st, "zzb", [128, KD, 1])
        S.dma("sp", ones_bf.all(), W["ones_bf"].all())
        S.dma("sp", fws.all(), W["fw"].all())
        S.memset("dve", zb.all(), 0.0)
        X = [P.sb(st, f"zX{i}", [128, KD, 512]) for i in range(2)]
        XN = [P.sb(st, f"zXN{i}", [128, KD, 512]) for i in range(2)]
        sqb = P.sb(st, "zsqb", [128, KD, 512], BF16)
        tmpf = P.sb(st, "ztmpf", [128, KD, 512])
        rstd = P.sb(st, "zrstd", [128, 512])
        psn = P.ps(st, "zpsn", [128, 512])
        xv = xT2.ap.rearrange("k p t -> p k t")
        yv = yT.ap.rearrange("k p t -> p k t")
        for i, (t0, Wd) in enumerate(P.tiles[1:]):
            S.dma("sp", X[i % 2].all(), V(xv[:, :, t0:t0 + Wd], xT2.k(t0, slice(None)).res))
            norm_mod(P, S, X[i % 2], XN[i % 2], sqb, psn, rstd, tmpf, ones_bf, fws, zb, 0, 512)
            S.dma("pool", V(yv[:, :, t0 - CTX:t0 - CTX + Wd], yT.res), XN[i % 2].all())
        S.barrier()
        P.flush()


W_ST = [("init_att", lambda P: [2, 128, 6, 64], F32), ("init_s5", lambda P: [2, 128, 2, 8], F32)]


def build_La(TL, l, NL=4):
    P = Prog(TL, NL=NL)
    W = declare(P, W_A)
    sc = alloc_scratch(P)
    st_att = P.dout("st_att", [4, 128, 6, 64], F32)
    st_s5 = P.dout("st_s5", [4, 128, 2, 8], F32)
    with ExitStack() as st:
        MOD = phase_mod(P, st, W, 16)
        lbv = compute_lbv(P, st, W, l)
        phase_A(P, W, MOD, sc, lbv, so=True)
        with ExitStack() as st2:
            A = attn_alloc(P, st2, W)
            for d in range(2):
                attn_pass(P, A, sc, d, False, None, V(st_att.ap[d], st_att.res), V(st_att.ap[2 + d], st_att.res), None)
            P.S.barrier()
            P.flush()
        with ExitStack() as st2:
            B = s5_alloc(P, st2, W)
            for d in range(2):
                s5_pass(P, B, sc, d, False, None, V(st_s5.ap[d], st_s5.res), V(st_s5.ap[2 + d], st_s5.res), None)
            P.S.barrier()
            P.flush()
    return P.nc


def build_Lb(TL, l, moe, last, NL=4):
    P = Prog(TL, NL=NL)
    NT = P.NT
    specs = W_A + W_ST + W_C
    if moe:
        specs = specs + W_MOE + [("ffn_w1", lambda P: [8, D, DFF], F32), ("ffn_w3", lambda P: [8, D, DFF], F32), ("ffn_w2", lambda P: [8, DFF, D], F32)]
    else:
        specs = specs + [("ffn_w1", lambda P: [D, DFF], F32), ("ffn_w3", lambda P: [D, DFF], F32), ("ffn_w2", lambda P: [DFF, D], F32)]
    if last:
        specs = specs + [("fw", lambda P: [128, KD, 1], F32)]
    W = declare(P, specs)
    sc = alloc_scratch(P)
    YA = P.dscr("YA", [6, 128, NT], F32)
    YS = P.dscr("YS", [2, 128, NT], F32)
    XN2 = P.dscr("XN2", [KD, 128, NT], BF16)
    GTS = P.dscr("GTS", [8, NT], BF16)
    if last:
        xT2 = P.dscr("xT2", [KD, 128, NT], F32)
        yT = P.dout("yT", [KD, 128, P.TL], F32)
    else:
        xT2 = P.dout("xT2", [KD, 128, NT], F32)
    with ExitStack() as st:
        MOD = phase_mod(P, st, W, 48)
        lbv = compute_lbv(P, st, W, l)
        phase_A(P, W, MOD, sc, lbv)
        with ExitStack() as st2:
            A = attn_alloc(P, st2, W)
            for d in range(2):
                attn_pass(P, A, sc, d, True, V(W["init_att"].ap[d], W["init_att"].res), None, None, YA)
            P.S.barrier()
            P.flush()
        with ExitStack() as st2:
            B = s5_alloc(P, st2, W)
            for d in range(2):
                s5_pass(P, B, sc, d, True, V(W["init_s5"].ap[d], W["init_s5"].res), None, None, YS)
            P.S.barrier()
            P.flush()
        phase_C(P, W, MOD, sc, YA, YS, xT2, XN2, GTS, moe, not last)
        phase_D(P, W, MOD, xT2, XN2, GTS, moe, not last)
        if last:
            phase_final(P, W, xT2, yT)
    return P.nc


def consts_all():
    c = consts_np()
    c.update(attn_consts_np())
    bm = np.zeros((128, 128), np.float32)
    bm[:64, :64] = 1.0 / 64
    bm[64:, 64:] = 1.0 / 64
    c["blockmean"] = bm.astype(ml_dtypes.bfloat16)
    sel = np.zeros((8, 8, 128), np.float32)
    for e in range(8):
        sel[e, e, :] = 1.0
    c["selE"] = sel.astype(ml_dtypes.bfloat16)
    return c


def layer_inputs(inp, l, depth):
    d = layer_consts(inp, l)
    d.update(s5_layout_np(inp, l))
    d.update(consts_all())
    d["w_out"] = np.ascontiguousarray(inp["w_out"][l])
    d["hn_w"] = np.ascontiguousarray(np.concatenate([fm_vec(inp["ret_gn_w"][l]), fm_vec(inp["hg_norm_w"][l]), fm_vec(inp["gla_norm_w"][l])], axis=1))
    j = l // 2
    if l % 2 == 0:
        d["ffn_w1"] = np.ascontiguousarray(inp["ffn_w1"][j])
        d["ffn_w3"] = np.ascontiguousarray(inp["ffn_w3"][j])
        d["ffn_w2"] = np.ascontiguousarray(inp["ffn_w2"][j])
    else:
        d["ffn_w1"] = np.ascontiguousarray(inp["moe_w1"][j])
        d["ffn_w3"] = np.ascontiguousarray(inp["moe_w3"][j])
        d["ffn_w2"] = np.ascontiguousarray(inp["moe_w2"][j])
        d["router_w"] = np.ascontiguousarray(inp["router_w"][j])
        d["router_b"] = np.ascontiguousarray(np.asarray(inp["router_b"][j]).reshape(1, 8))
    d["fw"] = np.ascontiguousarray(fm_vec(inp["final_norm_w"]).reshape(128, KD, 1))
    return d


def names_of(specs):
    return [s[0] for s in specs]


def kernel(**inputs):
    inp = {k: np.asarray(v) for k, v in inputs.items()}
    x = inp["x"]
    B, SEQ, _ = x.shape
    depth = inp["ada_w"].shape[0]
    TL = SEQ // 2
    ncore = 2 * B
    cores = [(b, s) for b in range(B) for s in range(2)]
    xT = [to_fm(np.concatenate([inp["ctx"][b], x[b, s * TL:(s + 1) * TL]], axis=0)) for (b, s) in cores]
    ropes = [rope_tables(TL, s) for s in range(2)]
    conds = [core_cond(inp, b) for b in range(B)]
    out = np.empty_like(x)
    for l in range(depth):
        moe = (l % 2 == 1)
        last = (l == depth - 1)
        li = layer_inputs(inp, l, depth)
        nca = build_La(TL, l, NL=depth)
        maps = []
        for ci, (b, s) in enumerate(cores):
            m = {k: li[k] for k in names_of(W_A) if k in li}
            m["xT"] = xT[ci]
            m["cond"] = conds[b]
            m["rope"] = ropes[s]
            maps.append(m)
        ra = run_bass_kernel_spmd(nca, maps, core_ids=list(range(ncore))).results
        nb_specs = W_A + W_ST + W_C + (W_MOE if moe else [])
        ncb = build_Lb(TL, l, moe, last, NL=depth)
        maps = []
        for ci, (b, s) in enumerate(cores):
            m = {k: li[k] for k in names_of(nb_specs) if k in li}
            for k in ("ffn_w1", "ffn_w3", "ffn_w2"):
                m[k] = li[k]
            if last:
                m["fw"] = li["fw"]
            m["xT"] = xT[ci]
            m["cond"] = conds[b]
            m["rope"] = ropes[s]
            pa = ra[ci ^ 1]
            own = ra[ci]
            if s == 0:
                m["init_att"] = np.stack([np.asarray(own["st_att"])[0], np.asarray(pa["st_att"])[3]])
                m["init_s5"] = np.stack([np.asarray(own["st_s5"])[0], np.asarray(pa["st_s5"])[3]])
            else:
                m["init_att"] = np.stack([np.asarray(pa["st_att"])[2], np.asarray(own["st_att"])[1]])
                m["init_s5"] = np.stack([np.asarray(pa["st_s5"])[2], np.asarray(own["st_s5"])[1]])
            maps.append(m)
        rb = run_bass_kernel_spmd(ncb, maps, core_ids=list(range(ncore))).results
        if last:
            for ci, (b, s) in enumerate(cores):
                out[b, s * TL:(s + 1) * TL] = from_fm(np.asarray(rb[ci]["yT"]))
        else:
            xT = [np.asarray(rb[ci]["xT2"]) for ci in range(ncore)]
    return out
```

```python
import numpy as np
import ml_dtypes
import concourse.bass as bass
import concourse.mybir as mybir
from concourse.bass_utils import run_bass_kernel_spmd

F32 = mybir.dt.float32
BF16 = mybir.dt.bfloat16
AF = mybir.ActivationFunctionType
ALU = mybir.AluOpType
AX = mybir.AxisListType

D = 1024
KD = 8
CTX = 256
L = 128
NPT = 27
NCOL = NPT * 128
DFF = 2816
HFF = 1408
NFT = 11


class Res:
    __slots__ = ("w", "r")

    def __init__(self):
        self.w = None
        self.r = {}


class V:
    __slots__ = ("ap", "res")

    def __init__(self, ap, res):
        self.ap = ap
        self.res = res

    def __getitem__(self, idx):
        return V(self.ap[idx], self.res)

    def bc(self, shape):
        return V(self.ap.broadcast_to(shape), self.res)

    def re(self, pat, **kw):
        return V(self.ap.rearrange(pat, **kw), self.res)


class T:
    def __init__(self, ap):
        self.ap = ap
        self.res = Res()
        self.keyed = {}

    def __getitem__(self, idx):
        return V(self.ap[idx], self.res)

    def k(self, key, idx):
        r = self.keyed.get(key)
        if r is None:
            r = self.keyed[key] = Res()
        return V(self.ap[idx], r)

    def all(self):
        return V(self.ap, self.res)


class Eng:
    def __init__(self, key, sem):
        self.key = key
        self.sem = sem
        self.cnt = 0
        self.seen = {}
        self.ops = []


class Sched:
    def __init__(self, nc, stack, ndma=12):
        self.nc = nc
        self.stack = stack
        self.eng = {}
        for k in ("pe", "act", "dve", "pool", "sp"):
            self.eng[k] = Eng(k, stack.enter_context(nc.semaphore("s_" + k)))
        self.dsem = {}
        self.dq = {}
        for q in ("sp", "pool", "act"):
            self.dq[q] = 0
            for i in range(ndma):
                self.dsem[(q, i)] = [stack.enter_context(nc.semaphore(f"d_{q}_{i}")), 0]
        self.ndma = ndma
        self.n_inst = 0

    def semof(self, key):
        if isinstance(key, tuple):
            return self.dsem[key][0]
        return self.eng[key].sem

    def _deps(self, e, reads, writes):
        deps = {}

        def add(k, c):
            if deps.get(k, 0) < c:
                deps[k] = c

        for v in reads:
            if v.res.w is not None:
                add(*v.res.w)
        for v in writes:
            if v.res.w is not None:
                add(*v.res.w)
            for k, c in v.res.r.items():
                add(k, c)
        for k, c in deps.items():
            if k == "pe" and e.key == "pe":
                continue
            if e.seen.get(k, 0) < c:
                e.ops.append(("w", self.semof(k), c))
                e.seen[k] = c
                self.n_inst += 1

    def emit(self, ek, fn, reads, writes):
        e = self.eng[ek]
        self._deps(e, reads, writes)
        e.cnt += 1
        e.ops.append(("o", fn))
        self.n_inst += 1
        for v in writes:
            v.res.w = (ek, e.cnt)
            v.res.r = {}
        for v in reads:
            v.res.r[ek] = e.cnt

    def dma(self, q, out, in_, **kw):
        e = self.eng[q]
        self._deps(e, [in_], [out])
        i = self.dq[q]
        self.dq[q] = (i + 1) % self.ndma
        key = (q, i)
        ds = self.dsem[key]
        if e.seen.get(key, 0) < ds[1]:
            e.ops.append(("w", ds[0], ds[1]))
            e.seen[key] = ds[1]
        ds[1] += 16
        e.ops.append(("d", out.ap, in_.ap, ds[0], kw))
        self.n_inst += 1
        out.res.w = (key, ds[1])
        out.res.r = {}
        in_.res.r[key] = ds[1]

    def barrier(self):
        for e in self.eng.values():
            for o in self.eng.values():
                if o.key != e.key and o.cnt > 0 and e.seen.get(o.key, 0) < o.cnt:
                    e.ops.append(("w", o.sem, o.cnt))
                    e.seen[o.key] = o.cnt
            for key, ds in self.dsem.items():
                if ds[1] > 0 and e.seen.get(key, 0) < ds[1]:
                    e.ops.append(("w", ds[0], ds[1]))
                    e.seen[key] = ds[1]

    def finish(self):
        self.barrier()
        nc = self.nc
        with nc.Block() as block:
            def run(e, h):
                for op in e.ops:
                    if op[0] == "w":
                        h.wait_ge(op[1], op[2])
                    elif op[0] == "o":
                        op[1](h).then_inc(e.sem, 1)
                    else:
                        h.dma_start(out=op[1], in_=op[2], **op[4]).then_inc(op[3], 16)

            @block.tensor
            def _(h):
                run(self.eng["pe"], h)

            @block.scalar
            def _(h):
                run(self.eng["act"], h)

            @block.vector
            def _(h):
                run(self.eng["dve"], h)

            @block.gpsimd
            def _(h):
                run(self.eng["pool"], h)

            @block.sync
            def _(h):
                run(self.eng["sp"], h)

    def mm(self, out, lhsT, rhs, start=True, stop=True):
        self.emit("pe", lambda h: h.matmul(out.ap, lhsT.ap, rhs.ap, start=start, stop=stop),
                  [lhsT, rhs] + ([] if start else [out]), [out])

    def tr(self, out, in_, ident):
        self.emit("pe", lambda h: h.transpose(out.ap, in_.ap, ident.ap), [in_, ident], [out])

    def act(self, out, in_, func, bias=None, scale=None, accum=None):
        reads = [in_]
        kw = {}
        if bias is not None:
            if isinstance(bias, V):
                reads.append(bias)
                kw["bias"] = bias.ap
            else:
                kw["bias"] = float(bias)
        if scale is not None:
            if isinstance(scale, V):
                reads.append(scale)
                kw["scale"] = scale.ap
            else:
                kw["scale"] = float(scale)
        writes = [out]
        if accum is not None:
            kw["accum_out"] = accum.ap
            writes.append(accum)
        self.emit("act", lambda h: h.activation(out.ap, in_.ap, func, **kw), reads, writes)

    def ts(self, ek, out, in0, s1, op0, s2=None, op1=None):
        reads = [in0]
        a1 = s1
        a2 = s2
        if isinstance(s1, V):
            reads.append(s1)
            a1 = s1.ap
        if isinstance(s2, V):
            reads.append(s2)
            a2 = s2.ap
        if op1 is None:
            self.emit(ek, lambda h: h.tensor_scalar(out.ap, in0.ap, a1, None, op0), reads, [out])
        else:
            self.emit(ek, lambda h: h.tensor_scalar(out.ap, in0.ap, a1, a2, op0, op1), reads, [out])

    def tt(self, ek, out, a, b, op):
        self.emit(ek, lambda h: h.tensor_tensor(out.ap, a.ap, b.ap, op), [a, b], [out])

    def stt(self, out, in0, sc, in1, op0, op1):
        reads = [in0, in1]
        a = sc
        if isinstance(sc, V):
            reads.append(sc)
            a = sc.ap
        self.emit("dve", lambda h: h.scalar_tensor_tensor(out.ap, in0.ap, a, in1.ap, op0, op1), reads, [out])

    def scan(self, out, d0, d1, init, op0=ALU.mult, op1=ALU.add):
        reads = [d0, d1]
        a = init
        if isinstance(init, V):
            reads.append(init)
            a = init.ap
        self.emit("dve", lambda h: h.tensor_tensor_scan(out.ap, d0.ap, d1.ap, a, op0, op1), reads, [out])

    def copy(self, ek, out, in_):
        if ek == "act":
            self.emit("act", lambda h: h.copy(out.ap, in_.ap), [in_], [out])
        else:
            self.emit(ek, lambda h: h.tensor_copy(out.ap, in_.ap), [in_], [out])

    def memset(self, ek, out, val):
        self.emit(ek, lambda h: h.memset(out.ap, val), [], [out])

    def recip(self, out, in_):
        self.emit("dve", lambda h: h.reciprocal(out.ap, in_.ap), [in_], [out])

    def red(self, out, in_, op):
        self.emit("dve", lambda h: h.tensor_reduce(out.ap, in_.ap, AX.X, op), [in_], [out])


from contextlib import ExitStack


class Prog:
    def __init__(self, TL, debug=False, NL=4):
        self.NL = NL
        self.nc = bass.Bass("TRN2", target_bir_lowering=False)
        self.stack = ExitStack()
        self.S = Sched(self.nc, self.stack)
        self.TL = TL
        self.NT = CTX + TL
        self.tiles = [(0, CTX)] + [(CTX + 512 * i, 512) for i in range(TL // 512)]
        self.debug = debug
        self.scr_kind = "ExternalOutput" if debug else "Internal"

    def din(self, name, shape, dt=F32):
        return T(self.nc.dram_tensor(name, list(shape), dt, kind="ExternalInput").ap())

    def dout(self, name, shape, dt=F32):
        return T(self.nc.dram_tensor(name, list(shape), dt, kind="ExternalOutput").ap())

    def dscr(self, name, shape, dt=F32):
        return T(self.nc.dram_tensor(name, list(shape), dt, kind=self.scr_kind).ap())

    def sb(self, st, name, shape, dt=F32):
        self._uid = getattr(self, "_uid", 0) + 1
        t = st.enter_context(self.nc.sbuf_tensor(f"sb{self._uid}_{name}", list(shape), dt))
        return T(t[tuple(slice(None) for _ in shape)])

    def ps(self, st, name, shape, dt=F32):
        self._uid = getattr(self, "_uid", 0) + 1
        t = st.enter_context(self.nc.psum_tensor(f"ps{self._uid}_{name}", list(shape), dt))
        return T(t[tuple(slice(None) for _ in shape)])

    def flush(self):
        self.S.finish()
        for e in self.S.eng.values():
            e.ops = []


def load_cast(P, st_name, dst, src_ap_fn, npieces, piece_shape, stg, eng_cycle=("dve", "pool")):
    raise NotImplementedError


def phase_mod(P, st, W, nj):
    S = P.S
    cond = P.sb(st, "cond", [128, KD, 2])
    csl = P.sb(st, "csl", [128, KD, 2])
    adab = P.sb(st, "adab", [128, 48])
    MOD = P.sb(st, "MOD", [128, 48, 2])
    S.dma("sp", cond.all(), W["cond"].all())
    S.dma("sp", adab.all(), W["ada_b"].all())
    S.act(csl.all(), cond.all(), AF.Silu)
    with ExitStack() as st2:
        pm = P.ps(st2, "pm", [128, 48, 2])
        stg = [P.sb(st2, f"adaw{i}", [128, KD, 768]) for i in range(2)]
        aw = W["ada_w"].ap.rearrange("(kc p) n -> p kc n", p=128)
        for pc in range((nj + 5) // 6):
            sg = stg[pc % 2]
            S.dma("sp" if pc % 2 == 0 else "pool", sg.all(), V(aw[:, :, pc * 768:(pc + 1) * 768], W["ada_w"].res))
            for jj in range(6):
                j = pc * 6 + jj
                if j >= nj:
                    break
                for kc in range(KD):
                    S.mm(pm[:, j, :], sg[:, kc, jj * 128:(jj + 1) * 128], csl[:, kc, :], start=(kc == 0), stop=(kc == KD - 1))
        S.tt("dve", MOD[:, 0:nj, :], pm[:, 0:nj, :], adab[:, 0:nj].re("p (j o) -> p j o", o=1).bc([128, nj, 2]), ALU.add)
        S.barrier()
        P.flush()
    return MOD


def norm_mod(P, S, X, XN, sqb, psn, rstd, tmpf, ones_bf, s_vec, b_vec, col, Wd):
    S.act(sqb[:, :, :Wd], X[:, :, :Wd], AF.Square)
    for kc in range(KD):
        S.mm(psn[:, :Wd], ones_bf.all(), sqb[:, kc, :Wd], start=(kc == 0), stop=(kc == KD - 1))
    S.ts("dve", rstd[:, :Wd], psn[:, :Wd], 1.0 / D, ALU.mult, 1e-6, ALU.add)
    S.act(rstd[:, :Wd], rstd[:, :Wd], AF.Sqrt)
    S.recip(rstd[:, :Wd], rstd[:, :Wd])
    for kc in range(KD):
        S.stt(tmpf[:, kc, :Wd], X[:, kc, :Wd], s_vec[:, kc, col:col + 1], rstd[:, :Wd], ALU.mult, ALU.mult)
        S.act(XN[:, kc, :Wd], tmpf[:, kc, :Wd], AF.Identity, bias=b_vec[:, kc, col:col + 1])


def consts_np():
    c = {}
    c["ones_bf"] = np.ones((128, 128), ml_dtypes.bfloat16)
    c["ident_f"] = np.eye(128, dtype=np.float32)
    c["ident_bf"] = np.eye(128).astype(ml_dtypes.bfloat16)
    return c


def to_fm(a):
    T_ = a.shape[0]
    return np.ascontiguousarray(a.T.reshape(KD, 128, T_))


def from_fm(a):
    return np.ascontiguousarray(a.reshape(D, -1).T)


def alloc_scratch(P):
    NT = P.NT
    sc = {}
    sc["QS"] = P.dscr("QS", [18, 128, NT], BF16)
    sc["GS"] = P.dscr("GS", [8, 128, NT], F32)
    sc["GT"] = P.dscr("GT", [8, 128, NT], BF16)
    sc["VS"] = P.dscr("VS", [NT, 768], BF16)
    return sc


def phase_A(P, W, MOD, sc, lbv, so=False):
    S = P.S
    with ExitStack() as st:
        Win = P.sb(st, "Win", [128, KD, NCOL], BF16)
        Wv = P.sb(st, "Wv", [128, KD, 768], BF16)
        wa2 = P.sb(st, "wa2", [128, 2, 256], BF16)
        wa2f = P.sb(st, "wa2f", [128, 2, 256])
        gba = P.sb(st, "gba", [128, 2, 2])
        n1w = P.sb(st, "n1w", [128, KD])
        s1 = P.sb(st, "s1", [128, KD, 2])
        ones_bf = P.sb(st, "ones", [128, 128], BF16)
        S.dma("sp", ones_bf.all(), W["ones_bf"].all())
        S.dma("sp", wa2f.all(), W["wa2p"].all())
        S.dma("sp", gba.all(), W["gla_ba"].all())
        S.dma("sp", n1w.all(), W["n1w"].all())
        S.copy("pool", wa2.all(), wa2f.all())
        S.ts("dve", s1.all(), MOD[:, 8:16, :], 1.0, ALU.add)
        S.tt("dve", s1.all(), s1.all(), n1w.all().re("p (k o) -> p k o", o=1).bc([128, KD, 2]), ALU.mult)
        win_v = W["w_in_arr"].ap.rearrange("(kc p) n -> p kc n", p=128)
        wv_v = W["w_v"].ap.rearrange("(kc p) n -> p kc n", p=128)
        with ExitStack() as st2:
            stg = [P.sb(st2, f"wstg{i}", [128, KD, 432]) for i in range(2)]
            for pc in range(8):
                sg = stg[pc % 2]
                S.dma("sp" if pc % 2 == 0 else "pool", sg.all(), V(win_v[:, :, pc * 432:(pc + 1) * 432], W["w_in_arr"].res))
                S.copy("dve" if pc % 2 == 0 else "act", Win[:, :, pc * 432:(pc + 1) * 432], sg.all())
            for pc in range(2):
                sg = stg[pc % 2]
                S.dma("sp", sg[:, :, 0:384], V(wv_v[:, :, pc * 384:(pc + 1) * 384], W["w_v"].res))
                S.copy("dve", Wv[:, :, pc * 384:(pc + 1) * 384], sg[:, :, 0:384])
            S.barrier()
            P.flush()
        X = [P.sb(st, f"X{i}", [128, KD, 512]) for i in range(2)]
        XN = P.sb(st, "XN", [128, KD, 512], BF16)
        sqb = P.sb(st, "sqb", [128, KD, 512], BF16)
        tmpf = P.sb(st, "tmpf", [128, KD, 512])
        rstd = P.sb(st, "rstd", [128, 512])
        rope = [P.sb(st, f"rope{i}", [128, 4, 512]) for i in range(1)]
        QSs = [P.sb(st, f"QSs{i}", [128, 18, 512], BF16) for i in range(1)]
        GSs = [P.sb(st, f"GSs{i}", [128, 8, 512]) for i in range(1)]
        GTs = [P.sb(st, f"GTs{i}", [128, 8, 512], BF16) for i in range(1)]
        Vs = [P.sb(st, f"Vs{i}", [128, 768], BF16) for i in range(3)]
        low = P.sb(st, "low", [128, 512], BF16)
        t1 = [P.sb(st, f"t1_{i}", [128, 512]) for i in range(2)]
        t2 = [P.sb(st, f"t2_{i}", [128, 512]) for i in range(2)]
        sg_ = [P.sb(st, f"sg_{i}", [128, 512]) for i in range(2)]
        psn = P.ps(st, "psn", [128, 512])
        pp = [P.ps(st, f"pp{i}", [128, 512]) for i in range(5)]
        pv = P.ps(st, "pv", [128, 1024])
        xv = W["xT"].ap.rearrange("k p t -> p k t")
        qsv = sc["QS"].ap.rearrange("n p t -> p n t")
        gsv = sc["GS"].ap.rearrange("n p t -> p n t")
        gtv = sc["GT"].ap.rearrange("n p t -> p n t")
        ppi = [0]

        def proj(pt, Wd):
            p = pp[ppi[0] % 5]
            ppi[0] += 1
            for kc in range(KD):
                S.mm(p[:, :Wd], Win[:, kc, pt * 128:(pt + 1) * 128], XN[:, kc, :Wd], start=(kc == 0), stop=(kc == KD - 1))
            return p

        for ti, (t0, Wd) in enumerate(P.tiles):
            col = 1 if ti == 0 else 0
            Xt = X[ti % 2]
            rp = rope[0]
            QSt, GSt, GTt = QSs[0], GSs[0], GTs[0]
            S.dma("sp", Xt[:, :, :Wd], V(xv[:, :, t0:t0 + Wd], W["xT"].res))
            S.dma("sp", rp[:, :, :Wd], V(W["rope"].ap[:, :, t0:t0 + Wd], W["rope"].res))
            norm_mod(P, S, Xt, XN, sqb, psn, rstd, tmpf, ones_bf, s1, MOD[:, 0:8, :], col, Wd)
            for which, (pt_a, pt_s, dst) in enumerate(((0, 4, 0), (2, 6, 6))):
                if so and which == 0:
                    continue
                for j in range(2):
                    pa = proj(pt_a + j, Wd)
                    pb = proj(pt_s + j, Wd)
                    a = t1[j]
                    b = t2[j]
                    S.tt("dve", a[:, :Wd], pa[:, :Wd], rp[:, 2 * which, :Wd], ALU.mult)
                    S.tt("dve", b[:, :Wd], pb[:, :Wd], rp[:, 2 * which + 1, :Wd], ALU.mult)
                    S.tt("pool", QSt[:, dst + j, :Wd], a[:, :Wd], b[:, :Wd], ALU.add)
                    if which == 1:
                        S.copy("pool", QSt[:, 12 + j, :Wd], QSt[:, 6 + j, :Wd])
            for j in range(2):
                if not so:
                    S.act(GTt[:, 0 + j, :Wd], proj(8 + j, Wd)[:, :Wd], AF.Silu)
                    S.act(GTt[:, 2 + j, :Wd], proj(18 + j, Wd)[:, :Wd], AF.Silu)
                    S.act(GTt[:, 4 + j, :Wd], proj(24 + j, Wd)[:, :Wd], AF.Silu)
                    S.copy("dve", QSt[:, 2 + j, :Wd], proj(12 + j, Wd)[:, :Wd])
                    S.copy("act", QSt[:, 4 + j, :Wd], proj(20 + j, Wd)[:, :Wd])
                S.copy("dve", GTt[:, 6 + j, :Wd], proj(10 + j, Wd)[:, :Wd])
                pk = proj(22 + j, Wd)
                S.ts("dve", QSt[:, 10 + j, :Wd], pk[:, :Wd], 32.0 ** -0.5, ALU.mult)
                S.copy("pool", QSt[:, 16 + j, :Wd], QSt[:, 10 + j, :Wd])
            for d in range(2):
                for j in range(2):
                    pz = proj(14 + 2 * d + j, Wd)
                    sg = sg_[j]
                    S.act(sg[:, :Wd], pz[:, :Wd], AF.Sigmoid)
                    f = t1[j]
                    S.ts("dve", f[:, :Wd], sg[:, :Wd], lbv[:, j, 0:1], ALU.mult, lbv[:, j, 1:2], ALU.add)
                    S.act(GSt[:, 4 * d + j, :Wd], f[:, :Wd], AF.Ln)
                    S.ts("dve", QSt[:, (8 if d == 0 else 14) + j, :Wd], sg[:, :Wd], lbv[:, j, 2:3], ALU.mult, lbv[:, j, 0:1], ALU.add)
            S.copy("dve", low[:, :Wd], proj(26, Wd)[:, :Wd])
            for d in range(2):
                for j in range(2):
                    p = pp[ppi[0] % 5]
                    ppi[0] += 1
                    S.mm(p[:, :Wd], wa2[32 * d:32 * d + 16, d, j * 128:(j + 1) * 128], low[32 * d:32 * d + 16, :Wd])
                    sg = sg_[j]
                    S.act(sg[:, :Wd], p[:, :Wd], AF.Sigmoid, bias=gba[:, d, j:j + 1])
                    S.act(sg[:, :Wd], sg[:, :Wd], AF.Ln)
                    S.ts("pool", GSt[:, 4 * d + 2 + j, :Wd], sg[:, :Wd], 1.0 / 16.0, ALU.mult)
            for sub in range(Wd // 128):
                for kc in range(KD):
                    S.mm(pv[:, 0:512], XN[:, kc, sub * 128:(sub + 1) * 128], Wv[:, kc, 0:512], start=(kc == 0), stop=(kc == KD - 1))
                for kc in range(KD):
                    S.mm(pv[:, 512:768], XN[:, kc, sub * 128:(sub + 1) * 128], Wv[:, kc, 512:768], start=(kc == 0), stop=(kc == KD - 1))
                vt = Vs[sub % 3]
                S.copy("act", vt.all(), pv[:, 0:768])
                S.dma("pool", V(sc["VS"].ap[t0 + sub * 128:t0 + (sub + 1) * 128, :], sc["VS"].res), vt.all())
            if so:
                S.dma("pool", V(qsv[:, 6:18, t0:t0 + Wd], sc["QS"].res), QSt[:, 6:18, :Wd])
                S.dma("pool", V(gtv[:, 6:8, t0:t0 + Wd], sc["GT"].res), GTt[:, 6:8, :Wd])
            else:
                S.dma("pool", V(qsv[:, :, t0:t0 + Wd], sc["QS"].res), QSt[:, :, :Wd])
                S.dma("pool", V(gtv[:, :, t0:t0 + Wd], sc["GT"].res), GTt[:, :, :Wd])
            S.dma("pool", V(gsv[:, :, t0:t0 + Wd], sc["GS"].res), GSt[:, :, :Wd])
        S.barrier()
        P.flush()


def compute_lbv(P, st, W, l):
    S = P.S
    NL = P.NL
    lg = P.sb(st, "lbl", [128, 2, NL])
    ex = P.sb(st, "lbe", [128, 2, NL])
    mx = P.sb(st, "lbm", [128, 2])
    sm = P.sb(st, "lbs", [128, 2])
    pa = P.sb(st, "lbp", [128, 2])
    lbv = P.sb(st, "lbv", [128, 2, 3])
    S.dma("sp", lg.all(), W["hg_lb"].all())
    S.red(mx.all(), lg.all(), ALU.max)
    S.tt("dve", ex.all(), lg.all(), mx.all().re("p (j o) -> p j o", o=1).bc([128, 2, NL]), ALU.subtract)
    S.act(ex.all(), ex.all(), AF.Exp)
    S.red(sm.all(), ex.all(), ALU.add)
    S.recip(sm.all(), sm.all())
    if l == 0:
        S.memset("dve", pa.all(), 0.0)
    else:
        S.red(pa.all(), ex[:, :, 1:l + 1], ALU.add)
        S.tt("dve", pa.all(), pa.all(), sm.all(), ALU.mult)
        S.ts("dve", pa.all(), pa.all(), 0.0, ALU.max, 1.0 - 1e-6, ALU.min)
    S.copy("dve", lbv[:, :, 1], pa.all())
    S.ts("dve", lbv[:, :, 0], pa.all(), -1.0, ALU.mult, 1.0, ALU.add)
    S.ts("dve", lbv[:, :, 2], pa.all(), 1.0, ALU.mult, -1.0, ALU.add)
    return lbv


def fm_vec(v, n=None):
    v = np.asarray(v, np.float32)
    return np.ascontiguousarray(v.reshape(-1, 128).T)


def pad_gla(a):
    out = np.zeros(a.shape[:-1] + (256,), a.dtype)
    for h in range(4):
        out[..., h * 64:h * 64 + 32] = a[..., h * 32:(h + 1) * 32]
    return out


def rope_tables(TL, half):
    NT = CTX + TL
    r = np.arange(128)
    w = r % 64
    part = w // 32
    u = w % 32
    f = u % 16
    freqs = (10000.0 ** (-(f.astype(np.float64)) / 16.0))
    tg = half * TL + np.arange(TL)
    pos = np.where(part[:, None] == 0, (tg // 64)[None, :], (tg % 64)[None, :]).astype(np.float64)
    ang = pos.astype(np.float32).astype(np.float64) * freqs.astype(np.float32).astype(np.float64)[:, None]
    cos = np.cos(ang)
    sin = np.sin(ang) * np.where(u < 16, -1.0, 1.0)[:, None]
    tab = np.zeros((128, 4, NT), np.float32)
    tab[:, 0, :CTX] = 1.0
    tab[:, 2, :CTX] = 0.125
    tab[:, 0, CTX:] = cos
    tab[:, 1, CTX:] = sin
    tab[:, 2, CTX:] = 0.125 * cos
    tab[:, 3, CTX:] = 0.125 * sin
    return tab


def arrange_w_in(w_in):
    w_in = np.asarray(w_in, np.float32)
    c = np.arange(256)
    h, w = c // 64, c % 64
    perm = h * 64 + (w // 32) * 32 + ((w % 32) + 16) % 32
    lowt = np.zeros((D, 128), np.float32)
    lowt[:, 0:16] = w_in[:, 3328:3344]
    lowt[:, 32:48] = w_in[:, 3344:3360]
    parts = [w_in[:, 0:256], w_in[:, 256:512], w_in[:, 0:256][:, perm], w_in[:, 256:512][:, perm],
             w_in[:, 768:1024], w_in[:, 1024:1280], w_in[:, 1280:1536], w_in[:, 1536:1792], w_in[:, 1792:2048],
             w_in[:, 2304:2560], pad_gla(w_in[:, 2560:2688]), pad_gla(w_in[:, 2688:2816]), w_in[:, 3072:3328], lowt]
    arr = np.ascontiguousarray(np.concatenate(parts, axis=1))
    assert arr.shape == (D, NCOL)
    wv = np.ascontiguousarray(np.concatenate([w_in[:, 512:768], w_in[:, 2048:2304], w_in[:, 2816:3072]], axis=1))
    return arr, wv


def layer_consts(inp, l):
    d = {}
    d["ada_w"] = np.ascontiguousarray(inp["ada_w"][l])
    d["ada_b"] = fm_vec(inp["ada_b"][l])
    d["n1w"] = fm_vec(inp["norm1_w"][l])
    d["n2w"] = fm_vec(inp["norm2_w"][l])
    d["w_in_arr"], d["w_v"] = arrange_w_in(inp["w_in"][l])
    wa2p = np.zeros((128, 2, 256), np.float32)
    wa2p[0:16, 0, :] = pad_gla(np.asarray(inp["gla_wa2"][l][0]))
    wa2p[32:48, 1, :] = pad_gla(np.asarray(inp["gla_wa2"][l][1]))
    d["wa2p"] = wa2p
    ba = pad_gla(np.asarray(inp["gla_ba"][l]))
    d["gla_ba"] = np.ascontiguousarray(ba.reshape(2, 2, 128).transpose(2, 0, 1))
    d["hg_lb"] = np.ascontiguousarray(np.asarray(inp["hg_lb_logits"]).reshape(-1, 2, 128).transpose(2, 1, 0))
    d.update(consts_np())
    return d


def core_cond(inp, b):
    c = np.stack([fm_vec(inp["c"][b]), fm_vec(inp["c_ctx"])], axis=-1)
    return np.ascontiguousarray(c)


def build_test_A(TL, l, NL=4):
    P = Prog(TL, debug=True, NL=NL)
    W = {}
    NT = P.NT
    for name, shape, dt in [("xT", [KD, 128, NT], F32), ("cond", [128, KD, 2], F32), ("ada_w", [D, 6 * D], F32),
                            ("ada_b", [128, 48], F32), ("n1w", [128, KD], F32), ("w_in_arr", [D, NCOL], F32),
                            ("w_v", [D, 768], F32), ("wa2p", [128, 2, 256], F32), ("gla_ba", [128, 2, 2], F32),
                            ("hg_lb", [128, 2, P.NL], F32), ("rope", [128, 4, NT], F32), ("ones_bf", [128, 128], BF16)]:
        W[name] = P.din(name, shape, dt)
    sc = alloc_scratch(P)
    with ExitStack() as st:
        MOD = phase_mod(P, st, W, 16)
        lbv = compute_lbv(P, st, W, l)
        phase_A(P, W, MOD, sc, lbv)
    return P.nc


def attn_consts_np(mirror=False):
    lg = np.log1p(-(2.0 ** (-5.0 - np.arange(4, dtype=np.float32)))).astype(np.float32)
    if mirror:
        lg = lg[::-1]
    bret = np.zeros((128, 2, 2, 128), np.float32)
    t = np.arange(128, dtype=np.float32)
    for d in range(2):
        lgd = lg if d == 0 else lg[::-1]
        for tile in range(2):
            for hh in range(2):
                h = tile * 2 + hh
                cnt = (t + 1) if d == 0 else (128 - t)
                bret[hh * 64:(hh + 1) * 64, d, tile, :] = (cnt * lgd[h])[None, :]
    j = np.arange(128)[:, None]
    i = np.arange(128)[None, :]
    masks = np.stack([(j <= i), (j >= i)], axis=1).astype(np.float32)
    return {"bret": bret, "masks": np.ascontiguousarray(masks)}


class AttnBufs:
    pass


def attn_alloc(P, st, W):
    S = P.S
    A = AttnBufs()
    A.ones = P.sb(st, "a_ones", [128, 128])
    S.memset("dve", A.ones.all(), 1.0)
    A.masks = P.sb(st, "a_masks", [128, 2, 128])
    S.dma("sp", A.masks.all(), W["masks"].all())
    A.identb = P.sb(st, "a_identb", [128, 128], BF16)
    S.dma("sp", A.identb.all(), W["ident_bf"].all())
    A.B = []
    bretv = W["bret"]
    for d in range(2):
        bl = []
        for pp_ in range(2):
            b = P.sb(st, f"a_B{d}{pp_}", [128, 6, 128])
            S.dma("sp", b[:, 0:2, :], bretv[:, d, :, :])
            bl.append(b)
        A.B.append(bl)
    A.cc = 0
    A.E_ = [P.sb(st, f"a_E{i}", [128, 6, 128]) for i in range(2)]
    A.D1_ = [P.sb(st, f"a_D1{i}", [128, 6, 128]) for i in range(2)]
    A.Eq1_ = [P.sb(st, f"a_Eq1{i}", [128, 6, 64]) for i in range(2)]
    A.Ek1_ = [P.sb(st, f"a_Ek1{i}", [128, 6, 128]) for i in range(2)]
    A.Ek0_ = [P.sb(st, f"a_Ek0{i}", [128, 6, 64]) for i in range(2)]
    A.qh_ = [P.sb(st, f"a_qh{i}", [128, 6, 128], BF16) for i in range(2)]
    A.q1_ = [P.sb(st, f"a_q1{i}", [128, 6, 64], BF16) for i in range(2)]
    A.k1_ = [P.sb(st, f"a_k1{i}", [128, 6, 128], BF16) for i in range(2)]
    A.k0_ = [P.sb(st, f"a_k0{i}", [128, 6, 64], BF16) for i in range(2)]
    A.k1T_ = [P.sb(st, f"a_k1T{i}", [128, 6, 128], BF16) for i in range(2)]
    A.tS_ = [P.sb(st, f"a_tS{i}", [128, 6, 64]) for i in range(2)]
    A.Asb = [[[P.sb(st, f"a_A{d}{m}{i}", [128, 4, 128], BF16) for i in range(2)] for m in range(3)] for d in range(2)]
    for d in range(2):
        for m in range(3):
            for i in range(2):
                S.memset("pool", A.Asb[d][m][i].all(), 0.0)
    A.S = P.sb(st, "a_S", [128, 6, 64])
    A.Sbf = P.sb(st, "a_Sbf", [128, 6, 64], BF16)
    A.Q = [P.sb(st, f"a_Q{i}", [128, 6, 512], BF16) for i in range(2)]
    A.K = [P.sb(st, f"a_K{i}", [128, 6, 512], BF16) for i in range(2)]
    A.G = [P.sb(st, f"a_G{i}", [128, 4, 512]) for i in range(2)]
    A.Vt = [P.sb(st, f"a_V{i}", [128, 4, 768], BF16) for i in range(2)]
    A.Yo = [P.sb(st, f"a_Yo{i}", [128, 6, 512]) for i in range(2)]
    A.Yf = [P.sb(st, f"a_Yf{i}", [128, 6, 512]) for i in range(2)]
    A.psA = [P.ps(st, f"a_psA{m}", [128, 4, 128]) for m in range(3)]
    A.psO = P.ps(st, "a_psO", [128, 6, 128])
    A.psT = P.ps(st, "a_psT", [128, 6, 128], BF16)
    A.psS = P.ps(st, "a_psS", [128, 6, 64])
    return A


def attn_dirs(d):
    if d == 0:
        return slice(0, 64), slice(64, 128), 63, 127, 63
    return slice(64, 128), slice(0, 64), 64, 0, 0


def attn_stage1(P, A, d, par, Q, K, G, c0, full):
    S = P.S
    B = A.B[d][par]
    E, D1, Eq1, Ek1, Ek0 = A.E_[par], A.D1_[par], A.Eq1_[par], A.Ek1_[par], A.Ek0_[par]
    qh, q1, k1, k0, k1T = A.qh_[par], A.q1_[par], A.k1_[par], A.k0_[par], A.k1T_[par]
    cs = slice(c0, c0 + 128)
    H0, H1, ref, last, last1 = attn_dirs(d)
    rv = (lambda v: v) if d == 0 else (lambda v: v[:, ::-1])
    for j in range(4):
        S.scan(rv(B[:, 2 + j, :]), rv(A.ones.all()), rv(G[:, j, cs]), 0.0)
    bref = B[:, :, ref:ref + 1]
    S.tt("dve", D1.all(), B.all(), bref.bc([128, 6, 128]), ALU.subtract)
    S.act(Eq1.all(), D1[:, :, H1], AF.Exp)
    S.ts("dve", D1.all(), D1.all(), -1.0, ALU.mult, 80.0, ALU.min)
    S.act(Ek1.all(), D1.all(), AF.Exp)
    S.tt("dve", k1.all(), K[:, :, cs], Ek1.all(), ALU.mult)
    for j in range(6):
        S.tr(A.psT[:, j, :], k1[:, j, :], A.identb.all())
    S.copy("act", k1T.all(), A.psT.all())
    if full:
        S.act(E.all(), B.all(), AF.Exp)
        S.ts("pool", Ek0.all(), B[:, :, H0], -1.0, ALU.mult, 80.0, ALU.min)
        S.act(Ek0.all(), Ek0.all(), AF.Exp)
        S.tt("dve", qh.all(), Q[:, :, cs], E.all(), ALU.mult)
        S.tt("pool", q1.all(), Q[:, :, cs][:, :, H1], Eq1.all(), ALU.mult)
        S.tt("pool", k0.all(), K[:, :, cs][:, :, H0], Ek0.all(), ALU.mult)
        mask = A.masks[:, d, :]
        for m in range(3):
            psA = A.psA[m]
            for h in range(4):
                tile = 2 * m + h // 2
                rows = slice((h % 2) * 64, (h % 2) * 64 + 64)
                S.mm(psA[H0, h, H0], k0[rows, tile, :], qh[rows, tile, H0])
                S.mm(psA[:, h, H1], k1[rows, tile, :], q1[rows, tile, :])
            Asb = A.Asb[d][m][par]
            S.tt("dve", Asb[H0, :, H0], psA[H0, :, H0], mask[H0, H0].re("p (o i) -> p o i", o=1).bc([64, 4, 64]), ALU.mult)
            S.tt("dve", Asb[:, :, H1], psA[:, :, H1], mask[:, H1].re("p (o i) -> p o i", o=1).bc([128, 4, 64]), ALU.mult)
    else:
        S.act(E[:, :, last:last + 1], B[:, :, last:last + 1], AF.Exp)


def attn_stage2(P, A, d, par, Vt, c0, sub, full, Yo):
    S = P.S
    E, Eq1, qh, k1T, tS = A.E_[par], A.Eq1_[par], A.qh_[par], A.k1T_[par], A.tS_[par]
    cs = slice(c0, c0 + 128)
    H0, H1, ref, last, last1 = attn_dirs(d)
    if full:
        for m in range(3):
            Asb = A.Asb[d][m][par]
            for h in range(4):
                tile = 2 * m + h // 2
                rows = slice((h % 2) * 64, (h % 2) * 64 + 64)
                vc = slice(m * 256 + h * 64, m * 256 + h * 64 + 64)
                S.mm(A.psO[rows, tile, :], Vt[:, sub, vc], Asb[:, h, :], start=True, stop=False)
                S.mm(A.psO[rows, tile, :], A.Sbf[rows, tile, :], qh[rows, tile, :], start=False, stop=True)
        S.copy("act", Yo[:, :, cs], A.psO.all())
    for m in range(3):
        for h in range(4):
            tile = 2 * m + h // 2
            rows = slice((h % 2) * 64, (h % 2) * 64 + 64)
            vc = slice(m * 256 + h * 64, m * 256 + h * 64 + 64)
            S.mm(A.psS[rows, tile, :], k1T[:, tile, rows], Vt[:, sub, vc])
    e1 = E[:, :, last:last + 1].bc([128, 6, 64])
    e2 = Eq1[:, :, last1:last1 + 1].bc([128, 6, 64])
    S.tt("dve", tS.all(), A.psS.all(), e2, ALU.mult)
    S.tt("pool", A.S.all(), A.S.all(), e1, ALU.mult)
    S.tt("pool", A.S.all(), A.S.all(), tS.all(), ALU.add)
    S.copy("pool", A.Sbf.all(), A.S.all())


def attn_pass(P, A, sc, d, full, init_lat, out_ctx, out_fin, YA, Ysrc=None):
    S = P.S
    qsv = sc["QS"].ap.rearrange("n p t -> p n t")
    gsv = sc["GS"].ap.rearrange("n p t -> p n t")
    yav = YA.ap.rearrange("n p t -> p n t") if YA is not None else None
    if Ysrc is None:
        Ysrc = YA
    ysrcv = Ysrc.ap.rearrange("n p t -> p n t") if Ysrc is not None else None
    S.memset("pool", A.S.all(), 0.0)
    S.memset("pool", A.Sbf.all(), 0.0)
    lat = P.tiles[1:]
    order = [P.tiles[0]] + (lat if d == 0 else lat[::-1])
    chunks = []
    for n, (t0, Wd) in enumerate(order):
        nch = Wd // 128
        cis = list(range(nch)) if d == 0 else list(range(nch - 1, -1, -1))
        for idx, ci in enumerate(cis):
            chunks.append((n, t0, Wd, ci, idx == 0, idx == nch - 1))

    def s1(i):
        n, t0, Wd, ci, first, lastc = chunks[i]
        Q, K, G, Vt = A.Q[n % 2], A.K[n % 2], A.G[n % 2], A.Vt[n % 2]
        if first:
            if full:
                S.dma("sp", Q[:, :, :Wd], V(qsv[:, 0:6, t0:t0 + Wd], sc["QS"].res))
            ko = 6 if d == 0 else 12
            S.dma("sp", K[:, :, :Wd], V(qsv[:, ko:ko + 6, t0:t0 + Wd], sc["QS"].res))
            S.dma("sp", G[:, :, :Wd], V(gsv[:, 4 * d:4 * d + 4, t0:t0 + Wd], sc["GS"].res))
            S.dma("sp", Vt[:, :Wd // 128, :], V(sc["VS"].ap[t0:t0 + Wd, :].rearrange("(s p) c -> p s c", p=128), sc["VS"].res))
            if full and d == 1:
                S.dma("sp", A.Yf[n % 2][:, :, :Wd], V(ysrcv[:, :, t0:t0 + Wd], Ysrc.res))
        attn_stage1(P, A, d, i % 2, Q, K, G, ci * 128, full)

    def s2(i):
        n, t0, Wd, ci, first, lastc = chunks[i]
        Vt, Yo = A.Vt[n % 2], A.Yo[n % 2]
        attn_stage2(P, A, d, i % 2, Vt, ci * 128, ci, full, Yo)
        if lastc:
            if full:
                if d == 1:
                    S.tt("dve", Yo[:, :, :Wd], Yo[:, :, :Wd], A.Yf[n % 2][:, :, :Wd], ALU.add)
                S.dma("pool", V(yav[:, :, t0:t0 + Wd], YA.res), Yo[:, :, :Wd])
            if n == 0:
                if out_ctx is not None:
                    S.dma("pool", out_ctx, A.S.all())
                if init_lat is not None:
                    S.dma("sp", A.S.all(), init_lat)
                    S.copy("pool", A.Sbf.all(), A.S.all())

    s1(0)
    for i in range(len(chunks)):
        if i + 1 < len(chunks):
            s1(i + 1)
        s2(i)
    if out_fin is not None:
        S.dma("pool", out_fin, A.S.all())


def build_test_B(TL, l, NL=4, full=True):
    P = Prog(TL, debug=True, NL=NL)
    W = {}
    NT = P.NT
    for name, shape, dt in [("xT", [KD, 128, NT], F32), ("cond", [128, KD, 2], F32), ("ada_w", [D, 6 * D], F32),
                            ("ada_b", [128, 48], F32), ("n1w", [128, KD], F32), ("w_in_arr", [D, NCOL], F32),
                            ("w_v", [D, 768], F32), ("wa2p", [128, 2, 256], F32), ("gla_ba", [128, 2, 2], F32),
                            ("hg_lb", [128, 2, P.NL], F32), ("rope", [128, 4, NT], F32), ("ones_bf", [128, 128], BF16),
                            ("ident_bf", [128, 128], BF16), ("bret", [128, 2, 2, 128], F32), ("masks", [128, 2, 128], F32),
                            ("init_att", [2, 128, 6, 64], F32)]:
        W[name] = P.din(name, shape, dt)
    sc = alloc_scratch(P)
    YA = P.dscr("YA", [6, 128, NT], F32)
    st_out = P.dout("st_out", [4, 128, 6, 64], F32)
    with ExitStack() as st:
        MOD = phase_mod(P, st, W, 16)
        lbv = compute_lbv(P, st, W, l)
        phase_A(P, W, MOD, sc, lbv)
        with ExitStack() as st2:
            A = attn_alloc(P, st2, W)
            for d in range(2):
                attn_pass(P, A, sc, d, full, V(W["init_att"].ap[d], W["init_att"].res) if full else None,
                          V(st_out.ap[d], st_out.res), V(st_out.ap[2 + d], st_out.res), YA if full else None)
            P.S.barrier()
            P.flush()
    return P.nc


def s5_layout_np(inp, l):
    d = {}
    G, Pn, Cg = 16, 64, 16

    def sm(a):
        return np.ascontiguousarray(np.asarray(a, np.float32).reshape(2, 8, 128).transpose(2, 0, 1))

    d["s5_lre"] = sm(inp["s5_lam_re"][l])
    d["s5_lim"] = sm(inp["s5_lam_im"][l])
    dt = np.repeat(np.asarray(inp["s5_log_dt"][l], np.float32)[:, :, None], Pn, axis=2)
    d["s5_ldt"] = sm(dt)
    BT = np.zeros((2, 2, 8, 128, 128), np.float32)
    CT = np.zeros((2, 2, 8, 128, 128), np.float32)
    for dd in range(2):
        for ri, (bn, cn) in enumerate((("s5_b_re", "s5_c_re"), ("s5_b_im", "s5_c_im"))):
            Bm = np.asarray(inp[bn][l][dd], np.float32)
            Cm = np.asarray(inp[cn][l][dd], np.float32)
            for k in range(8):
                for gg in range(2):
                    g = 2 * k + gg
                    r0 = (g % 8) * 16
                    BT[dd, ri, k, r0:r0 + 16, gg * 64:(gg + 1) * 64] = Bm[g].T
                    CT[dd, ri, k, gg * 64:(gg + 1) * 64, r0:r0 + 16] = Cm[g].T
    d["s5_BT"] = np.ascontiguousarray(BT.transpose(3, 0, 1, 2, 4).reshape(128, 32, 128))
    d["s5_CT"] = np.ascontiguousarray(CT.transpose(3, 0, 1, 2, 4).reshape(128, 32, 128))
    d["s5_d"] = fm_vec(inp["s5_d"][l])
    d["glu_w"] = np.ascontiguousarray(inp["s5_glu_w"][l])
    d["glu_b"] = fm_vec(inp["s5_glu_b"][l])
    return d


class S5Bufs:
    pass


def cmul(S, ek, o_re, o_im, a_re, a_im, b_re, b_im, t1, t2):
    S.tt(ek, t1, a_re, b_re, ALU.mult)
    S.tt(ek, t2, a_im, b_im, ALU.mult)
    S.tt(ek, o_im, a_re, b_im, ALU.mult)
    S.tt(ek, o_re, t1, t2, ALU.subtract)
    S.tt(ek, t1, a_im, b_re, ALU.mult)
    S.tt(ek, o_im, o_im, t1, ALU.add)


def s5_alloc(P, st, W):
    S = P.S
    B = S5Bufs()
    B.BT = P.sb(st, "s5BT", [128, 32, 128], BF16)
    B.CT = P.sb(st, "s5CT", [128, 32, 128], BF16)
    with ExitStack() as st2:
        stg = P.sb(st2, "s5stg", [128, 32, 128])
        S.dma("sp", stg.all(), W["s5_BT"].all())
        S.copy("dve", B.BT.all(), stg.all())
        stg2 = P.sb(st2, "s5stg2", [128, 32, 128])
        S.dma("sp", stg2.all(), W["s5_CT"].all())
        S.copy("act", B.CT.all(), stg2.all())
        S.barrier()
        P.flush()
    B.lre = P.sb(st, "s5lre", [128, 2, 8])
    B.lim = P.sb(st, "s5lim", [128, 2, 8])
    B.ldt = P.sb(st, "s5ldt", [128, 2, 8])
    S.dma("sp", B.lre.all(), W["s5_lre"].all())
    S.dma("sp", B.lim.all(), W["s5_lim"].all())
    S.dma("sp", B.ldt.all(), W["s5_ldt"].all())
    B.dt = P.sb(st, "s5dt", [128, 2, 8])
    S.act(B.dt.all(), B.ldt.all(), AF.Exp)
    B.r = P.sb(st, "s5r", [128, 2, 8])
    B.u = P.sb(st, "s5u", [128, 2, 2, 8])
    B.coef = P.sb(st, "s5coef", [128, 2, 2, 8])
    B.uW = P.sb(st, "s5uW", [128, 2, 2, 8])
    tmp = [P.sb(st, f"s5tmp{i}", [128, 2, 8]) for i in range(6)]
    th = tmp[0]
    S.tt("dve", th.all(), B.lim.all(), B.dt.all(), ALU.mult)
    S.tt("dve", tmp[1].all(), B.lre.all(), B.dt.all(), ALU.mult)
    S.act(B.r.all(), tmp[1].all(), AF.Exp)
    x, x2, pa, pb_ = tmp[2], tmp[3], tmp[4], tmp[5]
    ure, uim = B.u[:, :, 0, :], B.u[:, :, 1, :]
    S.ts("dve", x.all(), th.all(), 1.0 / 64, ALU.mult)
    S.tt("dve", x2.all(), x.all(), x.all(), ALU.mult)
    S.ts("dve", pa.all(), x2.all(), -1.0 / 5040, ALU.mult)
    S.stt(pa.all(), pa.all(), 1.0 / 120, x2.all(), ALU.add, ALU.mult)
    S.stt(pa.all(), pa.all(), -1.0 / 6, x2.all(), ALU.add, ALU.mult)
    S.stt(uim, pa.all(), 1.0, x.all(), ALU.add, ALU.mult)
    S.ts("dve", pb_.all(), x2.all(), 1.0 / 40320, ALU.mult)
    S.stt(pb_.all(), pb_.all(), -1.0 / 720, x2.all(), ALU.add, ALU.mult)
    S.stt(pb_.all(), pb_.all(), 1.0 / 24, x2.all(), ALU.add, ALU.mult)
    S.stt(pb_.all(), pb_.all(), -0.5, x2.all(), ALU.add, ALU.mult)
    S.ts("dve", ure, pb_.all(), 1.0, ALU.add)
    for _ in range(6):
        S.tt("dve", pa.all(), ure, ure, ALU.mult)
        S.tt("dve", pb_.all(), uim, uim, ALU.mult)
        S.stt(x.all(), ure, 2.0, uim, ALU.mult, ALU.mult)
        S.tt("dve", ure, pa.all(), pb_.all(), ALU.subtract)
        S.copy("dve", uim, x.all())
    S.tt("dve", pa.all(), ure, ure, ALU.mult)
    S.tt("dve", pb_.all(), uim, uim, ALU.mult)
    S.tt("dve", pa.all(), pa.all(), pb_.all(), ALU.add)
    S.act(pa.all(), pa.all(), AF.Sqrt)
    S.recip(pa.all(), pa.all())
    S.tt("dve", ure, ure, pa.all(), ALU.mult)
    S.tt("dve", uim, uim, pa.all(), ALU.mult)
    are, aim = tmp[0], tmp[1]
    S.tt("dve", are.all(), B.u[:, :, 0, :], B.r.all(), ALU.mult)
    S.tt("dve", aim.all(), B.u[:, :, 1, :], B.r.all(), ALU.mult)
    S.ts("dve", are.all(), are.all(), -1.0, ALU.add)
    den = tmp[2]
    S.tt("dve", den.all(), B.lre.all(), B.lre.all(), ALU.mult)
    S.tt("dve", tmp[3].all(), B.lim.all(), B.lim.all(), ALU.mult)
    S.tt("dve", den.all(), den.all(), tmp[3].all(), ALU.add)
    S.recip(den.all(), den.all())
    nl = tmp[3]
    S.ts("dve", nl.all(), B.lim.all(), -1.0, ALU.mult)
    cmul(S, "dve", B.coef[:, :, 0, :], B.coef[:, :, 1, :], are.all(), aim.all(), B.lre.all(), nl.all(), tmp[4].all(), tmp[5].all())
    S.tt("dve", B.coef[:, :, 0, :], B.coef[:, :, 0, :], den.all(), ALU.mult)
    S.tt("dve", B.coef[:, :, 1, :], B.coef[:, :, 1, :], den.all(), ALU.mult)
    B.POST = P.sb(st, "s5POST", [128, 8, 2, 512])
    B.PRE = P.sb(st, "s5PRE", [128, 8, 2, 512])
    B.tt1 = P.sb(st, "s5tt1", [128, 8, 256])
    B.tt2 = P.sb(st, "s5tt2", [128, 8, 256])
    B.G = P.sb(st, "s5G", [128, 8, 2, 512])
    B.Tp1 = [P.sb(st, f"s5Tp1_{i}", [128, 2, 512]) for i in range(2)]
    B.Tp2 = [P.sb(st, f"s5Tp2_{i}", [128, 2, 512]) for i in range(2)]
    B.Tq1 = [P.sb(st, f"s5Tq1_{i}", [128, 2, 512]) for i in range(2)]
    B.Tq2 = [P.sb(st, f"s5Tq2_{i}", [128, 2, 512]) for i in range(2)]
    B.Hb = [P.sb(st, f"s5Hb{i}", [128, 2, 512], BF16) for i in range(2)]
    B.U = [P.sb(st, f"s5U{i}", [128, 2, 512], BF16) for i in range(2)]
    B.Wt = [P.sb(st, f"s5Wt{i}", [128, 2, 512]) for i in range(2)]
    B.Yo = [P.sb(st, f"s5Yo{i}", [128, 2, 512]) for i in range(2)]
    B.Yf = P.sb(st, "s5Yf", [128, 2, 512])
    B.h = P.sb(st, "s5h", [128, 2, 8])
    B.gi = P.sb(st, "s5gi", [128, 2, 8])
    B.sm = [P.sb(st, f"s5sm{i}", [128, 8]) for i in range(3)]
    B.pb = [P.ps(st, f"s5pb{i}", [128, 2, 512]) for i in range(2)]
    B.py = [P.ps(st, f"s5py{i}", [128, 512]) for i in range(2)]
    return B


def s5_tables(P, B, d):
    S = P.S
    S.memset("dve", B.POST[:, :, 0, 0:1], 1.0)
    S.memset("dve", B.POST[:, :, 1, 0:1], 0.0)
    S.copy("dve", B.uW[:, d, :, :], B.u[:, d, :, :])
    n = 1
    while n < 512:
        wre = B.uW[:, d, 0, :].re("p (k o) -> p k o", o=1).bc([128, 8, n])
        wim = B.uW[:, d, 1, :].re("p (k o) -> p k o", o=1).bc([128, 8, n])
        cmul(S, "dve", B.POST[:, :, 0, n:2 * n], B.POST[:, :, 1, n:2 * n], B.POST[:, :, 0, 0:n], B.POST[:, :, 1, 0:n],
             wre, wim, B.tt1[:, :, 0:n], B.tt2[:, :, 0:n])
        n *= 2
        if n < 512:
            cmul(S, "dve", B.sm[0].all(), B.sm[1].all(), B.uW[:, d, 0, :], B.uW[:, d, 1, :], B.uW[:, d, 0, :], B.uW[:, d, 1, :], B.sm[2].all(), B.gi[:, 0, :])
            S.copy("dve", B.uW[:, d, 0, :], B.sm[0].all())
            S.copy("dve", B.uW[:, d, 1, :], B.sm[1].all())
    for hlf in range(2):
        sl = slice(hlf * 256, (hlf + 1) * 256)
        cre = B.coef[:, d, 0, :].re("p (k o) -> p k o", o=1).bc([128, 8, 256])
        cim = B.coef[:, d, 1, :].re("p (k o) -> p k o", o=1).bc([128, 8, 256])
        S.tt("dve", B.tt1.all(), B.POST[:, :, 0, sl], cre, ALU.mult)
        S.tt("dve", B.tt2.all(), B.POST[:, :, 1, sl], cim, ALU.mult)
        S.tt("dve", B.PRE[:, :, 0, sl], B.tt1.all(), B.tt2.all(), ALU.add)
        S.tt("dve", B.tt1.all(), B.POST[:, :, 0, sl], cim, ALU.mult)
        S.tt("dve", B.tt2.all(), B.POST[:, :, 1, sl], cre, ALU.mult)
        S.tt("dve", B.PRE[:, :, 1, sl], B.tt1.all(), B.tt2.all(), ALU.subtract)


def s5_pass(P, B, sc, d, full, init_lat, out_ctx, out_fin, YS, Ysrc=None):
    S = P.S
    gtv = sc["GT"].ap.rearrange("n p t -> p n t")
    ysv = YS.ap.rearrange("n p t -> p n t") if YS is not None else None
    if Ysrc is None:
        Ysrc = YS
    ysrcv = Ysrc.ap.rearrange("n p t -> p n t") if Ysrc is not None else None
    s5_tables(P, B, d)
    S.memset("dve", B.h.all(), 0.0)
    lat = P.tiles[1:]
    order = [P.tiles[0]] + (lat if d == 0 else lat[::-1])
    rv = (lambda v: v) if d == 0 else (lambda v: v[:, ::-1])
    rv3 = (lambda v: v) if d == 0 else (lambda v: v[:, :, ::-1])
    for n, (t0, Wd) in enumerate(order):
        U, Yo = B.U[n % 2], B.Yo[n % 2]
        S.dma("sp", U[:, :, :Wd], V(gtv[:, 6:8, t0:t0 + Wd], sc["GT"].res))
        if full and d == 1:
            S.dma("sp", B.Yf[:, :, :Wd], V(ysrcv[:, :, t0:t0 + Wd], Ysrc.res))
        cmul(S, "dve", B.gi[:, 0, :], B.gi[:, 1, :], B.u[:, d, 0, :], B.u[:, d, 1, :], B.h[:, 0, :], B.h[:, 1, :], B.sm[0].all(), B.sm[1].all())
        def Gk(k, idx):
            return B.G.k(k, (slice(None), k) + idx)

        def pre(k):
            pb = B.pb[k % 2]
            for ri in range(2):
                S.mm(pb[:, ri, :Wd], B.BT[:, (d * 2 + ri) * 8 + k, :], U[:, k // 4, :Wd])
            pre_re = rv3(B.PRE[:, k, 0:1, :Wd]).bc([128, 2, Wd])
            pre_im = rv3(B.PRE[:, k, 1:2, :Wd]).bc([128, 2, Wd])
            Wt = B.Wt[k % 2]
            T1, T2 = B.Tp1[k % 2], B.Tp2[k % 2]
            S.tt("dve", T1[:, :, :Wd], pb[:, :, :Wd], pre_re, ALU.mult)
            S.tt("dve", T2[:, :, :Wd], pb[:, ::-1, :Wd], pre_im, ALU.mult)
            S.tt("pool", Wt[:, 0, :Wd], T1[:, 0, :Wd], T2[:, 0, :Wd], ALU.subtract)
            S.tt("pool", Wt[:, 1, :Wd], T1[:, 1, :Wd], T2[:, 1, :Wd], ALU.add)

        def mid(k):
            Wt = B.Wt[k % 2]
            rbc = B.r[:, d, k:k + 1].bc([128, Wd])
            for ri in range(2):
                S.scan(rv(Gk(k, (ri, slice(0, Wd)))), rbc, rv(Wt[:, ri, :Wd]), B.gi[:, ri, k:k + 1])

        def post(k):
            post_re = rv3(B.POST[:, k, 0:1, :Wd]).bc([128, 2, Wd])
            post_im = rv3(B.POST[:, k, 1:2, :Wd]).bc([128, 2, Wd])
            Hb = B.Hb[k % 2]
            T1, T2 = B.Tq1[k % 2], B.Tq2[k % 2]
            S.tt("dve", T1[:, :, :Wd], Gk(k, (slice(None), slice(0, Wd))), post_re, ALU.mult)
            S.tt("dve", T2[:, :, :Wd], Gk(k, (slice(None, None, -1), slice(0, Wd))), post_im, ALU.mult)
            S.tt("pool", Hb[:, 0, :Wd], T1[:, 0, :Wd], T2[:, 0, :Wd], ALU.subtract)
            S.stt(Hb[:, 1, :Wd], T1[:, 1, :Wd], -1.0, T2[:, 1, :Wd], ALU.mult, ALU.subtract)
            o = k // 4
            for ri in range(2):
                S.mm(B.py[o][:, :Wd], B.CT[:, (d * 2 + ri) * 8 + k, :], Hb[:, ri, :Wd],
                     start=(k % 4 == 0 and ri == 0), stop=(k % 4 == 3 and ri == 1))

        pre(0)
        for k in range(8):
            if k + 1 < 8:
                pre(k + 1)
            mid(k)
            if full:
                post(k)
        mi = Wd - 1 if d == 0 else 0
        S.eng["dve"].ops.append(("w", S.eng["dve"].sem, S.eng["dve"].cnt))
        S.eng["dve"].seen["dve"] = S.eng["dve"].cnt
        cmul(S, "dve", B.h[:, 0, :], B.h[:, 1, :], B.POST[:, :, 0, Wd - 1], B.POST[:, :, 1, Wd - 1],
             B.G[:, :, 0, mi], B.G[:, :, 1, mi], B.sm[0].all(), B.sm[1].all())
        for k_ in range(8):
            B.G.k(k_, (slice(None),)).res.r["dve"] = S.eng["dve"].cnt
        if full:
            for o in range(2):
                if d == 1:
                    S.tt("dve", Yo[:, o, :Wd], B.py[o][:, :Wd], B.Yf[:, o, :Wd], ALU.add)
                else:
                    S.copy("act", Yo[:, o, :Wd], B.py[o][:, :Wd])
            S.dma("pool", V(ysv[:, :, t0:t0 + Wd], YS.res), Yo[:, :, :Wd])
        if n == 0:
            if out_ctx is not None:
                S.dma("pool", out_ctx, B.h.all())
            if init_lat is not None:
                S.dma("sp", B.h.all(), init_lat)
    if out_fin is not None:
        S.dma("pool", out_fin, B.h.all())


W_A = [("xT", lambda P: [KD, 128, P.NT], F32), ("cond", lambda P: [128, KD, 2], F32), ("ada_w", lambda P: [D, 6 * D], F32),
       ("ada_b", lambda P: [128, 48], F32), ("n1w", lambda P: [128, KD], F32), ("w_in_arr", lambda P: [D, NCOL], F32),
       ("w_v", lambda P: [D, 768], F32), ("wa2p", lambda P: [128, 2, 256], F32), ("gla_ba", lambda P: [128, 2, 2], F32),
       ("hg_lb", lambda P: [128, 2, P.NL], F32), ("rope", lambda P: [128, 4, P.NT], F32), ("ones_bf", lambda P: [128, 128], BF16),
       ("ident_bf", lambda P: [128, 128], BF16), ("bret", lambda P: [128, 2, 2, 128], F32), ("masks", lambda P: [128, 2, 128], F32),
       ("s5_lre", lambda P: [128, 2, 8], F32), ("s5_lim", lambda P: [128, 2, 8], F32), ("s5_ldt", lambda P: [128, 2, 8], F32),
       ("s5_BT", lambda P: [128, 32, 128], F32), ("s5_CT", lambda P: [128, 32, 128], F32)]


def declare(P, specs):
    W = {}
    for name, shp, dt in specs:
        W[name] = P.din(name, shp(P), dt)
    return W


def build_test_S(TL, l, NL=4, full=True):
    P = Prog(TL, debug=True, NL=NL)
    W = declare(P, W_A + [("init_s5", lambda P: [2, 128, 2, 8], F32)])
    NT = P.NT
    sc = alloc_scratch(P)
    YS = P.dscr("YS", [2, 128, NT], F32)
    st_out = P.dout("st_out", [4, 128, 2, 8], F32)
    with ExitStack() as st:
        MOD = phase_mod(P, st, W, 16)
        lbv = compute_lbv(P, st, W, l)
        phase_A(P, W, MOD, sc, lbv)
        with ExitStack() as st2:
            B = s5_alloc(P, st2, W)
            for d in range(2):
                s5_pass(P, B, sc, d, full, V(W["init_s5"].ap[d], W["init_s5"].res) if full else None,
                        V(st_out.ap[d], st_out.res), V(st_out.ap[2 + d], st_out.res), YS if full else None)
            P.S.barrier()
            P.flush()
    return P.nc


def norm_mod2(P, S, X, XN, XNf, sqb, psn, rstd, tmpf, ones_bf, s_vec, b_vec, col, Wd):
    S.act(sqb[:, :, :Wd], X[:, :, :Wd], AF.Square)
    for kc in range(KD):
        S.mm(psn[:, :Wd], ones_bf.all(), sqb[:, kc, :Wd], start=(kc == 0), stop=(kc == KD - 1))
    S.ts("dve", rstd[:, :Wd], psn[:, :Wd], 1.0 / D, ALU.mult, 1e-6, ALU.add)
    S.act(rstd[:, :Wd], rstd[:, :Wd], AF.Sqrt)
    S.recip(rstd[:, :Wd], rstd[:, :Wd])
    for kc in range(KD):
        S.stt(tmpf[:, kc, :Wd], X[:, kc, :Wd], s_vec[:, kc, col:col + 1], rstd[:, :Wd], ALU.mult, ALU.mult)
        S.act(XNf[:, kc, :Wd], tmpf[:, kc, :Wd], AF.Identity, bias=b_vec[:, kc, col:col + 1])
    S.copy("pool", XN[:, :, :Wd], XNf[:, :, :Wd])


W_C = [("w_out", lambda P: [D, D], F32), ("hn_w", lambda P: [128, 6], F32), ("s5_d", lambda P: [128, 2], F32),
       ("glu_w", lambda P: [256, 256], F32), ("glu_b", lambda P: [128, 2], F32), ("n2w", lambda P: [128, KD], F32),
       ("blockmean", lambda P: [128, 128], BF16), ("ident_f", lambda P: [128, 128], F32)]
W_MOE = [("router_w", lambda P: [D, 8], F32), ("router_b", lambda P: [1, 8], F32), ("selE", lambda P: [8, 8, 128], BF16)]


def phase_C(P, W, MOD, sc, YA, YS, xT2, XN2, GTS, moe, do_ctx):
    S = P.S
    with ExitStack() as st:
        Wout = P.sb(st, "Wout", [128, KD, D], BF16)
        gluw = P.sb(st, "gluw", [128, 2, 256], BF16)
        Bm = P.sb(st, "Bm", [128, 128], BF16)
        ones_bf = P.sb(st, "onesC", [128, 128], BF16)
        hnw = P.sb(st, "hnw", [128, 6])
        s5d = P.sb(st, "s5d", [128, 2])
        glub = P.sb(st, "glub", [128, 2])
        n2w = P.sb(st, "n2w", [128, KD])
        s2 = P.sb(st, "s2", [128, KD, 2])
        S.dma("sp", Bm.all(), W["blockmean"].all())
        S.dma("sp", ones_bf.all(), W["ones_bf"].all())
        S.dma("sp", hnw.all(), W["hn_w"].all())
        S.dma("sp", s5d.all(), W["s5_d"].all())
        S.dma("sp", glub.all(), W["glu_b"].all())
        S.dma("sp", n2w.all(), W["n2w"].all())
        S.ts("dve", s2.all(), MOD[:, 32:40, :], 1.0, ALU.add)
        S.tt("dve", s2.all(), s2.all(), n2w.all().re("p (k o) -> p k o", o=1).bc([128, KD, 2]), ALU.mult)
        if moe:
            rw = P.sb(st, "rw", [128, KD, 8])
            rb = P.sb(st, "rb", [128, 8])
            identf = P.sb(st, "identf", [128, 128])
            S.dma("sp", rw.all(), V(W["router_w"].ap.rearrange("(kc p) n -> p kc n", p=128), W["router_w"].res))
            S.dma("sp", rb.all(), V(W["router_b"].ap.partition_broadcast(128), W["router_b"].res), allow_slow_non_contiguous=True)
            S.dma("sp", identf.all(), W["ident_f"].all())
        with ExitStack() as st2:
            stg = P.sb(st2, "wostg", [128, KD, D])
            S.dma("sp", stg.all(), V(W["w_out"].ap.rearrange("(kc p) n -> p kc n", p=128), W["w_out"].res))
            S.copy("dve", Wout[:, 0:4, :], stg[:, 0:4, :])
            S.copy("act", Wout[:, 4:8, :], stg[:, 4:8, :])
            stg2 = P.sb(st2, "glstg", [128, 2, 256])
            S.dma("sp", stg2.all(), V(W["glu_w"].ap.rearrange("(kc p) n -> p kc n", p=128), W["glu_w"].res))
            S.copy("pool", gluw.all(), stg2.all())
            S.barrier()
            P.flush()
        X = [P.sb(st, f"cX{i}", [128, KD, 512]) for i in range(2)]
        YAt = [P.sb(st, f"cYA{i}", [128, 6, 512]) for i in range(2)]
        YSt = [P.sb(st, f"cYS{i}", [128, 2, 512]) for i in range(2)]
        GTt = [P.sb(st, f"cGT{i}", [128, 8, 512], BF16) for i in range(2)]
        Y = P.sb(st, "cY", [128, KD, 512], BF16)
        ybf = P.sb(st, "cybf", [128, 512], BF16)
        sqh = P.sb(st, "csqh", [128, 512], BF16)
        yc = P.sb(st, "cyc", [128, 512])
        rs = P.sb(st, "crs", [128, 512])
        o_ = P.sb(st, "co", [128, 512])
        zz = [P.sb(st, f"cz{i}", [128, 512]) for i in range(2)]
        zb = P.sb(st, "czb", [128, 2, 512], BF16)
        ta = P.sb(st, "cta", [128, 512])
        tb = P.sb(st, "ctb", [128, 512])
        sqb = P.sb(st, "csqb", [128, KD, 512], BF16)
        tmpf = P.sb(st, "ctmpf", [128, KD, 512])
        XNf = P.sb(st, "cXNf", [128, KD, 512])
        XN = P.sb(st, "cXN", [128, KD, 512], BF16)
        rstd = P.sb(st, "crstd", [128, 512])
        pp = [P.ps(st, f"cpp{i}", [128, 512]) for i in range(3)]
        po = [P.ps(st, f"cpo{i}", [128, 512]) for i in range(2)]
        psn = P.ps(st, "cpsn", [128, 512])
        if moe:
            pr = P.ps(st, "cpr", [128, 8])
            ptr = P.ps(st, "cptr", [8, 128])
            lg = P.sb(st, "clg", [128, 8])
            l2 = P.sb(st, "cl2", [128, 8])
            ee = P.sb(st, "cee", [128, 8])
            sm = [P.sb(st, f"csm{i}", [128, 1]) for i in range(4)]
            gT = P.sb(st, "cgT", [8, 512], BF16)
        ppi = [0]

        def nextpp():
            p = pp[ppi[0] % 3]
            ppi[0] += 1
            return p

        xv = W["xT"].ap.rearrange("k p t -> p k t")
        x2v = xT2.ap.rearrange("k p t -> p k t")
        xnv = XN2.ap.rearrange("k p t -> p k t")
        yav = YA.ap.rearrange("n p t -> p n t")
        ysv = YS.ap.rearrange("n p t -> p n t")
        gtv = sc["GT"].ap.rearrange("n p t -> p n t")
        tiles = P.tiles if do_ctx else P.tiles[1:]
        for n, (t0, Wd) in enumerate(tiles):
            col = 1 if t0 == 0 else 0
            Xt, ya, ys, gt = X[n % 2], YAt[n % 2], YSt[n % 2], GTt[n % 2]
            S.dma("sp", Xt[:, :, :Wd], V(xv[:, :, t0:t0 + Wd], W["xT"].res))
            S.dma("sp", ya[:, :, :Wd], V(yav[:, :, t0:t0 + Wd], YA.res))
            S.dma("sp", ys[:, :, :Wd], V(ysv[:, :, t0:t0 + Wd], YS.res))
            S.dma("sp", gt[:, :, :Wd], V(gtv[:, :, t0:t0 + Wd], sc["GT"].res))
            for j in range(6):
                m = j // 2
                kc = (0, 4, 6)[m] + (j % 2)
                y = ya[:, j, :Wd]
                if m == 0:
                    S.copy("act", ybf[:, :Wd], y)
                    pm = nextpp()
                    S.mm(pm[:, :Wd], Bm.all(), ybf[:, :Wd])
                    S.tt("dve", yc[:, :Wd], y, pm[:, :Wd], ALU.subtract)
                    y = yc[:, :Wd]
                S.act(sqh[:, :Wd], y, AF.Square)
                pv = nextpp()
                S.mm(pv[:, :Wd], Bm.all(), sqh[:, :Wd])
                S.ts("dve", rs[:, :Wd], pv[:, :Wd], 1e-6, ALU.add)
                S.act(rs[:, :Wd], rs[:, :Wd], AF.Sqrt)
                S.recip(rs[:, :Wd], rs[:, :Wd])
                S.stt(o_[:, :Wd], y, hnw[:, j:j + 1], rs[:, :Wd], ALU.mult, ALU.mult)
                S.tt("pool", Y[:, kc, :Wd], o_[:, :Wd], gt[:, j, :Wd], ALU.mult)
            for j in range(2):
                z = zz[j]
                S.stt(z[:, :Wd], gt[:, 6 + j, :Wd], s5d[:, j:j + 1], ys[:, j, :Wd], ALU.mult, ALU.add)
                S.act(ta[:, :Wd], z[:, :Wd], AF.Square)
                S.ts("dve", ta[:, :Wd], ta[:, :Wd], 0.044715, ALU.mult, 1.0, ALU.add)
                S.tt("dve", ta[:, :Wd], ta[:, :Wd], z[:, :Wd], ALU.mult)
                S.act(tb[:, :Wd], ta[:, :Wd], AF.Sigmoid, scale=1.5957691216057308)
                S.tt("dve", z[:, :Wd], z[:, :Wd], tb[:, :Wd], ALU.mult)
                S.copy("pool", zb[:, j, :Wd], z[:, :Wd])
            for j in range(2):
                pg = nextpp()
                for kk in range(2):
                    S.mm(pg[:, :Wd], gluw[:, kk, j * 128:(j + 1) * 128], zb[:, kk, :Wd], start=(kk == 0), stop=(kk == 1))
                S.act(tb[:, :Wd], pg[:, :Wd], AF.Sigmoid, bias=glub[:, j:j + 1])
                S.tt("dve", Y[:, 2 + j, :Wd], zz[j][:, :Wd], tb[:, :Wd], ALU.mult)
            for dt in range(KD):
                p = po[dt % 2]
                for kc in range(KD):
                    S.mm(p[:, :Wd], Wout[:, kc, dt * 128:(dt + 1) * 128], Y[:, kc, :Wd], start=(kc == 0), stop=(kc == KD - 1))
                S.stt(Xt[:, dt, :Wd], p[:, :Wd], MOD[:, 16 + dt, col:col + 1], Xt[:, dt, :Wd], ALU.mult, ALU.add)
            S.dma("pool", V(x2v[:, :, t0:t0 + Wd], xT2.k(t0, slice(None)).res), Xt[:, :, :Wd])
            norm_mod2(P, S, Xt, XN, XNf, sqb, psn, rstd, tmpf, ones_bf, s2, MOD[:, 24:32, :], col, Wd)
            S.dma("pool", V(xnv[:, :, t0:t0 + Wd], XN2.res), XN[:, :, :Wd])
            if moe:
                for sub in range(Wd // 128):
                    ss = slice(sub * 128, (sub + 1) * 128)
                    for kc in range(KD):
                        S.mm(pr.all(), XNf[:, kc, ss], rw[:, kc, :], start=(kc == 0), stop=(kc == KD - 1))
                    S.tt("dve", lg.all(), pr.all(), rb.all(), ALU.add)
                    S.red(sm[0].all(), lg.all(), ALU.max)
                    S.ts("dve", l2.all(), lg.all(), sm[0].all(), ALU.is_equal)
                    S.stt(l2.all(), l2.all(), -1e30, lg.all(), ALU.mult, ALU.add)
                    S.red(sm[1].all(), l2.all(), ALU.max)
                    S.ts("dve", l2.all(), lg.all(), sm[1].all(), ALU.is_ge)
                    S.ts("dve", sm[2].all(), sm[0].all(), -1.0, ALU.mult)
                    S.act(ee.all(), lg.all(), AF.Exp, bias=sm[2].all())
                    S.tt("dve", ee.all(), ee.all(), l2.all(), ALU.mult)
                    S.red(sm[3].all(), ee.all(), ALU.add)
                    S.recip(sm[3].all(), sm[3].all())
                    S.ts("dve", ee.all(), ee.all(), sm[3].all(), ALU.mult)
                    S.tr(ptr.all(), ee.all(), identf.all())
                    S.copy("act", gT[:, ss], ptr.all())
                S.dma("pool", V(GTS.ap[:, t0:t0 + Wd], GTS.res), gT[:, :Wd])
        S.barrier()
        P.flush()


def phase_D(P, W, MOD, xT2, XN2, GTS, moe, do_ctx):
    S = P.S
    nE = 8 if moe else 1
    with ExitStack() as st:
        w1 = P.sb(st, "fw1", [128, KD, HFF], BF16)
        w3 = P.sb(st, "fw3", [128, KD, HFF], BF16)
        w2 = P.sb(st, "fw2", [128, NFT, D], BF16)
        stg = [P.sb(st, f"fstg{i}", [128, 2816]) for i in range(2)]
        XNt = [P.sb(st, f"fXN{i}", [128, KD, 512], BF16) for i in range(2)]
        Xa = [P.sb(st, f"fXa{i}", [128, KD, 512]) for i in range(2)]
        hT = P.sb(st, "fhT", [128, NFT, 512], BF16)
        sl = [P.sb(st, f"fsl{i}", [128, 512]) for i in range(2)]
        tg = [P.sb(st, f"ftg{i}", [128, 512]) for i in range(2)]
        p1 = [P.ps(st, f"fp1_{i}", [128, 512]) for i in range(2)]
        p3 = [P.ps(st, f"fp3_{i}", [128, 512]) for i in range(2)]
        po = [P.ps(st, f"fpo{i}", [128, 512]) for i in range(2)]
        if moe:
            gTs = P.sb(st, "fgTs", [8, P.NT], BF16)
            selE = P.sb(st, "fselE", [8, 8, 128], BF16)
            gateB = P.sb(st, "fgateB", [128, 512])
            pg = P.ps(st, "fpg", [128, 512])
            c0_ = 0 if do_ctx else CTX
            S.dma("sp", gTs[:, c0_:], GTS[:, c0_:])
            S.dma("sp", selE.all(), W["selE"].all())
        x2v = xT2.ap.rearrange("k p t -> p k t")
        xnv = XN2.ap.rearrange("k p t -> p k t")
        tiles = P.tiles if do_ctx else P.tiles[1:]
        cast_eng = ("dve", "act", "pool")
        ci = 0
        qi = 0
        for e in range(nE):
            for hh in range(2):
                hs = slice(hh * HFF, (hh + 1) * HFF)
                if moe:
                    a1, a3, a2 = W["ffn_w1"].ap[e], W["ffn_w3"].ap[e], W["ffn_w2"].ap[e]
                else:
                    a1, a3, a2 = W["ffn_w1"].ap, W["ffn_w3"].ap, W["ffn_w2"].ap
                for (src, dst, rw_) in ((a1, w1, W["ffn_w1"]), (a3, w3, W["ffn_w3"])):
                    sv = src[:, hs].rearrange("(kc p) n -> p kc n", p=128)
                    for pc in range(4):
                        sg = stg[qi % 2]
                        qi += 1
                        cs = slice(pc * 352, (pc + 1) * 352)
                        S.dma("sp" if qi % 2 == 0 else "act", sg.all().re("p (k n) -> p k n", k=KD), V(sv[:, :, cs], rw_.res))
                        S.copy(cast_eng[ci % 3], dst[:, :, cs], sg.all().re("p (k n) -> p k n", k=KD))
                        ci += 1
                sv = a2[hs, :].rearrange("(ft p) n -> p ft n", p=128)
                for pc in range(4):
                    sg = stg[qi % 2]
                    qi += 1
                    cs = slice(pc * 256, (pc + 1) * 256)
                    S.dma("sp" if qi % 2 == 0 else "act", sg.all().re("p (k n) -> p k n", k=NFT), V(sv[:, :, cs], W["ffn_w2"].res))
                    S.copy(cast_eng[ci % 3], w2[:, :, cs], sg.all().re("p (k n) -> p k n", k=NFT))
                    ci += 1
                for n, (t0, Wd) in enumerate(tiles):
                    col = 1 if t0 == 0 else 0
                    xn, xa = XNt[n % 2], Xa[n % 2]
                    S.dma("sp", xn[:, :, :Wd], V(xnv[:, :, t0:t0 + Wd], XN2.res))
                    S.dma("sp", xa[:, :, :Wd], V(x2v[:, :, t0:t0 + Wd], xT2.k(t0, slice(None)).res))
                    if moe:
                        S.mm(pg[:, :Wd], selE[:, e, :], gTs[:, t0:t0 + Wd])
                        S.copy("act", gateB[:, :Wd], pg[:, :Wd])
                    for ft in range(NFT):
                        a, b = p1[ft % 2], p3[ft % 2]
                        fs = slice(ft * 128, (ft + 1) * 128)
                        for kc in range(KD):
                            S.mm(a[:, :Wd], w1[:, kc, fs], xn[:, kc, :Wd], start=(kc == 0), stop=(kc == KD - 1))
                        for kc in range(KD):
                            S.mm(b[:, :Wd], w3[:, kc, fs], xn[:, kc, :Wd], start=(kc == 0), stop=(kc == KD - 1))
                        s_ = sl[ft % 2]
                        S.act(s_[:, :Wd], a[:, :Wd], AF.Silu)
                        S.tt("dve", hT[:, ft, :Wd], s_[:, :Wd], b[:, :Wd], ALU.mult)
                    for dt in range(KD):
                        p = po[dt % 2]
                        for ft in range(NFT):
                            S.mm(p[:, :Wd], w2[:, ft, dt * 128:(dt + 1) * 128], hT[:, ft, :Wd], start=(ft == 0), stop=(ft == NFT - 1))
                        if moe:
                            t_ = tg[dt % 2]
                            S.tt("dve", t_[:, :Wd], p[:, :Wd], gateB[:, :Wd], ALU.mult)
                            S.stt(xa[:, dt, :Wd], t_[:, :Wd], MOD[:, 40 + dt, col:col + 1], xa[:, dt, :Wd], ALU.mult, ALU.add)
                        else:
                            S.stt(xa[:, dt, :Wd], p[:, :Wd], MOD[:, 40 + dt, col:col + 1], xa[:, dt, :Wd], ALU.mult, ALU.add)
                    S.dma("pool", V(x2v[:, :, t0:t0 + Wd], xT2.k(t0, slice(None)).res), xa[:, :, :Wd])
        S.barrier()
        P.flush()


def phase_final(P, W, xT2, yT):
    S = P.S
    with ExitStack() as st:
        ones_bf = P.sb(st, "zones", [128, 128], BF16)
        fws = P.sb(st, "zfw", [128, KD, 1])
        zb = P.sb(st, "zzb", [128, KD, 1])
        S.dma("sp", ones_bf.all(), W["ones_bf"].all())
        S.dma("sp", fws.all(), W["fw"].all())
        S.memset("dve", zb.all(), 0.0)
        X = [P.sb(st, f"zX{i}", [128, KD, 512]) for i in range(2)]
        XN = [P.sb(st, f"zXN{i}", [128, KD, 512]) for i in range(2)]
        sqb = P.sb(st, "zsqb", [128, KD, 512], BF16)
        tmpf = P.sb(st, "ztmpf", [128, KD, 512])
        rstd = P.sb(st, "zrstd", [128, 512])
        psn = P.ps(st, "zpsn", [128, 512])
        xv = xT2.ap.rearrange("k p t -> p k t")
        yv = yT.ap.rearrange("k p t -> p k t")
        for i, (t0, Wd) in enumerate(P.tiles[1:]):
            S.dma("sp", X[i % 2].all(), V(xv[:, :, t0:t0 + Wd], xT2.k(t0, slice(None)).res))
            norm_mod(P, S, X[i % 2], XN[i % 2], sqb, psn, rstd, tmpf, ones_bf, fws, zb, 0, 512)
            S.dma("pool", V(yv[:, :, t0 - CTX:t0 - CTX + Wd], yT.res), XN[i % 2].all())
        S.barrier()
        P.flush()


W_ST = [("init_att", lambda P: [128, 6, 64], F32), ("init_s5", lambda P: [128, 2, 8], F32)]


def build_La(TL, l, NL=4):
    P = Prog(TL, NL=NL)
    W = declare(P, W_A)
    sc = alloc_scratch(P)
    st_att = P.dout("st_att", [4, 128, 6, 64], F32)
    st_s5 = P.dout("st_s5", [4, 128, 2, 8], F32)
    with ExitStack() as st:
        MOD = phase_mod(P, st, W, 16)
        lbv = compute_lbv(P, st, W, l)
        phase_A(P, W, MOD, sc, lbv, so=True)
        with ExitStack() as st2:
            A = attn_alloc(P, st2, W)
            for d in range(2):
                attn_pass(P, A, sc, d, False, None, V(st_att.ap[d], st_att.res), V(st_att.ap[2 + d], st_att.res), None)
            P.S.barrier()
            P.flush()
        with ExitStack() as st2:
            B = s5_alloc(P, st2, W)
            for d in range(2):
                s5_pass(P, B, sc, d, False, None, V(st_s5.ap[d], st_s5.res), V(st_s5.ap[2 + d], st_s5.res), None)
            P.S.barrier()
            P.flush()
    return P.nc


def build_Lb(TL, l, moe, last, NL=4):
    P = Prog(TL, NL=NL)
    NT = P.NT
    specs = W_A + W_ST + W_C
    if moe:
        specs = specs + W_MOE + [("ffn_w1", lambda P: [8, D, DFF], F32), ("ffn_w3", lambda P: [8, D, DFF], F32), ("ffn_w2", lambda P: [8, DFF, D], F32)]
    else:
        specs = specs + [("ffn_w1", lambda P: [D, DFF], F32), ("ffn_w3", lambda P: [D, DFF], F32), ("ffn_w2", lambda P: [DFF, D], F32)]
    if last:
        specs = specs + [("fw", lambda P: [128, KD, 1], F32)]
    W = declare(P, specs)
    sc = alloc_scratch(P)
    YA = P.dscr("YA", [6, 128, NT], F32)
    YS = P.dscr("YS", [2, 128, NT], F32)
    XN2 = P.dscr("XN2", [KD, 128, NT], BF16)
    GTS = P.dscr("GTS", [8, NT], BF16)
    if last:
        xT2 = P.dscr("xT2", [KD, 128, NT], F32)
        yT = P.dout("yT", [KD, 128, P.TL], F32)
    else:
        xT2 = P.dout("xT2", [KD, 128, NT], F32)
    with ExitStack() as st:
        MOD = phase_mod(P, st, W, 48)
        lbv = compute_lbv(P, st, W, l)
        phase_A(P, W, MOD, sc, lbv)
        with ExitStack() as st2:
            A = attn_alloc(P, st2, W)
            for d in range(2):
                attn_pass(P, A, sc, d, True, V(W["init_att"].ap[d], W["init_att"].res), None, None, YA)
            P.S.barrier()
            P.flush()
        with ExitStack() as st2:
            B = s5_alloc(P, st2, W)
            for d in range(2):
                s5_pass(P, B, sc, d, True, V(W["init_s5"].ap[d], W["init_s5"].res), None, None, YS)
            P.S.barrier()
            P.flush()
        phase_C(P, W, MOD, sc, YA, YS, xT2, XN2, GTS, moe, not last)
        phase_D(P, W, MOD, xT2, XN2, GTS, moe, not last)
        if last:
            phase_final(P, W, xT2, yT)
    return P.nc


def consts_all():
    c = consts_np()
    c.update(attn_consts_np())
    bm = np.zeros((128, 128), np.float32)
    bm[:64, :64] = 1.0 / 64
    bm[64:, 64:] = 1.0 / 64
    c["blockmean"] = bm.astype(ml_dtypes.bfloat16)
    sel = np.zeros((8, 8, 128), np.float32)
    for e in range(8):
        sel[e, e, :] = 1.0
    c["selE"] = sel.astype(ml_dtypes.bfloat16)
    return c


def layer_inputs(inp, l, depth):
    d = layer_consts(inp, l)
    d.update(s5_layout_np(inp, l))
    d.update(consts_all())
    d["w_out"] = np.ascontiguousarray(inp["w_out"][l])
    d["hn_w"] = np.ascontiguousarray(np.concatenate([fm_vec(inp["ret_gn_w"][l]), fm_vec(inp["hg_norm_w"][l]), fm_vec(inp["gla_norm_w"][l])], axis=1))
    j = l // 2
    if l % 2 == 0:
        d["ffn_w1"] = np.ascontiguousarray(inp["ffn_w1"][j])
        d["ffn_w3"] = np.ascontiguousarray(inp["ffn_w3"][j])
        d["ffn_w2"] = np.ascontiguousarray(inp["ffn_w2"][j])
    else:
        d["ffn_w1"] = np.ascontiguousarray(inp["moe_w1"][j])
        d["ffn_w3"] = np.ascontiguousarray(inp["moe_w3"][j])
        d["ffn_w2"] = np.ascontiguousarray(inp["moe_w2"][j])
        d["router_w"] = np.ascontiguousarray(inp["router_w"][j])
        d["router_b"] = np.ascontiguousarray(np.asarray(inp["router_b"][j]).reshape(1, 8))
    d["fw"] = np.ascontiguousarray(fm_vec(inp["final_norm_w"]).reshape(128, KD, 1))
    return d


W_1 = W_A
W_2 = [("xT", lambda P: [KD, 128, P.NT], F32), ("MODin", lambda P: [128, 48, 2], F32),
       ("QSin", lambda P: [18, 128, P.NT], BF16), ("GSin", lambda P: [8, 128, P.NT], F32),
       ("GTin", lambda P: [8, 128, P.NT], BF16), ("VSin", lambda P: [P.NT, 768], BF16),
       ("YAin", lambda P: [6, 128, P.NT], F32), ("YSin", lambda P: [2, 128, P.NT], F32),
       ("ones_bf", lambda P: [128, 128], BF16), ("ident_bf", lambda P: [128, 128], BF16),
       ("bret", lambda P: [128, 2, 2, 128], F32), ("masks", lambda P: [128, 2, 128], F32),
       ("s5_lre", lambda P: [128, 2, 8], F32), ("s5_lim", lambda P: [128, 2, 8], F32), ("s5_ldt", lambda P: [128, 2, 8], F32),
       ("s5_BT", lambda P: [128, 32, 128], F32), ("s5_CT", lambda P: [128, 32, 128], F32)] + W_ST + W_C


def build_L1(TL, l, NL=4):
    P = Prog(TL, debug=True, NL=NL)
    W = declare(P, W_1)
    NT = P.NT
    sc = alloc_scratch(P)
    YA = P.dout("YA", [6, 128, NT], F32)
    YS = P.dout("YS", [2, 128, NT], F32)
    MODo = P.dout("MODo", [128, 48, 2], F32)
    f_att = P.dout("f_att", [128, 6, 64], F32)
    f_s5 = P.dout("f_s5", [128, 2, 8], F32)
    with ExitStack() as st:
        MOD = phase_mod(P, st, W, 48)
        P.S.dma("pool", MODo.all(), MOD.all())
        lbv = compute_lbv(P, st, W, l)
        phase_A(P, W, MOD, sc, lbv)
        with ExitStack() as st2:
            A = attn_alloc(P, st2, W)
            attn_pass(P, A, sc, 0, True, None, None, f_att.all(), YA)
            P.S.barrier()
            P.flush()
        with ExitStack() as st2:
            B = s5_alloc(P, st2, W)
            s5_pass(P, B, sc, 0, True, None, None, f_s5.all(), YS)
            P.S.barrier()
            P.flush()
    return P.nc


def build_L2(TL, l, moe, last, NL=4):
    P = Prog(TL, NL=NL)
    NT = P.NT
    specs = list(W_2)
    if moe:
        specs = specs + W_MOE + [("ffn_w1", lambda P: [8, D, DFF], F32), ("ffn_w3", lambda P: [8, D, DFF], F32), ("ffn_w2", lambda P: [8, DFF, D], F32)]
    else:
        specs = specs + [("ffn_w1", lambda P: [D, DFF], F32), ("ffn_w3", lambda P: [D, DFF], F32), ("ffn_w2", lambda P: [DFF, D], F32)]
    if last:
        specs = specs + [("fw", lambda P: [128, KD, 1], F32)]
    W = declare(P, specs)
    sc = {"QS": W["QSin"], "GS": W["GSin"], "GT": W["GTin"], "VS": W["VSin"]}
    YA = P.dscr("YA", [6, 128, NT], F32)
    YS = P.dscr("YS", [2, 128, NT], F32)
    XN2 = P.dscr("XN2", [KD, 128, NT], BF16)
    GTS = P.dscr("GTS", [8, NT], BF16)
    if last:
        xT2 = P.dscr("xT2", [KD, 128, NT], F32)
        yT = P.dout("yT", [KD, 128, P.TL], F32)
    else:
        xT2 = P.dout("xT2", [KD, 128, NT], F32)
    with ExitStack() as st:
        MOD = P.sb(st, "MOD2", [128, 48, 2])
        P.S.dma("sp", MOD.all(), W["MODin"].all())
        with ExitStack() as st2:
            A = attn_alloc(P, st2, W)
            attn_pass(P, A, sc, 1, True, W["init_att"].all(), None, None, YA, Ysrc=W["YAin"])
            P.S.barrier()
            P.flush()
        with ExitStack() as st2:
            B = s5_alloc(P, st2, W)
            s5_pass(P, B, sc, 1, True, W["init_s5"].all(), None, None, YS, Ysrc=W["YSin"])
            P.S.barrier()
            P.flush()
        phase_C(P, W, MOD, sc, YA, YS, xT2, XN2, GTS, moe, not last)
        phase_D(P, W, MOD, xT2, XN2, GTS, moe, not last)
        if last:
            phase_final(P, W, xT2, yT)
    return P.nc


def mirror_layer_inputs(li):
    m = dict(li)
    w = li["w_in_arr"].copy()
    w[:, 14 * 128:16 * 128] = li["w_in_arr"][:, 16 * 128:18 * 128]
    w[:, 16 * 128:18 * 128] = li["w_in_arr"][:, 14 * 128:16 * 128]
    lo = 26 * 128
    w[:, lo:lo + 16] = li["w_in_arr"][:, lo + 32:lo + 48]
    w[:, lo + 32:lo + 48] = li["w_in_arr"][:, lo:lo + 16]
    m["w_in_arr"] = w
    wa = np.zeros_like(li["wa2p"])
    wa[0:16, 0, :] = li["wa2p"][32:48, 1, :]
    wa[32:48, 1, :] = li["wa2p"][0:16, 0, :]
    m["wa2p"] = wa
    m["gla_ba"] = np.ascontiguousarray(li["gla_ba"][:, ::-1, :])
    for k in ("s5_lre", "s5_lim", "s5_ldt"):
        m[k] = np.ascontiguousarray(li[k][:, ::-1, :])
    for k in ("s5_BT", "s5_CT"):
        a = li[k].reshape(128, 2, 16, 128)
        m[k] = np.ascontiguousarray(a[:, ::-1]).reshape(128, 32, 128)
    m["bret"] = attn_consts_np(mirror=True)["bret"]
    return m


def names_of(specs):
    return [s[0] for s in specs]


def kernel(**inputs):
    inp = {k: np.asarray(v) for k, v in inputs.items()}
    x = inp["x"]
    B, SEQ, _ = x.shape
    depth = inp["ada_w"].shape[0]
    TL = SEQ // 2
    ncore = 2 * B
    cores = [(b, s) for b in range(B) for s in range(2)]
    xT = []
    ropes = []
    for (b, s) in cores:
        toks = np.concatenate([inp["ctx"][b], x[b, s * TL:(s + 1) * TL]], axis=0)
        rp = rope_tables(TL, s)
        if s == 1:
            toks = np.concatenate([inp["ctx"][b][::-1], x[b, s * TL:(s + 1) * TL][::-1]], axis=0)
            rp = np.ascontiguousarray(np.concatenate([rp[:, :, :CTX], rp[:, :, CTX:][:, :, ::-1]], axis=2))
        xT.append(to_fm(toks))
        ropes.append(rp)
    conds = [core_cond(inp, b) for b in range(B)]
    out = np.empty_like(x)
    for l in range(depth):
        moe = (l % 2 == 1)
        last = (l == depth - 1)
        li = [layer_inputs(inp, l, depth)]
        li.append(mirror_layer_inputs(li[0]))
        nc1 = build_L1(TL, l, NL=depth)
        maps = []
        for ci, (b, s) in enumerate(cores):
            m = {k: li[s][k] for k in names_of(W_1) if k in li[s]}
            m["xT"] = xT[ci]
            m["cond"] = conds[b]
            m["rope"] = ropes[ci]
            maps.append(m)
        r1 = run_bass_kernel_spmd(nc1, maps, core_ids=list(range(ncore))).results
        nc2 = build_L2(TL, l, moe, last, NL=depth)
        specs2 = W_2 + (W_MOE if moe else [])
        maps = []
        for ci, (b, s) in enumerate(cores):
            m = {k: li[s][k] for k in names_of(specs2) if k in li[s]}
            for k in ("ffn_w1", "ffn_w3", "ffn_w2"):
                m[k] = li[s][k]
            if last:
                m["fw"] = li[s]["fw"]
            m["xT"] = xT[ci]
            own, pa = r1[ci], r1[ci ^ 1]
            m["MODin"] = np.asarray(own["MODo"])
            for k in ("QS", "GS", "GT", "VS", "YA", "YS"):
                m[k + "in"] = np.asarray(own[k])
            m["init_att"] = np.asarray(pa["f_att"])
            m["init_s5"] = np.asarray(pa["f_s5"])
            maps.append(m)
        r2 = run_bass_kernel_spmd(nc2, maps, core_ids=list(range(ncore))).results
        if last:
            for ci, (b, s) in enumerate(cores):
                y = from_fm(np.asarray(r2[ci]["yT"]))
                out[b, s * TL:(s + 1) * TL] = y[::-1] if s == 1 else y
        else:
            xT = [np.asarray(r2[ci]["xT2"]) for ci in range(ncore)]
    return out
```

```python
import numpy as np
import ml_dtypes
import concourse.bass as bass
import concourse.mybir as mybir
from concourse.bass_utils import run_bass_kernel_spmd

F32 = mybir.dt.float32
BF16 = mybir.dt.bfloat16
AF = mybir.ActivationFunctionType
ALU = mybir.AluOpType
AX = mybir.AxisListType

D = 1024
KD = 8
CTX = 256
L = 128
NPT = 27
NCOL = NPT * 128
DFF = 2816
HFF = 1408
NFT = 11


class Res:
    __slots__ = ("w", "r")

    def __init__(self):
        self.w = None
        self.r = {}


class V:
    __slots__ = ("ap", "res")

    def __init__(self, ap, res):
        self.ap = ap
        self.res = res

    def __getitem__(self, idx):
        return V(self.ap[idx], self.res)

    def bc(self, shape):
        return V(self.ap.broadcast_to(shape), self.res)

    def re(self, pat, **kw):
        return V(self.ap.rearrange(pat, **kw), self.res)


class T:
    def __init__(self, ap):
        self.ap = ap
        self.res = Res()
        self.keyed = {}

    def __getitem__(self, idx):
        return V(self.ap[idx], self.res)

    def k(self, key, idx):
        r = self.keyed.get(key)
        if r is None:
            r = self.keyed[key] = Res()
        return V(self.ap[idx], r)

    def all(self):
        return V(self.ap, self.res)


class Eng:
    def __init__(self, key, sem):
        self.key = key
        self.sem = sem
        self.cnt = 0
        self.seen = {}
        self.ops = []


class Sched:
    def __init__(self, nc, stack, ndma=12):
        self.nc = nc
        self.stack = stack
        self.eng = {}
        for k in ("pe", "act", "dve", "pool", "sp"):
            self.eng[k] = Eng(k, stack.enter_context(nc.semaphore("s_" + k)))
        self.dsem = {}
        self.dq = {}
        for q in ("sp", "pool", "act"):
            self.dq[q] = 0
            for i in range(ndma):
                self.dsem[(q, i)] = [stack.enter_context(nc.semaphore(f"d_{q}_{i}")), 0]
        self.ndma = ndma
        self.n_inst = 0

    def semof(self, key):
        if isinstance(key, tuple):
            return self.dsem[key][0]
        return self.eng[key].sem

    def _deps(self, e, reads, writes):
        deps = {}

        def add(k, c):
            if deps.get(k, 0) < c:
                deps[k] = c

        for v in reads:
            if v.res.w is not None:
                add(*v.res.w)
        for v in writes:
            if v.res.w is not None:
                add(*v.res.w)
            for k, c in v.res.r.items():
                add(k, c)
        for k, c in deps.items():
            if k == "pe" and e.key == "pe":
                continue
            if e.seen.get(k, 0) < c:
                e.ops.append(("w", self.semof(k), c))
                e.seen[k] = c
                self.n_inst += 1

    def emit(self, ek, fn, reads, writes):
        e = self.eng[ek]
        self._deps(e, reads, writes)
        e.cnt += 1
        e.ops.append(("o", fn))
        self.n_inst += 1
        for v in writes:
            v.res.w = (ek, e.cnt)
            v.res.r = {}
        for v in reads:
            v.res.r[ek] = e.cnt

    def dma(self, q, out, in_, **kw):
        e = self.eng[q]
        self._deps(e, [in_], [out])
        i = self.dq[q]
        self.dq[q] = (i + 1) % self.ndma
        key = (q, i)
        ds = self.dsem[key]
        if e.seen.get(key, 0) < ds[1]:
            e.ops.append(("w", ds[0], ds[1]))
            e.seen[key] = ds[1]
        ds[1] += 16
        e.ops.append(("d", out.ap, in_.ap, ds[0], kw))
        self.n_inst += 1
        out.res.w = (key, ds[1])
        out.res.r = {}
        in_.res.r[key] = ds[1]

    def barrier(self):
        for e in self.eng.values():
            for o in self.eng.values():
                if o.key != e.key and o.cnt > 0 and e.seen.get(o.key, 0) < o.cnt:
                    e.ops.append(("w", o.sem, o.cnt))
                    e.seen[o.key] = o.cnt
            for key, ds in self.dsem.items():
                if ds[1] > 0 and e.seen.get(key, 0) < ds[1]:
                    e.ops.append(("w", ds[0], ds[1]))
                    e.seen[key] = ds[1]

    def finish(self):
        self.barrier()
        nc = self.nc
        with nc.Block() as block:
            def run(e, h):
                for op in e.ops:
                    if op[0] == "w":
                        h.wait_ge(op[1], op[2])
                    elif op[0] == "o":
                        op[1](h).then_inc(e.sem, 1)
                    else:
                        h.dma_start(out=op[1], in_=op[2], **op[4]).then_inc(op[3], 16)

            @block.tensor
            def _(h):
                run(self.eng["pe"], h)

            @block.scalar
            def _(h):
                run(self.eng["act"], h)

            @block.vector
            def _(h):
                run(self.eng["dve"], h)

            @block.gpsimd
            def _(h):
                run(self.eng["pool"], h)

            @block.sync
            def _(h):
                run(self.eng["sp"], h)

    def mm(self, out, lhsT, rhs, start=True, stop=True):
        self.emit("pe", lambda h: h.matmul(out.ap, lhsT.ap, rhs.ap, start=start, stop=stop),
                  [lhsT, rhs] + ([] if start else [out]), [out])

    def tr(self, out, in_, ident):
        self.emit("pe", lambda h: h.transpose(out.ap, in_.ap, ident.ap), [in_, ident], [out])

    def act(self, out, in_, func, bias=None, scale=None, accum=None):
        reads = [in_]
        kw = {}
        if bias is not None:
            if isinstance(bias, V):
                reads.append(bias)
                kw["bias"] = bias.ap
            else:
                kw["bias"] = float(bias)
        if scale is not None:
            if isinstance(scale, V):
                reads.append(scale)
                kw["scale"] = scale.ap
            else:
                kw["scale"] = float(scale)
        writes = [out]
        if accum is not None:
            kw["accum_out"] = accum.ap
            writes.append(accum)
        self.emit("act", lambda h: h.activation(out.ap, in_.ap, func, **kw), reads, writes)

    def ts(self, ek, out, in0, s1, op0, s2=None, op1=None):
        reads = [in0]
        a1 = s1
        a2 = s2
        if isinstance(s1, V):
            reads.append(s1)
            a1 = s1.ap
        if isinstance(s2, V):
            reads.append(s2)
            a2 = s2.ap
        if op1 is None:
            self.emit(ek, lambda h: h.tensor_scalar(out.ap, in0.ap, a1, None, op0), reads, [out])
        else:
            self.emit(ek, lambda h: h.tensor_scalar(out.ap, in0.ap, a1, a2, op0, op1), reads, [out])

    def tt(self, ek, out, a, b, op):
        self.emit(ek, lambda h: h.tensor_tensor(out.ap, a.ap, b.ap, op), [a, b], [out])

    def stt(self, out, in0, sc, in1, op0, op1):
        reads = [in0, in1]
        a = sc
        if isinstance(sc, V):
            reads.append(sc)
            a = sc.ap
        self.emit("dve", lambda h: h.scalar_tensor_tensor(out.ap, in0.ap, a, in1.ap, op0, op1), reads, [out])

    def scan(self, out, d0, d1, init, op0=ALU.mult, op1=ALU.add):
        reads = [d0, d1]
        a = init
        if isinstance(init, V):
            reads.append(init)
            a = init.ap
        self.emit("dve", lambda h: h.tensor_tensor_scan(out.ap, d0.ap, d1.ap, a, op0, op1), reads, [out])

    def copy(self, ek, out, in_):
        if ek == "act":
            self.emit("act", lambda h: h.copy(out.ap, in_.ap), [in_], [out])
        else:
            self.emit(ek, lambda h: h.tensor_copy(out.ap, in_.ap), [in_], [out])

    def memset(self, ek, out, val):
        self.emit(ek, lambda h: h.memset(out.ap, val), [], [out])

    def recip(self, out, in_):
        self.emit("dve", lambda h: h.reciprocal(out.ap, in_.ap), [in_], [out])

    def red(self, out, in_, op):
        self.emit("dve", lambda h: h.tensor_reduce(out.ap, in_.ap, AX.X, op), [in_], [out])


from contextlib import ExitStack


class Prog:
    def __init__(self, TL, debug=False, NL=4):
        self.NL = NL
        self.nc = bass.Bass("TRN2", target_bir_lowering=False)
        self.stack = ExitStack()
        self.S = Sched(self.nc, self.stack)
        self.TL = TL
        self.NT = CTX + TL
        self.tiles = [(0, CTX)] + [(CTX + 512 * i, 512) for i in range(TL // 512)]
        self.debug = debug
        self.scr_kind = "ExternalOutput" if debug else "Internal"

    def din(self, name, shape, dt=F32):
        return T(self.nc.dram_tensor(name, list(shape), dt, kind="ExternalInput").ap())

    def dout(self, name, shape, dt=F32):
        return T(self.nc.dram_tensor(name, list(shape), dt, kind="ExternalOutput").ap())

    def dscr(self, name, shape, dt=F32):
        return T(self.nc.dram_tensor(name, list(shape), dt, kind=self.scr_kind).ap())

    def sb(self, st, name, shape, dt=F32):
        self._uid = getattr(self, "_uid", 0) + 1
        t = st.enter_context(self.nc.sbuf_tensor(f"sb{self._uid}_{name}", list(shape), dt))
        return T(t[tuple(slice(None) for _ in shape)])

    def ps(self, st, name, shape, dt=F32):
        self._uid = getattr(self, "_uid", 0) + 1
        t = st.enter_context(self.nc.psum_tensor(f"ps{self._uid}_{name}", list(shape), dt))
        return T(t[tuple(slice(None) for _ in shape)])

    def flush(self):
        self.S.finish()
        for e in self.S.eng.values():
            e.ops = []


def load_cast(P, st_name, dst, src_ap_fn, npieces, piece_shape, stg, eng_cycle=("dve", "pool")):
    raise NotImplementedError


def phase_mod(P, st, W, nj):
    S = P.S
    cond = P.sb(st, "cond", [128, KD, 2])
    csl = P.sb(st, "csl", [128, KD, 2])
    adab = P.sb(st, "adab", [128, 48])
    MOD = P.sb(st, "MOD", [128, 48, 2])
    S.dma("sp", cond.all(), W["cond"].all())
    S.dma("sp", adab.all(), W["ada_b"].all())
    S.act(csl.all(), cond.all(), AF.Silu)
    with ExitStack() as st2:
        pm = P.ps(st2, "pm", [128, 48, 2])
        stg = [P.sb(st2, f"adaw{i}", [128, KD, 768]) for i in range(2)]
        aw = W["ada_w"].ap.rearrange("(kc p) n -> p kc n", p=128)
        for pc in range((nj + 5) // 6):
            sg = stg[pc % 2]
            S.dma("sp" if pc % 2 == 0 else "pool", sg.all(), V(aw[:, :, pc * 768:(pc + 1) * 768], W["ada_w"].res))
            for jj in range(6):
                j = pc * 6 + jj
                if j >= nj:
                    break
                for kc in range(KD):
                    S.mm(pm[:, j, :], sg[:, kc, jj * 128:(jj + 1) * 128], csl[:, kc, :], start=(kc == 0), stop=(kc == KD - 1))
        S.tt("dve", MOD[:, 0:nj, :], pm[:, 0:nj, :], adab[:, 0:nj].re("p (j o) -> p j o", o=1).bc([128, nj, 2]), ALU.add)
        S.barrier()
        P.flush()
    return MOD


def norm_mod(P, S, X, XN, sqb, psn, rstd, tmpf, ones_bf, s_vec, b_vec, col, Wd):
    S.act(sqb[:, :, :Wd], X[:, :, :Wd], AF.Square)
    for kc in range(KD):
        S.mm(psn[:, :Wd], ones_bf.all(), sqb[:, kc, :Wd], start=(kc == 0), stop=(kc == KD - 1))
    S.ts("dve", rstd[:, :Wd], psn[:, :Wd], 1.0 / D, ALU.mult, 1e-6, ALU.add)
    S.act(rstd[:, :Wd], rstd[:, :Wd], AF.Sqrt)
    S.recip(rstd[:, :Wd], rstd[:, :Wd])
    for kc in range(KD):
        S.stt(tmpf[:, kc, :Wd], X[:, kc, :Wd], s_vec[:, kc, col:col + 1], rstd[:, :Wd], ALU.mult, ALU.mult)
        S.act(XN[:, kc, :Wd], tmpf[:, kc, :Wd], AF.Identity, bias=b_vec[:, kc, col:col + 1])


def consts_np():
    c = {}
    c["ones_bf"] = np.ones((128, 128), ml_dtypes.bfloat16)
    c["ident_f"] = np.eye(128, dtype=np.float32)
    c["ident_bf"] = np.eye(128).astype(ml_dtypes.bfloat16)
    return c


def to_fm(a):
    T_ = a.shape[0]
    return np.ascontiguousarray(a.T.reshape(KD, 128, T_))


def from_fm(a):
    return np.ascontiguousarray(a.reshape(D, -1).T)


def alloc_scratch(P, out=False):
    NT = P.NT
    sc = {}
    mk = P.dout if out else P.dscr
    sc["QS"] = mk("QS", [18, 128, NT], BF16)
    sc["GS"] = mk("GS", [8, 128, NT], F32)
    sc["GT"] = mk("GT", [8, 128, NT], BF16)
    sc["VS"] = mk("VS", [NT, 768], BF16)
    return sc


def phase_A(P, W, MOD, sc, lbv, so=False):
    S = P.S
    with ExitStack() as st:
        Win = P.sb(st, "Win", [128, KD, NCOL], BF16)
        Wv = P.sb(st, "Wv", [128, KD, 768], BF16)
        wa2 = P.sb(st, "wa2", [128, 2, 256], BF16)
        wa2f = P.sb(st, "wa2f", [128, 2, 256])
        gba = P.sb(st, "gba", [128, 2, 2])
        n1w = P.sb(st, "n1w", [128, KD])
        s1 = P.sb(st, "s1", [128, KD, 2])
        ones_bf = P.sb(st, "ones", [128, 128], BF16)
        S.dma("sp", ones_bf.all(), W["ones_bf"].all())
        S.dma("sp", wa2f.all(), W["wa2p"].all())
        S.dma("sp", gba.all(), W["gla_ba"].all())
        S.dma("sp", n1w.all(), W["n1w"].all())
        S.copy("pool", wa2.all(), wa2f.all())
        S.ts("dve", s1.all(), MOD[:, 8:16, :], 1.0, ALU.add)
        S.tt("dve", s1.all(), s1.all(), n1w.all().re("p (k o) -> p k o", o=1).bc([128, KD, 2]), ALU.mult)
        win_v = W["w_in_arr"].ap.rearrange("(kc p) n -> p kc n", p=128)
        wv_v = W["w_v"].ap.rearrange("(kc p) n -> p kc n", p=128)
        with ExitStack() as st2:
            stg = [P.sb(st2, f"wstg{i}", [128, KD, 432]) for i in range(2)]
            for pc in range(8):
                sg = stg[pc % 2]
                S.dma("sp" if pc % 2 == 0 else "pool", sg.all(), V(win_v[:, :, pc * 432:(pc + 1) * 432], W["w_in_arr"].res))
                S.copy("dve" if pc % 2 == 0 else "act", Win[:, :, pc * 432:(pc + 1) * 432], sg.all())
            for pc in range(2):
                sg = stg[pc % 2]
                S.dma("sp", sg[:, :, 0:384], V(wv_v[:, :, pc * 384:(pc + 1) * 384], W["w_v"].res))
                S.copy("dve", Wv[:, :, pc * 384:(pc + 1) * 384], sg[:, :, 0:384])
            S.barrier()
            P.flush()
        X = [P.sb(st, f"X{i}", [128, KD, 512]) for i in range(2)]
        XN = P.sb(st, "XN", [128, KD, 512], BF16)
        sqb = P.sb(st, "sqb", [128, KD, 512], BF16)
        tmpf = P.sb(st, "tmpf", [128, KD, 512])
        rstd = P.sb(st, "rstd", [128, 512])
        rope = [P.sb(st, f"rope{i}", [128, 4, 512]) for i in range(1)]
        QSs = [P.sb(st, f"QSs{i}", [128, 18, 512], BF16) for i in range(1)]
        GSs = [P.sb(st, f"GSs{i}", [128, 8, 512]) for i in range(1)]
        GTs = [P.sb(st, f"GTs{i}", [128, 8, 512], BF16) for i in range(1)]
        Vs = [P.sb(st, f"Vs{i}", [128, 768], BF16) for i in range(3)]
        low = P.sb(st, "low", [128, 512], BF16)
        t1 = [P.sb(st, f"t1_{i}", [128, 512]) for i in range(2)]
        t2 = [P.sb(st, f"t2_{i}", [128, 512]) for i in range(2)]
        sg_ = [P.sb(st, f"sg_{i}", [128, 512]) for i in range(2)]
        psn = P.ps(st, "psn", [128, 512])
        pp = [P.ps(st, f"pp{i}", [128, 512]) for i in range(5)]
        pv = P.ps(st, "pv", [128, 1024])
        xv = W["xT"].ap.rearrange("k p t -> p k t")
        qsv = sc["QS"].ap.rearrange("n p t -> p n t")
        gsv = sc["GS"].ap.rearrange("n p t -> p n t")
        gtv = sc["GT"].ap.rearrange("n p t -> p n t")
        ppi = [0]

        def proj(pt, Wd):
            p = pp[ppi[0] % 5]
            ppi[0] += 1
            for kc in range(KD):
                S.mm(p[:, :Wd], Win[:, kc, pt * 128:(pt + 1) * 128], XN[:, kc, :Wd], start=(kc == 0), stop=(kc == KD - 1))
            return p

        for ti, (t0, Wd) in enumerate(P.tiles):
            col = 1 if ti == 0 else 0
            Xt = X[ti % 2]
            rp = rope[0]
            QSt, GSt, GTt = QSs[0], GSs[0], GTs[0]
            S.dma("sp", Xt[:, :, :Wd], V(xv[:, :, t0:t0 + Wd], W["xT"].res))
            S.dma("sp", rp[:, :, :Wd], V(W["rope"].ap[:, :, t0:t0 + Wd], W["rope"].res))
            norm_mod(P, S, Xt, XN, sqb, psn, rstd, tmpf, ones_bf, s1, MOD[:, 0:8, :], col, Wd)
            for which, (pt_a, pt_s, dst) in enumerate(((0, 4, 0), (2, 6, 6))):
                if so and which == 0:
                    continue
                for j in range(2):
                    pa = proj(pt_a + j, Wd)
                    pb = proj(pt_s + j, Wd)
                    a = t1[j]
                    b = t2[j]
                    S.tt("dve", a[:, :Wd], pa[:, :Wd], rp[:, 2 * which, :Wd], ALU.mult)
                    S.tt("dve", b[:, :Wd], pb[:, :Wd], rp[:, 2 * which + 1, :Wd], ALU.mult)
                    S.tt("pool", QSt[:, dst + j, :Wd], a[:, :Wd], b[:, :Wd], ALU.add)
                    if which == 1:
                        S.copy("pool", QSt[:, 12 + j, :Wd], QSt[:, 6 + j, :Wd])
            for j in range(2):
                if not so:
                    S.act(GTt[:, 0 + j, :Wd], proj(8 + j, Wd)[:, :Wd], AF.Silu)
                    S.act(GTt[:, 2 + j, :Wd], proj(18 + j, Wd)[:, :Wd], AF.Silu)
                    S.act(GTt[:, 4 + j, :Wd], proj(24 + j, Wd)[:, :Wd], AF.Silu)
                    S.copy("dve", QSt[:, 2 + j, :Wd], proj(12 + j, Wd)[:, :Wd])
                    S.copy("act", QSt[:, 4 + j, :Wd], proj(20 + j, Wd)[:, :Wd])
                S.copy("dve", GTt[:, 6 + j, :Wd], proj(10 + j, Wd)[:, :Wd])
                pk = proj(22 + j, Wd)
                S.ts("dve", QSt[:, 10 + j, :Wd], pk[:, :Wd], 32.0 ** -0.5, ALU.mult)
                S.copy("pool", QSt[:, 16 + j, :Wd], QSt[:, 10 + j, :Wd])
            for d in range(2):
                for j in range(2):
                    pz = proj(14 + 2 * d + j, Wd)
                    sg = sg_[j]
                    S.act(sg[:, :Wd], pz[:, :Wd], AF.Sigmoid)
                    f = t1[j]
                    S.ts("dve", f[:, :Wd], sg[:, :Wd], lbv[:, j, 0:1], ALU.mult, lbv[:, j, 1:2], ALU.add)
                    S.act(GSt[:, 4 * d + j, :Wd], f[:, :Wd], AF.Ln)
                    S.ts("dve", QSt[:, (8 if d == 0 else 14) + j, :Wd], sg[:, :Wd], lbv[:, j, 2:3], ALU.mult, lbv[:, j, 0:1], ALU.add)
            S.copy("dve", low[:, :Wd], proj(26, Wd)[:, :Wd])
            for d in range(2):
                for j in range(2):
                    p = pp[ppi[0] % 5]
                    ppi[0] += 1
                    S.mm(p[:, :Wd], wa2[32 * d:32 * d + 16, d, j * 128:(j + 1) * 128], low[32 * d:32 * d + 16, :Wd])
                    sg = sg_[j]
                    S.act(sg[:, :Wd], p[:, :Wd], AF.Sigmoid, bias=gba[:, d, j:j + 1])
                    S.act(sg[:, :Wd], sg[:, :Wd], AF.Ln)
                    S.ts("pool", GSt[:, 4 * d + 2 + j, :Wd], sg[:, :Wd], 1.0 / 16.0, ALU.mult)
            for sub in range(Wd // 128):
                for kc in range(KD):
                    S.mm(pv[:, 0:512], XN[:, kc, sub * 128:(sub + 1) * 128], Wv[:, kc, 0:512], start=(kc == 0), stop=(kc == KD - 1))
                for kc in range(KD):
                    S.mm(pv[:, 512:768], XN[:, kc, sub * 128:(sub + 1) * 128], Wv[:, kc, 512:768], start=(kc == 0), stop=(kc == KD - 1))
                vt = Vs[sub % 3]
                S.copy("act", vt.all(), pv[:, 0:768])
                S.dma("pool", V(sc["VS"].ap[t0 + sub * 128:t0 + (sub + 1) * 128, :], sc["VS"].res), vt.all())
            if so:
                S.dma("pool", V(qsv[:, 6:18, t0:t0 + Wd], sc["QS"].res), QSt[:, 6:18, :Wd])
                S.dma("pool", V(gtv[:, 6:8, t0:t0 + Wd], sc["GT"].res), GTt[:, 6:8, :Wd])
            else:
                S.dma("pool", V(qsv[:, :, t0:t0 + Wd], sc["QS"].res), QSt[:, :, :Wd])
                S.dma("pool", V(gtv[:, :, t0:t0 + Wd], sc["GT"].res), GTt[:, :, :Wd])
            S.dma("pool", V(gsv[:, :, t0:t0 + Wd], sc["GS"].res), GSt[:, :, :Wd])
        S.barrier()
        P.flush()


def compute_lbv(P, st, W, l):
    S = P.S
    NL = P.NL
    lg = P.sb(st, "lbl", [128, 2, NL])
    ex = P.sb(st, "lbe", [128, 2, NL])
    mx = P.sb(st, "lbm", [128, 2])
    sm = P.sb(st, "lbs", [128, 2])
    pa = P.sb(st, "lbp", [128, 2])
    lbv = P.sb(st, "lbv", [128, 2, 3])
    S.dma("sp", lg.all(), W["hg_lb"].all())
    S.red(mx.all(), lg.all(), ALU.max)
    S.tt("dve", ex.all(), lg.all(), mx.all().re("p (j o) -> p j o", o=1).bc([128, 2, NL]), ALU.subtract)
    S.act(ex.all(), ex.all(), AF.Exp)
    S.red(sm.all(), ex.all(), ALU.add)
    S.recip(sm.all(), sm.all())
    if l == 0:
        S.memset("dve", pa.all(), 0.0)
    else:
        S.red(pa.all(), ex[:, :, 1:l + 1], ALU.add)
        S.tt("dve", pa.all(), pa.all(), sm.all(), ALU.mult)
        S.ts("dve", pa.all(), pa.all(), 0.0, ALU.max, 1.0 - 1e-6, ALU.min)
    S.copy("dve", lbv[:, :, 1], pa.all())
    S.ts("dve", lbv[:, :, 0], pa.all(), -1.0, ALU.mult, 1.0, ALU.add)
    S.ts("dve", lbv[:, :, 2], pa.all(), 1.0, ALU.mult, -1.0, ALU.add)
    return lbv


def fm_vec(v, n=None):
    v = np.asarray(v, np.float32)
    return np.ascontiguousarray(v.reshape(-1, 128).T)


def pad_gla(a):
    out = np.zeros(a.shape[:-1] + (256,), a.dtype)
    for h in range(4):
        out[..., h * 64:h * 64 + 32] = a[..., h * 32:(h + 1) * 32]
    return out


def rope_tables(TL, half):
    NT = CTX + TL
    r = np.arange(128)
    w = r % 64
    part = w // 32
    u = w % 32
    f = u % 16
    freqs = (10000.0 ** (-(f.astype(np.float64)) / 16.0))
    tg = half * TL + np.arange(TL)
    pos = np.where(part[:, None] == 0, (tg // 64)[None, :], (tg % 64)[None, :]).astype(np.float64)
    ang = pos.astype(np.float32).astype(np.float64) * freqs.astype(np.float32).astype(np.float64)[:, None]
    cos = np.cos(ang)
    sin = np.sin(ang) * np.where(u < 16, -1.0, 1.0)[:, None]
    tab = np.zeros((128, 4, NT), np.float32)
    tab[:, 0, :CTX] = 1.0
    tab[:, 2, :CTX] = 0.125
    tab[:, 0, CTX:] = cos
    tab[:, 1, CTX:] = sin
    tab[:, 2, CTX:] = 0.125 * cos
    tab[:, 3, CTX:] = 0.125 * sin
    return tab


def arrange_w_in(w_in):
    w_in = np.asarray(w_in, np.float32)
    c = np.arange(256)
    h, w = c // 64, c % 64
    perm = h * 64 + (w // 32) * 32 + ((w % 32) + 16) % 32
    lowt = np.zeros((D, 128), np.float32)
    lowt[:, 0:16] = w_in[:, 3328:3344]
    lowt[:, 32:48] = w_in[:, 3344:3360]
    parts = [w_in[:, 0:256], w_in[:, 256:512], w_in[:, 0:256][:, perm], w_in[:, 256:512][:, perm],
             w_in[:, 768:1024], w_in[:, 1024:1280], w_in[:, 1280:1536], w_in[:, 1536:1792], w_in[:, 1792:2048],
             w_in[:, 2304:2560], pad_gla(w_in[:, 2560:2688]), pad_gla(w_in[:, 2688:2816]), w_in[:, 3072:3328], lowt]
    arr = np.ascontiguousarray(np.concatenate(parts, axis=1))
    assert arr.shape == (D, NCOL)
    wv = np.ascontiguousarray(np.concatenate([w_in[:, 512:768], w_in[:, 2048:2304], w_in[:, 2816:3072]], axis=1))
    return arr, wv


def layer_consts(inp, l):
    d = {}
    d["ada_w"] = np.ascontiguousarray(inp["ada_w"][l])
    d["ada_b"] = fm_vec(inp["ada_b"][l])
    d["n1w"] = fm_vec(inp["norm1_w"][l])
    d["n2w"] = fm_vec(inp["norm2_w"][l])
    d["w_in_arr"], d["w_v"] = arrange_w_in(inp["w_in"][l])
    wa2p = np.zeros((128, 2, 256), np.float32)
    wa2p[0:16, 0, :] = pad_gla(np.asarray(inp["gla_wa2"][l][0]))
    wa2p[32:48, 1, :] = pad_gla(np.asarray(inp["gla_wa2"][l][1]))
    d["wa2p"] = wa2p
    ba = pad_gla(np.asarray(inp["gla_ba"][l]))
    d["gla_ba"] = np.ascontiguousarray(ba.reshape(2, 2, 128).transpose(2, 0, 1))
    d["hg_lb"] = np.ascontiguousarray(np.asarray(inp["hg_lb_logits"]).reshape(-1, 2, 128).transpose(2, 1, 0))
    d.update(consts_np())
    return d


def core_cond(inp, b):
    c = np.stack([fm_vec(inp["c"][b]), fm_vec(inp["c_ctx"])], axis=-1)
    return np.ascontiguousarray(c)


def build_test_A(TL, l, NL=4):
    P = Prog(TL, debug=True, NL=NL)
    W = {}
    NT = P.NT
    for name, shape, dt in [("xT", [KD, 128, NT], F32), ("cond", [128, KD, 2], F32), ("ada_w", [D, 6 * D], F32),
                            ("ada_b", [128, 48], F32), ("n1w", [128, KD], F32), ("w_in_arr", [D, NCOL], F32),
                            ("w_v", [D, 768], F32), ("wa2p", [128, 2, 256], F32), ("gla_ba", [128, 2, 2], F32),
                            ("hg_lb", [128, 2, P.NL], F32), ("rope", [128, 4, NT], F32), ("ones_bf", [128, 128], BF16)]:
        W[name] = P.din(name, shape, dt)
    sc = alloc_scratch(P)
    with ExitStack() as st:
        MOD = phase_mod(P, st, W, 16)
        lbv = compute_lbv(P, st, W, l)
        phase_A(P, W, MOD, sc, lbv)
    return P.nc


def attn_consts_np(mirror=False):
    lg = np.log1p(-(2.0 ** (-5.0 - np.arange(4, dtype=np.float32)))).astype(np.float32)
    if mirror:
        lg = lg[::-1]
    bret = np.zeros((128, 2, 2, 128), np.float32)
    t = np.arange(128, dtype=np.float32)
    for d in range(2):
        lgd = lg if d == 0 else lg[::-1]
        for tile in range(2):
            for hh in range(2):
                h = tile * 2 + hh
                cnt = (t + 1) if d == 0 else (128 - t)
                bret[hh * 64:(hh + 1) * 64, d, tile, :] = (cnt * lgd[h])[None, :]
    j = np.arange(128)[:, None]
    i = np.arange(128)[None, :]
    masks = np.stack([(j <= i), (j >= i)], axis=1).astype(np.float32)
    return {"bret": bret, "masks": np.ascontiguousarray(masks)}


class AttnBufs:
    pass


def attn_alloc(P, st, W):
    S = P.S
    A = AttnBufs()
    A.ones = P.sb(st, "a_ones", [128, 128])
    S.memset("dve", A.ones.all(), 1.0)
    A.masks = P.sb(st, "a_masks", [128, 2, 128])
    S.dma("sp", A.masks.all(), W["masks"].all())
    A.identb = P.sb(st, "a_identb", [128, 128], BF16)
    S.dma("sp", A.identb.all(), W["ident_bf"].all())
    A.B = []
    bretv = W["bret"]
    for d in range(2):
        bl = []
        for pp_ in range(2):
            b = P.sb(st, f"a_B{d}{pp_}", [128, 6, 128])
            S.dma("sp", b[:, 0:2, :], bretv[:, d, :, :])
            bl.append(b)
        A.B.append(bl)
    A.cc = 0
    A.E_ = [P.sb(st, f"a_E{i}", [128, 6, 128]) for i in range(2)]
    A.D1_ = [P.sb(st, f"a_D1{i}", [128, 6, 128]) for i in range(2)]
    A.Eq1_ = [P.sb(st, f"a_Eq1{i}", [128, 6, 64]) for i in range(2)]
    A.Ek1_ = [P.sb(st, f"a_Ek1{i}", [128, 6, 128]) for i in range(2)]
    A.Ek0_ = [P.sb(st, f"a_Ek0{i}", [128, 6, 64]) for i in range(2)]
    A.qh_ = [P.sb(st, f"a_qh{i}", [128, 6, 128], BF16) for i in range(2)]
    A.q1_ = [P.sb(st, f"a_q1{i}", [128, 6, 64], BF16) for i in range(2)]
    A.k1_ = [P.sb(st, f"a_k1{i}", [128, 6, 128], BF16) for i in range(2)]
    A.k0_ = [P.sb(st, f"a_k0{i}", [128, 6, 64], BF16) for i in range(2)]
    A.k1T_ = [P.sb(st, f"a_k1T{i}", [128, 6, 128], BF16) for i in range(2)]
    A.tS_ = [P.sb(st, f"a_tS{i}", [128, 6, 64]) for i in range(2)]
    A.Asb = [[[P.sb(st, f"a_A{d}{m}{i}", [128, 4, 128], BF16) for i in range(2)] for m in range(3)] for d in range(2)]
    for d in range(2):
        for m in range(3):
            for i in range(2):
                S.memset("pool", A.Asb[d][m][i].all(), 0.0)
    A.S = P.sb(st, "a_S", [128, 6, 64])
    A.Sbf = P.sb(st, "a_Sbf", [128, 6, 64], BF16)
    A.Q = [P.sb(st, f"a_Q{i}", [128, 6, 512], BF16) for i in range(2)]
    A.K = [P.sb(st, f"a_K{i}", [128, 6, 512], BF16) for i in range(2)]
    A.G = [P.sb(st, f"a_G{i}", [128, 4, 512]) for i in range(2)]
    A.Vt = [P.sb(st, f"a_V{i}", [128, 4, 768], BF16) for i in range(2)]
    A.Yo = [P.sb(st, f"a_Yo{i}", [128, 6, 512]) for i in range(2)]
    A.Yf = [P.sb(st, f"a_Yf{i}", [128, 6, 512]) for i in range(2)]
    A.psA = [P.ps(st, f"a_psA{m}", [128, 4, 128]) for m in range(3)]
    A.psO = P.ps(st, "a_psO", [128, 6, 128])
    A.psT = P.ps(st, "a_psT", [128, 6, 128], BF16)
    A.psS = P.ps(st, "a_psS", [128, 6, 64])
    return A


def attn_dirs(d):
    if d == 0:
        return slice(0, 64), slice(64, 128), 63, 127, 63
    return slice(64, 128), slice(0, 64), 64, 0, 0


def attn_stage1(P, A, d, par, Q, K, G, c0, full):
    S = P.S
    B = A.B[d][par]
    E, D1, Eq1, Ek1, Ek0 = A.E_[par], A.D1_[par], A.Eq1_[par], A.Ek1_[par], A.Ek0_[par]
    qh, q1, k1, k0, k1T = A.qh_[par], A.q1_[par], A.k1_[par], A.k0_[par], A.k1T_[par]
    cs = slice(c0, c0 + 128)
    H0, H1, ref, last, last1 = attn_dirs(d)
    rv = (lambda v: v) if d == 0 else (lambda v: v[:, ::-1])
    for j in range(4):
        S.scan(rv(B[:, 2 + j, :]), rv(A.ones.all()), rv(G[:, j, cs]), 0.0)
    bref = B[:, :, ref:ref + 1]
    S.tt("dve", D1.all(), B.all(), bref.bc([128, 6, 128]), ALU.subtract)
    S.act(Eq1.all(), D1[:, :, H1], AF.Exp)
    S.ts("dve", D1.all(), D1.all(), -1.0, ALU.mult, 80.0, ALU.min)
    S.act(Ek1.all(), D1.all(), AF.Exp)
    S.tt("dve", k1.all(), K[:, :, cs], Ek1.all(), ALU.mult)
    for j in range(6):
        S.tr(A.psT[:, j, :], k1[:, j, :], A.identb.all())
    S.copy("act", k1T.all(), A.psT.all())
    if full:
        S.act(E.all(), B.all(), AF.Exp)
        S.ts("pool", Ek0.all(), B[:, :, H0], -1.0, ALU.mult, 80.0, ALU.min)
        S.act(Ek0.all(), Ek0.all(), AF.Exp)
        S.tt("dve", qh.all(), Q[:, :, cs], E.all(), ALU.mult)
        S.tt("pool", q1.all(), Q[:, :, cs][:, :, H1], Eq1.all(), ALU.mult)
        S.tt("pool", k0.all(), K[:, :, cs][:, :, H0], Ek0.all(), ALU.mult)
        mask = A.masks[:, d, :]
        for m in range(3):
            psA = A.psA[m]
            for h in range(4):
                tile = 2 * m + h // 2
                rows = slice((h % 2) * 64, (h % 2) * 64 + 64)
                S.mm(psA[H0, h, H0], k0[rows, tile, :], qh[rows, tile, H0])
                S.mm(psA[:, h, H1], k1[rows, tile, :], q1[rows, tile, :])
            Asb = A.Asb[d][m][par]
            S.tt("dve", Asb[H0, :, H0], psA[H0, :, H0], mask[H0, H0].re("p (o i) -> p o i", o=1).bc([64, 4, 64]), ALU.mult)
            S.tt("dve", Asb[:, :, H1], psA[:, :, H1], mask[:, H1].re("p (o i) -> p o i", o=1).bc([128, 4, 64]), ALU.mult)
    else:
        S.act(E[:, :, last:last + 1], B[:, :, last:last + 1], AF.Exp)


def attn_stage2(P, A, d, par, Vt, c0, sub, full, Yo):
    S = P.S
    E, Eq1, qh, k1T, tS = A.E_[par], A.Eq1_[par], A.qh_[par], A.k1T_[par], A.tS_[par]
    cs = slice(c0, c0 + 128)
    H0, H1, ref, last, last1 = attn_dirs(d)
    if full:
        for m in range(3):
            Asb = A.Asb[d][m][par]
            for h in range(4):
                tile = 2 * m + h // 2
                rows = slice((h % 2) * 64, (h % 2) * 64 + 64)
                vc = slice(m * 256 + h * 64, m * 256 + h * 64 + 64)
                S.mm(A.psO[rows, tile, :], Vt[:, sub, vc], Asb[:, h, :], start=True, stop=False)
                S.mm(A.psO[rows, tile, :], A.Sbf[rows, tile, :], qh[rows, tile, :], start=False, stop=True)
        S.copy("act", Yo[:, :, cs], A.psO.all())
    for m in range(3):
        for h in range(4):
            tile = 2 * m + h // 2
            rows = slice((h % 2) * 64, (h % 2) * 64 + 64)
            vc = slice(m * 256 + h * 64, m * 256 + h * 64 + 64)
            S.mm(A.psS[rows, tile, :], k1T[:, tile, rows], Vt[:, sub, vc])
    e1 = E[:, :, last:last + 1].bc([128, 6, 64])
    e2 = Eq1[:, :, last1:last1 + 1].bc([128, 6, 64])
    S.tt("dve", tS.all(), A.psS.all(), e2, ALU.mult)
    S.tt("pool", A.S.all(), A.S.all(), e1, ALU.mult)
    S.tt("pool", A.S.all(), A.S.all(), tS.all(), ALU.add)
    S.copy("pool", A.Sbf.all(), A.S.all())


def attn_pass(P, A, sc, d, full, init_lat, out_ctx, out_fin, YA, Ysrc=None):
    S = P.S
    qsv = sc["QS"].ap.rearrange("n p t -> p n t")
    gsv = sc["GS"].ap.rearrange("n p t -> p n t")
    yav = YA.ap.rearrange("n p t -> p n t") if YA is not None else None
    if Ysrc is None:
        Ysrc = YA
    ysrcv = Ysrc.ap.rearrange("n p t -> p n t") if Ysrc is not None else None
    S.memset("pool", A.S.all(), 0.0)
    S.memset("pool", A.Sbf.all(), 0.0)
    lat = P.tiles[1:]
    order = [P.tiles[0]] + (lat if d == 0 else lat[::-1])
    chunks = []
    for n, (t0, Wd) in enumerate(order):
        nch = Wd // 128
        cis = list(range(nch)) if d == 0 else list(range(nch - 1, -1, -1))
        for idx, ci in enumerate(cis):
            chunks.append((n, t0, Wd, ci, idx == 0, idx == nch - 1))

    def s1(i):
        n, t0, Wd, ci, first, lastc = chunks[i]
        Q, K, G, Vt = A.Q[n % 2], A.K[n % 2], A.G[n % 2], A.Vt[n % 2]
        if first:
            if full:
                S.dma("sp", Q[:, :, :Wd], V(qsv[:, 0:6, t0:t0 + Wd], sc["QS"].res))
            ko = 6 if d == 0 else 12
            S.dma("sp", K[:, :, :Wd], V(qsv[:, ko:ko + 6, t0:t0 + Wd], sc["QS"].res))
            S.dma("sp", G[:, :, :Wd], V(gsv[:, 4 * d:4 * d + 4, t0:t0 + Wd], sc["GS"].res))
            S.dma("sp", Vt[:, :Wd // 128, :], V(sc["VS"].ap[t0:t0 + Wd, :].rearrange("(s p) c -> p s c", p=128), sc["VS"].res))
            if full and d == 1:
                S.dma("sp", A.Yf[n % 2][:, :, :Wd], V(ysrcv[:, :, t0:t0 + Wd], Ysrc.res))
        attn_stage1(P, A, d, i % 2, Q, K, G, ci * 128, full)

    def s2(i):
        n, t0, Wd, ci, first, lastc = chunks[i]
        Vt, Yo = A.Vt[n % 2], A.Yo[n % 2]
        attn_stage2(P, A, d, i % 2, Vt, ci * 128, ci, full, Yo)
        if lastc:
            if full:
                if d == 1:
                    S.tt("dve", Yo[:, :, :Wd], Yo[:, :, :Wd], A.Yf[n % 2][:, :, :Wd], ALU.add)
                S.dma("pool", V(yav[:, :, t0:t0 + Wd], YA.res), Yo[:, :, :Wd])
            if n == 0:
                if out_ctx is not None:
                    S.dma("pool", out_ctx, A.S.all())
                if init_lat is not None:
                    S.dma("sp", A.S.all(), init_lat)
                    S.copy("pool", A.Sbf.all(), A.S.all())

    s1(0)
    for i in range(len(chunks)):
        if i + 1 < len(chunks):
            s1(i + 1)
        s2(i)
    if out_fin is not None:
        S.dma("pool", out_fin, A.S.all())


def build_test_B(TL, l, NL=4, full=True):
    P = Prog(TL, debug=True, NL=NL)
    W = {}
    NT = P.NT
    for name, shape, dt in [("xT", [KD, 128, NT], F32), ("cond", [128, KD, 2], F32), ("ada_w", [D, 6 * D], F32),
                            ("ada_b", [128, 48], F32), ("n1w", [128, KD], F32), ("w_in_arr", [D, NCOL], F32),
                            ("w_v", [D, 768], F32), ("wa2p", [128, 2, 256], F32), ("gla_ba", [128, 2, 2], F32),
                            ("hg_lb", [128, 2, P.NL], F32), ("rope", [128, 4, NT], F32), ("ones_bf", [128, 128], BF16),
                            ("ident_bf", [128, 128], BF16), ("bret", [128, 2, 2, 128], F32), ("masks", [128, 2, 128], F32),
                            ("init_att", [2, 128, 6, 64], F32)]:
        W[name] = P.din(name, shape, dt)
    sc = alloc_scratch(P)
    YA = P.dscr("YA", [6, 128, NT], F32)
    st_out = P.dout("st_out", [4, 128, 6, 64], F32)
    with ExitStack() as st:
        MOD = phase_mod(P, st, W, 16)
        lbv = compute_lbv(P, st, W, l)
        phase_A(P, W, MOD, sc, lbv)
        with ExitStack() as st2:
            A = attn_alloc(P, st2, W)
            for d in range(2):
                attn_pass(P, A, sc, d, full, V(W["init_att"].ap[d], W["init_att"].res) if full else None,
                          V(st_out.ap[d], st_out.res), V(st_out.ap[2 + d], st_out.res), YA if full else None)
            P.S.barrier()
            P.flush()
    return P.nc


def s5_layout_np(inp, l):
    d = {}
    G, Pn, Cg = 16, 64, 16

    def sm(a):
        return np.ascontiguousarray(np.asarray(a, np.float32).reshape(2, 8, 128).transpose(2, 0, 1))

    d["s5_lre"] = sm(inp["s5_lam_re"][l])
    d["s5_lim"] = sm(inp["s5_lam_im"][l])
    dt = np.repeat(np.asarray(inp["s5_log_dt"][l], np.float32)[:, :, None], Pn, axis=2)
    d["s5_ldt"] = sm(dt)
    BT = np.zeros((2, 2, 8, 128, 128), np.float32)
    CT = np.zeros((2, 2, 8, 128, 128), np.float32)
    for dd in range(2):
        for ri, (bn, cn) in enumerate((("s5_b_re", "s5_c_re"), ("s5_b_im", "s5_c_im"))):
            Bm = np.asarray(inp[bn][l][dd], np.float32)
            Cm = np.asarray(inp[cn][l][dd], np.float32)
            for k in range(8):
                for gg in range(2):
                    g = 2 * k + gg
                    r0 = (g % 8) * 16
                    BT[dd, ri, k, r0:r0 + 16, gg * 64:(gg + 1) * 64] = Bm[g].T
                    CT[dd, ri, k, gg * 64:(gg + 1) * 64, r0:r0 + 16] = Cm[g].T
    d["s5_BT"] = np.ascontiguousarray(BT.transpose(3, 0, 1, 2, 4).reshape(128, 32, 128))
    d["s5_CT"] = np.ascontiguousarray(CT.transpose(3, 0, 1, 2, 4).reshape(128, 32, 128))
    d["s5_d"] = fm_vec(inp["s5_d"][l])
    d["glu_w"] = np.ascontiguousarray(inp["s5_glu_w"][l])
    d["glu_b"] = fm_vec(inp["s5_glu_b"][l])
    return d


class S5Bufs:
    pass


def cmul(S, ek, o_re, o_im, a_re, a_im, b_re, b_im, t1, t2):
    S.tt(ek, t1, a_re, b_re, ALU.mult)
    S.tt(ek, t2, a_im, b_im, ALU.mult)
    S.tt(ek, o_im, a_re, b_im, ALU.mult)
    S.tt(ek, o_re, t1, t2, ALU.subtract)
    S.tt(ek, t1, a_im, b_re, ALU.mult)
    S.tt(ek, o_im, o_im, t1, ALU.add)


def s5_alloc(P, st, W):
    S = P.S
    B = S5Bufs()
    B.BT = P.sb(st, "s5BT", [128, 32, 128], BF16)
    B.CT = P.sb(st, "s5CT", [128, 32, 128], BF16)
    with ExitStack() as st2:
        stg = P.sb(st2, "s5stg", [128, 32, 128])
        S.dma("sp", stg.all(), W["s5_BT"].all())
        S.copy("dve", B.BT.all(), stg.all())
        stg2 = P.sb(st2, "s5stg2", [128, 32, 128])
        S.dma("sp", stg2.all(), W["s5_CT"].all())
        S.copy("act", B.CT.all(), stg2.all())
        S.barrier()
        P.flush()
    B.lre = P.sb(st, "s5lre", [128, 2, 8])
    B.lim = P.sb(st, "s5lim", [128, 2, 8])
    B.ldt = P.sb(st, "s5ldt", [128, 2, 8])
    S.dma("sp", B.lre.all(), W["s5_lre"].all())
    S.dma("sp", B.lim.all(), W["s5_lim"].all())
    S.dma("sp", B.ldt.all(), W["s5_ldt"].all())
    B.dt = P.sb(st, "s5dt", [128, 2, 8])
    S.act(B.dt.all(), B.ldt.all(), AF.Exp)
    B.r = P.sb(st, "s5r", [128, 2, 8])
    B.u = P.sb(st, "s5u", [128, 2, 2, 8])
    B.coef = P.sb(st, "s5coef", [128, 2, 2, 8])
    B.uW = P.sb(st, "s5uW", [128, 2, 2, 8])
    tmp = [P.sb(st, f"s5tmp{i}", [128, 2, 8]) for i in range(6)]
    th = tmp[0]
    S.tt("dve", th.all(), B.lim.all(), B.dt.all(), ALU.mult)
    S.tt("dve", tmp[1].all(), B.lre.all(), B.dt.all(), ALU.mult)
    S.act(B.r.all(), tmp[1].all(), AF.Exp)
    x, x2, pa, pb_ = tmp[2], tmp[3], tmp[4], tmp[5]
    ure, uim = B.u[:, :, 0, :], B.u[:, :, 1, :]
    S.ts("dve", x.all(), th.all(), 1.0 / 64, ALU.mult)
    S.tt("dve", x2.all(), x.all(), x.all(), ALU.mult)
    S.ts("dve", pa.all(), x2.all(), -1.0 / 5040, ALU.mult)
    S.stt(pa.all(), pa.all(), 1.0 / 120, x2.all(), ALU.add, ALU.mult)
    S.stt(pa.all(), pa.all(), -1.0 / 6, x2.all(), ALU.add, ALU.mult)
    S.stt(uim, pa.all(), 1.0, x.all(), ALU.add, ALU.mult)
    S.ts("dve", pb_.all(), x2.all(), 1.0 / 40320, ALU.mult)
    S.stt(pb_.all(), pb_.all(), -1.0 / 720, x2.all(), ALU.add, ALU.mult)
    S.stt(pb_.all(), pb_.all(), 1.0 / 24, x2.all(), ALU.add, ALU.mult)
    S.stt(pb_.all(), pb_.all(), -0.5, x2.all(), ALU.add, ALU.mult)
    S.ts("dve", ure, pb_.all(), 1.0, ALU.add)
    for _ in range(6):
        S.tt("dve", pa.all(), ure, ure, ALU.mult)
        S.tt("dve", pb_.all(), uim, uim, ALU.mult)
        S.stt(x.all(), ure, 2.0, uim, ALU.mult, ALU.mult)
        S.tt("dve", ure, pa.all(), pb_.all(), ALU.subtract)
        S.copy("dve", uim, x.all())
    S.tt("dve", pa.all(), ure, ure, ALU.mult)
    S.tt("dve", pb_.all(), uim, uim, ALU.mult)
    S.tt("dve", pa.all(), pa.all(), pb_.all(), ALU.add)
    S.act(pa.all(), pa.all(), AF.Sqrt)
    S.recip(pa.all(), pa.all())
    S.tt("dve", ure, ure, pa.all(), ALU.mult)
    S.tt("dve", uim, uim, pa.all(), ALU.mult)
    are, aim = tmp[0], tmp[1]
    S.tt("dve", are.all(), B.u[:, :, 0, :], B.r.all(), ALU.mult)
    S.tt("dve", aim.all(), B.u[:, :, 1, :], B.r.all(), ALU.mult)
    S.ts("dve", are.all(), are.all(), -1.0, ALU.add)
    den = tmp[2]
    S.tt("dve", den.all(), B.lre.all(), B.lre.all(), ALU.mult)
    S.tt("dve", tmp[3].all(), B.lim.all(), B.lim.all(), ALU.mult)
    S.tt("dve", den.all(), den.all(), tmp[3].all(), ALU.add)
    S.recip(den.all(), den.all())
    nl = tmp[3]
    S.ts("dve", nl.all(), B.lim.all(), -1.0, ALU.mult)
    cmul(S, "dve", B.coef[:, :, 0, :], B.coef[:, :, 1, :], are.all(), aim.all(), B.lre.all(), nl.all(), tmp[4].all(), tmp[5].all())
    S.tt("dve", B.coef[:, :, 0, :], B.coef[:, :, 0, :], den.all(), ALU.mult)
    S.tt("dve", B.coef[:, :, 1, :], B.coef[:, :, 1, :], den.all(), ALU.mult)
    B.POST = P.sb(st, "s5POST", [128, 8, 2, 512])
    B.PRE = P.sb(st, "s5PRE", [128, 8, 2, 512])
    B.tt1 = P.sb(st, "s5tt1", [128, 8, 256])
    B.tt2 = P.sb(st, "s5tt2", [128, 8, 256])
    B.G = P.sb(st, "s5G", [128, 8, 2, 512])
    B.Tp1 = [P.sb(st, f"s5Tp1_{i}", [128, 2, 512]) for i in range(2)]
    B.Tp2 = [P.sb(st, f"s5Tp2_{i}", [128, 2, 512]) for i in range(2)]
    B.Tq1 = [P.sb(st, f"s5Tq1_{i}", [128, 2, 512]) for i in range(2)]
    B.Tq2 = [P.sb(st, f"s5Tq2_{i}", [128, 2, 512]) for i in range(2)]
    B.Hb = [P.sb(st, f"s5Hb{i}", [128, 2, 512], BF16) for i in range(2)]
    B.U = [P.sb(st, f"s5U{i}", [128, 2, 512], BF16) for i in range(2)]
    B.Wt = [P.sb(st, f"s5Wt{i}", [128, 2, 512]) for i in range(2)]
    B.Yo = [P.sb(st, f"s5Yo{i}", [128, 2, 512]) for i in range(2)]
    B.Yf = P.sb(st, "s5Yf", [128, 2, 512])
    B.h = P.sb(st, "s5h", [128, 2, 8])
    B.gi = P.sb(st, "s5gi", [128, 2, 8])
    B.sm = [P.sb(st, f"s5sm{i}", [128, 8]) for i in range(3)]
    B.pb = [P.ps(st, f"s5pb{i}", [128, 2, 512]) for i in range(2)]
    B.py = [P.ps(st, f"s5py{i}", [128, 512]) for i in range(2)]
    return B


def s5_tables(P, B, d):
    S = P.S
    S.memset("dve", B.POST[:, :, 0, 0:1], 1.0)
    S.memset("dve", B.POST[:, :, 1, 0:1], 0.0)
    S.copy("dve", B.uW[:, d, :, :], B.u[:, d, :, :])
    n = 1
    while n < 512:
        wre = B.uW[:, d, 0, :].re("p (k o) -> p k o", o=1).bc([128, 8, n])
        wim = B.uW[:, d, 1, :].re("p (k o) -> p k o", o=1).bc([128, 8, n])
        cmul(S, "dve", B.POST[:, :, 0, n:2 * n], B.POST[:, :, 1, n:2 * n], B.POST[:, :, 0, 0:n], B.POST[:, :, 1, 0:n],
             wre, wim, B.tt1[:, :, 0:n], B.tt2[:, :, 0:n])
        n *= 2
        if n < 512:
            cmul(S, "dve", B.sm[0].all(), B.sm[1].all(), B.uW[:, d, 0, :], B.uW[:, d, 1, :], B.uW[:, d, 0, :], B.uW[:, d, 1, :], B.sm[2].all(), B.gi[:, 0, :])
            S.copy("dve", B.uW[:, d, 0, :], B.sm[0].all())
            S.copy("dve", B.uW[:, d, 1, :], B.sm[1].all())
    for hlf in range(2):
        sl = slice(hlf * 256, (hlf + 1) * 256)
        cre = B.coef[:, d, 0, :].re("p (k o) -> p k o", o=1).bc([128, 8, 256])
        cim = B.coef[:, d, 1, :].re("p (k o) -> p k o", o=1).bc([128, 8, 256])
        S.tt("dve", B.tt1.all(), B.POST[:, :, 0, sl], cre, ALU.mult)
        S.tt("dve", B.tt2.all(), B.POST[:, :, 1, sl], cim, ALU.mult)
        S.tt("dve", B.PRE[:, :, 0, sl], B.tt1.all(), B.tt2.all(), ALU.add)
        S.tt("dve", B.tt1.all(), B.POST[:, :, 0, sl], cim, ALU.mult)
        S.tt("dve", B.tt2.all(), B.POST[:, :, 1, sl], cre, ALU.mult)
        S.tt("dve", B.PRE[:, :, 1, sl], B.tt1.all(), B.tt2.all(), ALU.subtract)


def s5_pass(P, B, sc, d, full, init_lat, out_ctx, out_fin, YS, Ysrc=None):
    S = P.S
    gtv = sc["GT"].ap.rearrange("n p t -> p n t")
    ysv = YS.ap.rearrange("n p t -> p n t") if YS is not None else None
    if Ysrc is None:
        Ysrc = YS
    ysrcv = Ysrc.ap.rearrange("n p t -> p n t") if Ysrc is not None else None
    s5_tables(P, B, d)
    S.memset("dve", B.h.all(), 0.0)
    lat = P.tiles[1:]
    order = [P.tiles[0]] + (lat if d == 0 else lat[::-1])
    rv = (lambda v: v) if d == 0 else (lambda v: v[:, ::-1])
    rv3 = (lambda v: v) if d == 0 else (lambda v: v[:, :, ::-1])
    for n, (t0, Wd) in enumerate(order):
        U, Yo = B.U[n % 2], B.Yo[n % 2]
        S.dma("sp", U[:, :, :Wd], V(gtv[:, 6:8, t0:t0 + Wd], sc["GT"].res))
        if full and d == 1:
            S.dma("sp", B.Yf[:, :, :Wd], V(ysrcv[:, :, t0:t0 + Wd], Ysrc.res))
        cmul(S, "dve", B.gi[:, 0, :], B.gi[:, 1, :], B.u[:, d, 0, :], B.u[:, d, 1, :], B.h[:, 0, :], B.h[:, 1, :], B.sm[0].all(), B.sm[1].all())
        def Gk(k, idx):
            return B.G.k(k, (slice(None), k) + idx)

        def pre(k):
            pb = B.pb[k % 2]
            for ri in range(2):
                S.mm(pb[:, ri, :Wd], B.BT[:, (d * 2 + ri) * 8 + k, :], U[:, k // 4, :Wd])
            pre_re = rv3(B.PRE[:, k, 0:1, :Wd]).bc([128, 2, Wd])
            pre_im = rv3(B.PRE[:, k, 1:2, :Wd]).bc([128, 2, Wd])
            Wt = B.Wt[k % 2]
            T1, T2 = B.Tp1[k % 2], B.Tp2[k % 2]
            S.tt("dve", T1[:, :, :Wd], pb[:, :, :Wd], pre_re, ALU.mult)
            S.tt("dve", T2[:, :, :Wd], pb[:, ::-1, :Wd], pre_im, ALU.mult)
            S.tt("pool", Wt[:, 0, :Wd], T1[:, 0, :Wd], T2[:, 0, :Wd], ALU.subtract)
            S.tt("pool", Wt[:, 1, :Wd], T1[:, 1, :Wd], T2[:, 1, :Wd], ALU.add)

        def mid(k):
            Wt = B.Wt[k % 2]
            rbc = B.r[:, d, k:k + 1].bc([128, Wd])
            for ri in range(2):
                S.scan(rv(Gk(k, (ri, slice(0, Wd)))), rbc, rv(Wt[:, ri, :Wd]), B.gi[:, ri, k:k + 1])

        def post(k):
            post_re = rv3(B.POST[:, k, 0:1, :Wd]).bc([128, 2, Wd])
            post_im = rv3(B.POST[:, k, 1:2, :Wd]).bc([128, 2, Wd])
            Hb = B.Hb[k % 2]
            T1, T2 = B.Tq1[k % 2], B.Tq2[k % 2]
            S.tt("dve", T1[:, :, :Wd], Gk(k, (slice(None), slice(0, Wd))), post_re, ALU.mult)
            S.tt("dve", T2[:, :, :Wd], Gk(k, (slice(None, None, -1), slice(0, Wd))), post_im, ALU.mult)
            S.tt("pool", Hb[:, 0, :Wd], T1[:, 0, :Wd], T2[:, 0, :Wd], ALU.subtract)
            S.stt(Hb[:, 1, :Wd], T1[:, 1, :Wd], -1.0, T2[:, 1, :Wd], ALU.mult, ALU.subtract)
            o = k // 4
            for ri in range(2):
                S.mm(B.py[o][:, :Wd], B.CT[:, (d * 2 + ri) * 8 + k, :], Hb[:, ri, :Wd],
                     start=(k % 4 == 0 and ri == 0), stop=(k % 4 == 3 and ri == 1))

        pre(0)
        for k in range(8):
            if k + 1 < 8:
                pre(k + 1)
            mid(k)
            if full:
                post(k)
        mi = Wd - 1 if d == 0 else 0
        S.eng["dve"].ops.append(("w", S.eng["dve"].sem, S.eng["dve"].cnt))
        S.eng["dve"].seen["dve"] = S.eng["dve"].cnt
        cmul(S, "dve", B.h[:, 0, :], B.h[:, 1, :], B.POST[:, :, 0, Wd - 1], B.POST[:, :, 1, Wd - 1],
             B.G[:, :, 0, mi], B.G[:, :, 1, mi], B.sm[0].all(), B.sm[1].all())
        for k_ in range(8):
            B.G.k(k_, (slice(None),)).res.r["dve"] = S.eng["dve"].cnt
        if full:
            for o in range(2):
                if d == 1:
                    S.tt("dve", Yo[:, o, :Wd], B.py[o][:, :Wd], B.Yf[:, o, :Wd], ALU.add)
                else:
                    S.copy("act", Yo[:, o, :Wd], B.py[o][:, :Wd])
            S.dma("pool", V(ysv[:, :, t0:t0 + Wd], YS.res), Yo[:, :, :Wd])
        if n == 0:
            if out_ctx is not None:
                S.dma("pool", out_ctx, B.h.all())
            if init_lat is not None:
                S.dma("sp", B.h.all(), init_lat)
    if out_fin is not None:
        S.dma("pool", out_fin, B.h.all())


W_A = [("xT", lambda P: [KD, 128, P.NT], F32), ("cond", lambda P: [128, KD, 2], F32), ("ada_w", lambda P: [D, 6 * D], F32),
       ("ada_b", lambda P: [128, 48], F32), ("n1w", lambda P: [128, KD], F32), ("w_in_arr", lambda P: [D, NCOL], F32),
       ("w_v", lambda P: [D, 768], F32), ("wa2p", lambda P: [128, 2, 256], F32), ("gla_ba", lambda P: [128, 2, 2], F32),
       ("hg_lb", lambda P: [128, 2, P.NL], F32), ("rope", lambda P: [128, 4, P.NT], F32), ("ones_bf", lambda P: [128, 128], BF16),
       ("ident_bf", lambda P: [128, 128], BF16), ("bret", lambda P: [128, 2, 2, 128], F32), ("masks", lambda P: [128, 2, 128], F32),
       ("s5_lre", lambda P: [128, 2, 8], F32), ("s5_lim", lambda P: [128, 2, 8], F32), ("s5_ldt", lambda P: [128, 2, 8], F32),
       ("s5_BT", lambda P: [128, 32, 128], F32), ("s5_CT", lambda P: [128, 32, 128], F32)]


def declare(P, specs, prefix=""):
    W = {}
    for name, shp, dt in specs:
        W[name] = P.din(prefix + name, shp(P), dt)
    return W


def build_test_S(TL, l, NL=4, full=True):
    P = Prog(TL, debug=True, NL=NL)
    W = declare(P, W_A + [("init_s5", lambda P: [2, 128, 2, 8], F32)])
    NT = P.NT
    sc = alloc_scratch(P)
    YS = P.dscr("YS", [2, 128, NT], F32)
    st_out = P.dout("st_out", [4, 128, 2, 8], F32)
    with ExitStack() as st:
        MOD = phase_mod(P, st, W, 16)
        lbv = compute_lbv(P, st, W, l)
        phase_A(P, W, MOD, sc, lbv)
        with ExitStack() as st2:
            B = s5_alloc(P, st2, W)
            for d in range(2):
                s5_pass(P, B, sc, d, full, V(W["init_s5"].ap[d], W["init_s5"].res) if full else None,
                        V(st_out.ap[d], st_out.res), V(st_out.ap[2 + d], st_out.res), YS if full else None)
            P.S.barrier()
            P.flush()
    return P.nc


def norm_mod2(P, S, X, XN, XNf, sqb, psn, rstd, tmpf, ones_bf, s_vec, b_vec, col, Wd):
    S.act(sqb[:, :, :Wd], X[:, :, :Wd], AF.Square)
    for kc in range(KD):
        S.mm(psn[:, :Wd], ones_bf.all(), sqb[:, kc, :Wd], start=(kc == 0), stop=(kc == KD - 1))
    S.ts("dve", rstd[:, :Wd], psn[:, :Wd], 1.0 / D, ALU.mult, 1e-6, ALU.add)
    S.act(rstd[:, :Wd], rstd[:, :Wd], AF.Sqrt)
    S.recip(rstd[:, :Wd], rstd[:, :Wd])
    for kc in range(KD):
        S.stt(tmpf[:, kc, :Wd], X[:, kc, :Wd], s_vec[:, kc, col:col + 1], rstd[:, :Wd], ALU.mult, ALU.mult)
        S.act(XNf[:, kc, :Wd], tmpf[:, kc, :Wd], AF.Identity, bias=b_vec[:, kc, col:col + 1])
    S.copy("pool", XN[:, :, :Wd], XNf[:, :, :Wd])


W_C = [("w_out", lambda P: [D, D], F32), ("hn_w", lambda P: [128, 6], F32), ("s5_d", lambda P: [128, 2], F32),
       ("glu_w", lambda P: [256, 256], F32), ("glu_b", lambda P: [128, 2], F32), ("n2w", lambda P: [128, KD], F32),
       ("blockmean", lambda P: [128, 128], BF16), ("ident_f", lambda P: [128, 128], F32)]
W_MOE = [("router_w", lambda P: [D, 8], F32), ("router_b", lambda P: [1, 8], F32), ("selE", lambda P: [8, 8, 128], BF16)]


def phase_C(P, W, MOD, sc, YA, YS, xT2, XN2, GTS, moe, do_ctx):
    S = P.S
    with ExitStack() as st:
        Wout = P.sb(st, "Wout", [128, KD, D], BF16)
        gluw = P.sb(st, "gluw", [128, 2, 256], BF16)
        Bm = P.sb(st, "Bm", [128, 128], BF16)
        ones_bf = P.sb(st, "onesC", [128, 128], BF16)
        hnw = P.sb(st, "hnw", [128, 6])
        s5d = P.sb(st, "s5d", [128, 2])
        glub = P.sb(st, "glub", [128, 2])
        n2w = P.sb(st, "n2w", [128, KD])
        s2 = P.sb(st, "s2", [128, KD, 2])
        S.dma("sp", Bm.all(), W["blockmean"].all())
        S.dma("sp", ones_bf.all(), W["ones_bf"].all())
        S.dma("sp", hnw.all(), W["hn_w"].all())
        S.dma("sp", s5d.all(), W["s5_d"].all())
        S.dma("sp", glub.all(), W["glu_b"].all())
        S.dma("sp", n2w.all(), W["n2w"].all())
        S.ts("dve", s2.all(), MOD[:, 32:40, :], 1.0, ALU.add)
        S.tt("dve", s2.all(), s2.all(), n2w.all().re("p (k o) -> p k o", o=1).bc([128, KD, 2]), ALU.mult)
        if moe:
            rw = P.sb(st, "rw", [128, KD, 8])
            rb = P.sb(st, "rb", [128, 8])
            identf = P.sb(st, "identf", [128, 128])
            S.dma("sp", rw.all(), V(W["router_w"].ap.rearrange("(kc p) n -> p kc n", p=128), W["router_w"].res))
            S.dma("sp", rb.all(), V(W["router_b"].ap.partition_broadcast(128), W["router_b"].res), allow_slow_non_contiguous=True)
            S.dma("sp", identf.all(), W["ident_f"].all())
        with ExitStack() as st2:
            stg = P.sb(st2, "wostg", [128, KD, D])
            S.dma("sp", stg.all(), V(W["w_out"].ap.rearrange("(kc p) n -> p kc n", p=128), W["w_out"].res))
            S.copy("dve", Wout[:, 0:4, :], stg[:, 0:4, :])
            S.copy("act", Wout[:, 4:8, :], stg[:, 4:8, :])
            stg2 = P.sb(st2, "glstg", [128, 2, 256])
            S.dma("sp", stg2.all(), V(W["glu_w"].ap.rearrange("(kc p) n -> p kc n", p=128), W["glu_w"].res))
            S.copy("pool", gluw.all(), stg2.all())
            S.barrier()
            P.flush()
        X = [P.sb(st, f"cX{i}", [128, KD, 512]) for i in range(2)]
        YAt = [P.sb(st, f"cYA{i}", [128, 6, 512]) for i in range(2)]
        YSt = [P.sb(st, f"cYS{i}", [128, 2, 512]) for i in range(2)]
        GTt = [P.sb(st, f"cGT{i}", [128, 8, 512], BF16) for i in range(2)]
        Y = P.sb(st, "cY", [128, KD, 512], BF16)
        ybf = P.sb(st, "cybf", [128, 512], BF16)
        sqh = P.sb(st, "csqh", [128, 512], BF16)
        yc = P.sb(st, "cyc", [128, 512])
        rs = P.sb(st, "crs", [128, 512])
        o_ = P.sb(st, "co", [128, 512])
        zz = [P.sb(st, f"cz{i}", [128, 512]) for i in range(2)]
        zb = P.sb(st, "czb", [128, 2, 512], BF16)
        ta = P.sb(st, "cta", [128, 512])
        tb = P.sb(st, "ctb", [128, 512])
        sqb = P.sb(st, "csqb", [128, KD, 512], BF16)
        tmpf = P.sb(st, "ctmpf", [128, KD, 512])
        XNf = P.sb(st, "cXNf", [128, KD, 512])
        XN = P.sb(st, "cXN", [128, KD, 512], BF16)
        rstd = P.sb(st, "crstd", [128, 512])
        pp = [P.ps(st, f"cpp{i}", [128, 512]) for i in range(3)]
        po = [P.ps(st, f"cpo{i}", [128, 512]) for i in range(2)]
        psn = P.ps(st, "cpsn", [128, 512])
        if moe:
            pr = P.ps(st, "cpr", [128, 8])
            ptr = P.ps(st, "cptr", [8, 128])
            lg = P.sb(st, "clg", [128, 8])
            l2 = P.sb(st, "cl2", [128, 8])
            ee = P.sb(st, "cee", [128, 8])
            sm = [P.sb(st, f"csm{i}", [128, 1]) for i in range(4)]
            gT = P.sb(st, "cgT", [8, 512], BF16)
        ppi = [0]

        def nextpp():
            p = pp[ppi[0] % 3]
            ppi[0] += 1
            return p

        xv = W["xT"].ap.rearrange("k p t -> p k t")
        x2v = xT2.ap.rearrange("k p t -> p k t")
        xnv = XN2.ap.rearrange("k p t -> p k t")
        yav = YA.ap.rearrange("n p t -> p n t")
        ysv = YS.ap.rearrange("n p t -> p n t")
        gtv = sc["GT"].ap.rearrange("n p t -> p n t")
        tiles = P.tiles if do_ctx else P.tiles[1:]
        for n, (t0, Wd) in enumerate(tiles):
            col = 1 if t0 == 0 else 0
            Xt, ya, ys, gt = X[n % 2], YAt[n % 2], YSt[n % 2], GTt[n % 2]
            S.dma("sp", Xt[:, :, :Wd], V(xv[:, :, t0:t0 + Wd], W["xT"].res))
            S.dma("sp", ya[:, :, :Wd], V(yav[:, :, t0:t0 + Wd], YA.res))
            S.dma("sp", ys[:, :, :Wd], V(ysv[:, :, t0:t0 + Wd], YS.res))
            S.dma("sp", gt[:, :, :Wd], V(gtv[:, :, t0:t0 + Wd], sc["GT"].res))
            for j in range(6):
                m = j // 2
                kc = (0, 4, 6)[m] + (j % 2)
                y = ya[:, j, :Wd]
                if m == 0:
                    S.copy("act", ybf[:, :Wd], y)
                    pm = nextpp()
                    S.mm(pm[:, :Wd], Bm.all(), ybf[:, :Wd])
                    S.tt("dve", yc[:, :Wd], y, pm[:, :Wd], ALU.subtract)
                    y = yc[:, :Wd]
                S.act(sqh[:, :Wd], y, AF.Square)
                pv = nextpp()
                S.mm(pv[:, :Wd], Bm.all(), sqh[:, :Wd])
                S.ts("dve", rs[:, :Wd], pv[:, :Wd], 1e-6, ALU.add)
                S.act(rs[:, :Wd], rs[:, :Wd], AF.Sqrt)
                S.recip(rs[:, :Wd], rs[:, :Wd])
                S.stt(o_[:, :Wd], y, hnw[:, j:j + 1], rs[:, :Wd], ALU.mult, ALU.mult)
                S.tt("pool", Y[:, kc, :Wd], o_[:, :Wd], gt[:, j, :Wd], ALU.mult)
            for j in range(2):
                z = zz[j]
                S.stt(z[:, :Wd], gt[:, 6 + j, :Wd], s5d[:, j:j + 1], ys[:, j, :Wd], ALU.mult, ALU.add)
                S.act(ta[:, :Wd], z[:, :Wd], AF.Square)
                S.ts("dve", ta[:, :Wd], ta[:, :Wd], 0.044715, ALU.mult, 1.0, ALU.add)
                S.tt("dve", ta[:, :Wd], ta[:, :Wd], z[:, :Wd], ALU.mult)
                S.act(tb[:, :Wd], ta[:, :Wd], AF.Sigmoid, scale=1.5957691216057308)
                S.tt("dve", z[:, :Wd], z[:, :Wd], tb[:, :Wd], ALU.mult)
                S.copy("pool", zb[:, j, :Wd], z[:, :Wd])
            for j in range(2):
                pg = nextpp()
                for kk in range(2):
                    S.mm(pg[:, :Wd], gluw[:, kk, j * 128:(j + 1) * 128], zb[:, kk, :Wd], start=(kk == 0), stop=(kk == 1))
                S.act(tb[:, :Wd], pg[:, :Wd], AF.Sigmoid, bias=glub[:, j:j + 1])
                S.tt("dve", Y[:, 2 + j, :Wd], zz[j][:, :Wd], tb[:, :Wd], ALU.mult)
            for dt in range(KD):
                p = po[dt % 2]
                for kc in range(KD):
                    S.mm(p[:, :Wd], Wout[:, kc, dt * 128:(dt + 1) * 128], Y[:, kc, :Wd], start=(kc == 0), stop=(kc == KD - 1))
                S.stt(Xt[:, dt, :Wd], p[:, :Wd], MOD[:, 16 + dt, col:col + 1], Xt[:, dt, :Wd], ALU.mult, ALU.add)
            S.dma("pool", V(x2v[:, :, t0:t0 + Wd], xT2.k(t0, slice(None)).res), Xt[:, :, :Wd])
            norm_mod2(P, S, Xt, XN, XNf, sqb, psn, rstd, tmpf, ones_bf, s2, MOD[:, 24:32, :], col, Wd)
            S.dma("pool", V(xnv[:, :, t0:t0 + Wd], XN2.res), XN[:, :, :Wd])
            if moe:
                for sub in range(Wd // 128):
                    ss = slice(sub * 128, (sub + 1) * 128)
                    for kc in range(KD):
                        S.mm(pr.all(), XNf[:, kc, ss], rw[:, kc, :], start=(kc == 0), stop=(kc == KD - 1))
                    S.tt("dve", lg.all(), pr.all(), rb.all(), ALU.add)
                    S.red(sm[0].all(), lg.all(), ALU.max)
                    S.ts("dve", l2.all(), lg.all(), sm[0].all(), ALU.is_equal)
                    S.stt(l2.all(), l2.all(), -1e30, lg.all(), ALU.mult, ALU.add)
                    S.red(sm[1].all(), l2.all(), ALU.max)
                    S.ts("dve", l2.all(), lg.all(), sm[1].all(), ALU.is_ge)
                    S.ts("dve", sm[2].all(), sm[0].all(), -1.0, ALU.mult)
                    S.act(ee.all(), lg.all(), AF.Exp, bias=sm[2].all())
                    S.tt("dve", ee.all(), ee.all(), l2.all(), ALU.mult)
                    S.red(sm[3].all(), ee.all(), ALU.add)
                    S.recip(sm[3].all(), sm[3].all())
                    S.ts("dve", ee.all(), ee.all(), sm[3].all(), ALU.mult)
                    S.tr(ptr.all(), ee.all(), identf.all())
                    S.copy("act", gT[:, ss], ptr.all())
                S.dma("pool", V(GTS.ap[:, t0:t0 + Wd], GTS.res), gT[:, :Wd])
        S.barrier()
        P.flush()


def phase_D(P, W, MOD, xT2, XN2, GTS, moe, do_ctx):
    S = P.S
    nE = 8 if moe else 1
    with ExitStack() as st:
        w1 = P.sb(st, "fw1", [128, KD, HFF], BF16)
        w3 = P.sb(st, "fw3", [128, KD, HFF], BF16)
        w2 = P.sb(st, "fw2", [128, NFT, D], BF16)
        stg = [P.sb(st, f"fstg{i}", [128, 2816]) for i in range(2)]
        XNt = [P.sb(st, f"fXN{i}", [128, KD, 512], BF16) for i in range(2)]
        Xa = [P.sb(st, f"fXa{i}", [128, KD, 512]) for i in range(2)]
        hT = P.sb(st, "fhT", [128, NFT, 512], BF16)
        sl = [P.sb(st, f"fsl{i}", [128, 512]) for i in range(2)]
        tg = [P.sb(st, f"ftg{i}", [128, 512]) for i in range(2)]
        p1 = [P.ps(st, f"fp1_{i}", [128, 512]) for i in range(2)]
        p3 = [P.ps(st, f"fp3_{i}", [128, 512]) for i in range(2)]
        po = [P.ps(st, f"fpo{i}", [128, 512]) for i in range(2)]
        if moe:
            gTs = P.sb(st, "fgTs", [8, P.NT], BF16)
            selE = P.sb(st, "fselE", [8, 8, 128], BF16)
            gateB = P.sb(st, "fgateB", [128, 512])
            pg = P.ps(st, "fpg", [128, 512])
            c0_ = 0 if do_ctx else CTX
            S.dma("sp", gTs[:, c0_:], GTS[:, c0_:])
            S.dma("sp", selE.all(), W["selE"].all())
        x2v = xT2.ap.rearrange("k p t -> p k t")
        xnv = XN2.ap.rearrange("k p t -> p k t")
        tiles = P.tiles if do_ctx else P.tiles[1:]
        cast_eng = ("dve", "act", "pool")
        ci = 0
        qi = 0
        for e in range(nE):
            for hh in range(2):
                hs = slice(hh * HFF, (hh + 1) * HFF)
                if moe:
                    a1, a3, a2 = W["ffn_w1"].ap[e], W["ffn_w3"].ap[e], W["ffn_w2"].ap[e]
                else:
                    a1, a3, a2 = W["ffn_w1"].ap, W["ffn_w3"].ap, W["ffn_w2"].ap
                for (src, dst, rw_) in ((a1, w1, W["ffn_w1"]), (a3, w3, W["ffn_w3"])):
                    sv = src[:, hs].rearrange("(kc p) n -> p kc n", p=128)
                    for pc in range(4):
                        sg = stg[qi % 2]
                        qi += 1
                        cs = slice(pc * 352, (pc + 1) * 352)
                        S.dma("sp" if qi % 2 == 0 else "act", sg.all().re("p (k n) -> p k n", k=KD), V(sv[:, :, cs], rw_.res))
                        S.copy(cast_eng[ci % 3], dst[:, :, cs], sg.all().re("p (k n) -> p k n", k=KD))
                        ci += 1
                sv = a2[hs, :].rearrange("(ft p) n -> p ft n", p=128)
                for pc in range(4):
                    sg = stg[qi % 2]
                    qi += 1
                    cs = slice(pc * 256, (pc + 1) * 256)
                    S.dma("sp" if qi % 2 == 0 else "act", sg.all().re("p (k n) -> p k n", k=NFT), V(sv[:, :, cs], W["ffn_w2"].res))
                    S.copy(cast_eng[ci % 3], w2[:, :, cs], sg.all().re("p (k n) -> p k n", k=NFT))
                    ci += 1
                for n, (t0, Wd) in enumerate(tiles):
                    col = 1 if t0 == 0 else 0
                    xn, xa = XNt[n % 2], Xa[n % 2]
                    S.dma("sp", xn[:, :, :Wd], V(xnv[:, :, t0:t0 + Wd], XN2.res))
                    S.dma("sp", xa[:, :, :Wd], V(x2v[:, :, t0:t0 + Wd], xT2.k(t0, slice(None)).res))
                    if moe:
                        S.mm(pg[:, :Wd], selE[:, e, :], gTs[:, t0:t0 + Wd])
                        S.copy("act", gateB[:, :Wd], pg[:, :Wd])
                    for ft in range(NFT):
                        a, b = p1[ft % 2], p3[ft % 2]
                        fs = slice(ft * 128, (ft + 1) * 128)
                        for kc in range(KD):
                            S.mm(a[:, :Wd], w1[:, kc, fs], xn[:, kc, :Wd], start=(kc == 0), stop=(kc == KD - 1))
                        for kc in range(KD):
                            S.mm(b[:, :Wd], w3[:, kc, fs], xn[:, kc, :Wd], start=(kc == 0), stop=(kc == KD - 1))
                        s_ = sl[ft % 2]
                        S.act(s_[:, :Wd], a[:, :Wd], AF.Silu)
                        S.tt("dve", hT[:, ft, :Wd], s_[:, :Wd], b[:, :Wd], ALU.mult)
                    for dt in range(KD):
                        p = po[dt % 2]
                        for ft in range(NFT):
                            S.mm(p[:, :Wd], w2[:, ft, dt * 128:(dt + 1) * 128], hT[:, ft, :Wd], start=(ft == 0), stop=(ft == NFT - 1))
                        if moe:
                            t_ = tg[dt % 2]
                            S.tt("dve", t_[:, :Wd], p[:, :Wd], gateB[:, :Wd], ALU.mult)
                            S.stt(xa[:, dt, :Wd], t_[:, :Wd], MOD[:, 40 + dt, col:col + 1], xa[:, dt, :Wd], ALU.mult, ALU.add)
                        else:
                            S.stt(xa[:, dt, :Wd], p[:, :Wd], MOD[:, 40 + dt, col:col + 1], xa[:, dt, :Wd], ALU.mult, ALU.add)
                    S.dma("pool", V(x2v[:, :, t0:t0 + Wd], xT2.k(t0, slice(None)).res), xa[:, :, :Wd])
        S.barrier()
        P.flush()


def phase_final(P, W, xT2, yT):
    S = P.S
    with ExitStack() as st:
        ones_bf = P.sb(st, "zones", [128, 128], BF16)
        fws = P.sb(st, "zfw", [128, KD, 1])
        zb = P.sb(st, "zzb", [128, KD, 1])
        S.dma("sp", ones_bf.all(), W["ones_bf"].all())
        S.dma("sp", fws.all(), W["fw"].all())
        S.memset("dve", zb.all(), 0.0)
        X = [P.sb(st, f"zX{i}", [128, KD, 512]) for i in range(2)]
        XN = [P.sb(st, f"zXN{i}", [128, KD, 512]) for i in range(2)]
        sqb = P.sb(st, "zsqb", [128, KD, 512], BF16)
        tmpf = P.sb(st, "ztmpf", [128, KD, 512])
        rstd = P.sb(st, "zrstd", [128, 512])
        psn = P.ps(st, "zpsn", [128, 512])
        xv = xT2.ap.rearrange("k p t -> p k t")
        yv = yT.ap.rearrange("k p t -> p k t")
        for i, (t0, Wd) in enumerate(P.tiles[1:]):
            S.dma("sp", X[i % 2].all(), V(xv[:, :, t0:t0 + Wd], xT2.k(t0, slice(None)).res))
            norm_mod(P, S, X[i % 2], XN[i % 2], sqb, psn, rstd, tmpf, ones_bf, fws, zb, 0, 512)
            S.dma("pool", V(yv[:, :, t0 - CTX:t0 - CTX + Wd], yT.res), XN[i % 2].all())
        S.barrier()
        P.flush()


W_ST = [("init_att", lambda P: [128, 6, 64], F32), ("init_s5", lambda P: [128, 2, 8], F32)]


def build_La(TL, l, NL=4):
    P = Prog(TL, NL=NL)
    W = declare(P, W_A)
    sc = alloc_scratch(P)
    st_att = P.dout("st_att", [4, 128, 6, 64], F32)
    st_s5 = P.dout("st_s5", [4, 128, 2, 8], F32)
    with ExitStack() as st:
        MOD = phase_mod(P, st, W, 16)
        lbv = compute_lbv(P, st, W, l)
        phase_A(P, W, MOD, sc, lbv, so=True)
        with ExitStack() as st2:
            A = attn_alloc(P, st2, W)
            for d in range(2):
                attn_pass(P, A, sc, d, False, None, V(st_att.ap[d], st_att.res), V(st_att.ap[2 + d], st_att.res), None)
            P.S.barrier()
            P.flush()
        with ExitStack() as st2:
            B = s5_alloc(P, st2, W)
            for d in range(2):
                s5_pass(P, B, sc, d, False, None, V(st_s5.ap[d], st_s5.res), V(st_s5.ap[2 + d], st_s5.res), None)
            P.S.barrier()
            P.flush()
    return P.nc


def build_Lb(TL, l, moe, last, NL=4):
    P = Prog(TL, NL=NL)
    NT = P.NT
    specs = W_A + W_ST + W_C
    if moe:
        specs = specs + W_MOE + [("ffn_w1", lambda P: [8, D, DFF], F32), ("ffn_w3", lambda P: [8, D, DFF], F32), ("ffn_w2", lambda P: [8, DFF, D], F32)]
    else:
        specs = specs + [("ffn_w1", lambda P: [D, DFF], F32), ("ffn_w3", lambda P: [D, DFF], F32), ("ffn_w2", lambda P: [DFF, D], F32)]
    if last:
        specs = specs + [("fw", lambda P: [128, KD, 1], F32)]
    W = declare(P, specs)
    sc = alloc_scratch(P)
    YA = P.dscr("YA", [6, 128, NT], F32)
    YS = P.dscr("YS", [2, 128, NT], F32)
    XN2 = P.dscr("XN2", [KD, 128, NT], BF16)
    GTS = P.dscr("GTS", [8, NT], BF16)
    if last:
        xT2 = P.dscr("xT2", [KD, 128, NT], F32)
        yT = P.dout("yT", [KD, 128, P.TL], F32)
    else:
        xT2 = P.dout("xT2", [KD, 128, NT], F32)
    with ExitStack() as st:
        MOD = phase_mod(P, st, W, 48)
        lbv = compute_lbv(P, st, W, l)
        phase_A(P, W, MOD, sc, lbv)
        with ExitStack() as st2:
            A = attn_alloc(P, st2, W)
            for d in range(2):
                attn_pass(P, A, sc, d, True, V(W["init_att"].ap[d], W["init_att"].res), None, None, YA)
            P.S.barrier()
            P.flush()
        with ExitStack() as st2:
            B = s5_alloc(P, st2, W)
            for d in range(2):
                s5_pass(P, B, sc, d, True, V(W["init_s5"].ap[d], W["init_s5"].res), None, None, YS)
            P.S.barrier()
            P.flush()
        phase_C(P, W, MOD, sc, YA, YS, xT2, XN2, GTS, moe, not last)
        phase_D(P, W, MOD, xT2, XN2, GTS, moe, not last)
        if last:
            phase_final(P, W, xT2, yT)
    return P.nc


def consts_all():
    c = consts_np()
    c.update(attn_consts_np())
    bm = np.zeros((128, 128), np.float32)
    bm[:64, :64] = 1.0 / 64
    bm[64:, 64:] = 1.0 / 64
    c["blockmean"] = bm.astype(ml_dtypes.bfloat16)
    sel = np.zeros((8, 8, 128), np.float32)
    for e in range(8):
        sel[e, e, :] = 1.0
    c["selE"] = sel.astype(ml_dtypes.bfloat16)
    return c


def layer_inputs(inp, l, depth):
    d = layer_consts(inp, l)
    d.update(s5_layout_np(inp, l))
    d.update(consts_all())
    d["w_out"] = np.ascontiguousarray(inp["w_out"][l])
    d["hn_w"] = np.ascontiguousarray(np.concatenate([fm_vec(inp["ret_gn_w"][l]), fm_vec(inp["hg_norm_w"][l]), fm_vec(inp["gla_norm_w"][l])], axis=1))
    j = l // 2
    if l % 2 == 0:
        d["ffn_w1"] = np.ascontiguousarray(inp["ffn_w1"][j])
        d["ffn_w3"] = np.ascontiguousarray(inp["ffn_w3"][j])
        d["ffn_w2"] = np.ascontiguousarray(inp["ffn_w2"][j])
    else:
        d["ffn_w1"] = np.ascontiguousarray(inp["moe_w1"][j])
        d["ffn_w3"] = np.ascontiguousarray(inp["moe_w3"][j])
        d["ffn_w2"] = np.ascontiguousarray(inp["moe_w2"][j])
        d["router_w"] = np.ascontiguousarray(inp["router_w"][j])
        d["router_b"] = np.ascontiguousarray(np.asarray(inp["router_b"][j]).reshape(1, 8))
    d["fw"] = np.ascontiguousarray(fm_vec(inp["final_norm_w"]).reshape(128, KD, 1))
    return d


W_1 = W_A
W_2 = [("xT", lambda P: [KD, 128, P.NT], F32), ("MODin", lambda P: [128, 48, 2], F32),
       ("QSin", lambda P: [18, 128, P.NT], BF16), ("GSin", lambda P: [8, 128, P.NT], F32),
       ("GTin", lambda P: [8, 128, P.NT], BF16), ("VSin", lambda P: [P.NT, 768], BF16),
       ("YAin", lambda P: [6, 128, P.NT], F32), ("YSin", lambda P: [2, 128, P.NT], F32),
       ("ones_bf", lambda P: [128, 128], BF16), ("ident_bf", lambda P: [128, 128], BF16),
       ("bret", lambda P: [128, 2, 2, 128], F32), ("masks", lambda P: [128, 2, 128], F32),
       ("s5_lre", lambda P: [128, 2, 8], F32), ("s5_lim", lambda P: [128, 2, 8], F32), ("s5_ldt", lambda P: [128, 2, 8], F32),
       ("s5_BT", lambda P: [128, 32, 128], F32), ("s5_CT", lambda P: [128, 32, 128], F32)] + W_ST + W_C


def emit_L1(P, W, l):
    NT = P.NT
    sc = alloc_scratch(P, out=True)
    YA = P.dout("YA", [6, 128, NT], F32)
    YS = P.dout("YS", [2, 128, NT], F32)
    MODo = P.dout("MODo", [128, 48, 2], F32)
    f_att = P.dout("f_att", [128, 6, 64], F32)
    f_s5 = P.dout("f_s5", [128, 2, 8], F32)
    with ExitStack() as st:
        MOD = phase_mod(P, st, W, 48)
        P.S.dma("pool", MODo.all(), MOD.all())
        lbv = compute_lbv(P, st, W, l)
        phase_A(P, W, MOD, sc, lbv)
        with ExitStack() as st2:
            A = attn_alloc(P, st2, W)
            attn_pass(P, A, sc, 0, True, None, None, f_att.all(), YA)
            P.S.barrier()
            P.flush()
        with ExitStack() as st2:
            B = s5_alloc(P, st2, W)
            s5_pass(P, B, sc, 0, True, None, None, f_s5.all(), YS)
            P.S.barrier()
            P.flush()


def specs_L2(moe, last):
    specs = list(W_2)
    if moe:
        specs = specs + W_MOE + [("ffn_w1", lambda P: [8, D, DFF], F32), ("ffn_w3", lambda P: [8, D, DFF], F32), ("ffn_w2", lambda P: [8, DFF, D], F32)]
    else:
        specs = specs + [("ffn_w1", lambda P: [D, DFF], F32), ("ffn_w3", lambda P: [D, DFF], F32), ("ffn_w2", lambda P: [DFF, D], F32)]
    if last:
        specs = specs + [("fw", lambda P: [128, KD, 1], F32)]
    return specs


def emit_L2(P, W, l, moe, last):
    NT = P.NT
    sc = {"QS": W["QSin"], "GS": W["GSin"], "GT": W["GTin"], "VS": W["VSin"]}
    YA = P.dscr("YA2", [6, 128, NT], F32)
    YS = P.dscr("YS2", [2, 128, NT], F32)
    XN2 = P.dscr("XN2", [KD, 128, NT], BF16)
    GTS = P.dscr("GTS", [8, NT], BF16)
    if last:
        xT2 = P.dscr("xT2", [KD, 128, NT], F32)
        yT = P.dout("yT", [KD, 128, P.TL], F32)
    else:
        xT2 = P.dout("xT2", [KD, 128, NT], F32)
    with ExitStack() as st:
        MOD = P.sb(st, "MOD2", [128, 48, 2])
        P.S.dma("sp", MOD.all(), W["MODin"].all())
        with ExitStack() as st2:
            A = attn_alloc(P, st2, W)
            attn_pass(P, A, sc, 1, True, W["init_att"].all(), None, None, YA, Ysrc=W["YAin"])
            P.S.barrier()
            P.flush()
        with ExitStack() as st2:
            B = s5_alloc(P, st2, W)
            s5_pass(P, B, sc, 1, True, W["init_s5"].all(), None, None, YS, Ysrc=W["YSin"])
            P.S.barrier()
            P.flush()
        phase_C(P, W, MOD, sc, YA, YS, xT2, XN2, GTS, moe, not last)
        phase_D(P, W, MOD, xT2, XN2, GTS, moe, not last)
        if last:
            phase_final(P, W, xT2, yT)
    return xT2


def build_L1(TL, l, NL=4):
    P = Prog(TL, NL=NL)
    W = declare(P, W_1)
    emit_L1(P, W, l)
    return P.nc


def build_L2(TL, l, moe, last, NL=4):
    P = Prog(TL, NL=NL)
    W = declare(P, specs_L2(moe, last))
    emit_L2(P, W, l, moe, last)
    return P.nc


W_1N = [sp for sp in W_1 if sp[0] != "xT"]


def build_M(TL, l, moe, NL=4):
    P = Prog(TL, NL=NL)
    W2 = declare(P, specs_L2(moe, False))
    W1 = declare(P, W_1N, prefix="n_")
    xT2 = emit_L2(P, W2, l, moe, False)
    W1["xT"] = xT2
    emit_L1(P, W1, l + 1)
    return P.nc


def mirror_layer_inputs(li):
    m = dict(li)
    w = li["w_in_arr"].copy()
    w[:, 14 * 128:16 * 128] = li["w_in_arr"][:, 16 * 128:18 * 128]
    w[:, 16 * 128:18 * 128] = li["w_in_arr"][:, 14 * 128:16 * 128]
    lo = 26 * 128
    w[:, lo:lo + 16] = li["w_in_arr"][:, lo + 32:lo + 48]
    w[:, lo + 32:lo + 48] = li["w_in_arr"][:, lo:lo + 16]
    m["w_in_arr"] = w
    wa = np.zeros_like(li["wa2p"])
    wa[0:16, 0, :] = li["wa2p"][32:48, 1, :]
    wa[32:48, 1, :] = li["wa2p"][0:16, 0, :]
    m["wa2p"] = wa
    m["gla_ba"] = np.ascontiguousarray(li["gla_ba"][:, ::-1, :])
    for k in ("s5_lre", "s5_lim", "s5_ldt"):
        m[k] = np.ascontiguousarray(li[k][:, ::-1, :])
    for k in ("s5_BT", "s5_CT"):
        a = li[k].reshape(128, 2, 16, 128)
        m[k] = np.ascontiguousarray(a[:, ::-1]).reshape(128, 32, 128)
    m["bret"] = attn_consts_np(mirror=True)["bret"]
    return m


def names_of(specs):
    return [s[0] for s in specs]


def kernel(**inputs):
    inp = {k: np.asarray(v) for k, v in inputs.items()}
    x = inp["x"]
    B, SEQ, _ = x.shape
    depth = inp["ada_w"].shape[0]
    TL = SEQ // 2
    ncore = 2 * B
    cores = [(b, s) for b in range(B) for s in range(2)]
    xT = []
    ropes = []
    for (b, s) in cores:
        toks = np.concatenate([inp["ctx"][b], x[b, s * TL:(s + 1) * TL]], axis=0)
        rp = rope_tables(TL, s)
        if s == 1:
            toks = np.concatenate([inp["ctx"][b][::-1], x[b, s * TL:(s + 1) * TL][::-1]], axis=0)
            rp = np.ascontiguousarray(np.concatenate([rp[:, :, :CTX], rp[:, :, CTX:][:, :, ::-1]], axis=2))
        xT.append(to_fm(toks))
        ropes.append(rp)
    conds = [core_cond(inp, b) for b in range(B)]
    out = np.empty_like(x)
    ids = list(range(ncore))

    def lin(l):
        a = layer_inputs(inp, l, depth)
        return [a, mirror_layer_inputs(a)]

    def map_L1(li, ci, prefix=""):
        b, s = cores[ci]
        m = {prefix + k: li[s][k] for k in names_of(W_1N) if k in li[s]}
        m[prefix + "cond"] = conds[b]
        m[prefix + "rope"] = ropes[ci]
        return m

    def map_L2(li, ci, moe, last, r1):
        b, s = cores[ci]
        m = {k: li[s][k] for k in names_of(W_2 + (W_MOE if moe else [])) if k in li[s]}
        for k in ("ffn_w1", "ffn_w3", "ffn_w2"):
            m[k] = li[s][k]
        if last:
            m["fw"] = li[s]["fw"]
        m["xT"] = xT[ci]
        own, pa = r1[ci], r1[ci ^ 1]
        m["MODin"] = np.asarray(own["MODo"])
        for k in ("QS", "GS", "GT", "VS", "YA", "YS"):
            m[k + "in"] = np.asarray(own[k])
        m["init_att"] = np.asarray(pa["f_att"])
        m["init_s5"] = np.asarray(pa["f_s5"])
        return m

    li = lin(0)
    maps = []
    for ci in range(ncore):
        m = map_L1(li, ci)
        m["xT"] = xT[ci]
        maps.append(m)
    r1 = run_bass_kernel_spmd(build_L1(TL, 0, NL=depth), maps, core_ids=ids).results
    for l in range(depth - 1):
        moe = (l % 2 == 1)
        li_next = lin(l + 1)
        maps = []
        for ci in range(ncore):
            m = map_L2(li, ci, moe, False, r1)
            m.update(map_L1(li_next, ci, prefix="n_"))
            maps.append(m)
        r = run_bass_kernel_spmd(build_M(TL, l, moe, NL=depth), maps, core_ids=ids).results
        xT = [np.asarray(r[ci]["xT2"]) for ci in range(ncore)]
        r1 = r
        li = li_next
    l = depth - 1
    moe = (l % 2 == 1)
    maps = [map_L2(li, ci, moe, True, r1) for ci in range(ncore)]
    r2 = run_bass_kernel_spmd(build_L2(TL, l, moe, True, NL=depth), maps, core_ids=ids).results
    for ci, (b, s) in enumerate(cores):
        y = from_fm(np.asarray(r2[ci]["yT"]))
        out[b, s * TL:(s + 1) * TL] = y[::-1] if s == 1 else y
    return out
```

```python
import numpy as np
import ml_dtypes
import concourse.bass as bass
import concourse.mybir as mybir
from concourse.bass_utils import run_bass_kernel_spmd

F32 = mybir.dt.float32
BF16 = mybir.dt.bfloat16
AF = mybir.ActivationFunctionType
ALU = mybir.AluOpType
AX = mybir.AxisListType

D = 1024
KD = 8
CTX = 256
L = 128
NPT = 27
NCOL = NPT * 128
DFF = 2816
HFF = 1408
NFT = 11


class Res:
    __slots__ = ("w", "r")

    def __init__(self):
        self.w = None
        self.r = {}


class V:
    __slots__ = ("ap", "res")

    def __init__(self, ap, res):
        self.ap = ap
        self.res = res

    def __getitem__(self, idx):
        return V(self.ap[idx], self.res)

    def bc(self, shape):
        return V(self.ap.broadcast_to(shape), self.res)

    def re(self, pat, **kw):
        return V(self.ap.rearrange(pat, **kw), self.res)


class T:
    def __init__(self, ap):
        self.ap = ap
        self.res = Res()
        self.keyed = {}

    def __getitem__(self, idx):
        return V(self.ap[idx], self.res)

    def k(self, key, idx):
        r = self.keyed.get(key)
        if r is None:
            r = self.keyed[key] = Res()
        return V(self.ap[idx], r)

    def all(self):
        return V(self.ap, self.res)


class Eng:
    def __init__(self, key, sem):
        self.key = key
        self.sem = sem
        self.cnt = 0
        self.seen = {}
        self.ops = []


class Sched:
    def __init__(self, nc, stack, ndma=12):
        self.nc = nc
        self.stack = stack
        self.eng = {}
        for k in ("pe", "act", "dve", "pool", "sp"):
            self.eng[k] = Eng(k, stack.enter_context(nc.semaphore("s_" + k)))
        self.dsem = {}
        self.dq = {}
        for q in ("sp", "pool", "act"):
            self.dq[q] = 0
            for i in range(ndma):
                self.dsem[(q, i)] = [stack.enter_context(nc.semaphore(f"d_{q}_{i}")), 0]
        self.ndma = ndma
        self.n_inst = 0

    def semof(self, key):
        if isinstance(key, tuple):
            return self.dsem[key][0]
        return self.eng[key].sem

    def _deps(self, e, reads, writes):
        deps = {}

        def add(k, c):
            if deps.get(k, 0) < c:
                deps[k] = c

        for v in reads:
            if v.res.w is not None:
                add(*v.res.w)
        for v in writes:
            if v.res.w is not None:
                add(*v.res.w)
            for k, c in v.res.r.items():
                add(k, c)
        for k, c in deps.items():
            if k == "pe" and e.key == "pe":
                continue
            if e.seen.get(k, 0) < c:
                e.ops.append(("w", self.semof(k), c))
                e.seen[k] = c
                self.n_inst += 1

    def emit(self, ek, fn, reads, writes):
        e = self.eng[ek]
        self._deps(e, reads, writes)
        e.cnt += 1
        e.ops.append(("o", fn))
        self.n_inst += 1
        for v in writes:
            v.res.w = (ek, e.cnt)
            v.res.r = {}
        for v in reads:
            v.res.r[ek] = e.cnt

    def dma(self, q, out, in_, **kw):
        e = self.eng[q]
        self._deps(e, [in_], [out])
        i = self.dq[q]
        self.dq[q] = (i + 1) % self.ndma
        key = (q, i)
        ds = self.dsem[key]
        if e.seen.get(key, 0) < ds[1]:
            e.ops.append(("w", ds[0], ds[1]))
            e.seen[key] = ds[1]
        ds[1] += 16
        e.ops.append(("d", out.ap, in_.ap, ds[0], kw))
        self.n_inst += 1
        out.res.w = (key, ds[1])
        out.res.r = {}
        in_.res.r[key] = ds[1]

    def barrier(self):
        for e in self.eng.values():
            for o in self.eng.values():
                if o.key != e.key and o.cnt > 0 and e.seen.get(o.key, 0) < o.cnt:
                    e.ops.append(("w", o.sem, o.cnt))
                    e.seen[o.key] = o.cnt
            for key, ds in self.dsem.items():
                if ds[1] > 0 and e.seen.get(key, 0) < ds[1]:
                    e.ops.append(("w", ds[0], ds[1]))
                    e.seen[key] = ds[1]

    def finish(self):
        self.barrier()
        nc = self.nc
        with nc.Block() as block:
            def run(e, h):
                for op in e.ops:
                    if op[0] == "w":
                        h.wait_ge(op[1], op[2])
                    elif op[0] == "o":
                        op[1](h).then_inc(e.sem, 1)
                    else:
                        h.dma_start(out=op[1], in_=op[2], **op[4]).then_inc(op[3], 16)

            @block.tensor
            def _(h):
                run(self.eng["pe"], h)

            @block.scalar
            def _(h):
                run(self.eng["act"], h)

            @block.vector
            def _(h):
                run(self.eng["dve"], h)

            @block.gpsimd
            def _(h):
                run(self.eng["pool"], h)

            @block.sync
            def _(h):
                run(self.eng["sp"], h)

    def mm(self, out, lhsT, rhs, start=True, stop=True):
        self.emit("pe", lambda h: h.matmul(out.ap, lhsT.ap, rhs.ap, start=start, stop=stop),
                  [lhsT, rhs] + ([] if start else [out]), [out])

    def tr(self, out, in_, ident):
        self.emit("pe", lambda h: h.transpose(out.ap, in_.ap, ident.ap), [in_, ident], [out])

    def act(self, out, in_, func, bias=None, scale=None, accum=None):
        reads = [in_]
        kw = {}
        if bias is not None:
            if isinstance(bias, V):
                reads.append(bias)
                kw["bias"] = bias.ap
            else:
                kw["bias"] = float(bias)
        if scale is not None:
            if isinstance(scale, V):
                reads.append(scale)
                kw["scale"] = scale.ap
            else:
                kw["scale"] = float(scale)
        writes = [out]
        if accum is not None:
            kw["accum_out"] = accum.ap
            writes.append(accum)
        self.emit("act", lambda h: h.activation(out.ap, in_.ap, func, **kw), reads, writes)

    def ts(self, ek, out, in0, s1, op0, s2=None, op1=None):
        reads = [in0]
        a1 = s1
        a2 = s2
        if isinstance(s1, V):
            reads.append(s1)
            a1 = s1.ap
        if isinstance(s2, V):
            reads.append(s2)
            a2 = s2.ap
        if op1 is None:
            self.emit(ek, lambda h: h.tensor_scalar(out.ap, in0.ap, a1, None, op0), reads, [out])
        else:
            self.emit(ek, lambda h: h.tensor_scalar(out.ap, in0.ap, a1, a2, op0, op1), reads, [out])

    def tt(self, ek, out, a, b, op):
        self.emit(ek, lambda h: h.tensor_tensor(out.ap, a.ap, b.ap, op), [a, b], [out])

    def stt(self, out, in0, sc, in1, op0, op1):
        reads = [in0, in1]
        a = sc
        if isinstance(sc, V):
            reads.append(sc)
            a = sc.ap
        self.emit("dve", lambda h: h.scalar_tensor_tensor(out.ap, in0.ap, a, in1.ap, op0, op1), reads, [out])

    def scan(self, out, d0, d1, init, op0=ALU.mult, op1=ALU.add):
        reads = [d0, d1]
        a = init
        if isinstance(init, V):
            reads.append(init)
            a = init.ap
        self.emit("dve", lambda h: h.tensor_tensor_scan(out.ap, d0.ap, d1.ap, a, op0, op1), reads, [out])

    def copy(self, ek, out, in_):
        if ek == "act":
            self.emit("act", lambda h: h.copy(out.ap, in_.ap), [in_], [out])
        else:
            self.emit(ek, lambda h: h.tensor_copy(out.ap, in_.ap), [in_], [out])

    def memset(self, ek, out, val):
        self.emit(ek, lambda h: h.memset(out.ap, val), [], [out])

    def recip(self, out, in_):
        self.emit("dve", lambda h: h.reciprocal(out.ap, in_.ap), [in_], [out])

    def red(self, out, in_, op):
        self.emit("dve", lambda h: h.tensor_reduce(out.ap, in_.ap, AX.X, op), [in_], [out])


from contextlib import ExitStack


class Prog:
    def __init__(self, TL, debug=False, NL=4):
        self.NL = NL
        self.nc = bass.Bass("TRN2", target_bir_lowering=False)
        self.stack = ExitStack()
        self.S = Sched(self.nc, self.stack)
        self.TL = TL
        self.NT = CTX + TL
        self.tiles = [(0, CTX)] + [(CTX + 512 * i, 512) for i in range(TL // 512)]
        self.debug = debug
        self.scr_kind = "ExternalOutput" if debug else "Internal"

    def din(self, name, shape, dt=F32):
        return T(self.nc.dram_tensor(name, list(shape), dt, kind="ExternalInput").ap())

    def dout(self, name, shape, dt=F32):
        return T(self.nc.dram_tensor(name, list(shape), dt, kind="ExternalOutput").ap())

    def dscr(self, name, shape, dt=F32):
        return T(self.nc.dram_tensor(name, list(shape), dt, kind=self.scr_kind).ap())

    def sb(self, st, name, shape, dt=F32):
        self._uid = getattr(self, "_uid", 0) + 1
        t = st.enter_context(self.nc.sbuf_tensor(f"sb{self._uid}_{name}", list(shape), dt))
        return T(t[tuple(slice(None) for _ in shape)])

    def ps(self, st, name, shape, dt=F32):
        self._uid = getattr(self, "_uid", 0) + 1
        t = st.enter_context(self.nc.psum_tensor(f"ps{self._uid}_{name}", list(shape), dt))
        return T(t[tuple(slice(None) for _ in shape)])

    def flush(self):
        self.S.finish()
        for e in self.S.eng.values():
            e.ops = []


def load_cast(P, st_name, dst, src_ap_fn, npieces, piece_shape, stg, eng_cycle=("dve", "pool")):
    raise NotImplementedError


def phase_mod(P, st, W, nj):
    S = P.S
    cond = P.sb(st, "cond", [128, KD, 2])
    csl = P.sb(st, "csl", [128, KD, 2])
    adab = P.sb(st, "adab", [128, 48])
    MOD = P.sb(st, "MOD", [128, 48, 2])
    S.dma("sp", cond.all(), W["cond"].all())
    S.dma("sp", adab.all(), W["ada_b"].all())
    S.act(csl.all(), cond.all(), AF.Silu)
    with ExitStack() as st2:
        pm = P.ps(st2, "pm", [128, 48, 2])
        stg = [P.sb(st2, f"adaw{i}", [128, KD, 768]) for i in range(2)]
        aw = W["ada_w"].ap.rearrange("(kc p) n -> p kc n", p=128)
        for pc in range((nj + 5) // 6):
            sg = stg[pc % 2]
            S.dma("sp" if pc % 2 == 0 else "pool", sg.all(), V(aw[:, :, pc * 768:(pc + 1) * 768], W["ada_w"].res))
            for jj in range(6):
                j = pc * 6 + jj
                if j >= nj:
                    break
                for kc in range(KD):
                    S.mm(pm[:, j, :], sg[:, kc, jj * 128:(jj + 1) * 128], csl[:, kc, :], start=(kc == 0), stop=(kc == KD - 1))
        S.tt("dve", MOD[:, 0:nj, :], pm[:, 0:nj, :], adab[:, 0:nj].re("p (j o) -> p j o", o=1).bc([128, nj, 2]), ALU.add)
        S.barrier()
        P.flush()
    return MOD


def norm_mod(P, S, X, XN, sqb, psn, rstd, tmpf, ones_bf, s_vec, b_vec, col, Wd):
    S.act(sqb[:, :, :Wd], X[:, :, :Wd], AF.Square)
    for kc in range(KD):
        S.mm(psn[:, :Wd], ones_bf.all(), sqb[:, kc, :Wd], start=(kc == 0), stop=(kc == KD - 1))
    S.ts("dve", rstd[:, :Wd], psn[:, :Wd], 1.0 / D, ALU.mult, 1e-6, ALU.add)
    S.act(rstd[:, :Wd], rstd[:, :Wd], AF.Sqrt)
    S.recip(rstd[:, :Wd], rstd[:, :Wd])
    for kc in range(KD):
        S.stt(tmpf[:, kc, :Wd], X[:, kc, :Wd], s_vec[:, kc, col:col + 1], rstd[:, :Wd], ALU.mult, ALU.mult)
        S.act(XN[:, kc, :Wd], tmpf[:, kc, :Wd], AF.Identity, bias=b_vec[:, kc, col:col + 1])


def consts_np():
    c = {}
    c["ones_bf"] = np.ones((128, 128), ml_dtypes.bfloat16)
    c["ident_f"] = np.eye(128, dtype=np.float32)
    c["ident_bf"] = np.eye(128).astype(ml_dtypes.bfloat16)
    return c


def to_fm(a):
    T_ = a.shape[0]
    return np.ascontiguousarray(a.T.reshape(KD, 128, T_))


def from_fm(a):
    return np.ascontiguousarray(a.reshape(D, -1).T)


def alloc_scratch(P, out=False):
    NT = P.NT
    sc = {}
    mk = P.dout if out else P.dscr
    sc["QS"] = mk("QS", [18, 128, NT], BF16)
    sc["GS"] = mk("GS", [8, 128, NT], F32)
    sc["GT"] = mk("GT", [8, 128, NT], BF16)
    sc["VS"] = mk("VS", [NT, 768], BF16)
    return sc


def phase_A(P, W, MOD, sc, lbv, so=False):
    S = P.S
    with ExitStack() as st:
        Win = P.sb(st, "Win", [128, KD, NCOL], BF16)
        Wv = P.sb(st, "Wv", [128, KD, 768], BF16)
        wa2 = P.sb(st, "wa2", [128, 2, 256], BF16)
        wa2f = P.sb(st, "wa2f", [128, 2, 256])
        gba = P.sb(st, "gba", [128, 2, 2])
        n1w = P.sb(st, "n1w", [128, KD])
        s1 = P.sb(st, "s1", [128, KD, 2])
        ones_bf = P.sb(st, "ones", [128, 128], BF16)
        S.dma("sp", ones_bf.all(), W["ones_bf"].all())
        S.dma("sp", wa2f.all(), W["wa2p"].all())
        S.dma("sp", gba.all(), W["gla_ba"].all())
        S.dma("sp", n1w.all(), W["n1w"].all())
        S.copy("pool", wa2.all(), wa2f.all())
        S.ts("dve", s1.all(), MOD[:, 8:16, :], 1.0, ALU.add)
        S.tt("dve", s1.all(), s1.all(), n1w.all().re("p (k o) -> p k o", o=1).bc([128, KD, 2]), ALU.mult)
        win_v = W["w_in_arr"].ap.rearrange("(kc p) n -> p kc n", p=128)
        wv_v = W["w_v"].ap.rearrange("(kc p) n -> p kc n", p=128)
        with ExitStack() as st2:
            stg = [P.sb(st2, f"wstg{i}", [128, KD, 432]) for i in range(2)]
            for pc in range(8):
                sg = stg[pc % 2]
                S.dma("sp" if pc % 2 == 0 else "pool", sg.all(), V(win_v[:, :, pc * 432:(pc + 1) * 432], W["w_in_arr"].res))
                S.copy("dve" if pc % 2 == 0 else "act", Win[:, :, pc * 432:(pc + 1) * 432], sg.all())
            for pc in range(2):
                sg = stg[pc % 2]
                S.dma("sp", sg[:, :, 0:384], V(wv_v[:, :, pc * 384:(pc + 1) * 384], W["w_v"].res))
                S.copy("dve", Wv[:, :, pc * 384:(pc + 1) * 384], sg[:, :, 0:384])
            S.barrier()
            P.flush()
        X = [P.sb(st, f"X{i}", [128, KD, 512]) for i in range(2)]
        XN = P.sb(st, "XN", [128, KD, 512], BF16)
        sqb = P.sb(st, "sqb", [128, KD, 512], BF16)
        tmpf = P.sb(st, "tmpf", [128, KD, 512])
        rstd = P.sb(st, "rstd", [128, 512])
        rope = [P.sb(st, f"rope{i}", [128, 4, 512]) for i in range(1)]
        QSs = [P.sb(st, f"QSs{i}", [128, 18, 512], BF16) for i in range(1)]
        GSs = [P.sb(st, f"GSs{i}", [128, 8, 512]) for i in range(1)]
        GTs = [P.sb(st, f"GTs{i}", [128, 8, 512], BF16) for i in range(1)]
        Vs = [P.sb(st, f"Vs{i}", [128, 768], BF16) for i in range(3)]
        low = P.sb(st, "low", [128, 512], BF16)
        t1 = [P.sb(st, f"t1_{i}", [128, 512]) for i in range(2)]
        t2 = [P.sb(st, f"t2_{i}", [128, 512]) for i in range(2)]
        sg_ = [P.sb(st, f"sg_{i}", [128, 512]) for i in range(2)]
        psn = P.ps(st, "psn", [128, 512])
        pp = [P.ps(st, f"pp{i}", [128, 512]) for i in range(5)]
        pv = P.ps(st, "pv", [128, 1024])
        xv = W["xT"].ap.rearrange("k p t -> p k t")
        qsv = sc["QS"].ap.rearrange("n p t -> p n t")
        gsv = sc["GS"].ap.rearrange("n p t -> p n t")
        gtv = sc["GT"].ap.rearrange("n p t -> p n t")
        ppi = [0]

        def proj(pt, Wd):
            p = pp[ppi[0] % 5]
            ppi[0] += 1
            for kc in range(KD):
                S.mm(p[:, :Wd], Win[:, kc, pt * 128:(pt + 1) * 128], XN[:, kc, :Wd], start=(kc == 0), stop=(kc == KD - 1))
            return p

        for ti, (t0, Wd) in enumerate(P.tiles):
            col = 1 if ti == 0 else 0
            Xt = X[ti % 2]
            rp = rope[0]
            QSt, GSt, GTt = QSs[0], GSs[0], GTs[0]
            S.dma("sp", Xt[:, :, :Wd], V(xv[:, :, t0:t0 + Wd], W["xT"].res))
            S.dma("sp", rp[:, :, :Wd], V(W["rope"].ap[:, :, t0:t0 + Wd], W["rope"].res))
            norm_mod(P, S, Xt, XN, sqb, psn, rstd, tmpf, ones_bf, s1, MOD[:, 0:8, :], col, Wd)
            for which, (pt_a, pt_s, dst) in enumerate(((0, 4, 0), (2, 6, 6))):
                if so and which == 0:
                    continue
                for j in range(2):
                    pa = proj(pt_a + j, Wd)
                    pb = proj(pt_s + j, Wd)
                    a = t1[j]
                    b = t2[j]
                    S.tt("dve", a[:, :Wd], pa[:, :Wd], rp[:, 2 * which, :Wd], ALU.mult)
                    S.tt("dve", b[:, :Wd], pb[:, :Wd], rp[:, 2 * which + 1, :Wd], ALU.mult)
                    S.tt("pool", QSt[:, dst + j, :Wd], a[:, :Wd], b[:, :Wd], ALU.add)
                    if which == 1:
                        S.copy("pool", QSt[:, 12 + j, :Wd], QSt[:, 6 + j, :Wd])
            for j in range(2):
                if not so:
                    S.act(GTt[:, 0 + j, :Wd], proj(8 + j, Wd)[:, :Wd], AF.Silu)
                    S.act(GTt[:, 2 + j, :Wd], proj(18 + j, Wd)[:, :Wd], AF.Silu)
                    S.act(GTt[:, 4 + j, :Wd], proj(24 + j, Wd)[:, :Wd], AF.Silu)
                    S.copy("dve", QSt[:, 2 + j, :Wd], proj(12 + j, Wd)[:, :Wd])
                    S.copy("act", QSt[:, 4 + j, :Wd], proj(20 + j, Wd)[:, :Wd])
                S.copy("dve", GTt[:, 6 + j, :Wd], proj(10 + j, Wd)[:, :Wd])
                pk = proj(22 + j, Wd)
                S.ts("dve", QSt[:, 10 + j, :Wd], pk[:, :Wd], 32.0 ** -0.5, ALU.mult)
                S.copy("pool", QSt[:, 16 + j, :Wd], QSt[:, 10 + j, :Wd])
            for d in range(2):
                for j in range(2):
                    pz = proj(14 + 2 * d + j, Wd)
                    sg = sg_[j]
                    S.act(sg[:, :Wd], pz[:, :Wd], AF.Sigmoid)
                    f = t1[j]
                    S.ts("dve", f[:, :Wd], sg[:, :Wd], lbv[:, j, 0:1], ALU.mult, lbv[:, j, 1:2], ALU.add)
                    S.act(GSt[:, 4 * d + j, :Wd], f[:, :Wd], AF.Ln)
                    S.ts("dve", QSt[:, (8 if d == 0 else 14) + j, :Wd], sg[:, :Wd], lbv[:, j, 2:3], ALU.mult, lbv[:, j, 0:1], ALU.add)
            S.copy("dve", low[:, :Wd], proj(26, Wd)[:, :Wd])
            for d in range(2):
                for j in range(2):
                    p = pp[ppi[0] % 5]
                    ppi[0] += 1
                    S.mm(p[:, :Wd], wa2[32 * d:32 * d + 16, d, j * 128:(j + 1) * 128], low[32 * d:32 * d + 16, :Wd])
                    sg = sg_[j]
                    S.act(sg[:, :Wd], p[:, :Wd], AF.Sigmoid, bias=gba[:, d, j:j + 1])
                    S.act(sg[:, :Wd], sg[:, :Wd], AF.Ln)
                    S.ts("pool", GSt[:, 4 * d + 2 + j, :Wd], sg[:, :Wd], 1.0 / 16.0, ALU.mult)
            for sub in range(Wd // 128):
                for kc in range(KD):
                    S.mm(pv[:, 0:512], XN[:, kc, sub * 128:(sub + 1) * 128], Wv[:, kc, 0:512], start=(kc == 0), stop=(kc == KD - 1))
                for kc in range(KD):
                    S.mm(pv[:, 512:768], XN[:, kc, sub * 128:(sub + 1) * 128], Wv[:, kc, 512:768], start=(kc == 0), stop=(kc == KD - 1))
                vt = Vs[sub % 3]
                S.copy("act", vt.all(), pv[:, 0:768])
                S.dma("pool", V(sc["VS"].ap[t0 + sub * 128:t0 + (sub + 1) * 128, :], sc["VS"].res), vt.all())
            if so:
                S.dma("pool", V(qsv[:, 6:18, t0:t0 + Wd], sc["QS"].res), QSt[:, 6:18, :Wd])
                S.dma("pool", V(gtv[:, 6:8, t0:t0 + Wd], sc["GT"].res), GTt[:, 6:8, :Wd])
            else:
                S.dma("pool", V(qsv[:, :, t0:t0 + Wd], sc["QS"].res), QSt[:, :, :Wd])
                S.dma("pool", V(gtv[:, :, t0:t0 + Wd], sc["GT"].res), GTt[:, :, :Wd])
            S.dma("pool", V(gsv[:, :, t0:t0 + Wd], sc["GS"].res), GSt[:, :, :Wd])
        S.barrier()
        P.flush()


def compute_lbv(P, st, W, l):
    S = P.S
    NL = P.NL
    lg = P.sb(st, "lbl", [128, 2, NL])
    ex = P.sb(st, "lbe", [128, 2, NL])
    mx = P.sb(st, "lbm", [128, 2])
    sm = P.sb(st, "lbs", [128, 2])
    pa = P.sb(st, "lbp", [128, 2])
    lbv = P.sb(st, "lbv", [128, 2, 3])
    S.dma("sp", lg.all(), W["hg_lb"].all())
    S.red(mx.all(), lg.all(), ALU.max)
    S.tt("dve", ex.all(), lg.all(), mx.all().re("p (j o) -> p j o", o=1).bc([128, 2, NL]), ALU.subtract)
    S.act(ex.all(), ex.all(), AF.Exp)
    S.red(sm.all(), ex.all(), ALU.add)
    S.recip(sm.all(), sm.all())
    if l == 0:
        S.memset("dve", pa.all(), 0.0)
    else:
        S.red(pa.all(), ex[:, :, 1:l + 1], ALU.add)
        S.tt("dve", pa.all(), pa.all(), sm.all(), ALU.mult)
        S.ts("dve", pa.all(), pa.all(), 0.0, ALU.max, 1.0 - 1e-6, ALU.min)
    S.copy("dve", lbv[:, :, 1], pa.all())
    S.ts("dve", lbv[:, :, 0], pa.all(), -1.0, ALU.mult, 1.0, ALU.add)
    S.ts("dve", lbv[:, :, 2], pa.all(), 1.0, ALU.mult, -1.0, ALU.add)
    return lbv


def fm_vec(v, n=None):
    v = np.asarray(v, np.float32)
    return np.ascontiguousarray(v.reshape(-1, 128).T)


def pad_gla(a):
    out = np.zeros(a.shape[:-1] + (256,), a.dtype)
    for h in range(4):
        out[..., h * 64:h * 64 + 32] = a[..., h * 32:(h + 1) * 32]
    return out


def rope_tables(TL, half):
    NT = CTX + TL
    r = np.arange(128)
    w = r % 64
    part = w // 32
    u = w % 32
    f = u % 16
    freqs = (10000.0 ** (-(f.astype(np.float64)) / 16.0))
    tg = half * TL + np.arange(TL)
    pos = np.where(part[:, None] == 0, (tg // 64)[None, :], (tg % 64)[None, :]).astype(np.float64)
    ang = pos.astype(np.float32).astype(np.float64) * freqs.astype(np.float32).astype(np.float64)[:, None]
    cos = np.cos(ang)
    sin = np.sin(ang) * np.where(u < 16, -1.0, 1.0)[:, None]
    tab = np.zeros((128, 4, NT), np.float32)
    tab[:, 0, :CTX] = 1.0
    tab[:, 2, :CTX] = 0.125
    tab[:, 0, CTX:] = cos
    tab[:, 1, CTX:] = sin
    tab[:, 2, CTX:] = 0.125 * cos
    tab[:, 3, CTX:] = 0.125 * sin
    return tab


def arrange_w_in(w_in):
    w_in = np.asarray(w_in, np.float32)
    c = np.arange(256)
    h, w = c // 64, c % 64
    perm = h * 64 + (w // 32) * 32 + ((w % 32) + 16) % 32
    lowt = np.zeros((D, 128), np.float32)
    lowt[:, 0:16] = w_in[:, 3328:3344]
    lowt[:, 32:48] = w_in[:, 3344:3360]
    parts = [w_in[:, 0:256], w_in[:, 256:512], w_in[:, 0:256][:, perm], w_in[:, 256:512][:, perm],
             w_in[:, 768:1024], w_in[:, 1024:1280], w_in[:, 1280:1536], w_in[:, 1536:1792], w_in[:, 1792:2048],
             w_in[:, 2304:2560], pad_gla(w_in[:, 2560:2688]), pad_gla(w_in[:, 2688:2816]), w_in[:, 3072:3328], lowt]
    arr = np.ascontiguousarray(np.concatenate(parts, axis=1))
    assert arr.shape == (D, NCOL)
    wv = np.ascontiguousarray(np.concatenate([w_in[:, 512:768], w_in[:, 2048:2304], w_in[:, 2816:3072]], axis=1))
    return arr, wv


def layer_consts(inp, l):
    d = {}
    d["ada_w"] = np.ascontiguousarray(inp["ada_w"][l])
    d["ada_b"] = fm_vec(inp["ada_b"][l])
    d["n1w"] = fm_vec(inp["norm1_w"][l])
    d["n2w"] = fm_vec(inp["norm2_w"][l])
    d["w_in_arr"], d["w_v"] = arrange_w_in(inp["w_in"][l])
    wa2p = np.zeros((128, 2, 256), np.float32)
    wa2p[0:16, 0, :] = pad_gla(np.asarray(inp["gla_wa2"][l][0]))
    wa2p[32:48, 1, :] = pad_gla(np.asarray(inp["gla_wa2"][l][1]))
    d["wa2p"] = wa2p
    ba = pad_gla(np.asarray(inp["gla_ba"][l]))
    d["gla_ba"] = np.ascontiguousarray(ba.reshape(2, 2, 128).transpose(2, 0, 1))
    d["hg_lb"] = np.ascontiguousarray(np.asarray(inp["hg_lb_logits"]).reshape(-1, 2, 128).transpose(2, 1, 0))
    d.update(consts_np())
    return d


def core_cond(inp, b):
    c = np.stack([fm_vec(inp["c"][b]), fm_vec(inp["c_ctx"])], axis=-1)
    return np.ascontiguousarray(c)


def build_test_A(TL, l, NL=4):
    P = Prog(TL, debug=True, NL=NL)
    W = {}
    NT = P.NT
    for name, shape, dt in [("xT", [KD, 128, NT], F32), ("cond", [128, KD, 2], F32), ("ada_w", [D, 6 * D], F32),
                            ("ada_b", [128, 48], F32), ("n1w", [128, KD], F32), ("w_in_arr", [D, NCOL], F32),
                            ("w_v", [D, 768], F32), ("wa2p", [128, 2, 256], F32), ("gla_ba", [128, 2, 2], F32),
                            ("hg_lb", [128, 2, P.NL], F32), ("rope", [128, 4, NT], F32), ("ones_bf", [128, 128], BF16)]:
        W[name] = P.din(name, shape, dt)
    sc = alloc_scratch(P)
    with ExitStack() as st:
        MOD = phase_mod(P, st, W, 16)
        lbv = compute_lbv(P, st, W, l)
        phase_A(P, W, MOD, sc, lbv)
    return P.nc


def attn_consts_np(mirror=False):
    lg = np.log1p(-(2.0 ** (-5.0 - np.arange(4, dtype=np.float32)))).astype(np.float32)
    if mirror:
        lg = lg[::-1]
    bret = np.zeros((128, 2, 2, 128), np.float32)
    t = np.arange(128, dtype=np.float32)
    for d in range(2):
        lgd = lg if d == 0 else lg[::-1]
        for tile in range(2):
            for hh in range(2):
                h = tile * 2 + hh
                cnt = (t + 1) if d == 0 else (128 - t)
                bret[hh * 64:(hh + 1) * 64, d, tile, :] = (cnt * lgd[h])[None, :]
    j = np.arange(128)[:, None]
    i = np.arange(128)[None, :]
    masks = np.stack([(j <= i), (j >= i)], axis=1).astype(np.float32)
    return {"bret": bret, "masks": np.ascontiguousarray(masks)}


class AttnBufs:
    pass


def attn_alloc(P, st, W):
    S = P.S
    A = AttnBufs()
    A.ones = P.sb(st, "a_ones", [128, 128])
    S.memset("dve", A.ones.all(), 1.0)
    A.masks = P.sb(st, "a_masks", [128, 2, 128])
    S.dma("sp", A.masks.all(), W["masks"].all())
    A.identb = P.sb(st, "a_identb", [128, 128], BF16)
    S.dma("sp", A.identb.all(), W["ident_bf"].all())
    A.B = []
    bretv = W["bret"]
    for d in range(2):
        bl = []
        for pp_ in range(3):
            b = P.sb(st, f"a_B{d}{pp_}", [128, 6, 128])
            S.dma("sp", b[:, 0:2, :], bretv[:, d, :, :])
            bl.append(b)
        A.B.append(bl)
    A.cc = 0
    A.E_ = [P.sb(st, f"a_E{i}", [128, 6, 128]) for i in range(3)]
    A.D1_ = [P.sb(st, f"a_D1{i}", [128, 6, 128]) for i in range(3)]
    A.Eq1_ = [P.sb(st, f"a_Eq1{i}", [128, 6, 64]) for i in range(3)]
    A.Ek1_ = [P.sb(st, f"a_Ek1{i}", [128, 6, 128]) for i in range(3)]
    A.Ek0_ = [P.sb(st, f"a_Ek0{i}", [128, 6, 64]) for i in range(3)]
    A.qh_ = [P.sb(st, f"a_qh{i}", [128, 6, 128], BF16) for i in range(3)]
    A.q1_ = [P.sb(st, f"a_q1{i}", [128, 6, 64], BF16) for i in range(3)]
    A.k1_ = [P.sb(st, f"a_k1{i}", [128, 6, 128], BF16) for i in range(3)]
    A.k0_ = [P.sb(st, f"a_k0{i}", [128, 6, 64], BF16) for i in range(3)]
    A.k1T_ = [P.sb(st, f"a_k1T{i}", [128, 6, 128], BF16) for i in range(2)]
    A.tS_ = [P.sb(st, f"a_tS{i}", [128, 6, 64]) for i in range(2)]
    A.Asb = [[[P.sb(st, f"a_A{d}{m}{i}", [128, 4, 128], BF16) for i in range(2)] for m in range(3)] for d in range(2)]
    for d in range(2):
        for m in range(3):
            for i in range(2):
                S.memset("pool", A.Asb[d][m][i].all(), 0.0)
    A.S = P.sb(st, "a_S", [128, 6, 64])
    A.Sbf = P.sb(st, "a_Sbf", [128, 6, 64], BF16)
    A.Q = [P.sb(st, f"a_Q{i}", [128, 6, 512], BF16) for i in range(2)]
    A.K = [P.sb(st, f"a_K{i}", [128, 6, 512], BF16) for i in range(2)]
    A.G = [P.sb(st, f"a_G{i}", [128, 4, 512]) for i in range(2)]
    A.Vt = [P.sb(st, f"a_V{i}", [128, 4, 768], BF16) for i in range(2)]
    A.Yo = [P.sb(st, f"a_Yo{i}", [128, 6, 512]) for i in range(2)]
    A.Yf = [P.sb(st, f"a_Yf{i}", [128, 6, 512]) for i in range(2)]
    A.psA = [P.ps(st, f"a_psA{m}", [128, 4, 128]) for m in range(3)]
    A.psO = P.ps(st, "a_psO", [128, 6, 128])
    A.psT = P.ps(st, "a_psT", [128, 6, 128], BF16)
    A.psS = P.ps(st, "a_psS", [128, 6, 64])
    return A


def attn_dirs(d):
    if d == 0:
        return slice(0, 64), slice(64, 128), 63, 127, 63
    return slice(64, 128), slice(0, 64), 64, 0, 0


def attn_stage1a(P, A, d, p3, Q, K, G, c0, full):
    S = P.S
    B = A.B[d][p3]
    E, D1, Eq1, Ek1, Ek0 = A.E_[p3], A.D1_[p3], A.Eq1_[p3], A.Ek1_[p3], A.Ek0_[p3]
    qh, q1, k1, k0 = A.qh_[p3], A.q1_[p3], A.k1_[p3], A.k0_[p3]
    cs = slice(c0, c0 + 128)
    H0, H1, ref, last, last1 = attn_dirs(d)
    rv = (lambda v: v) if d == 0 else (lambda v: v[:, ::-1])
    for j in range(4):
        S.scan(rv(B[:, 2 + j, :]), rv(A.ones.all()), rv(G[:, j, cs]), 0.0)
    bref = B[:, :, ref:ref + 1]
    S.tt("dve", D1.all(), B.all(), bref.bc([128, 6, 128]), ALU.subtract)
    S.act(Eq1.all(), D1[:, :, H1], AF.Exp)
    S.ts("dve", D1.all(), D1.all(), -1.0, ALU.mult, 80.0, ALU.min)
    S.act(Ek1.all(), D1.all(), AF.Exp)
    S.tt("dve", k1.all(), K[:, :, cs], Ek1.all(), ALU.mult)
    if full:
        S.act(E.all(), B.all(), AF.Exp)
        S.ts("pool", Ek0.all(), B[:, :, H0], -1.0, ALU.mult, 80.0, ALU.min)
        S.act(Ek0.all(), Ek0.all(), AF.Exp)
        S.tt("dve", qh.all(), Q[:, :, cs], E.all(), ALU.mult)
        S.tt("pool", q1.all(), Q[:, :, cs][:, :, H1], Eq1.all(), ALU.mult)
        S.tt("pool", k0.all(), K[:, :, cs][:, :, H0], Ek0.all(), ALU.mult)
    else:
        S.act(E[:, :, last:last + 1], B[:, :, last:last + 1], AF.Exp)


def attn_stage1b(P, A, d, p3, par, full):
    S = P.S
    qh, q1, k1, k0, k1T = A.qh_[p3], A.q1_[p3], A.k1_[p3], A.k0_[p3], A.k1T_[par]
    H0, H1, ref, last, last1 = attn_dirs(d)
    for j in range(6):
        S.tr(A.psT[:, j, :], k1[:, j, :], A.identb.all())
    S.copy("act", k1T.all(), A.psT.all())
    if full:
        mask = A.masks[:, d, :]
        for m in range(3):
            psA = A.psA[m]
            for h in range(4):
                tile = 2 * m + h // 2
                rows = slice((h % 2) * 64, (h % 2) * 64 + 64)
                S.mm(psA[H0, h, H0], k0[rows, tile, :], qh[rows, tile, H0])
                S.mm(psA[:, h, H1], k1[rows, tile, :], q1[rows, tile, :])
            Asb = A.Asb[d][m][par]
            S.tt("dve", Asb[H0, :, H0], psA[H0, :, H0], mask[H0, H0].re("p (o i) -> p o i", o=1).bc([64, 4, 64]), ALU.mult)
            S.tt("dve", Asb[:, :, H1], psA[:, :, H1], mask[:, H1].re("p (o i) -> p o i", o=1).bc([128, 4, 64]), ALU.mult)


def attn_stage2(P, A, d, p3, par, Vt, c0, sub, full, Yo):
    S = P.S
    E, Eq1, qh, k1T, tS = A.E_[p3], A.Eq1_[p3], A.qh_[p3], A.k1T_[par], A.tS_[par]
    cs = slice(c0, c0 + 128)
    H0, H1, ref, last, last1 = attn_dirs(d)
    if full:
        for m in range(3):
            Asb = A.Asb[d][m][par]
            for h in range(4):
                tile = 2 * m + h // 2
                rows = slice((h % 2) * 64, (h % 2) * 64 + 64)
                vc = slice(m * 256 + h * 64, m * 256 + h * 64 + 64)
                S.mm(A.psO[rows, tile, :], Vt[:, sub, vc], Asb[:, h, :], start=True, stop=False)
                S.mm(A.psO[rows, tile, :], A.Sbf[rows, tile, :], qh[rows, tile, :], start=False, stop=True)
        S.copy("act", Yo[:, :, cs], A.psO.all())
    for m in range(3):
        for h in range(4):
            tile = 2 * m + h // 2
            rows = slice((h % 2) * 64, (h % 2) * 64 + 64)
            vc = slice(m * 256 + h * 64, m * 256 + h * 64 + 64)
            S.mm(A.psS[rows, tile, :], k1T[:, tile, rows], Vt[:, sub, vc])
    e1 = E[:, :, last:last + 1].bc([128, 6, 64])
    e2 = Eq1[:, :, last1:last1 + 1].bc([128, 6, 64])
    S.tt("dve", tS.all(), A.psS.all(), e2, ALU.mult)
    S.tt("pool", A.S.all(), A.S.all(), e1, ALU.mult)
    S.tt("pool", A.S.all(), A.S.all(), tS.all(), ALU.add)
    S.copy("pool", A.Sbf.all(), A.S.all())


def attn_pass(P, A, sc, d, full, init_lat, out_ctx, out_fin, YA, Ysrc=None):
    S = P.S
    qsv = sc["QS"].ap.rearrange("n p t -> p n t")
    gsv = sc["GS"].ap.rearrange("n p t -> p n t")
    yav = YA.ap.rearrange("n p t -> p n t") if YA is not None else None
    if Ysrc is None:
        Ysrc = YA
    ysrcv = Ysrc.ap.rearrange("n p t -> p n t") if Ysrc is not None else None
    S.memset("pool", A.S.all(), 0.0)
    S.memset("pool", A.Sbf.all(), 0.0)
    lat = P.tiles[1:]
    order = [P.tiles[0]] + (lat if d == 0 else lat[::-1])
    chunks = []
    for n, (t0, Wd) in enumerate(order):
        nch = Wd // 128
        cis = list(range(nch)) if d == 0 else list(range(nch - 1, -1, -1))
        for idx, ci in enumerate(cis):
            chunks.append((n, t0, Wd, ci, idx == 0, idx == nch - 1))

    def s1(i):
        n, t0, Wd, ci, first, lastc = chunks[i]
        Q, K, G, Vt = A.Q[n % 2], A.K[n % 2], A.G[n % 2], A.Vt[n % 2]
        if first:
            if full:
                S.dma("sp", Q[:, :, :Wd], V(qsv[:, 0:6, t0:t0 + Wd], sc["QS"].res))
            ko = 6 if d == 0 else 12
            S.dma("sp", K[:, :, :Wd], V(qsv[:, ko:ko + 6, t0:t0 + Wd], sc["QS"].res))
            S.dma("sp", G[:, :, :Wd], V(gsv[:, 4 * d:4 * d + 4, t0:t0 + Wd], sc["GS"].res))
            S.dma("sp", Vt[:, :Wd // 128, :], V(sc["VS"].ap[t0:t0 + Wd, :].rearrange("(s p) c -> p s c", p=128), sc["VS"].res))
            if full and d == 1:
                S.dma("sp", A.Yf[n % 2][:, :, :Wd], V(ysrcv[:, :, t0:t0 + Wd], Ysrc.res))
        attn_stage1a(P, A, d, i % 3, Q, K, G, ci * 128, full)

    def s1b(i):
        attn_stage1b(P, A, d, i % 3, i % 2, full)

    def s2(i):
        n, t0, Wd, ci, first, lastc = chunks[i]
        Vt, Yo = A.Vt[n % 2], A.Yo[n % 2]
        attn_stage2(P, A, d, i % 3, i % 2, Vt, ci * 128, ci, full, Yo)
        if lastc:
            if full:
                if d == 1:
                    S.tt("dve", Yo[:, :, :Wd], Yo[:, :, :Wd], A.Yf[n % 2][:, :, :Wd], ALU.add)
                S.dma("pool", V(yav[:, :, t0:t0 + Wd], YA.res), Yo[:, :, :Wd])
            if n == 0:
                if out_ctx is not None:
                    S.dma("pool", out_ctx, A.S.all())
                if init_lat is not None:
                    S.dma("sp", A.S.all(), init_lat)
                    S.copy("pool", A.Sbf.all(), A.S.all())

    nchk = len(chunks)
    s1(0)
    if nchk > 1:
        s1(1)
    s1b(0)
    for i in range(nchk):
        if i + 2 < nchk:
            s1(i + 2)
        if i + 1 < nchk:
            s1b(i + 1)
        s2(i)
    if out_fin is not None:
        S.dma("pool", out_fin, A.S.all())


def build_test_B(TL, l, NL=4, full=True):
    P = Prog(TL, debug=True, NL=NL)
    W = {}
    NT = P.NT
    for name, shape, dt in [("xT", [KD, 128, NT], F32), ("cond", [128, KD, 2], F32), ("ada_w", [D, 6 * D], F32),
                            ("ada_b", [128, 48], F32), ("n1w", [128, KD], F32), ("w_in_arr", [D, NCOL], F32),
                            ("w_v", [D, 768], F32), ("wa2p", [128, 2, 256], F32), ("gla_ba", [128, 2, 2], F32),
                            ("hg_lb", [128, 2, P.NL], F32), ("rope", [128, 4, NT], F32), ("ones_bf", [128, 128], BF16),
                            ("ident_bf", [128, 128], BF16), ("bret", [128, 2, 2, 128], F32), ("masks", [128, 2, 128], F32),
                            ("init_att", [2, 128, 6, 64], F32)]:
        W[name] = P.din(name, shape, dt)
    sc = alloc_scratch(P)
    YA = P.dscr("YA", [6, 128, NT], F32)
    st_out = P.dout("st_out", [4, 128, 6, 64], F32)
    with ExitStack() as st:
        MOD = phase_mod(P, st, W, 16)
        lbv = compute_lbv(P, st, W, l)
        phase_A(P, W, MOD, sc, lbv)
        with ExitStack() as st2:
            A = attn_alloc(P, st2, W)
            for d in range(2):
                attn_pass(P, A, sc, d, full, V(W["init_att"].ap[d], W["init_att"].res) if full else None,
                          V(st_out.ap[d], st_out.res), V(st_out.ap[2 + d], st_out.res), YA if full else None)
            P.S.barrier()
            P.flush()
    return P.nc


def s5_layout_np(inp, l):
    d = {}
    G, Pn, Cg = 16, 64, 16

    def sm(a):
        return np.ascontiguousarray(np.asarray(a, np.float32).reshape(2, 8, 128).transpose(2, 0, 1))

    d["s5_lre"] = sm(inp["s5_lam_re"][l])
    d["s5_lim"] = sm(inp["s5_lam_im"][l])
    dt = np.repeat(np.asarray(inp["s5_log_dt"][l], np.float32)[:, :, None], Pn, axis=2)
    d["s5_ldt"] = sm(dt)
    BT = np.zeros((2, 2, 8, 128, 128), np.float32)
    CT = np.zeros((2, 2, 8, 128, 128), np.float32)
    for dd in range(2):
        for ri, (bn, cn) in enumerate((("s5_b_re", "s5_c_re"), ("s5_b_im", "s5_c_im"))):
            Bm = np.asarray(inp[bn][l][dd], np.float32)
            Cm = np.asarray(inp[cn][l][dd], np.float32)
            for k in range(8):
                for gg in range(2):
                    g = 2 * k + gg
                    r0 = (g % 8) * 16
                    BT[dd, ri, k, r0:r0 + 16, gg * 64:(gg + 1) * 64] = Bm[g].T
                    CT[dd, ri, k, gg * 64:(gg + 1) * 64, r0:r0 + 16] = Cm[g].T
    d["s5_BT"] = np.ascontiguousarray(BT.transpose(3, 0, 1, 2, 4).reshape(128, 32, 128))
    d["s5_CT"] = np.ascontiguousarray(CT.transpose(3, 0, 1, 2, 4).reshape(128, 32, 128))
    d["s5_d"] = fm_vec(inp["s5_d"][l])
    d["glu_w"] = np.ascontiguousarray(inp["s5_glu_w"][l])
    d["glu_b"] = fm_vec(inp["s5_glu_b"][l])
    return d


class S5Bufs:
    pass


def cmul(S, ek, o_re, o_im, a_re, a_im, b_re, b_im, t1, t2):
    S.tt(ek, t1, a_re, b_re, ALU.mult)
    S.tt(ek, t2, a_im, b_im, ALU.mult)
    S.tt(ek, o_im, a_re, b_im, ALU.mult)
    S.tt(ek, o_re, t1, t2, ALU.subtract)
    S.tt(ek, t1, a_im, b_re, ALU.mult)
    S.tt(ek, o_im, o_im, t1, ALU.add)


def s5_alloc(P, st, W):
    S = P.S
    B = S5Bufs()
    B.BT = P.sb(st, "s5BT", [128, 32, 128], BF16)
    B.CT = P.sb(st, "s5CT", [128, 32, 128], BF16)
    with ExitStack() as st2:
        stg = P.sb(st2, "s5stg", [128, 32, 128])
        S.dma("sp", stg.all(), W["s5_BT"].all())
        S.copy("dve", B.BT.all(), stg.all())
        stg2 = P.sb(st2, "s5stg2", [128, 32, 128])
        S.dma("sp", stg2.all(), W["s5_CT"].all())
        S.copy("act", B.CT.all(), stg2.all())
        S.barrier()
        P.flush()
    B.lre = P.sb(st, "s5lre", [128, 2, 8])
    B.lim = P.sb(st, "s5lim", [128, 2, 8])
    B.ldt = P.sb(st, "s5ldt", [128, 2, 8])
    S.dma("sp", B.lre.all(), W["s5_lre"].all())
    S.dma("sp", B.lim.all(), W["s5_lim"].all())
    S.dma("sp", B.ldt.all(), W["s5_ldt"].all())
    B.dt = P.sb(st, "s5dt", [128, 2, 8])
    S.act(B.dt.all(), B.ldt.all(), AF.Exp)
    B.r = P.sb(st, "s5r", [128, 2, 8])
    B.u = P.sb(st, "s5u", [128, 2, 2, 8])
    B.coef = P.sb(st, "s5coef", [128, 2, 2, 8])
    B.uW = P.sb(st, "s5uW", [128, 2, 2, 8])
    tmp = [P.sb(st, f"s5tmp{i}", [128, 2, 8]) for i in range(6)]
    th = tmp[0]
    S.tt("dve", th.all(), B.lim.all(), B.dt.all(), ALU.mult)
    S.tt("dve", tmp[1].all(), B.lre.all(), B.dt.all(), ALU.mult)
    S.act(B.r.all(), tmp[1].all(), AF.Exp)
    x, x2, pa, pb_ = tmp[2], tmp[3], tmp[4], tmp[5]
    ure, uim = B.u[:, :, 0, :], B.u[:, :, 1, :]
    S.ts("dve", x.all(), th.all(), 1.0 / 64, ALU.mult)
    S.tt("dve", x2.all(), x.all(), x.all(), ALU.mult)
    S.ts("dve", pa.all(), x2.all(), -1.0 / 5040, ALU.mult)
    S.stt(pa.all(), pa.all(), 1.0 / 120, x2.all(), ALU.add, ALU.mult)
    S.stt(pa.all(), pa.all(), -1.0 / 6, x2.all(), ALU.add, ALU.mult)
    S.stt(uim, pa.all(), 1.0, x.all(), ALU.add, ALU.mult)
    S.ts("dve", pb_.all(), x2.all(), 1.0 / 40320, ALU.mult)
    S.stt(pb_.all(), pb_.all(), -1.0 / 720, x2.all(), ALU.add, ALU.mult)
    S.stt(pb_.all(), pb_.all(), 1.0 / 24, x2.all(), ALU.add, ALU.mult)
    S.stt(pb_.all(), pb_.all(), -0.5, x2.all(), ALU.add, ALU.mult)
    S.ts("dve", ure, pb_.all(), 1.0, ALU.add)
    for _ in range(6):
        S.tt("dve", pa.all(), ure, ure, ALU.mult)
        S.tt("dve", pb_.all(), uim, uim, ALU.mult)
        S.stt(x.all(), ure, 2.0, uim, ALU.mult, ALU.mult)
        S.tt("dve", ure, pa.all(), pb_.all(), ALU.subtract)
        S.copy("dve", uim, x.all())
    S.tt("dve", pa.all(), ure, ure, ALU.mult)
    S.tt("dve", pb_.all(), uim, uim, ALU.mult)
    S.tt("dve", pa.all(), pa.all(), pb_.all(), ALU.add)
    S.act(pa.all(), pa.all(), AF.Sqrt)
    S.recip(pa.all(), pa.all())
    S.tt("dve", ure, ure, pa.all(), ALU.mult)
    S.tt("dve", uim, uim, pa.all(), ALU.mult)
    are, aim = tmp[0], tmp[1]
    S.tt("dve", are.all(), B.u[:, :, 0, :], B.r.all(), ALU.mult)
    S.tt("dve", aim.all(), B.u[:, :, 1, :], B.r.all(), ALU.mult)
    S.ts("dve", are.all(), are.all(), -1.0, ALU.add)
    den = tmp[2]
    S.tt("dve", den.all(), B.lre.all(), B.lre.all(), ALU.mult)
    S.tt("dve", tmp[3].all(), B.lim.all(), B.lim.all(), ALU.mult)
    S.tt("dve", den.all(), den.all(), tmp[3].all(), ALU.add)
    S.recip(den.all(), den.all())
    nl = tmp[3]
    S.ts("dve", nl.all(), B.lim.all(), -1.0, ALU.mult)
    cmul(S, "dve", B.coef[:, :, 0, :], B.coef[:, :, 1, :], are.all(), aim.all(), B.lre.all(), nl.all(), tmp[4].all(), tmp[5].all())
    S.tt("dve", B.coef[:, :, 0, :], B.coef[:, :, 0, :], den.all(), ALU.mult)
    S.tt("dve", B.coef[:, :, 1, :], B.coef[:, :, 1, :], den.all(), ALU.mult)
    B.POST = P.sb(st, "s5POST", [128, 8, 2, 512])
    B.PRE = P.sb(st, "s5PRE", [128, 8, 2, 512])
    B.tt1 = P.sb(st, "s5tt1", [128, 8, 256])
    B.tt2 = P.sb(st, "s5tt2", [128, 8, 256])
    B.G = P.sb(st, "s5G", [128, 8, 2, 512])
    B.Tp1 = [P.sb(st, f"s5Tp1_{i}", [128, 2, 512]) for i in range(2)]
    B.Tp2 = [P.sb(st, f"s5Tp2_{i}", [128, 2, 512]) for i in range(2)]
    B.Tq1 = [P.sb(st, f"s5Tq1_{i}", [128, 2, 512]) for i in range(2)]
    B.Tq2 = [P.sb(st, f"s5Tq2_{i}", [128, 2, 512]) for i in range(2)]
    B.Hb = [P.sb(st, f"s5Hb{i}", [128, 2, 512], BF16) for i in range(2)]
    B.U = [P.sb(st, f"s5U{i}", [128, 2, 512], BF16) for i in range(2)]
    B.Wt = [P.sb(st, f"s5Wt{i}", [128, 2, 512]) for i in range(2)]
    B.Yo = [P.sb(st, f"s5Yo{i}", [128, 2, 512]) for i in range(2)]
    B.Yf = P.sb(st, "s5Yf", [128, 2, 512])
    B.h = P.sb(st, "s5h", [128, 2, 8])
    B.gi = P.sb(st, "s5gi", [128, 2, 8])
    B.sm = [P.sb(st, f"s5sm{i}", [128, 8]) for i in range(3)]
    B.pb = [P.ps(st, f"s5pb{i}", [128, 2, 512]) for i in range(2)]
    B.py = [P.ps(st, f"s5py{i}", [128, 512]) for i in range(2)]
    return B


def s5_tables(P, B, d):
    S = P.S
    S.memset("dve", B.POST[:, :, 0, 0:1], 1.0)
    S.memset("dve", B.POST[:, :, 1, 0:1], 0.0)
    S.copy("dve", B.uW[:, d, :, :], B.u[:, d, :, :])
    n = 1
    while n < 512:
        wre = B.uW[:, d, 0, :].re("p (k o) -> p k o", o=1).bc([128, 8, n])
        wim = B.uW[:, d, 1, :].re("p (k o) -> p k o", o=1).bc([128, 8, n])
        cmul(S, "dve", B.POST[:, :, 0, n:2 * n], B.POST[:, :, 1, n:2 * n], B.POST[:, :, 0, 0:n], B.POST[:, :, 1, 0:n],
             wre, wim, B.tt1[:, :, 0:n], B.tt2[:, :, 0:n])
        n *= 2
        if n < 512:
            cmul(S, "dve", B.sm[0].all(), B.sm[1].all(), B.uW[:, d, 0, :], B.uW[:, d, 1, :], B.uW[:, d, 0, :], B.uW[:, d, 1, :], B.sm[2].all(), B.gi[:, 0, :])
            S.copy("dve", B.uW[:, d, 0, :], B.sm[0].all())
            S.copy("dve", B.uW[:, d, 1, :], B.sm[1].all())
    for hlf in range(2):
        sl = slice(hlf * 256, (hlf + 1) * 256)
        cre = B.coef[:, d, 0, :].re("p (k o) -> p k o", o=1).bc([128, 8, 256])
        cim = B.coef[:, d, 1, :].re("p (k o) -> p k o", o=1).bc([128, 8, 256])
        S.tt("dve", B.tt1.all(), B.POST[:, :, 0, sl], cre, ALU.mult)
        S.tt("dve", B.tt2.all(), B.POST[:, :, 1, sl], cim, ALU.mult)
        S.tt("dve", B.PRE[:, :, 0, sl], B.tt1.all(), B.tt2.all(), ALU.add)
        S.tt("dve", B.tt1.all(), B.POST[:, :, 0, sl], cim, ALU.mult)
        S.tt("dve", B.tt2.all(), B.POST[:, :, 1, sl], cre, ALU.mult)
        S.tt("dve", B.PRE[:, :, 1, sl], B.tt1.all(), B.tt2.all(), ALU.subtract)


def s5_pass(P, B, sc, d, full, init_lat, out_ctx, out_fin, YS, Ysrc=None):
    S = P.S
    gtv = sc["GT"].ap.rearrange("n p t -> p n t")
    ysv = YS.ap.rearrange("n p t -> p n t") if YS is not None else None
    if Ysrc is None:
        Ysrc = YS
    ysrcv = Ysrc.ap.rearrange("n p t -> p n t") if Ysrc is not None else None
    s5_tables(P, B, d)
    S.memset("dve", B.h.all(), 0.0)
    lat = P.tiles[1:]
    order = [P.tiles[0]] + (lat if d == 0 else lat[::-1])
    rv = (lambda v: v) if d == 0 else (lambda v: v[:, ::-1])
    rv3 = (lambda v: v) if d == 0 else (lambda v: v[:, :, ::-1])
    for n, (t0, Wd) in enumerate(order):
        U, Yo = B.U[n % 2], B.Yo[n % 2]
        S.dma("sp", U[:, :, :Wd], V(gtv[:, 6:8, t0:t0 + Wd], sc["GT"].res))
        if full and d == 1:
            S.dma("sp", B.Yf[:, :, :Wd], V(ysrcv[:, :, t0:t0 + Wd], Ysrc.res))
        cmul(S, "dve", B.gi[:, 0, :], B.gi[:, 1, :], B.u[:, d, 0, :], B.u[:, d, 1, :], B.h[:, 0, :], B.h[:, 1, :], B.sm[0].all(), B.sm[1].all())
        def Gk(k, idx):
            return B.G.k(k, (slice(None), k) + idx)

        def pre(k):
            pb = B.pb[k % 2]
            for ri in range(2):
                S.mm(pb[:, ri, :Wd], B.BT[:, (d * 2 + ri) * 8 + k, :], U[:, k // 4, :Wd])
            pre_re = rv3(B.PRE[:, k, 0:1, :Wd]).bc([128, 2, Wd])
            pre_im = rv3(B.PRE[:, k, 1:2, :Wd]).bc([128, 2, Wd])
            Wt = B.Wt[k % 2]
            T1, T2 = B.Tp1[k % 2], B.Tp2[k % 2]
            S.tt("dve", T1[:, :, :Wd], pb[:, :, :Wd], pre_re, ALU.mult)
            S.tt("dve", T2[:, :, :Wd], pb[:, ::-1, :Wd], pre_im, ALU.mult)
            S.tt("pool", Wt[:, 0, :Wd], T1[:, 0, :Wd], T2[:, 0, :Wd], ALU.subtract)
            S.tt("pool", Wt[:, 1, :Wd], T1[:, 1, :Wd], T2[:, 1, :Wd], ALU.add)

        def mid(k):
            Wt = B.Wt[k % 2]
            rbc = B.r[:, d, k:k + 1].bc([128, Wd])
            for ri in range(2):
                S.scan(rv(Gk(k, (ri, slice(0, Wd)))), rbc, rv(Wt[:, ri, :Wd]), B.gi[:, ri, k:k + 1])

        def post(k):
            post_re = rv3(B.POST[:, k, 0:1, :Wd]).bc([128, 2, Wd])
            post_im = rv3(B.POST[:, k, 1:2, :Wd]).bc([128, 2, Wd])
            Hb = B.Hb[k % 2]
            T1, T2 = B.Tq1[k % 2], B.Tq2[k % 2]
            S.tt("dve", T1[:, :, :Wd], Gk(k, (slice(None), slice(0, Wd))), post_re, ALU.mult)
            S.tt("dve", T2[:, :, :Wd], Gk(k, (slice(None, None, -1), slice(0, Wd))), post_im, ALU.mult)
            S.tt("pool", Hb[:, 0, :Wd], T1[:, 0, :Wd], T2[:, 0, :Wd], ALU.subtract)
            S.stt(Hb[:, 1, :Wd], T1[:, 1, :Wd], -1.0, T2[:, 1, :Wd], ALU.mult, ALU.subtract)
            o = k // 4
            for ri in range(2):
                S.mm(B.py[o][:, :Wd], B.CT[:, (d * 2 + ri) * 8 + k, :], Hb[:, ri, :Wd],
                     start=(k % 4 == 0 and ri == 0), stop=(k % 4 == 3 and ri == 1))

        pre(0)
        for k in range(8):
            if k + 1 < 8:
                pre(k + 1)
            mid(k)
            if full:
                post(k)
        mi = Wd - 1 if d == 0 else 0
        S.eng["dve"].ops.append(("w", S.eng["dve"].sem, S.eng["dve"].cnt))
        S.eng["dve"].seen["dve"] = S.eng["dve"].cnt
        cmul(S, "dve", B.h[:, 0, :], B.h[:, 1, :], B.POST[:, :, 0, Wd - 1], B.POST[:, :, 1, Wd - 1],
             B.G[:, :, 0, mi], B.G[:, :, 1, mi], B.sm[0].all(), B.sm[1].all())
        for k_ in range(8):
            B.G.k(k_, (slice(None),)).res.r["dve"] = S.eng["dve"].cnt
        if full:
            for o in range(2):
                if d == 1:
                    S.tt("dve", Yo[:, o, :Wd], B.py[o][:, :Wd], B.Yf[:, o, :Wd], ALU.add)
                else:
                    S.copy("act", Yo[:, o, :Wd], B.py[o][:, :Wd])
            S.dma("pool", V(ysv[:, :, t0:t0 + Wd], YS.res), Yo[:, :, :Wd])
        if n == 0:
            if out_ctx is not None:
                S.dma("pool", out_ctx, B.h.all())
            if init_lat is not None:
                S.dma("sp", B.h.all(), init_lat)
    if out_fin is not None:
        S.dma("pool", out_fin, B.h.all())


W_A = [("xT", lambda P: [KD, 128, P.NT], F32), ("cond", lambda P: [128, KD, 2], F32), ("ada_w", lambda P: [D, 6 * D], F32),
       ("ada_b", lambda P: [128, 48], F32), ("n1w", lambda P: [128, KD], F32), ("w_in_arr", lambda P: [D, NCOL], F32),
       ("w_v", lambda P: [D, 768], F32), ("wa2p", lambda P: [128, 2, 256], F32), ("gla_ba", lambda P: [128, 2, 2], F32),
       ("hg_lb", lambda P: [128, 2, P.NL], F32), ("rope", lambda P: [128, 4, P.NT], F32), ("ones_bf", lambda P: [128, 128], BF16),
       ("ident_bf", lambda P: [128, 128], BF16), ("bret", lambda P: [128, 2, 2, 128], F32), ("masks", lambda P: [128, 2, 128], F32),
       ("s5_lre", lambda P: [128, 2, 8], F32), ("s5_lim", lambda P: [128, 2, 8], F32), ("s5_ldt", lambda P: [128, 2, 8], F32),
       ("s5_BT", lambda P: [128, 32, 128], F32), ("s5_CT", lambda P: [128, 32, 128], F32)]


def declare(P, specs, prefix=""):
    W = {}
    for name, shp, dt in specs:
        W[name] = P.din(prefix + name, shp(P), dt)
    return W


def build_test_S(TL, l, NL=4, full=True):
    P = Prog(TL, debug=True, NL=NL)
    W = declare(P, W_A + [("init_s5", lambda P: [2, 128, 2, 8], F32)])
    NT = P.NT
    sc = alloc_scratch(P)
    YS = P.dscr("YS", [2, 128, NT], F32)
    st_out = P.dout("st_out", [4, 128, 2, 8], F32)
    with ExitStack() as st:
        MOD = phase_mod(P, st, W, 16)
        lbv = compute_lbv(P, st, W, l)
        phase_A(P, W, MOD, sc, lbv)
        with ExitStack() as st2:
            B = s5_alloc(P, st2, W)
            for d in range(2):
                s5_pass(P, B, sc, d, full, V(W["init_s5"].ap[d], W["init_s5"].res) if full else None,
                        V(st_out.ap[d], st_out.res), V(st_out.ap[2 + d], st_out.res), YS if full else None)
            P.S.barrier()
            P.flush()
    return P.nc


def norm_mod2(P, S, X, XN, XNf, sqb, psn, rstd, tmpf, ones_bf, s_vec, b_vec, col, Wd):
    S.act(sqb[:, :, :Wd], X[:, :, :Wd], AF.Square)
    for kc in range(KD):
        S.mm(psn[:, :Wd], ones_bf.all(), sqb[:, kc, :Wd], start=(kc == 0), stop=(kc == KD - 1))
    S.ts("dve", rstd[:, :Wd], psn[:, :Wd], 1.0 / D, ALU.mult, 1e-6, ALU.add)
    S.act(rstd[:, :Wd], rstd[:, :Wd], AF.Sqrt)
    S.recip(rstd[:, :Wd], rstd[:, :Wd])
    for kc in range(KD):
        S.stt(tmpf[:, kc, :Wd], X[:, kc, :Wd], s_vec[:, kc, col:col + 1], rstd[:, :Wd], ALU.mult, ALU.mult)
        S.act(XNf[:, kc, :Wd], tmpf[:, kc, :Wd], AF.Identity, bias=b_vec[:, kc, col:col + 1])
    S.copy("pool", XN[:, :, :Wd], XNf[:, :, :Wd])


W_C = [("w_out", lambda P: [D, D], F32), ("hn_w", lambda P: [128, 6], F32), ("s5_d", lambda P: [128, 2], F32),
       ("glu_w", lambda P: [256, 256], F32), ("glu_b", lambda P: [128, 2], F32), ("n2w", lambda P: [128, KD], F32),
       ("blockmean", lambda P: [128, 128], BF16), ("ident_f", lambda P: [128, 128], F32)]
W_MOE = [("router_w", lambda P: [D, 8], F32), ("router_b", lambda P: [1, 8], F32), ("selE", lambda P: [8, 8, 128], BF16)]


def phase_C(P, W, MOD, sc, YA, YS, xT2, XN2, GTS, moe, do_ctx):
    S = P.S
    with ExitStack() as st:
        Wout = P.sb(st, "Wout", [128, KD, D], BF16)
        gluw = P.sb(st, "gluw", [128, 2, 256], BF16)
        Bm = P.sb(st, "Bm", [128, 128], BF16)
        ones_bf = P.sb(st, "onesC", [128, 128], BF16)
        hnw = P.sb(st, "hnw", [128, 6])
        s5d = P.sb(st, "s5d", [128, 2])
        glub = P.sb(st, "glub", [128, 2])
        n2w = P.sb(st, "n2w", [128, KD])
        s2 = P.sb(st, "s2", [128, KD, 2])
        S.dma("sp", Bm.all(), W["blockmean"].all())
        S.dma("sp", ones_bf.all(), W["ones_bf"].all())
        S.dma("sp", hnw.all(), W["hn_w"].all())
        S.dma("sp", s5d.all(), W["s5_d"].all())
        S.dma("sp", glub.all(), W["glu_b"].all())
        S.dma("sp", n2w.all(), W["n2w"].all())
        S.ts("dve", s2.all(), MOD[:, 32:40, :], 1.0, ALU.add)
        S.tt("dve", s2.all(), s2.all(), n2w.all().re("p (k o) -> p k o", o=1).bc([128, KD, 2]), ALU.mult)
        if moe:
            rw = P.sb(st, "rw", [128, KD, 8])
            rb = P.sb(st, "rb", [128, 8])
            identf = P.sb(st, "identf", [128, 128])
            S.dma("sp", rw.all(), V(W["router_w"].ap.rearrange("(kc p) n -> p kc n", p=128), W["router_w"].res))
            S.dma("sp", rb.all(), V(W["router_b"].ap.partition_broadcast(128), W["router_b"].res), allow_slow_non_contiguous=True)
            S.dma("sp", identf.all(), W["ident_f"].all())
        with ExitStack() as st2:
            stg = P.sb(st2, "wostg", [128, KD, D])
            S.dma("sp", stg.all(), V(W["w_out"].ap.rearrange("(kc p) n -> p kc n", p=128), W["w_out"].res))
            S.copy("dve", Wout[:, 0:4, :], stg[:, 0:4, :])
            S.copy("act", Wout[:, 4:8, :], stg[:, 4:8, :])
            stg2 = P.sb(st2, "glstg", [128, 2, 256])
            S.dma("sp", stg2.all(), V(W["glu_w"].ap.rearrange("(kc p) n -> p kc n", p=128), W["glu_w"].res))
            S.copy("pool", gluw.all(), stg2.all())
            S.barrier()
            P.flush()
        X = [P.sb(st, f"cX{i}", [128, KD, 512]) for i in range(2)]
        YAt = [P.sb(st, f"cYA{i}", [128, 6, 512]) for i in range(2)]
        YSt = [P.sb(st, f"cYS{i}", [128, 2, 512]) for i in range(2)]
        GTt = [P.sb(st, f"cGT{i}", [128, 8, 512], BF16) for i in range(2)]
        Y = P.sb(st, "cY", [128, KD, 512], BF16)
        ybf = P.sb(st, "cybf", [128, 512], BF16)
        sqh = P.sb(st, "csqh", [128, 512], BF16)
        yc = P.sb(st, "cyc", [128, 512])
        rs = P.sb(st, "crs", [128, 512])
        o_ = P.sb(st, "co", [128, 512])
        zz = [P.sb(st, f"cz{i}", [128, 512]) for i in range(2)]
        zb = P.sb(st, "czb", [128, 2, 512], BF16)
        ta = P.sb(st, "cta", [128, 512])
        tb = P.sb(st, "ctb", [128, 512])
        sqb = P.sb(st, "csqb", [128, KD, 512], BF16)
        tmpf = P.sb(st, "ctmpf", [128, KD, 512])
        XNf = P.sb(st, "cXNf", [128, KD, 512])
        XN = P.sb(st, "cXN", [128, KD, 512], BF16)
        rstd = P.sb(st, "crstd", [128, 512])
        pp = [P.ps(st, f"cpp{i}", [128, 512]) for i in range(3)]
        po = [P.ps(st, f"cpo{i}", [128, 512]) for i in range(2)]
        psn = P.ps(st, "cpsn", [128, 512])
        if moe:
            pr = P.ps(st, "cpr", [128, 8])
            ptr = P.ps(st, "cptr", [8, 128])
            lg = P.sb(st, "clg", [128, 8])
            l2 = P.sb(st, "cl2", [128, 8])
            ee = P.sb(st, "cee", [128, 8])
            sm = [P.sb(st, f"csm{i}", [128, 1]) for i in range(4)]
            gT = P.sb(st, "cgT", [8, 512], BF16)
        ppi = [0]

        def nextpp():
            p = pp[ppi[0] % 3]
            ppi[0] += 1
            return p

        xv = W["xT"].ap.rearrange("k p t -> p k t")
        x2v = xT2.ap.rearrange("k p t -> p k t")
        xnv = XN2.ap.rearrange("k p t -> p k t")
        yav = YA.ap.rearrange("n p t -> p n t")
        ysv = YS.ap.rearrange("n p t -> p n t")
        gtv = sc["GT"].ap.rearrange("n p t -> p n t")
        tiles = P.tiles if do_ctx else P.tiles[1:]
        for n, (t0, Wd) in enumerate(tiles):
            col = 1 if t0 == 0 else 0
            Xt, ya, ys, gt = X[n % 2], YAt[n % 2], YSt[n % 2], GTt[n % 2]
            S.dma("sp", Xt[:, :, :Wd], V(xv[:, :, t0:t0 + Wd], W["xT"].res))
            S.dma("sp", ya[:, :, :Wd], V(yav[:, :, t0:t0 + Wd], YA.res))
            S.dma("sp", ys[:, :, :Wd], V(ysv[:, :, t0:t0 + Wd], YS.res))
            S.dma("sp", gt[:, :, :Wd], V(gtv[:, :, t0:t0 + Wd], sc["GT"].res))
            for j in range(6):
                m = j // 2
                kc = (0, 4, 6)[m] + (j % 2)
                y = ya[:, j, :Wd]
                if m == 0:
                    S.copy("act", ybf[:, :Wd], y)
                    pm = nextpp()
                    S.mm(pm[:, :Wd], Bm.all(), ybf[:, :Wd])
                    S.tt("dve", yc[:, :Wd], y, pm[:, :Wd], ALU.subtract)
                    y = yc[:, :Wd]
                S.act(sqh[:, :Wd], y, AF.Square)
                pv = nextpp()
                S.mm(pv[:, :Wd], Bm.all(), sqh[:, :Wd])
                S.ts("dve", rs[:, :Wd], pv[:, :Wd], 1e-6, ALU.add)
                S.act(rs[:, :Wd], rs[:, :Wd], AF.Sqrt)
                S.recip(rs[:, :Wd], rs[:, :Wd])
                S.stt(o_[:, :Wd], y, hnw[:, j:j + 1], rs[:, :Wd], ALU.mult, ALU.mult)
                S.tt("pool", Y[:, kc, :Wd], o_[:, :Wd], gt[:, j, :Wd], ALU.mult)
            for j in range(2):
                z = zz[j]
                S.stt(z[:, :Wd], gt[:, 6 + j, :Wd], s5d[:, j:j + 1], ys[:, j, :Wd], ALU.mult, ALU.add)
                S.act(ta[:, :Wd], z[:, :Wd], AF.Square)
                S.ts("dve", ta[:, :Wd], ta[:, :Wd], 0.044715, ALU.mult, 1.0, ALU.add)
                S.tt("dve", ta[:, :Wd], ta[:, :Wd], z[:, :Wd], ALU.mult)
                S.act(tb[:, :Wd], ta[:, :Wd], AF.Sigmoid, scale=1.5957691216057308)
                S.tt("dve", z[:, :Wd], z[:, :Wd], tb[:, :Wd], ALU.mult)
                S.copy("pool", zb[:, j, :Wd], z[:, :Wd])
            for j in range(2):
                pg = nextpp()
                for kk in range(2):
                    S.mm(pg[:, :Wd], gluw[:, kk, j * 128:(j + 1) * 128], zb[:, kk, :Wd], start=(kk == 0), stop=(kk == 1))
                S.act(tb[:, :Wd], pg[:, :Wd], AF.Sigmoid, bias=glub[:, j:j + 1])
                S.tt("dve", Y[:, 2 + j, :Wd], zz[j][:, :Wd], tb[:, :Wd], ALU.mult)
            for dt in range(KD):
                p = po[dt % 2]
                for kc in range(KD):
                    S.mm(p[:, :Wd], Wout[:, kc, dt * 128:(dt + 1) * 128], Y[:, kc, :Wd], start=(kc == 0), stop=(kc == KD - 1))
                S.stt(Xt[:, dt, :Wd], p[:, :Wd], MOD[:, 16 + dt, col:col + 1], Xt[:, dt, :Wd], ALU.mult, ALU.add)
            S.dma("pool", V(x2v[:, :, t0:t0 + Wd], xT2.k(t0, slice(None)).res), Xt[:, :, :Wd])
            norm_mod2(P, S, Xt, XN, XNf, sqb, psn, rstd, tmpf, ones_bf, s2, MOD[:, 24:32, :], col, Wd)
            S.dma("pool", V(xnv[:, :, t0:t0 + Wd], XN2.res), XN[:, :, :Wd])
            if moe:
                for sub in range(Wd // 128):
                    ss = slice(sub * 128, (sub + 1) * 128)
                    for kc in range(KD):
                        S.mm(pr.all(), XNf[:, kc, ss], rw[:, kc, :], start=(kc == 0), stop=(kc == KD - 1))
                    S.tt("dve", lg.all(), pr.all(), rb.all(), ALU.add)
                    S.red(sm[0].all(), lg.all(), ALU.max)
                    S.ts("dve", l2.all(), lg.all(), sm[0].all(), ALU.is_equal)
                    S.stt(l2.all(), l2.all(), -1e30, lg.all(), ALU.mult, ALU.add)
                    S.red(sm[1].all(), l2.all(), ALU.max)
                    S.ts("dve", l2.all(), lg.all(), sm[1].all(), ALU.is_ge)
                    S.ts("dve", sm[2].all(), sm[0].all(), -1.0, ALU.mult)
                    S.act(ee.all(), lg.all(), AF.Exp, bias=sm[2].all())
                    S.tt("dve", ee.all(), ee.all(), l2.all(), ALU.mult)
                    S.red(sm[3].all(), ee.all(), ALU.add)
                    S.recip(sm[3].all(), sm[3].all())
                    S.ts("dve", ee.all(), ee.all(), sm[3].all(), ALU.mult)
                    S.tr(ptr.all(), ee.all(), identf.all())
                    S.copy("act", gT[:, ss], ptr.all())
                S.dma("pool", V(GTS.ap[:, t0:t0 + Wd], GTS.res), gT[:, :Wd])
        S.barrier()
        P.flush()


def phase_D(P, W, MOD, xT2, XN2, GTS, moe, do_ctx):
    S = P.S
    nE = 8 if moe else 1
    with ExitStack() as st:
        w1 = P.sb(st, "fw1", [128, KD, HFF], BF16)
        w3 = P.sb(st, "fw3", [128, KD, HFF], BF16)
        w2 = P.sb(st, "fw2", [128, NFT, D], BF16)
        stg = [P.sb(st, f"fstg{i}", [128, 2816]) for i in range(2)]
        XNt = [P.sb(st, f"fXN{i}", [128, KD, 512], BF16) for i in range(2)]
        Xa = [P.sb(st, f"fXa{i}", [128, KD, 512]) for i in range(2)]
        hT = P.sb(st, "fhT", [128, NFT, 512], BF16)
        sl = [P.sb(st, f"fsl{i}", [128, 512]) for i in range(2)]
        tg = [P.sb(st, f"ftg{i}", [128, 512]) for i in range(2)]
        p1 = [P.ps(st, f"fp1_{i}", [128, 512]) for i in range(2)]
        p3 = [P.ps(st, f"fp3_{i}", [128, 512]) for i in range(2)]
        po = [P.ps(st, f"fpo{i}", [128, 512]) for i in range(2)]
        if moe:
            gTs = P.sb(st, "fgTs", [8, P.NT], BF16)
            selE = P.sb(st, "fselE", [8, 8, 128], BF16)
            gateB = P.sb(st, "fgateB", [128, 512])
            pg = P.ps(st, "fpg", [128, 512])
            c0_ = 0 if do_ctx else CTX
            S.dma("sp", gTs[:, c0_:], GTS[:, c0_:])
            S.dma("sp", selE.all(), W["selE"].all())
        x2v = xT2.ap.rearrange("k p t -> p k t")
        xnv = XN2.ap.rearrange("k p t -> p k t")
        tiles = P.tiles if do_ctx else P.tiles[1:]
        cast_eng = ("dve", "act", "pool")
        ci = 0
        qi = 0
        for e in range(nE):
            for hh in range(2):
                hs = slice(hh * HFF, (hh + 1) * HFF)
                if moe:
                    a1, a3, a2 = W["ffn_w1"].ap[e], W["ffn_w3"].ap[e], W["ffn_w2"].ap[e]
                else:
                    a1, a3, a2 = W["ffn_w1"].ap, W["ffn_w3"].ap, W["ffn_w2"].ap
                for (src, dst, rw_) in ((a1, w1, W["ffn_w1"]), (a3, w3, W["ffn_w3"])):
                    sv = src[:, hs].rearrange("(kc p) n -> p kc n", p=128)
                    for pc in range(4):
                        sg = stg[qi % 2]
                        qi += 1
                        cs = slice(pc * 352, (pc + 1) * 352)
                        S.dma("sp" if qi % 2 == 0 else "act", sg.all().re("p (k n) -> p k n", k=KD), V(sv[:, :, cs], rw_.res))
                        S.copy(cast_eng[ci % 3], dst[:, :, cs], sg.all().re("p (k n) -> p k n", k=KD))
                        ci += 1
                sv = a2[hs, :].rearrange("(ft p) n -> p ft n", p=128)
                for pc in range(4):
                    sg = stg[qi % 2]
                    qi += 1
                    cs = slice(pc * 256, (pc + 1) * 256)
                    S.dma("sp" if qi % 2 == 0 else "act", sg.all().re("p (k n) -> p k n", k=NFT), V(sv[:, :, cs], W["ffn_w2"].res))
                    S.copy(cast_eng[ci % 3], w2[:, :, cs], sg.all().re("p (k n) -> p k n", k=NFT))
                    ci += 1
                for n, (t0, Wd) in enumerate(tiles):
                    col = 1 if t0 == 0 else 0
                    xn, xa = XNt[n % 2], Xa[n % 2]
                    S.dma("sp", xn[:, :, :Wd], V(xnv[:, :, t0:t0 + Wd], XN2.res))
                    S.dma("sp", xa[:, :, :Wd], V(x2v[:, :, t0:t0 + Wd], xT2.k(t0, slice(None)).res))
                    if moe:
                        S.mm(pg[:, :Wd], selE[:, e, :], gTs[:, t0:t0 + Wd])
                        S.copy("act", gateB[:, :Wd], pg[:, :Wd])
                    for ft in range(NFT):
                        a, b = p1[ft % 2], p3[ft % 2]
                        fs = slice(ft * 128, (ft + 1) * 128)
                        for kc in range(KD):
                            S.mm(a[:, :Wd], w1[:, kc, fs], xn[:, kc, :Wd], start=(kc == 0), stop=(kc == KD - 1))
                        for kc in range(KD):
                            S.mm(b[:, :Wd], w3[:, kc, fs], xn[:, kc, :Wd], start=(kc == 0), stop=(kc == KD - 1))
                        s_ = sl[ft % 2]
                        S.act(s_[:, :Wd], a[:, :Wd], AF.Silu)
                        S.tt("dve", hT[:, ft, :Wd], s_[:, :Wd], b[:, :Wd], ALU.mult)
                    for dt in range(KD):
                        p = po[dt % 2]
                        for ft in range(NFT):
                            S.mm(p[:, :Wd], w2[:, ft, dt * 128:(dt + 1) * 128], hT[:, ft, :Wd], start=(ft == 0), stop=(ft == NFT - 1))
                        if moe:
                            t_ = tg[dt % 2]
                            S.tt("dve", t_[:, :Wd], p[:, :Wd], gateB[:, :Wd], ALU.mult)
                            S.stt(xa[:, dt, :Wd], t_[:, :Wd], MOD[:, 40 + dt, col:col + 1], xa[:, dt, :Wd], ALU.mult, ALU.add)
                        else:
                            S.stt(xa[:, dt, :Wd], p[:, :Wd], MOD[:, 40 + dt, col:col + 1], xa[:, dt, :Wd], ALU.mult, ALU.add)
                    S.dma("pool", V(x2v[:, :, t0:t0 + Wd], xT2.k(t0, slice(None)).res), xa[:, :, :Wd])
        S.barrier()
        P.flush()


def phase_final(P, W, xT2, yT):
    S = P.S
    with ExitStack() as st:
        ones_bf = P.sb(st, "zones", [128, 128], BF16)
        fws = P.sb(st, "zfw", [128, KD, 1])
        zb = P.sb(st, "zzb", [128, KD, 1])
        S.dma("sp", ones_bf.all(), W["ones_bf"].all())
        S.dma("sp", fws.all(), W["fw"].all())
        S.memset("dve", zb.all(), 0.0)
        X = [P.sb(st, f"zX{i}", [128, KD, 512]) for i in range(2)]
        XN = [P.sb(st, f"zXN{i}", [128, KD, 512]) for i in range(2)]
        sqb = P.sb(st, "zsqb", [128, KD, 512], BF16)
        tmpf = P.sb(st, "ztmpf", [128, KD, 512])
        rstd = P.sb(st, "zrstd", [128, 512])
        psn = P.ps(st, "zpsn", [128, 512])
        xv = xT2.ap.rearrange("k p t -> p k t")
        yv = yT.ap.rearrange("k p t -> p k t")
        for i, (t0, Wd) in enumerate(P.tiles[1:]):
            S.dma("sp", X[i % 2].all(), V(xv[:, :, t0:t0 + Wd], xT2.k(t0, slice(None)).res))
            norm_mod(P, S, X[i % 2], XN[i % 2], sqb, psn, rstd, tmpf, ones_bf, fws, zb, 0, 512)
            S.dma("pool", V(yv[:, :, t0 - CTX:t0 - CTX + Wd], yT.res), XN[i % 2].all())
        S.barrier()
        P.flush()


W_ST = [("init_att", lambda P: [128, 6, 64], F32), ("init_s5", lambda P: [128, 2, 8], F32)]


def build_La(TL, l, NL=4):
    P = Prog(TL, NL=NL)
    W = declare(P, W_A)
    sc = alloc_scratch(P)
    st_att = P.dout("st_att", [4, 128, 6, 64], F32)
    st_s5 = P.dout("st_s5", [4, 128, 2, 8], F32)
    with ExitStack() as st:
        MOD = phase_mod(P, st, W, 16)
        lbv = compute_lbv(P, st, W, l)
        phase_A(P, W, MOD, sc, lbv, so=True)
        with ExitStack() as st2:
            A = attn_alloc(P, st2, W)
            for d in range(2):
                attn_pass(P, A, sc, d, False, None, V(st_att.ap[d], st_att.res), V(st_att.ap[2 + d], st_att.res), None)
            P.S.barrier()
            P.flush()
        with ExitStack() as st2:
            B = s5_alloc(P, st2, W)
            for d in range(2):
                s5_pass(P, B, sc, d, False, None, V(st_s5.ap[d], st_s5.res), V(st_s5.ap[2 + d], st_s5.res), None)
            P.S.barrier()
            P.flush()
    return P.nc


def build_Lb(TL, l, moe, last, NL=4):
    P = Prog(TL, NL=NL)
    NT = P.NT
    specs = W_A + W_ST + W_C
    if moe:
        specs = specs + W_MOE + [("ffn_w1", lambda P: [8, D, DFF], F32), ("ffn_w3", lambda P: [8, D, DFF], F32), ("ffn_w2", lambda P: [8, DFF, D], F32)]
    else:
        specs = specs + [("ffn_w1", lambda P: [D, DFF], F32), ("ffn_w3", lambda P: [D, DFF], F32), ("ffn_w2", lambda P: [DFF, D], F32)]
    if last:
        specs = specs + [("fw", lambda P: [128, KD, 1], F32)]
    W = declare(P, specs)
    sc = alloc_scratch(P)
    YA = P.dscr("YA", [6, 128, NT], F32)
    YS = P.dscr("YS", [2, 128, NT], F32)
    XN2 = P.dscr("XN2", [KD, 128, NT], BF16)
    GTS = P.dscr("GTS", [8, NT], BF16)
    if last:
        xT2 = P.dscr("xT2", [KD, 128, NT], F32)
        yT = P.dout("yT", [KD, 128, P.TL], F32)
    else:
        xT2 = P.dout("xT2", [KD, 128, NT], F32)
    with ExitStack() as st:
        MOD = phase_mod(P, st, W, 48)
        lbv = compute_lbv(P, st, W, l)
        phase_A(P, W, MOD, sc, lbv)
        with ExitStack() as st2:
            A = attn_alloc(P, st2, W)
            for d in range(2):
                attn_pass(P, A, sc, d, True, V(W["init_att"].ap[d], W["init_att"].res), None, None, YA)
            P.S.barrier()
            P.flush()
        with ExitStack() as st2:
            B = s5_alloc(P, st2, W)
            for d in range(2):
                s5_pass(P, B, sc, d, True, V(W["init_s5"].ap[d], W["init_s5"].res), None, None, YS)
            P.S.barrier()
            P.flush()
        phase_C(P, W, MOD, sc, YA, YS, xT2, XN2, GTS, moe, not last)
        phase_D(P, W, MOD, xT2, XN2, GTS, moe, not last)
        if last:
            phase_final(P, W, xT2, yT)
    return P.nc


def consts_all():
    c = consts_np()
    c.update(attn_consts_np())
    bm = np.zeros((128, 128), np.float32)
    bm[:64, :64] = 1.0 / 64
    bm[64:, 64:] = 1.0 / 64
    c["blockmean"] = bm.astype(ml_dtypes.bfloat16)
    sel = np.zeros((8, 8, 128), np.float32)
    for e in range(8):
        sel[e, e, :] = 1.0
    c["selE"] = sel.astype(ml_dtypes.bfloat16)
    return c


def layer_inputs(inp, l, depth):
    d = layer_consts(inp, l)
    d.update(s5_layout_np(inp, l))
    d.update(consts_all())
    d["w_out"] = np.ascontiguousarray(inp["w_out"][l])
    d["hn_w"] = np.ascontiguousarray(np.concatenate([fm_vec(inp["ret_gn_w"][l]), fm_vec(inp["hg_norm_w"][l]), fm_vec(inp["gla_norm_w"][l])], axis=1))
    j = l // 2
    if l % 2 == 0:
        d["ffn_w1"] = np.ascontiguousarray(inp["ffn_w1"][j])
        d["ffn_w3"] = np.ascontiguousarray(inp["ffn_w3"][j])
        d["ffn_w2"] = np.ascontiguousarray(inp["ffn_w2"][j])
    else:
        d["ffn_w1"] = np.ascontiguousarray(inp["moe_w1"][j])
        d["ffn_w3"] = np.ascontiguousarray(inp["moe_w3"][j])
        d["ffn_w2"] = np.ascontiguousarray(inp["moe_w2"][j])
        d["router_w"] = np.ascontiguousarray(inp["router_w"][j])
        d["router_b"] = np.ascontiguousarray(np.asarray(inp["router_b"][j]).reshape(1, 8))
    d["fw"] = np.ascontiguousarray(fm_vec(inp["final_norm_w"]).reshape(128, KD, 1))
    return d


W_1 = W_A
W_2 = [("xT", lambda P: [KD, 128, P.NT], F32), ("MODin", lambda P: [128, 48, 2], F32),
       ("QSin", lambda P: [18, 128, P.NT], BF16), ("GSin", lambda P: [8, 128, P.NT], F32),
       ("GTin", lambda P: [8, 128, P.NT], BF16), ("VSin", lambda P: [P.NT, 768], BF16),
       ("YAin", lambda P: [6, 128, P.NT], F32), ("YSin", lambda P: [2, 128, P.NT], F32),
       ("ones_bf", lambda P: [128, 128], BF16), ("ident_bf", lambda P: [128, 128], BF16),
       ("bret", lambda P: [128, 2, 2, 128], F32), ("masks", lambda P: [128, 2, 128], F32),
       ("s5_lre", lambda P: [128, 2, 8], F32), ("s5_lim", lambda P: [128, 2, 8], F32), ("s5_ldt", lambda P: [128, 2, 8], F32),
       ("s5_BT", lambda P: [128, 32, 128], F32), ("s5_CT", lambda P: [128, 32, 128], F32)] + W_ST + W_C


def emit_L1(P, W, l):
    NT = P.NT
    sc = alloc_scratch(P, out=True)
    YA = P.dout("YA", [6, 128, NT], F32)
    YS = P.dout("YS", [2, 128, NT], F32)
    MODo = P.dout("MODo", [128, 48, 2], F32)
    f_att = P.dout("f_att", [128, 6, 64], F32)
    f_s5 = P.dout("f_s5", [128, 2, 8], F32)
    with ExitStack() as st:
        MOD = phase_mod(P, st, W, 48)
        P.S.dma("pool", MODo.all(), MOD.all())
        lbv = compute_lbv(P, st, W, l)
        phase_A(P, W, MOD, sc, lbv)
        with ExitStack() as st2:
            A = attn_alloc(P, st2, W)
            attn_pass(P, A, sc, 0, True, None, None, f_att.all(), YA)
            P.S.barrier()
            P.flush()
        with ExitStack() as st2:
            B = s5_alloc(P, st2, W)
            s5_pass(P, B, sc, 0, True, None, None, f_s5.all(), YS)
            P.S.barrier()
            P.flush()


def specs_L2(moe, last):
    specs = list(W_2)
    if moe:
        specs = specs + W_MOE + [("ffn_w1", lambda P: [8, D, DFF], F32), ("ffn_w3", lambda P: [8, D, DFF], F32), ("ffn_w2", lambda P: [8, DFF, D], F32)]
    else:
        specs = specs + [("ffn_w1", lambda P: [D, DFF], F32), ("ffn_w3", lambda P: [D, DFF], F32), ("ffn_w2", lambda P: [DFF, D], F32)]
    if last:
        specs = specs + [("fw", lambda P: [128, KD, 1], F32)]
    return specs


def emit_L2(P, W, l, moe, last):
    NT = P.NT
    sc = {"QS": W["QSin"], "GS": W["GSin"], "GT": W["GTin"], "VS": W["VSin"]}
    YA = P.dscr("YA2", [6, 128, NT], F32)
    YS = P.dscr("YS2", [2, 128, NT], F32)
    XN2 = P.dscr("XN2", [KD, 128, NT], BF16)
    GTS = P.dscr("GTS", [8, NT], BF16)
    if last:
        xT2 = P.dscr("xT2", [KD, 128, NT], F32)
        yT = P.dout("yT", [KD, 128, P.TL], F32)
    else:
        xT2 = P.dout("xT2", [KD, 128, NT], F32)
    with ExitStack() as st:
        MOD = P.sb(st, "MOD2", [128, 48, 2])
        P.S.dma("sp", MOD.all(), W["MODin"].all())
        with ExitStack() as st2:
            A = attn_alloc(P, st2, W)
            attn_pass(P, A, sc, 1, True, W["init_att"].all(), None, None, YA, Ysrc=W["YAin"])
            P.S.barrier()
            P.flush()
        with ExitStack() as st2:
            B = s5_alloc(P, st2, W)
            s5_pass(P, B, sc, 1, True, W["init_s5"].all(), None, None, YS, Ysrc=W["YSin"])
            P.S.barrier()
            P.flush()
        phase_C(P, W, MOD, sc, YA, YS, xT2, XN2, GTS, moe, not last)
        phase_D(P, W, MOD, xT2, XN2, GTS, moe, not last)
        if last:
            phase_final(P, W, xT2, yT)
    return xT2


def build_L1(TL, l, NL=4):
    P = Prog(TL, NL=NL)
    W = declare(P, W_1)
    emit_L1(P, W, l)
    return P.nc


def build_L2(TL, l, moe, last, NL=4):
    P = Prog(TL, NL=NL)
    W = declare(P, specs_L2(moe, last))
    emit_L2(P, W, l, moe, last)
    return P.nc


W_1N = [sp for sp in W_1 if sp[0] != "xT"]


def build_M(TL, l, moe, NL=4):
    P = Prog(TL, NL=NL)
    W2 = declare(P, specs_L2(moe, False))
    W1 = declare(P, W_1N, prefix="n_")
    xT2 = emit_L2(P, W2, l, moe, False)
    W1["xT"] = xT2
    emit_L1(P, W1, l + 1)
    return P.nc


def mirror_layer_inputs(li):
    m = dict(li)
    w = li["w_in_arr"].copy()
    w[:, 14 * 128:16 * 128] = li["w_in_arr"][:, 16 * 128:18 * 128]
    w[:, 16 * 128:18 * 128] = li["w_in_arr"][:, 14 * 128:16 * 128]
    lo = 26 * 128
    w[:, lo:lo + 16] = li["w_in_arr"][:, lo + 32:lo + 48]
    w[:, lo + 32:lo + 48] = li["w_in_arr"][:, lo:lo + 16]
    m["w_in_arr"] = w
    wa = np.zeros_like(li["wa2p"])
    wa[0:16, 0, :] = li["wa2p"][32:48, 1, :]
    wa[32:48, 1, :] = li["wa2p"][0:16, 0, :]
    m["wa2p"] = wa
    m["gla_ba"] = np.ascontiguousarray(li["gla_ba"][:, ::-1, :])
    for k in ("s5_lre", "s5_lim", "s5_ldt"):
        m[k] = np.ascontiguousarray(li[k][:, ::-1, :])
    for k in ("s5_BT", "s5_CT"):
        a = li[k].reshape(128, 2, 16, 128)
        m[k] = np.ascontiguousarray(a[:, ::-1]).reshape(128, 32, 128)
    m["bret"] = attn_consts_np(mirror=True)["bret"]
    return m


def names_of(specs):
    return [s[0] for s in specs]


def kernel(**inputs):
    inp = {k: np.asarray(v) for k, v in inputs.items()}
    x = inp["x"]
    B, SEQ, _ = x.shape
    depth = inp["ada_w"].shape[0]
    TL = SEQ // 2
    ncore = 2 * B
    cores = [(b, s) for b in range(B) for s in range(2)]
    xT = []
    ropes = []
    for (b, s) in cores:
        toks = np.concatenate([inp["ctx"][b], x[b, s * TL:(s + 1) * TL]], axis=0)
        rp = rope_tables(TL, s)
        if s == 1:
            toks = np.concatenate([inp["ctx"][b][::-1], x[b, s * TL:(s + 1) * TL][::-1]], axis=0)
            rp = np.ascontiguousarray(np.concatenate([rp[:, :, :CTX], rp[:, :, CTX:][:, :, ::-1]], axis=2))
        xT.append(to_fm(toks))
        ropes.append(rp)
    conds = [core_cond(inp, b) for b in range(B)]
    out = np.empty_like(x)
    ids = list(range(ncore))

    def lin(l):
        a = layer_inputs(inp, l, depth)
        return [a, mirror_layer_inputs(a)]

    def map_L1(li, ci, prefix=""):
        b, s = cores[ci]
        m = {prefix + k: li[s][k] for k in names_of(W_1N) if k in li[s]}
        m[prefix + "cond"] = conds[b]
        m[prefix + "rope"] = ropes[ci]
        return m

    def map_L2(li, ci, moe, last, r1):
        b, s = cores[ci]
        m = {k: li[s][k] for k in names_of(W_2 + (W_MOE if moe else [])) if k in li[s]}
        for k in ("ffn_w1", "ffn_w3", "ffn_w2"):
            m[k] = li[s][k]
        if last:
            m["fw"] = li[s]["fw"]
        m["xT"] = xT[ci]
        own, pa = r1[ci], r1[ci ^ 1]
        m["MODin"] = np.asarray(own["MODo"])
        for k in ("QS", "GS", "GT", "VS", "YA", "YS"):
            m[k + "in"] = np.asarray(own[k])
        m["init_att"] = np.asarray(pa["f_att"])
        m["init_s5"] = np.asarray(pa["f_s5"])
        return m

    li = lin(0)
    maps = []
    for ci in range(ncore):
        m = map_L1(li, ci)
        m["xT"] = xT[ci]
        maps.append(m)
    r1 = run_bass_kernel_spmd(build_L1(TL, 0, NL=depth), maps, core_ids=ids).results
    for l in range(depth - 1):
        moe = (l % 2 == 1)
        li_next = lin(l + 1)
        maps = []
        for ci in range(ncore):
            m = map_L2(li, ci, moe, False, r1)
            m.update(map_L1(li_next, ci, prefix="n_"))
            maps.append(m)
        r = run_bass_kernel_spmd(build_M(TL, l, moe, NL=depth), maps, core_ids=ids).results
        xT = [np.asarray(r[ci]["xT2"]) for ci in range(ncore)]
        r1 = r
        li = li_next
    l = depth - 1
    moe = (l % 2 == 1)
    maps = [map_L2(li, ci, moe, True, r1) for ci in range(ncore)]
    r2 = run_bass_kernel_spmd(build_L2(TL, l, moe, True, NL=depth), maps, core_ids=ids).results
    for ci, (b, s) in enumerate(cores):
        y = from_fm(np.asarray(r2[ci]["yT"]))
        out[b, s * TL:(s + 1) * TL] = y[::-1] if s == 1 else y
    return out
```
